# Optimizing a Trainium2 kernel written in Bass

```python
import math
import jax
import jax.numpy as jnp
from jax import lax
import numpy as np

D_MODEL = 1024
BATCH = 2
SEQ = 8192
DEPTH = 4

GRID_W = 64
CTX_LEN = 256
HEAD_DIM = 64
MIX_W = D_MODEL // 2
N_BRANCH = 4
HY_W = MIX_W
HY_ORDER = 2
HY_SHORT = 3
HY_BANDS = 16
HY_EMB = 2 * HY_BANDS + 1
HY_HID = 64
HY_MOD_SHIFT = 0.05
HY_FAST_DECAY = 0.3
HY_SLOW_DECAY = 1.5
HY_TARGET = 1e-2
GDN_H = MIX_W // HEAD_DIM
GDN_CONV = 3
GDN_CHUNK = 64
DIFF_VD = 2 * HEAD_DIM
DIFF_H = MIX_W // DIFF_VD
NA_H = MIX_W // HEAD_DIM
NA_ROWS = 8
NA_COLS = 16
N_EXPERTS = 16
N_GROUPS = 4
TOP_K = 2
EXPERT_FF = D_MODEL // 2
MOE_BLOCK = 128
Q_BLOCK = 128
ROPE_THETA = 10000.0
LN_EPS = 1e-5
RMS_EPS = 1e-6
DN_ALPHA = (2 * DEPTH) ** 0.25
DN_BETA = (8 * DEPTH) ** -0.25
SPLIT_SIZES = (3 * HY_W, 3 * MIX_W, MIX_W, 2 * GDN_H, 2 * GDN_H, 3 * MIX_W, 3 * MIX_W, N_BRANCH * D_MODEL)
SPLIT_IDX = tuple(sum(SPLIT_SIZES[:i + 1]) for i in range(len(SPLIT_SIZES) - 1))
IN_TOTAL = sum(SPLIT_SIZES)

kernel_name = 'hybrid_diffusion_parallel_mixers_grouped_moe'


def layer_norm(x, g=None, b=None):
    xf = x.astype(jnp.float32)
    mu = jnp.mean(xf, -1, keepdims=True)
    var = jnp.mean(jnp.square(xf - mu), -1, keepdims=True)
    y = (xf - mu) * lax.rsqrt(var + LN_EPS)
    if g is not None:
        y = y * g.astype(jnp.float32) + b.astype(jnp.float32)
    return y.astype(x.dtype)


def rms_norm(x, w):
    xf = x.astype(jnp.float32)
    return xf * lax.rsqrt(jnp.mean(jnp.square(xf), -1, keepdims=True) + RMS_EPS) * w.astype(jnp.float32)


def l2_normalize(x):
    return x * lax.rsqrt(jnp.sum(jnp.square(x), -1, keepdims=True) + RMS_EPS)


def modulate(x, shift, scale):
    return layer_norm(x) * (1.0 + scale) + shift


def centred_dwconv(u, w):
    K = w.shape[-1]
    T = u.shape[1]
    up = jnp.pad(u, ((0, 0), (K // 2, K // 2), (0, 0)))
    out = up[:, 0:T, :] * w[:, 0]
    for j in range(1, K):
        out = out + up[:, j:j + T, :] * w[:, j]
    return out


def hyena_filters(L, w1, b1, w2, b2, w3, freq, deltas):
    f32 = jnp.float32
    t = jnp.linspace(0.0, 1.0, L, dtype=f32)[:, None]
    ang = 2.0 * math.pi * jnp.arange(L, dtype=f32)[:, None] / L
    bands = jnp.linspace(1e-4, HY_BANDS - 1, HY_BANDS, dtype=f32)[None, :]
    feats = jnp.concatenate([t, jnp.cos(bands * ang), -jnp.sin(bands * ang)], axis=-1)
    fr = freq.astype(f32)
    h = jnp.sin(fr * (feats @ w1.astype(f32) + b1.astype(f32)))
    h = jnp.sin(fr * (h @ w2.astype(f32) + b2.astype(f32)))
    h = (h @ w3.astype(f32)).reshape(L, 2, HY_ORDER, HY_W)
    window = jnp.exp(-t[:, :, None, None] * jnp.abs(deltas.astype(f32))) + HY_MOD_SHIFT
    h = h * window
    h_fwd, h_bwd = h[:, 0], h[:, 1]
    taps = jnp.concatenate([h_fwd, jnp.zeros_like(h_fwd[:1]), jnp.flip(h_bwd[1:], 0)], axis=0)
    taps = taps / jnp.sum(jnp.abs(taps), axis=0, keepdims=True)
    return jnp.fft.rfft(taps, axis=0)


def hyena_mix(proj, conv_w, skip, filt_f):
    L = proj.shape[1]
    u = centred_dwconv(proj, conv_w).astype(jnp.float32)
    v, x1, x2 = jnp.split(u, 3, axis=-1)
    sk = skip.astype(jnp.float32)
    z = v
    for o, gate in enumerate((x1, x2)):
        zf = jnp.fft.rfft(z, n=2 * L, axis=1)
        y = jnp.fft.irfft(zf * filt_f[:, o], n=2 * L, axis=1)[:, :L]
        z = gate * (y + z * sk[o])
    return z.astype(proj.dtype)


def gated_delta_chunks(q, k, v, g, beta, s0):
    B, T, H, _ = q.shape
    C = GDN_CHUNK
    N = T // C

    def chunks(a):
        return jnp.moveaxis(a.reshape(B, N, C, H, *a.shape[3:]), 3, 1)

    q, k, v, g, beta = chunks(q), chunks(k), chunks(v), chunks(g), chunks(beta)
    gc = jnp.cumsum(g, axis=-1)
    tri = jnp.tril(jnp.ones((C, C), bool))
    strict = jnp.tril(jnp.ones((C, C), bool), -1)
    decay = jnp.exp(jnp.where(tri, gc[..., :, None] - gc[..., None, :], -jnp.inf))
    kb = k * beta[..., None]
    A = jnp.where(strict, jnp.einsum('bhnid,bhnjd->bhnij', kb, k) * decay, 0.0)
    eye = jnp.eye(C, dtype=jnp.float32)
    Tm = lax.linalg.triangular_solve(eye + A, jnp.broadcast_to(eye, A.shape),
                                     left_side=True, lower=True, unit_diagonal=True)
    u_val = jnp.einsum('bhnij,bhnjd->bhnid', Tm, v * beta[..., None])
    w_val = jnp.einsum('bhnij,bhnjd->bhnid', Tm, kb * jnp.exp(gc)[..., None])
    intra = jnp.einsum('bhnid,bhnjd->bhnij', q, k) * decay
    q_dec = q * jnp.exp(gc)[..., None]
    g_last = gc[..., -1]
    k_dec = k * jnp.exp(g_last[..., None] - gc)[..., None]

    def step(S, xs):
        qd, w, u, att, kd, gl = xs
        v_new = u - jnp.einsum('bhcd,bhde->bhce', w, S)
        o = jnp.einsum('bhcd,bhde->bhce', qd, S) + jnp.einsum('bhij,bhje->bhie', att, v_new)
        S = S * jnp.exp(gl)[..., None, None] + jnp.einsum('bhcd,bhce->bhde', kd, v_new)
        return S, o

    xs = tuple(jnp.moveaxis(a, 2, 0) for a in (q_dec, w_val, u_val, intra, k_dec, g_last))
    s_fin, o = lax.scan(step, s0, xs)
    o = jnp.transpose(o, (1, 0, 3, 2, 4)).reshape(B, T, H, -1)
    return o, s_fin


def gdn_mix(qkv, z, a, b, conv_w, a_log, dt_bias, norm_w, init_states, with_output):
    f32 = jnp.float32
    B, T, _ = qkv.shape
    u = jax.nn.silu(centred_dwconv(qkv, conv_w).astype(f32))
    q, k, v = [t.reshape(B, T, GDN_H, HEAD_DIM) for t in jnp.split(u, 3, axis=-1)]
    q = l2_normalize(q) * HEAD_DIM ** -0.5
    k = l2_normalize(k)
    g = -jnp.exp(a_log.astype(f32)) * jax.nn.softplus(a.astype(f32).reshape(B, T, 2, GDN_H) + dt_bias.astype(f32))
    beta = jax.nn.sigmoid(b.astype(f32).reshape(B, T, 2, GDN_H))
    outs, finals = [], []
    for d in range(2):
        rev = (lambda t: jnp.flip(t, 1)) if d == 1 else (lambda t: t)
        o, s_fin = gated_delta_chunks(rev(q), rev(k), rev(v), rev(g[:, :, d]), rev(beta[:, :, d]), init_states[d])
        finals.append(s_fin)
        if with_output:
            outs.append(rev(o))
    if not with_output:
        return None, (finals[0], finals[1])
    o = rms_norm(outs[0] + outs[1], norm_w) * jax.nn.silu(z.astype(f32).reshape(B, T, GDN_H, HEAD_DIM))
    return o.reshape(B, T, GDN_H * HEAD_DIM).astype(qkv.dtype), (finals[0], finals[1])


def axial_rope_angles(n):
    t = jnp.arange(n)
    row = (t // GRID_W).astype(jnp.float32)
    col = (t % GRID_W).astype(jnp.float32)
    nf = HEAD_DIM // 4
    inv = ROPE_THETA ** (-jnp.arange(nf, dtype=jnp.float32) / nf)
    return row[:, None] * inv, col[:, None] * inv


def rope_half(x, ang):
    x1, x2 = jnp.split(x, 2, axis=-1)
    cos, sin = jnp.cos(ang).astype(x.dtype), jnp.sin(ang).astype(x.dtype)
    return jnp.concatenate([x1 * cos - x2 * sin, x1 * sin + x2 * cos], axis=-1)


def axial_rope(x, ang_r, ang_c):
    half = HEAD_DIM // 2
    ar = ang_r[None, :, None, None, :]
    ac = ang_c[None, :, None, None, :]
    return jnp.concatenate([rope_half(x[..., :half], ar), rope_half(x[..., half:], ac)], axis=-1)


def diff_attend(q, k, v, lam):
    s = jnp.einsum('bqhcd,bkhcd->bhcqk', q, k).astype(jnp.float32) * HEAD_DIM ** -0.5
    p = jax.nn.softmax(s, axis=-1)
    a = p[:, :, 0] - lam * p[:, :, 1]
    return jnp.einsum('bhqk,bkhd->bqhd', a.astype(v.dtype), v)


def diff_latent(q, k, v, kc, vc, lam, ang_r, ang_c):
    B, n = q.shape[:2]
    q = axial_rope(q, ang_r, ang_c)
    k_all = jnp.concatenate([axial_rope(k, ang_r, ang_c), kc], axis=1)
    v_all = jnp.concatenate([v, vc], axis=1)
    nb = n // Q_BLOCK
    qb = jnp.swapaxes(q.reshape(B, nb, Q_BLOCK, *q.shape[2:]), 0, 1)
    o = lax.map(lambda qi: diff_attend(qi, k_all, v_all, lam), qb)
    return jnp.swapaxes(o, 0, 1).reshape(B, n, DIFF_H, DIFF_VD)


def diff_finish(o, norm_w, lam_init):
    B, T = o.shape[:2]
    return (rms_norm(o, norm_w) * (1.0 - lam_init)).reshape(B, T, DIFF_H * DIFF_VD).astype(o.dtype)


def split_diff(p):
    B, T, _ = p.shape
    q, k, v = jnp.split(p, 3, axis=-1)
    return (q.reshape(B, T, DIFF_H, 2, HEAD_DIM), k.reshape(B, T, DIFF_H, 2, HEAD_DIM),
            v.reshape(B, T, DIFF_H, DIFF_VD))


def dense_attend(q, k, v):
    s = jnp.einsum('bqhd,bkhd->bhqk', q, k).astype(jnp.float32) * HEAD_DIM ** -0.5
    p = jax.nn.softmax(s, axis=-1)
    return jnp.einsum('bhqk,bkhd->bqhd', p.astype(v.dtype), v)


def na_latent(q, k, v, kc, vc, rpb):
    B, n, H, dh = q.shape
    R = n // GRID_W
    kh, kw = min(NA_ROWS, R), NA_COLS
    grid = lambda t: t.reshape(B, R, GRID_W, H, dh)
    kg, vg = grid(k), grid(v)
    cols = jnp.arange(GRID_W)
    cstart = jnp.clip(cols - kw // 2, 0, GRID_W - kw)
    col_ok = (cols[None, :] >= cstart[:, None]) & (cols[None, :] < cstart[:, None] + kw)
    mask = jnp.broadcast_to(col_ok[:, None, :], (GRID_W, kh, GRID_W)).reshape(GRID_W, kh * GRID_W)
    dc = jnp.clip(cols[None, :] - cols[:, None], -(kw - 1), kw - 1) + (kw - 1)
    rpb_c = rpb.astype(jnp.float32)[:, :, dc]
    scale = dh ** -0.5

    def row(args):
        r, qr = args
        rs = jnp.clip(r - kh // 2, 0, R - kh)
        kr = lax.dynamic_slice_in_dim(kg, rs, kh, axis=1).reshape(B, kh * GRID_W, H, dh)
        vr = lax.dynamic_slice_in_dim(vg, rs, kh, axis=1).reshape(B, kh * GRID_W, H, dh)
        dr = rs + jnp.arange(kh) - r + (NA_ROWS - 1)
        bias = jnp.transpose(rpb_c[:, dr], (0, 2, 1, 3)).reshape(H, GRID_W, kh * GRID_W)
        s_win = jnp.einsum('bqhd,bkhd->bhqk', qr, kr).astype(jnp.float32) * scale + bias[None]
        s_win = jnp.where(mask, s_win, -jnp.inf)
        s_ctx = jnp.einsum('bqhd,bkhd->bhqk', qr, kc).astype(jnp.float32) * scale
        p = jax.nn.softmax(jnp.concatenate([s_win, s_ctx], axis=-1), axis=-1).astype(v.dtype)
        nw = kh * GRID_W
        return (jnp.einsum('bhqk,bkhd->bqhd', p[..., :nw], vr)
                + jnp.einsum('bhqk,bkhd->bqhd', p[..., nw:], vc))

    o = lax.map(row, (jnp.arange(R), jnp.swapaxes(grid(q), 0, 1)))
    return jnp.swapaxes(o, 0, 1).reshape(B, n, H * dh)


def merge_branches(ys, gate_cols, proj, w_o):
    br = jnp.einsum('btmc,mcd->btmd', jnp.stack(ys, axis=2), proj)
    g = jax.nn.sigmoid(gate_cols.reshape(gate_cols.shape[0], gate_cols.shape[1], N_BRANCH, -1))
    return jnp.sum(g * br, axis=2) @ w_o


def moe_ffn(u, router_w, router_b, w1, w3, w2):
    n_tok, d = u.shape
    s = jax.nn.sigmoid((u @ router_w).astype(jnp.float32))
    sel = s + router_b.astype(jnp.float32)
    per = N_EXPERTS // N_GROUPS
    group_score = lax.top_k(sel.reshape(n_tok, N_GROUPS, per), TOP_K)[0].sum(-1)
    best_group = jnp.argmax(group_score, axis=-1)
    in_group = (jnp.arange(N_EXPERTS) // per)[None, :] == best_group[:, None]
    _, idx = lax.top_k(jnp.where(in_group, sel, -jnp.inf), TOP_K)
    wts = jnp.take_along_axis(s, idx, axis=-1)
    wts = wts / jnp.sum(wts, -1, keepdims=True)
    n_slot = n_tok * TOP_K
    e_flat = idx.reshape(-1).astype(jnp.int32)
    t_flat = jnp.repeat(jnp.arange(n_tok, dtype=jnp.int32), TOP_K)
    w_flat = wts.reshape(-1)
    order = jnp.argsort(e_flat)
    e_s, t_s, w_s = e_flat[order], t_flat[order], w_flat[order]
    counts = jax.ops.segment_sum(jnp.ones_like(e_flat), e_flat, num_segments=N_EXPERTS)
    padded = (counts + MOE_BLOCK - 1) // MOE_BLOCK * MOE_BLOCK
    start = jnp.cumsum(counts) - counts
    pend = jnp.cumsum(padded)
    pstart = pend - padded
    dest = pstart[e_s] + jnp.arange(n_slot, dtype=jnp.int32) - start[e_s]
    n_blk = -(-n_slot // MOE_BLOCK) + N_EXPERTS
    cap = n_blk * MOE_BLOCK
    buf_tok = jnp.full((cap,), n_tok, jnp.int32).at[dest].set(t_s)
    buf_w = jnp.zeros((cap,), jnp.float32).at[dest].set(w_s)
    blk_exp = jnp.minimum(jnp.searchsorted(pend, jnp.arange(n_blk, dtype=jnp.int32) * MOE_BLOCK, side='right'),
                          N_EXPERTS - 1)
    u_pad = jnp.concatenate([u, jnp.zeros((1, d), u.dtype)], axis=0)
    xb = u_pad[buf_tok].reshape(n_blk, MOE_BLOCK, d)

    def expert_block(args):
        xi, e = args
        return (jax.nn.silu(xi @ w1[e]) * (xi @ w3[e])) @ w2[e]

    yb = lax.map(expert_block, (xb, blk_exp)).reshape(cap, d)
    y = jax.ops.segment_sum(yb * buf_w[:, None].astype(yb.dtype), buf_tok, num_segments=n_tok + 1)
    return y[:n_tok]


def setup_inputs(seed: int = 0) -> dict:
    key = jax.random.key(seed)
    ks = iter(jax.random.split(key, 48))
    f32 = jnp.float32
    nrm = lambda shape, s: jax.random.normal(next(ks), shape, f32) * s
    D = D_MODEL
    max_decay = math.log(HY_TARGET) / HY_FAST_DECAY
    min_decay = math.log(HY_TARGET) / HY_SLOW_DECAY
    base_decay = jnp.linspace(min_decay, max_decay, HY_W, dtype=f32)
    dt = jnp.exp(jax.random.uniform(next(ks), (DEPTH, 2, GDN_H), f32, math.log(1e-3), math.log(1e-1)))
    return {
        'x': nrm((BATCH, SEQ, D), 1.0),
        'c': nrm((BATCH, D), 1.0),
        'ctx': nrm((BATCH, CTX_LEN, D), 1.0),
        'c_ctx': nrm((D,), 1.0),
        'w_mod': nrm((DEPTH, D, 6 * D), D ** -0.5),
        'b_mod': nrm((DEPTH, 6 * D), 0.02),
        'w_in': nrm((DEPTH, D, IN_TOTAL), D ** -0.5),
        'hy_conv': nrm((DEPTH, 3 * HY_W, HY_SHORT), HY_SHORT ** -0.5),
        'hy_w1': nrm((DEPTH, HY_EMB, HY_HID), HY_EMB ** -0.5),
        'hy_b1': nrm((DEPTH, HY_HID), 0.02),
        'hy_w2': nrm((DEPTH, HY_HID, HY_HID), HY_HID ** -0.5),
        'hy_b2': nrm((DEPTH, HY_HID), 0.02),
        'hy_w3': nrm((DEPTH, HY_HID, 2 * HY_ORDER * HY_W), HY_HID ** -0.5),
        'hy_freq': 1.0 + nrm((DEPTH, HY_HID), 0.02),
        'hy_deltas': base_decay * (1.0 + nrm((DEPTH, 2, HY_ORDER, HY_W), 0.02)),
        'hy_skip': nrm((DEPTH, HY_ORDER, HY_W), 0.5),
        'gdn_conv': nrm((DEPTH, 3 * MIX_W, GDN_CONV), GDN_CONV ** -0.5),
        'gdn_a_log': jnp.log(jax.random.uniform(next(ks), (DEPTH, 2, GDN_H), f32, 1.0, 16.0)),
        'gdn_dt_bias': dt + jnp.log(-jnp.expm1(-dt)),
        'gdn_norm': 1.0 + nrm((DEPTH, HEAD_DIM), 0.02),
        'diff_lam': nrm((DEPTH, 4, HEAD_DIM), 0.1),
        'diff_norm': 1.0 + nrm((DEPTH, DIFF_VD), 0.02),
        'na_rpb': nrm((DEPTH, NA_H, 2 * NA_ROWS - 1, 2 * NA_COLS - 1), 0.02),
        'branch_proj': nrm((DEPTH, N_BRANCH, MIX_W, D), MIX_W ** -0.5 * DN_BETA),
        'w_out': nrm((DEPTH, D, D), D ** -0.5 * DN_BETA),
        'ln_g': 1.0 + nrm((DEPTH, 2, D), 0.02),
        'ln_b': nrm((DEPTH, 2, D), 0.02),
        'router_w': nrm((D, N_EXPERTS), D ** -0.5),
        'router_b': nrm((N_EXPERTS,), 0.01),
        'exp_w1': nrm((DEPTH, N_EXPERTS, D, EXPERT_FF), D ** -0.5),
        'exp_w3': nrm((DEPTH, N_EXPERTS, D, EXPERT_FF), D ** -0.5),
        'exp_w2': nrm((DEPTH, N_EXPERTS, EXPERT_FF, D), EXPERT_FF ** -0.5 * DN_BETA),
    }


def reference(x, c, ctx, c_ctx, w_mod, b_mod, w_in, hy_conv, hy_w1, hy_b1, hy_w2, hy_b2, hy_w3, hy_freq,
              hy_deltas, hy_skip, gdn_conv, gdn_a_log, gdn_dt_bias, gdn_norm, diff_lam, diff_norm, na_rpb,
              branch_proj, w_out, ln_g, ln_b, router_w, router_b, exp_w1, exp_w3, exp_w2):
    B, n, D = x.shape
    m = ctx.shape[1]
    ang_r, ang_c = axial_rope_angles(n)
    zero_state = jnp.zeros((B, GDN_H, HEAD_DIM, HEAD_DIM), jnp.float32)
    xc = ctx
    for l in range(DEPTH):
        ctx_out = l < DEPTH - 1
        mx = jnp.split((jax.nn.silu(c) @ w_mod[l] + b_mod[l])[:, None, :], 6, axis=-1)
        mc = jnp.split((jax.nn.silu(c_ctx) @ w_mod[l] + b_mod[l])[None, None, :], 6, axis=-1)

        hx = modulate(x, mx[0], mx[1])
        hc = modulate(xc, mc[0], mc[1])
        hy_x, gqkv_x, gz_x, ga_x, gb_x, dqkv_x, nqkv_x, gate_x = jnp.split(hx @ w_in[l], SPLIT_IDX, axis=-1)
        hy_c, gqkv_c, gz_c, ga_c, gb_c, dqkv_c, nqkv_c, gate_c = jnp.split(hc @ w_in[l], SPLIT_IDX, axis=-1)
        filt = (hy_w1[l], hy_b1[l], hy_w2[l], hy_b2[l], hy_w3[l], hy_freq[l], hy_deltas[l])

        ya_x = hyena_mix(hy_x, hy_conv[l], hy_skip[l], hyena_filters(n, *filt))

        gdn_p = (gdn_conv[l], gdn_a_log[l], gdn_dt_bias[l], gdn_norm[l])
        yb_c, st_c = gdn_mix(gqkv_c, gz_c, ga_c, gb_c, *gdn_p, (zero_state, zero_state), ctx_out)
        yb_x, _ = gdn_mix(gqkv_x, gz_x, ga_x, gb_x, *gdn_p, st_c, True)

        lq1, lk1, lq2, lk2 = diff_lam[l].astype(jnp.float32)
        lam_init = 0.8 - 0.6 * math.exp(-0.3 * l)
        lam = jnp.exp(jnp.sum(lq1 * lk1)) - jnp.exp(jnp.sum(lq2 * lk2)) + lam_init
        dq_x, dk_x, dv_x = split_diff(dqkv_x)
        dq_c, dk_c, dv_c = split_diff(dqkv_c)
        yc_x = diff_finish(diff_latent(dq_x, dk_x, dv_x, dk_c, dv_c, lam, ang_r, ang_c), diff_norm[l], lam_init)

        nq_x, nk_x, nv_x = [t.reshape(B, n, NA_H, HEAD_DIM) for t in jnp.split(nqkv_x, 3, axis=-1)]
        nq_c, nk_c, nv_c = [t.reshape(B, m, NA_H, HEAD_DIM) for t in jnp.split(nqkv_c, 3, axis=-1)]
        yd_x = na_latent(nq_x, nk_x, nv_x, nk_c, nv_c, na_rpb[l])

        mix_x = merge_branches([ya_x, yb_x, yc_x, yd_x], gate_x, branch_proj[l], w_out[l])
        x = layer_norm(DN_ALPHA * x + mx[2] * mix_x, ln_g[l, 0], ln_b[l, 0])
        if ctx_out:
            ya_c = hyena_mix(hy_c, hy_conv[l], hy_skip[l], hyena_filters(m, *filt))
            yc_c = diff_finish(diff_attend(dq_c, dk_c, dv_c, lam), diff_norm[l], lam_init)
            yd_c = dense_attend(nq_c, nk_c, nv_c).reshape(B, m, NA_H * HEAD_DIM)
            mix_c = merge_branches([ya_c, yb_c, yc_c, yd_c], gate_c, branch_proj[l], w_out[l])
            xc = layer_norm(DN_ALPHA * xc + mc[2] * mix_c, ln_g[l, 0], ln_b[l, 0])

        h2x = modulate(x, mx[3], mx[4]).reshape(B * n, D)
        if ctx_out:
            h2c = modulate(xc, mc[3], mc[4]).reshape(B * m, D)
            y_all = moe_ffn(jnp.concatenate([h2x, h2c], axis=0), router_w, router_b, exp_w1[l], exp_w3[l], exp_w2[l])
            y_x = y_all[:B * n].reshape(B, n, D)
            y_c = y_all[B * n:].reshape(B, m, D)
            xc = layer_norm(DN_ALPHA * xc + mc[5] * y_c, ln_g[l, 1], ln_b[l, 1])
        else:
            y_x = moe_ffn(h2x, router_w, router_b, exp_w1[l], exp_w3[l], exp_w2[l]).reshape(B, n, D)
        x = layer_norm(DN_ALPHA * x + mx[5] * y_x, ln_g[l, 1], ln_b[l, 1])
    return x
```

```python
import math
from contextlib import ExitStack
import numpy as np
import concourse.bass as bass
import concourse.mybir as mybir
from concourse.bass_utils import run_bass_kernel_spmd

F32 = mybir.dt.float32
BF16 = mybir.dt.bfloat16
AF = mybir.ActivationFunctionType
ALU = mybir.AluOpType
AX = mybir.AxisListType

D = 1024
NL = 8192
NC = 256
NTOK = NL + NC
DEPTH = 4
GRID_W = 64
LN_EPS = 1e-5
RMS_EPS = 1e-6
DN_ALPHA = (2 * DEPTH) ** 0.25
QUADS = [[0, 1, 2, 3], [4, 5, 6, 7]]
NWG = 13 * 128 + 8


class T:
    __slots__ = ("h", "w", "r", "name", "excl")

    def __init__(self, h, name=None, excl=False):
        self.h = h; self.w = None; self.r = {}; self.name = name; self.excl = excl

    def __getitem__(self, idx):
        return self.h[idx]


class KB:
    def __init__(self, nc, n_lanes=24):
        self.nc = nc
        self.eng = {"pe": nc.tensor, "act": nc.scalar, "dve": nc.vector, "pool": nc.gpsimd, "sp": nc.sync}
        self.sem = {k: nc.alloc_semaphore("s_" + k) for k in self.eng}
        self.cnt = {k: 0 for k in self.eng}
        self.lanes = [nc.alloc_semaphore(f"lane{i}") for i in range(n_lanes)]
        self.lane_cnt = [0] * n_lanes
        self.lane_rr = 0
        self.cc = nc.alloc_semaphore("cc")
        self.cc_cnt = 0
        self.waited = {}
        self.nid = 0
        self.dq = 0
        self.stack = None

    def sb(self, shape, dtype, name="sb"):
        self.nid += 1
        nm = f"{name}_{self.nid}"
        if self.stack is not None:
            return T(self.stack.enter_context(self.nc.sbuf_tensor(nm, list(shape), dtype)), nm)
        return T(self.nc.alloc_sbuf_tensor(nm, list(shape), dtype), nm)

    def barrier(self):
        targets = [(self.sem[k], self.cnt[k]) for k in self.eng]
        targets += [(self.lanes[i], 16 * c) for i, c in enumerate(self.lane_cnt)]
        targets.append((self.cc, self.cc_cnt))
        for e in self.eng:
            for sm, v in targets:
                if v:
                    self._wait(e, sm, v)

    def scope(self):
        kb = self

        class _S:
            def __enter__(s2):
                s2.prev = kb.stack
                kb.stack = ExitStack()
                kb.stack.__enter__()
                return s2

            def __exit__(s2, *a):
                kb.barrier()
                kb.stack.__exit__(None, None, None)
                kb.stack = s2.prev
                return False
        return _S()

    def ps(self, shape, dtype=F32, name="ps"):
        self.nid += 1
        nm = f"{name}_{self.nid}"
        return T(self.nc.alloc_psum_tensor(nm, list(shape), dtype), nm, excl=True)

    def dram(self, shape, dtype, name="dr", kind="Internal"):
        self.nid += 1
        nm = name if kind != "Internal" else f"{name}_{self.nid}"
        return T(self.nc.dram_tensor(nm, list(shape), dtype, kind=kind), nm)

    def _wait(self, e, sem, val):
        key = (e, id(sem))
        if self.waited.get(key, 0) >= val:
            return
        self.waited[key] = val
        self.eng[e].wait_ge(sem, val)

    def _deps(self, e, reads, writes, skip_same=False):
        deps = {}

        def add(d):
            if d is None:
                return
            s, v = d
            if deps.get(id(s), (None, 0))[1] < v:
                deps[id(s)] = (s, v)
        for t in reads:
            add(t.w)
            if t.excl:
                for s_v in t.r.values():
                    add(s_v)
        for t in writes:
            add(t.w)
            for s_v in t.r.values():
                add(s_v)
        for s, v in deps.values():
            if skip_same and s is self.sem.get(e):
                continue
            self._wait(e, s, v)

    def _mark(self, me, reads, writes):
        s, v = me
        for t in reads:
            t.r[id(s)] = me
        for t in writes:
            t.w = me; t.r = {}

    def op(self, e, fn, reads=(), writes=()):
        self._deps(e, reads, writes, skip_same=(e == "pe"))
        ins = fn(self.eng[e])
        self.cnt[e] += 1
        ins.then_inc(self.sem[e], 1)
        self._mark((self.sem[e], self.cnt[e]), reads, writes)
        return ins

    def dma(self, out, in_, reads=(), writes=(), q=None, **kw):
        if q is None:
            q = ("sp", "act", "pool")[self.dq % 2]
            self.dq += 1
        self._deps(q, reads, writes)
        li = self.lane_rr; self.lane_rr = (self.lane_rr + 1) % len(self.lanes)
        ls = self.lanes[li]
        if self.lane_cnt[li]:
            self._wait(q, ls, 16 * self.lane_cnt[li])
        ins = self.eng[q].dma_start(out=out, in_=in_, **kw)
        self.lane_cnt[li] += 1
        ins.then_inc(ls, 16)
        self._mark((ls, 16 * self.lane_cnt[li]), reads, writes)
        return ins

    def allgather(self, src, dst, groups):
        self._deps("pool", [src], [dst])
        ins = self.nc.gpsimd.collective_compute("AllGather", ALU.bypass, replica_groups=groups,
                                                ins=[src.h.ap().opt()], outs=[dst.h.ap().opt()])
        self.cc_cnt += 1
        ins.then_inc(self.cc)
        self._mark((self.cc, self.cc_cnt), [src], [dst])

    def finish(self, tiles, e="sp"):
        for t in tiles:
            if t.w is not None:
                self._wait(e, t.w[0], t.w[1])

    def mm(self, out_t, out_ap, lhsT_t, lhsT_ap, rhs_t, rhs_ap, start=True, stop=True, skip=False):
        return self.op("pe", lambda e: e.matmul(out_ap, lhsT_ap, rhs_ap, start=start, stop=stop, skip_group_check=skip),
                       reads=[lhsT_t, rhs_t], writes=[out_t])

    def act(self, out_t, out_ap, in_t, in_ap, func, extra=(), **kw):
        return self.op("act", lambda e: e.activation(out=out_ap, in_=in_ap, func=func, **kw),
                       reads=[in_t, *extra], writes=[out_t])

    def tt(self, out_t, out_ap, a_t, a_ap, b_t, b_ap, op, e="dve"):
        return self.op(e, lambda en: en.tensor_tensor(out=out_ap, in0=a_ap, in1=b_ap, op=op),
                       reads=[a_t, b_t], writes=[out_t])

    def ts(self, out_t, out_ap, a_t, a_ap, s1, s2, op0, op1=None, extra=(), e="dve"):
        if op1 is None:
            return self.op(e, lambda en: en.tensor_scalar(out=out_ap, in0=a_ap, scalar1=s1, scalar2=None, op0=op0),
                           reads=[a_t, *extra], writes=[out_t])
        return self.op(e, lambda en: en.tensor_scalar(out=out_ap, in0=a_ap, scalar1=s1, scalar2=s2, op0=op0, op1=op1),
                       reads=[a_t, *extra], writes=[out_t])

    def stt(self, out_t, out_ap, a_t, a_ap, s, b_t, b_ap, op0, op1, extra=()):
        return self.op("dve", lambda en: en.scalar_tensor_tensor(out=out_ap, in0=a_ap, scalar=s, in1=b_ap, op0=op0, op1=op1),
                       reads=[a_t, b_t, *extra], writes=[out_t])

    def copy(self, out_t, out_ap, in_t, in_ap, e="dve"):
        if e == "act":
            return self.op("act", lambda en: en.copy(out=out_ap, in_=in_ap), reads=[in_t], writes=[out_t])
        return self.op(e, lambda en: en.tensor_copy(out=out_ap, in_=in_ap), reads=[in_t], writes=[out_t])

    def memset(self, t, ap, val, e="pool"):
        return self.op(e, lambda en: en.memset(ap, val), reads=[], writes=[t])


def bcast_ap(t, n, offset=0, parts=128):
    return bass.AP(tensor=t.h, offset=offset, ap=[[0, parts], [1, n]])


CHUNK = 2048


class CD:
    def __init__(self, kb, rows, ntok, dtype, name, chunk=CHUNK):
        self.ch = [(c0, cn, kb.dram([rows, cn], dtype, f"{name}{i}")) for i, (c0, cn) in enumerate(groups_of(ntok, chunk))]

    def cols(self, s, n):
        for c0, cn, t in self.ch:
            if c0 <= s and s + n <= c0 + cn:
                return t, t[:, s - c0:s - c0 + n]
        raise ValueError((s, n))


def groups_of(n, g):
    return [(i, min(g, n - i)) for i in range(0, n, g)]


class Prog:
    def __init__(self, nl=NL, depth=DEPTH, debug=None):
        self.nl = nl; self.ntok = nl + NC; self.depth = depth
        self.debug = debug or []
        self.nc = bass.Bass("TRN2", target_bir_lowering=False)
        self.kb = KB(self.nc)
        self.inputs = {}
        self.outs = {}

    def inp(self, name, shape, dtype=F32):
        if name in self.inputs:
            return self.inputs[name]
        t = self.kb.dram(shape, dtype, name, kind="ExternalInput")
        self.inputs[name] = t
        return t

    def out(self, name, shape, dtype=F32):
        t = self.kb.dram(shape, dtype, name, kind="ExternalOutput")
        self.outs[name] = t
        return t

    def tgroups(self, gs=512):
        res = [(s, n, False) for s, n in groups_of(self.nl, gs)]
        res += [(self.nl + s, n, True) for s, n in groups_of(NC, gs)]
        return res

    def setup_consts(self):
        kb = self.kb
        self.ones_col = kb.sb([128, 1], F32, "ones_col")
        kb.memset(self.ones_col, self.ones_col[:], 1.0)
        self.ones_row = kb.sb([1, 128], F32, "ones_row")
        kb.memset(self.ones_row, self.ones_row[:], 1.0)
        sel = self.inp("c_sel8", [8, 2])
        self.sel8 = kb.sb([8, 2], F32, "sel8")
        kb.dma(self.sel8[:], sel[:], reads=[sel], writes=[self.sel8])
        self.bank = [kb.ps([128, 512], F32, f"bank{i}") for i in range(8)]

    def phase_mod(self, l):
        kb = self.kb
        wm = self.inp(f"wmod{l}", [128, 8, 12 * 128])
        bm = self.inp(f"bmod{l}", [128, 12])
        modv = self.modv[l]
        bsb = self.bm_stage
        kb.dma(bsb[:], bm[:], reads=[bm], writes=[bsb])
        ps = self.bank[0]
        for k in range(8):
            wt = self.wm_stage[k % 2]
            kb.dma(wt[:], wm[:, k, :], reads=[wm], writes=[wt])
            for c in range(12):
                kb.mm(ps, ps[:, 2 * c:2 * c + 2], wt, wt[:, c * 128:(c + 1) * 128], self.csil, self.csil[:, k, :],
                      start=(k == 0 and c == 0), stop=(k == 7), skip=True)
        for v in range(2):
            kb.tt(modv, modv[:, :, v], ps, ps[:, 0:24].rearrange("p (c v) -> p c v", v=2)[:, :, v], bsb, bsb[:], ALU.add)
        for which in (1, 4):
            kb.ts(modv, modv[:, 2 * which:2 * which + 2, :], modv, modv[:, 2 * which:2 * which + 2, :], 1.0, None, ALU.add)
        return modv

    def setup_mod(self):
        kb = self.kb
        cT = self.inp("cT", [128, 8, 2])
        self.csil = kb.sb([128, 8, 2], F32, "csil")
        craw = kb.sb([128, 8, 2], F32, "craw")
        kb.dma(craw[:], cT[:], reads=[cT], writes=[craw])
        kb.act(self.csil, self.csil[:], craw, craw[:], AF.Silu)
        self.modv = [kb.sb([128, 12, 2], F32, f"modv{l}") for l in range(self.depth)]
        with kb.scope():
            self.wm_stage = [kb.sb([128, 12 * 128], F32, f"wmst{i}") for i in range(2)]
            self.bm_stage = kb.sb([128, 12], F32, "bmst")
            for l in range(self.depth):
                self.phase_mod(l)

    def ln_alloc(self):
        kb = self.kb
        self.st_in = kb.dram([2, self.ntok], F32, "st_in")
        self.st_all = kb.dram([8, self.ntok], F32, "st_all")
        self.ln_sq = kb.sb([128, 512], F32, "ln_sq")
        self.ln_st = kb.sb([1, 2, 512], F32, "ln_st")
        self.ln_sa = kb.sb([8, 512], F32, "ln_sa")
        self.ln_v = [kb.sb([1, 512], F32, f"ln_v{i}") for i in range(4)]

    def ln_stats(self, xt, s, n):
        kb = self.kb
        p1, p2 = self.bank[6], self.bank[7]
        for j in range(2):
            kb.mm(p1, p1[0:1, :n], self.ones_col, self.ones_col[:], xt, xt[:, j, :n], start=(j == 0), stop=(j == 1))
        for j in range(2):
            kb.act(self.ln_sq, self.ln_sq[:, :n], xt, xt[:, j, :n], AF.Square)
            kb.mm(p2, p2[0:1, :n], self.ones_col, self.ones_col[:], self.ln_sq, self.ln_sq[:, :n], start=(j == 0), stop=(j == 1))
        kb.copy(self.ln_st, self.ln_st[:, 0, :n], p1, p1[0:1, :n])
        kb.copy(self.ln_st, self.ln_st[:, 1, :n], p2, p2[0:1, :n], e="act")
        kb.dma(self.st_in[:, s:s + n].rearrange("(o r) n -> o r n", o=1), self.ln_st[:, :, :n], reads=[self.ln_st], writes=[self.st_in])

    def ln_gather(self):
        self.kb.allgather(self.st_in, self.st_all, QUADS)

    def ln_coef(self, s, n):
        kb = self.kb
        p1, p2 = self.bank[6], self.bank[7]
        kb.dma(self.ln_sa[:, :n], self.st_all[:, s:s + n], reads=[self.st_all], writes=[self.ln_sa])
        kb.mm(p1, p1[0:1, :n], self.sel8, self.sel8[:, 0:1], self.ln_sa, self.ln_sa[:, :n])
        kb.mm(p2, p2[0:1, :n], self.sel8, self.sel8[:, 1:2], self.ln_sa, self.ln_sa[:, :n])
        m, v, r, q = self.ln_v
        kb.ts(m, m[:, :n], p1, p1[0:1, :n], 1.0 / D, None, ALU.mult)
        kb.tt(v, v[:, :n], m, m[:, :n], m, m[:, :n], ALU.mult)
        kb.stt(v, v[:, :n], p2, p2[0:1, :n], 1.0 / D, v, v[:, :n], ALU.mult, ALU.subtract)
        kb.ts(v, v[:, :n], v, v[:, :n], LN_EPS, None, ALU.add)
        kb.act(v, v[:, :n], v, v[:, :n], AF.Sqrt)
        kb.op('dve', lambda en: en.reciprocal(out=r[:, :n], in_=v[:, :n]), reads=[v], writes=[r])
        kb.stt(q, q[:, :n], m, m[:, :n], -1.0, r, r[:, :n], ALU.mult, ALU.mult)
        kb.mm(p1, p1[:, :n], self.ones_row, self.ones_row[:], r, r[:, :n])
        kb.mm(p2, p2[:, :n], self.ones_row, self.ones_row[:], q, q[:, :n])
        return p1, p2

    def phase_A(self, l, xT, which_shift, which_scale, hT_my, hT_all, h32=None):
        kb = self.kb
        modv = self.modv[l]
        for (s, n, isc) in self.tgroups():
            xt = self.xa[0]
            kb.dma(xt[:, :, :n], xT[:, :, s:s + n].rearrange("j p n -> p j n"), reads=[xT], writes=[xt])
            self.ln_stats(xt, s, n)
        self.ln_gather()
        for gi, (s, n, isc) in enumerate(self.tgroups()):
            xt = self.xa[gi % 2]
            kb.dma(xt[:, :, :n], xT[:, :, s:s + n].rearrange("j p n -> p j n"), reads=[xT], writes=[xt])
            p1, p2 = self.ln_coef(s, n)
            hb = self.hb[gi % 2]
            v = 1 if isc else 0
            for j in range(2):
                tmp = self.xtmp
                kb.tt(tmp, tmp[:, :n], xt, xt[:, j, :n], p1, p1[:, :n], ALU.mult)
                kb.tt(tmp, tmp[:, :n], tmp, tmp[:, :n], p2, p2[:, :n], ALU.add)
                kb.act(hb, hb[:, j, :n], tmp, tmp[:, :n], AF.Identity, extra=[modv],
                       scale=modv[:, 2 * which_scale + j, v:v + 1], bias=modv[:, 2 * which_shift + j, v:v + 1])
                if h32 is not None:
                    kb.act(h32[1], h32[1][:, j, :n], tmp, tmp[:, :n], AF.Identity, extra=[modv],
                           scale=modv[:, 2 * which_scale + j, v:v + 1], bias=modv[:, 2 * which_shift + j, v:v + 1])
            ht, hap = hT_my.cols(s, n)
            kb.dma(hap.rearrange("(j p) n -> p j n", p=128), hb[:, :, :n], reads=[hb], writes=[ht])
            if h32 is not None:
                kb.dma(h32[0][:, :, s:s + n].rearrange("j p n -> p j n"), h32[1][:, :, :n], reads=[h32[1]], writes=[h32[0]])
        for (c0, cn, tm), (_, _, ta) in zip(hT_my.ch, hT_all.ch):
            kb.allgather(tm, ta, QUADS)

    def alloc_A(self):
        kb = self.kb
        self.xa = [kb.sb([128, 2, 512], F32, f"xa{i}") for i in range(2)]
        self.hb = [kb.sb([128, 2, 512], BF16, f"hb{i}") for i in range(2)]
        self.xtmp = kb.sb([128, 512], F32, "xtmp")
        self.hT_my = CD(kb, 256, self.ntok, BF16, "hT_my")
        self.hT_all = CD(kb, 1024, self.ntok, BF16, "hT_all")


    def alloc_mix(self):
        kb = self.kb; nt = self.ntok
        self.HY = kb.dram([3, 128, nt], F32, "HY")
        self.GP = kb.dram([3, 128, nt], F32, "GP")
        self.GZ = kb.dram([nt, 128], F32, "GZ")
        self.GAB = kb.dram([nt, 8], F32, "GAB")
        self.DQ = kb.dram([128, nt], BF16, "DQ"); self.DK = kb.dram([128, nt], BF16, "DK")
        self.DV = kb.dram([nt, 128], BF16, "DV")
        self.NQ = kb.dram([128, nt], BF16, "NQ"); self.NK = kb.dram([128, nt], BF16, "NK")
        self.NV = kb.dram([nt, 128], BF16, "NV")
        self.YT_my = CD(kb, 512, nt, BF16, "YT_my", chunk=1024)
        self.YT_allc = CD(kb, 2048, nt, BF16, "YT_all", chunk=1024)
        self.cosT = self.inp("c_cosT", [128, nt]); self.sinT = self.inp("c_sinT", [128, nt])
        self.alloc_mix_ident()

    def alloc_mix_ident(self):
        kb = self.kb
        if hasattr(self, "ident"):
            return
        identb = self.inp("c_ident", [128, 128])
        self.ident = kb.sb([128, 128], F32, "ident")
        kb.dma(self.ident[:], identb[:], reads=[identb], writes=[self.ident])
        self.identb = kb.sb([128, 128], BF16, "identb")
        kb.copy(self.identb, self.identb[:], self.ident, self.ident[:])

    def phase_B(self, l):
        kb = self.kb
        wg = self.inp(f"wg{l}", [128, 8, NWG])
        with kb.scope():
            wgb = kb.sb([128, 8, NWG + 256], BF16, "wgb")
            stg = [kb.sb([128, NWG], F32, "wgst") for _ in range(2)]
            for k in range(8):
                st = stg[k % 2]
                kb.dma(st[:], wg[:, k, :], reads=[wg], writes=[st])
                kb.copy(wgb, wgb[:, k, 0:NWG], st, st[:], e=("dve", "pool")[k % 2])
            for k in range(8):
                for si, src in enumerate((7, 8)):
                    sv = wgb[:, k, src * 128:(src + 1) * 128].rearrange("p (a t s) -> p a t s", a=4, t=2, s=16)
                    dv = wgb[:, k, NWG + si * 128:NWG + (si + 1) * 128].rearrange("p (a t s) -> p a t s", a=4, t=2, s=16)
                    kb.ts(wgb, dv[:, :, 0, :], wgb, sv[:, :, 1, :], -1.0, None, ALU.mult)
                    kb.copy(wgb, dv[:, :, 1, :], wgb, sv[:, :, 0, :])
            hsb = [kb.sb([128, 8, 512], BF16, "hsb") for _ in range(2)]
            cst = kb.sb([128, 512], F32, "cst"); snt = kb.sb([128, 512], F32, "snt")
            evf = [kb.sb([128, 512], F32, "evf") for _ in range(2)]
            t1 = kb.sb([128, 512], F32, "t1"); t2 = kb.sb([128, 512], F32, "t2")
            ob = [kb.sb([128, 512], BF16, "ob") for _ in range(2)]
            tmz = [kb.sb([128, 128], F32, "tmz") for _ in range(2)]
            tmv = [kb.sb([128, 2, 128], BF16, "tmv") for _ in range(2)]
            tmg = [kb.sb([128, 8], F32, "tmg") for _ in range(2)]
            hT_all = self.hT_all
            import os
            bstop = int(os.environ.get("BSTOP", "9"))
            for gi, (s, n, isc) in enumerate(self.tgroups()):
                if bstop == 0:
                    break
                hs = hsb[gi % 2]
                ht, hap = hT_all.cols(s, n)
                kb.dma(hs[:, :, :n], hap.rearrange("(k p) n -> p k n", p=128), reads=[ht], writes=[hs])
                kb.dma(cst[:, :n], self.cosT[:, s:s + n], reads=[self.cosT], writes=[cst])
                kb.dma(snt[:, :n], self.sinT[:, s:s + n], reads=[self.sinT], writes=[snt])

                def fm(cb, bk):
                    for k in range(8):
                        kb.mm(bk, bk[:, :n], wgb, wgb[:, k, cb:cb + 128], hs, hs[:, k, :n], start=(k == 0), stop=(k == 7))
                for ci in range(6):
                    if bstop == 5:
                        break
                    bk = self.bank[ci % 4]; fm(ci * 128, bk)
                    ev = evf[ci % 2]
                    kb.copy(ev, ev[:, :n], bk, bk[:, :n], e=("dve", "act")[ci % 2])
                    dst = self.HY if ci < 3 else self.GP
                    kb.dma(dst[ci % 3, :, s:s + n], ev[:, :n], reads=[ev], writes=[dst])
                if bstop == 1:
                    continue
                for qi, (cb, rb, dst) in enumerate(()) if bstop == 5 else []:
                    pass
                for qi, (cb, rb, dst) in enumerate(((7 * 128, NWG, self.DQ), (8 * 128, NWG + 128, self.DK)) if bstop != 5 else ()):
                    b0, b1 = self.bank[0 + 2 * qi], self.bank[1 + 2 * qi]
                    fm(cb, b0); fm(rb, b1)
                    kb.tt(t1, t1[:, :n], b0, b0[:, :n], cst, cst[:, :n], ALU.mult)
                    kb.tt(t2, t2[:, :n], b1, b1[:, :n], snt, snt[:, :n], ALU.mult)
                    o = ob[qi]
                    kb.tt(o, o[:, :n], t1, t1[:, :n], t2, t2[:, :n], ALU.add, e="pool")
                    kb.dma(dst[:, s:s + n], o[:, :n], reads=[o], writes=[dst])
                if bstop == 2:
                    continue
                for qi, (cb, dst) in enumerate(((10 * 128, self.NQ), (11 * 128, self.NK)) if bstop != 5 else ()):
                    bk = self.bank[qi]; fm(cb, bk)
                    o = ob[qi]
                    kb.copy(o, o[:, :n], bk, bk[:, :n], e=("dve", "act")[qi])
                    kb.dma(dst[:, s:s + n], o[:, :n], reads=[o], writes=[dst])
                if bstop == 3:
                    continue
                for tt_ in range(n // 128):
                    bk = self.bank[int(os.environ.get("TMB", "4")) + tt_ % 2]
                    tsl = slice(tt_ * 128, (tt_ + 1) * 128)
                    for (cb, w, off) in ((6 * 128, 128, 0), (9 * 128, 128, 128), (12 * 128, 128, 256), (13 * 128, 8, 384))[:int(os.environ.get('TMN', '4'))]:
                        for k in range(8):
                            if os.environ.get("TMX", "") == "nomm":
                                break
                            kb.mm(bk, bk[:, off:off + w], hs, hs[:, k, tsl], wgb, wgb[:, k, cb:cb + w], start=(k == 0), stop=(k == 7))
                    if os.environ.get("TMX", "") == "nocopy":
                        continue
                    tok0 = s + tt_ * 128
                    z = tmz[tt_ % 2]; v = tmv[tt_ % 2]; g_ = tmg[tt_ % 2]
                    tmd = int(os.environ.get("TMD", "15"))
                    kb.copy(z, z[:], bk, bk[:, 0:128], e=os.environ.get("TME", "act"))
                    if tmd & 1:
                        kb.dma(self.GZ[tok0:tok0 + 128, :], z[:], reads=[z], writes=[self.GZ])
                    kb.copy(v, v[:, 0, :], bk, bk[:, 128:256])
                    kb.copy(v, v[:, 1, :], bk, bk[:, 256:384])
                    if tmd & 2:
                        kb.dma(self.DV[tok0:tok0 + 128, :], v[:, 0, :], reads=[v], writes=[self.DV])
                    if tmd & 4:
                        kb.dma(self.NV[tok0:tok0 + 128, :], v[:, 1, :], reads=[v], writes=[self.NV])
                    kb.copy(g_, g_[:], bk, bk[:, 384:392], e=os.environ.get("TME", "act"))
                    if tmd & 8:
                        kb.dma(self.GAB[tok0:tok0 + 128, :], g_[:], reads=[g_], writes=[self.GAB])

    def store_T(self, src_t, src_ap, nq, row0, tok0, bank, stage):
        kb = self.kb
        pb = bank
        pv = pb[:].bitcast(BF16)
        kb.op("pe", lambda e: e.transpose(pv[:, 0:nq], src_ap, self.identb[0:nq, 0:nq]), reads=[src_t, self.identb], writes=[pb])
        kb.copy(stage, stage[:, 0:nq], pb, pv[:, 0:nq])
        yt, yap = self.YT_my.cols(tok0, nq)
        kb.dma(yap[row0:row0 + 128, :], stage[:, 0:nq], reads=[stage], writes=[yt])

    def phase_diff(self, l, lam_init):
        kb = self.kb; nt = self.ntok; ntile = nt // 128
        dl = self.inp(f"dlam{l}", [4, 64]); dn = self.inp(f"dnorm{l}", [1, 128])
        with kb.scope():
            kT = kb.sb([128, nt], BF16, "kT")
            kb.dma(kT[:], self.DK[:], reads=[self.DK], writes=[kT])
            vA = kb.sb([128, ntile, 130], BF16, "vA")
            kb.memset(vA, vA[:, :, 128:130], 1.0)
            for t0, tn in groups_of(ntile, 8):
                kb.dma(vA[:, t0:t0 + tn, 0:128], self.DV[t0 * 128:(t0 + tn) * 128, :].rearrange("(t p) c -> p t c", p=128), reads=[self.DV], writes=[vA])
            lm = kb.sb([128, 4, 64], F32, "lm")
            kb.dma(lm[:].rearrange("p a b -> p (a b)"), bcast_ap(dl, 256), reads=[dl], writes=[lm])
            lp = kb.sb([128, 2, 64], F32, "lp"); ls = kb.sb([128, 4], F32, "ls")
            kb.tt(lp, lp[:, 0, :], lm, lm[:, 0, :], lm, lm[:, 1, :], ALU.mult)
            kb.tt(lp, lp[:, 1, :], lm, lm[:, 2, :], lm, lm[:, 3, :], ALU.mult)
            kb.op("dve", lambda e: e.tensor_reduce(out=ls[:, 0:2], in_=lp[:], axis=AX.X, op=ALU.add), reads=[lp], writes=[ls])
            kb.act(ls, ls[:, 0:2], ls, ls[:, 0:2], AF.Exp)
            kb.tt(ls, ls[:, 2:3], ls, ls[:, 0:1], ls, ls[:, 1:2], ALU.subtract)
            kb.ts(ls, ls[:, 3:4], ls, ls[:, 2:3], lam_init, -1.0, ALU.add, ALU.mult)
            nw = kb.sb([128, 128], F32, "nw")
            kb.dma(nw[:], bcast_ap(dn, 128), reads=[dn], writes=[nw])
            kb.ts(nw, nw[:], nw, nw[:], 1.0 - lam_init, None, ALU.mult)
            qsb = [kb.sb([128, 512], BF16, "qsb") for _ in range(2)]
            pT = [kb.sb([128, 512], BF16, "pT") for _ in range(4)]
            rr = kb.sb([128, 4], F32, "rr"); of = kb.sb([128, 128], F32, "of"); osq = kb.sb([128, 128], F32, "osq")
            ob16 = [kb.sb([128, 128], BF16, "ob16") for _ in range(2)]
            stg = [kb.sb([128, 128], BF16, "stg") for _ in range(2)]
            sc = 64 ** -0.5
            pi = 0
            import os
            dstop = int(os.environ.get("DSTOP", "9"))
            for gi, (s, n, isc) in enumerate(self.tgroups()):
                if dstop == 0:
                    break
                q = qsb[gi % 2]
                kb.dma(q[:, :n], self.DQ[:, s:s + n], reads=[self.DQ], writes=[q])
                ktiles = list(range(self.nl // 128, ntile)) if isc else list(range(ntile))
                nqt = n // 128
                first = {}
                for ki, kt in enumerate(ktiles):
                    for c in range(2):
                        sb_ = self.bank[4 + (pi % 4)]
                        kb.mm(sb_, sb_[:, :n], kT, kT[c * 64:(c + 1) * 64, kt * 128:(kt + 1) * 128], q, q[c * 64:(c + 1) * 64, :n])
                        p = pT[pi % 4]; pi += 1
                        kb.act(p, p[:, :n], sb_, sb_[:, :n], AF.Exp, scale=sc)
                        for qt in range(nqt):
                            ab = self.bank[c * 2 + qt // 2]; off = (qt % 2) * 160
                            st = id(ab) not in first
                            first[id(ab)] = 1
                            kb.mm(ab, ab[:, off:off + 129], p, p[:, qt * 128:(qt + 1) * 128], vA, vA[:, kt, 0:129],
                                  start=st, stop=(ki == len(ktiles) - 1), skip=True)
                for qt in range(nqt):
                    if dstop == 1:
                        break
                    a0 = self.bank[0 + qt // 2]; a1 = self.bank[2 + qt // 2]; off = (qt % 2) * 160
                    kb.op("dve", lambda e: e.reciprocal(out=rr[:, 0:1], in_=a0[:, off + 128:off + 129]), reads=[a0], writes=[rr])
                    kb.op("dve", lambda e: e.reciprocal(out=rr[:, 1:2], in_=a1[:, off + 128:off + 129]), reads=[a1], writes=[rr])
                    kb.tt(rr, rr[:, 1:2], rr, rr[:, 1:2], ls, ls[:, 3:4], ALU.mult)
                    kb.ts(of, of[:], a0, a0[:, off:off + 128], rr[:, 0:1], None, ALU.mult, extra=[rr])
                    kb.stt(of, of[:], a1, a1[:, off:off + 128], rr[:, 1:2], of, of[:], ALU.mult, ALU.add, extra=[rr])
                    kb.op("act", lambda e: e.activation(out=osq[:], in_=of[:], func=AF.Square, accum_out=rr[:, 2:3]), reads=[of], writes=[osq, rr])
                    kb.ts(rr, rr[:, 2:3], rr, rr[:, 2:3], 1.0 / 128, RMS_EPS, ALU.mult, ALU.add)
                    kb.act(rr, rr[:, 2:3], rr, rr[:, 2:3], AF.Sqrt)
                    kb.op("dve", lambda e: e.reciprocal(out=rr[:, 3:4], in_=rr[:, 2:3]), reads=[rr], writes=[rr])
                    o16 = ob16[qt % 2]
                    kb.stt(o16, o16[:], of, of[:], rr[:, 3:4], nw, nw[:], ALU.mult, ALU.mult, extra=[rr])
                    if dstop == 2:
                        continue
                    self.store_T(o16, o16[:], 128, 256, s + qt * 128, self.bank[4 + qt % 2], stg[qt % 2])


    def phase_na(self, l):
        kb = self.kb; nt = self.ntok; nl = self.nl; R = nl // 64
        nb = self.inp(f"nabias{l}", [64, 2 * 15 * 64])
        nm = self.inp("c_namask", [64, 64])
        sc = 64 ** -0.5
        with kb.scope():
            kT = kb.sb([128, nt], BF16, "nkT"); qT = kb.sb([128, nt], BF16, "nqT")
            kb.dma(kT[:], self.NK[:], reads=[self.NK], writes=[kT])
            kb.dma(qT[:], self.NQ[:], reads=[self.NQ], writes=[qT])
            v64 = kb.sb([64, R, 2, 65], BF16, "v64")
            kb.memset(v64, v64[:, :, :, 64:65], 1.0)
            for r0, rn in groups_of(R, 16):
                for hh in range(2):
                    kb.dma(v64[:, r0:r0 + rn, hh, 0:64],
                           self.NV[r0 * 64:(r0 + rn) * 64, hh * 64:(hh + 1) * 64].rearrange("(r p) c -> p r c", p=64),
                           reads=[self.NV], writes=[v64])
            vcx = kb.sb([128, 2, 2, 65], BF16, "vcx")
            kb.memset(vcx, vcx[:, :, :, 64:65], 1.0)
            for hh in range(2):
                kb.dma(vcx[:, :, hh, 0:64], self.NV[nl:nt, hh * 64:(hh + 1) * 64].rearrange("(t p) c -> p t c", p=128), reads=[self.NV], writes=[vcx])
            EB = kb.sb([64, 2, 15, 64], F32, "EB"); msk = kb.sb([64, 64], F32, "msk")
            kb.dma(EB[:].rearrange("p a b c -> p (a b c)"), nb[:], reads=[nb], writes=[EB])
            kb.dma(msk[:], nm[:], reads=[nm], writes=[msk])
            kb.act(EB, EB[:].rearrange("p a b c -> p (a b c)"), EB, EB[:].rearrange("p a b c -> p (a b c)"), AF.Exp)
            for hh in range(2):
                for dr in range(15):
                    kb.tt(EB, EB[:, hh, dr, :], EB, EB[:, hh, dr, :], msk, msk[:], ALU.mult)
            pwf = [kb.sb([64, 512], F32, "pwf") for _ in range(2)]
            pwb = [kb.sb([64, 512], BF16, "pwb") for _ in range(2)]
            pcb = [kb.sb([128, 256], BF16, "pcb") for _ in range(2)]
            rr = kb.sb([128, 4], F32, "nrr")
            o16 = [kb.sb([128, 128], BF16, "no16") for _ in range(2)]
            stg = [kb.sb([128, 128], BF16, "nstg") for _ in range(2)]
            it = 0
            for r in range(R):
                rs = min(max(r - 4, 0), R - 8)
                dr0 = rs - r + 7
                o = o16[r % 2]
                for hh in range(2):
                    hp = slice(hh * 64, (hh + 1) * 64)
                    sbk = self.bank[4 + it % 2]; sck = self.bank[6 + it % 2]; ab = self.bank[it % 2]
                    qv = qT[hp, r * 64:(r + 1) * 64]
                    for j in range(8):
                        kb.mm(sbk, sbk[0:64, j * 64:(j + 1) * 64], kT, kT[hp, (rs + j) * 64:(rs + j + 1) * 64], qT, qv)
                    for t in range(2):
                        kb.mm(sck, sck[:, t * 64:(t + 1) * 64], kT, kT[hp, nl + t * 128:nl + (t + 1) * 128], qT, qv)
                    pf = pwf[it % 2]; pb = pwb[it % 2]; pc = pcb[it % 2]
                    kb.act(pf, pf[:], sbk, sbk[0:64, :], AF.Exp, scale=sc)
                    kb.tt(pb, pb[:], pf, pf[:], EB, EB[:, hh, dr0:dr0 + 8, :].rearrange("p a b -> p (a b)"), ALU.mult)
                    kb.act(pc, pc[:, 0:128], sck, sck[:, 0:128], AF.Exp, scale=sc)
                    for j in range(8):
                        kb.mm(ab, ab[0:64, 0:65], pb, pb[:, j * 64:(j + 1) * 64], v64, v64[:, rs + j, hh, :], start=(j == 0), stop=False)
                    for t in range(2):
                        kb.mm(ab, ab[0:64, 0:65], pc, pc[:, t * 64:(t + 1) * 64], vcx, vcx[:, t, hh, :], start=False, stop=(t == 1))
                    kb.op("dve", lambda e: e.reciprocal(out=rr[0:64, hh:hh + 1], in_=ab[0:64, 64:65]), reads=[ab], writes=[rr])
                    kb.ts(o, o[0:64, hp], ab, ab[0:64, 0:64], rr[0:64, hh:hh + 1], None, ALU.mult, extra=[rr])
                    it += 1
                self.store_T(o, o[0:64, :], 64, 384, r * 64, self.bank[2 + r % 2], stg[r % 2])
            for qt in range(2):
                o = o16[qt]
                for hh in range(2):
                    hp = slice(hh * 64, (hh + 1) * 64)
                    sck = self.bank[6 + it % 2]; ab = self.bank[it % 2]; pc = pcb[it % 2]
                    for t in range(2):
                        kb.mm(sck, sck[:, t * 128:(t + 1) * 128], kT, kT[hp, nl + t * 128:nl + (t + 1) * 128],
                              qT, qT[hp, nl + qt * 128:nl + (qt + 1) * 128])
                    kb.act(pc, pc[:, 0:256], sck, sck[:, 0:256], AF.Exp, scale=sc)
                    for t in range(2):
                        kb.mm(ab, ab[:, 0:65], pc, pc[:, t * 128:(t + 1) * 128], vcx, vcx[:, t, hh, :], start=(t == 0), stop=(t == 1))
                    kb.op("dve", lambda e: e.reciprocal(out=rr[:, hh:hh + 1], in_=ab[:, 64:65]), reads=[ab], writes=[rr])
                    kb.ts(o, o[:, hp], ab, ab[:, 0:64], rr[:, hh:hh + 1], None, ALU.mult, extra=[rr])
                    it += 1
                self.store_T(o, o[:], 128, 384, nl + qt * 128, self.bank[2 + qt], stg[qt])


    def alloc_tail(self):
        kb = self.kb; nt = self.ntok
        if not hasattr(self, "YT_allc"):
            self.YT_allc = CD(kb, 2048, nt, BF16, "YT_all", chunk=1024)
        self.mT_my = CD(kb, 256, nt, BF16, "mT_my"); self.mT_all = CD(kb, 1024, nt, BF16, "mT_all")
        self.preT = kb.dram([2, 128, nt], F32, "preT")
        self.x1T = kb.dram([2, 128, nt], F32, "x1T")

    def phase_merge(self, l, xT):
        kb = self.kb
        wgi = self.inp(f"wgate{l}", [128, 8, 1024]); bpi = self.inp(f"bp{l}", [128, 16, 256])
        woi = self.inp(f"wout{l}", [128, 8, 256]); lnp = self.inp(f"lnp{l}", [128, 2, 4])
        modv = self.modv[l]
        with kb.scope():
            wgt = kb.sb([128, 8, 1024], BF16, "wgt"); bpt = kb.sb([128, 16, 256], BF16, "bpt")
            wot = kb.sb([128, 8, 256], BF16, "wot"); lnt = kb.sb([128, 2, 4], F32, "lnt")
            stg = [kb.sb([128, 1024], F32, "mstg") for _ in range(2)]
            si = 0
            for k in range(8):
                st = stg[si % 2]; si += 1
                kb.dma(st[:], wgi[:, k, :], reads=[wgi], writes=[st])
                kb.copy(wgt, wgt[:, k, :], st, st[:], e=("dve", "pool")[k % 2])
            for q4 in range(4):
                st = stg[si % 2]; si += 1
                kb.dma(st[:].rearrange("p (a b) -> p a b", a=4), bpi[:, 4 * q4:4 * q4 + 4, :], reads=[bpi], writes=[st])
                kb.copy(bpt, bpt[:, 4 * q4:4 * q4 + 4, :].rearrange("p a b -> p (a b)"), st, st[:], e=("dve", "pool")[q4 % 2])
            for k2 in range(2):
                st = stg[si % 2]; si += 1
                kb.dma(st[:].rearrange("p (a b) -> p a b", a=4), woi[:, 4 * k2:4 * k2 + 4, :], reads=[woi], writes=[st])
                kb.copy(wot, wot[:, 4 * k2:4 * k2 + 4, :].rearrange("p a b -> p (a b)"), st, st[:], e=("dve", "pool")[k2 % 2])
            kb.dma(lnt[:], lnp[:], reads=[lnp], writes=[lnt])
            hsb = [kb.sb([128, 8, 512], BF16, "mhs") for _ in range(2)]
            ysb = [kb.sb([128, 16, 512], BF16, "mys") for _ in range(2)]
            sg = kb.sb([128, 512], F32, "msg"); tmp = kb.sb([128, 512], F32, "mtmp"); acc = kb.sb([128, 512], F32, "macc")
            mb = [kb.sb([128, 2, 512], BF16, "mmb") for _ in range(2)]
            it = 0
            for gi, (s, n, isc) in enumerate(self.tgroups()):
                hs = hsb[gi % 2]; ys = ysb[gi % 2]; mo = mb[gi % 2]
                ht, hap = self.hT_all.cols(s, n)
                kb.dma(hs[:, :, :n], hap.rearrange("(k p) n -> p k n", p=128), reads=[ht], writes=[hs])
                yt, yap = self.YT_allc.cols(s, n)
                for hh in range(2):
                    kb.dma(ys[:, 8 * hh:8 * hh + 8, :n], yap[1024 * hh:1024 * (hh + 1), :].rearrange("(q p) n -> p q n", p=128), reads=[yt], writes=[ys])
                for j in range(2):
                    for m in range(4):
                        A = self.bank[(2 * it) % 6]; B = self.bank[(2 * it + 1) % 6]; it += 1
                        cb = m * 256 + j * 128
                        for k in range(8):
                            kb.mm(A, A[:, :n], wgt, wgt[:, k, cb:cb + 128], hs, hs[:, k, :n], start=(k == 0), stop=(k == 7))
                        for g2 in range(4):
                            kb.mm(B, B[:, :n], bpt, bpt[:, g2 * 4 + m, j * 128:(j + 1) * 128], ys, ys[:, g2 * 4 + m, :n], start=(g2 == 0), stop=(g2 == 3))
                        kb.act(sg, sg[:, :n], A, A[:, :n], AF.Sigmoid)
                        if m == 0:
                            kb.tt(acc, acc[:, :n], sg, sg[:, :n], B, B[:, :n], ALU.mult)
                        else:
                            kb.tt(tmp, tmp[:, :n], sg, sg[:, :n], B, B[:, :n], ALU.mult)
                            if m < 3:
                                kb.tt(acc, acc[:, :n], acc, acc[:, :n], tmp, tmp[:, :n], ALU.add, e="pool")
                            else:
                                kb.tt(mo, mo[:, j, :n], acc, acc[:, :n], tmp, tmp[:, :n], ALU.add, e="pool")
                mt, map_ = self.mT_my.cols(s, n)
                kb.dma(map_.rearrange("(j p) n -> p j n", p=128), mo[:, :, :n], reads=[mo], writes=[mt])
            for (c0, cn, tm), (_, _, ta) in zip(self.mT_my.ch, self.mT_all.ch):
                kb.allgather(tm, ta, QUADS)
            pre = [kb.sb([128, 2, 512], F32, "mpre") for _ in range(2)]
            xts = [kb.sb([128, 2, 512], F32, "mxt") for _ in range(2)]
            for gi, (s, n, isc) in enumerate(self.tgroups()):
                ms = hsb[gi % 2]; xt = xts[gi % 2]; pr = pre[gi % 2]
                v = 1 if isc else 0
                mt, map_ = self.mT_all.cols(s, n)
                kb.dma(ms[:, :, :n], map_.rearrange("(k p) n -> p k n", p=128), reads=[mt], writes=[ms])
                kb.dma(xt[:, :, :n], xT[:, :, s:s + n].rearrange("j p n -> p j n"), reads=[xT], writes=[xt])
                for j in range(2):
                    A = self.bank[j]
                    for k in range(8):
                        kb.mm(A, A[:, :n], wot, wot[:, k, j * 128:(j + 1) * 128], ms, ms[:, k, :n], start=(k == 0), stop=(k == 7))
                    kb.act(tmp, tmp[:, :n], A, A[:, :n], AF.Identity, extra=[modv], scale=modv[:, 2 * 2 + j, v:v + 1])
                    kb.stt(pr, pr[:, j, :n], xt, xt[:, j, :n], DN_ALPHA, tmp, tmp[:, :n], ALU.mult, ALU.add)
                self.ln_stats(pr, s, n)
                kb.dma(self.preT[:, :, s:s + n].rearrange("j p n -> p j n"), pr[:, :, :n], reads=[pr], writes=[self.preT])
            self.ln_gather()
            for gi, (s, n, isc) in enumerate(self.tgroups()):
                pr = pre[gi % 2]; xo = xts[gi % 2]
                kb.dma(pr[:, :, :n], self.preT[:, :, s:s + n].rearrange("j p n -> p j n"), reads=[self.preT], writes=[pr])
                p1, p2 = self.ln_coef(s, n)
                for j in range(2):
                    kb.tt(tmp, tmp[:, :n], pr, pr[:, j, :n], p1, p1[:, :n], ALU.mult)
                    kb.tt(tmp, tmp[:, :n], tmp, tmp[:, :n], p2, p2[:, :n], ALU.add)
                    kb.act(xo, xo[:, j, :n], tmp, tmp[:, :n], AF.Identity, extra=[lnt], scale=lnt[:, j, 0:1], bias=lnt[:, j, 1:2])
                kb.dma(self.x1T[:, :, s:s + n].rearrange("j p n -> p j n"), xo[:, :, :n], reads=[xo], writes=[self.x1T])


    def alloc_moe(self):
        kb = self.kb; nt = self.ntok
        self.h2T_my = CD(kb, 256, nt, BF16, "h2T_my"); self.h2T_all = CD(kb, 1024, nt, BF16, "h2T_all")
        self.h2f = kb.dram([2, 128, nt], F32, "h2f")
        self.h32t = kb.sb([128, 2, 512], F32, "h32t")
        self.lg_my = kb.dram([16, nt], F32, "lg_my"); self.lg_all = kb.dram([64, nt], F32, "lg_all")
        self.GTd = kb.dram([16, nt], F32, "GTd")
        self.ypc = [(c0, cn, kb.dram([1024, cn], F32, f"ypc{i}"), kb.dram([256, cn], F32, f"yrc{i}"))
                    for i, (c0, cn) in enumerate(groups_of(nt, 1024))]
        self.x2T = kb.dram([2, 128, nt], F32, "x2T")

    def ypcols(self, s, n):
        for c0, cn, tp, tr in self.ypc:
            if c0 <= s and s + n <= c0 + cn:
                return tp, tr, s - c0
        raise ValueError((s, n))

    def phase_moe(self, l):
        kb = self.kb; nt = self.ntok
        modv = self.modv[l]
        rwi = self.inp("rw", [128, 2, 16]); rbi = self.inp("rb", [1, 16]); cmb = self.inp("c_comb", [64, 16])
        seli = self.inp("c_esel", [16, 4 * 128]); lnp = self.inp(f"lnp{l}", [128, 2, 4]) if f"lnp{l}" not in self.inputs else self.inputs[f"lnp{l}"]
        e1 = self.inp(f"ew1_{l}", [4, 128, 8 * 512]); e3 = self.inp(f"ew3_{l}", [4, 128, 8 * 512]); e2 = self.inp(f"ew2_{l}", [4, 128, 4 * 1024])
        self.phase_A(l, self.x1T, 3, 4, self.h2T_my, self.h2T_all, h32=(self.h2f, self.h32t))
        BIG = 1.0e9
        with kb.scope():
            rw = kb.sb([128, 2, 16], F32, "rw"); rb = kb.sb([128, 16], F32, "rb"); comb = kb.sb([64, 16], F32, "comb")
            esel = kb.sb([16, 4, 128], F32, "esel"); lnt = kb.sb([128, 2, 4], F32, "lnt2")
            kb.dma(rw[:], rwi[:], reads=[rwi], writes=[rw])
            kb.dma(rb[:], bcast_ap(rbi, 16), reads=[rbi], writes=[rb])
            kb.dma(comb[:], cmb[:], reads=[cmb], writes=[comb])
            kb.dma(esel[:].rearrange("p a b -> p (a b)"), seli[:], reads=[seli], writes=[esel])
            kb.dma(lnt[:], lnp[:], reads=[lnp], writes=[lnt])
            hf = [kb.sb([128, 2, 512], F32, "hf") for _ in range(2)]
            lgs = [kb.sb([16, 512], F32, "lgs") for _ in range(2)]
            for gi, (s, n, isc) in enumerate(self.tgroups()):
                h = hf[gi % 2]; lg = lgs[gi % 2]
                kb.dma(h[:, :, :n], self.h2f[:, :, s:s + n].rearrange("j p n -> p j n"), reads=[self.h2f], writes=[h])
                A = self.bank[gi % 2]
                for j in range(2):
                    kb.mm(A, A[0:16, :n], rw, rw[:, j, :], h, h[:, j, :n], start=(j == 0), stop=(j == 1))
                kb.copy(lg, lg[:, :n], A, A[0:16, :n])
                kb.dma(self.lg_my[:, s:s + n], lg[:, :n], reads=[lg], writes=[self.lg_my])
            kb.allgather(self.lg_my, self.lg_all, QUADS)
            lgt = [kb.sb([64, 128], F32, "lgt") for _ in range(2)]
            S = kb.sb([128, 16], F32, "rS"); sel = kb.sb([128, 16], F32, "rsel"); selm = kb.sb([128, 16], F32, "rselm")
            pr = kb.sb([128, 6, 4], F32, "rpr"); gs = kb.sb([128, 4], F32, "rgs"); sc1 = kb.sb([128, 8], F32, "rsc")
            eq = kb.sb([128, 4], F32, "req"); is1 = kb.sb([128, 16], F32, "ris1"); is2 = kb.sb([128, 16], F32, "ris2")
            G = kb.sb([128, 16], F32, "rG"); gtt = [kb.sb([16, 512], F32, "gtt") for _ in range(2)]
            ntile = nt // 128
            for ti in range(ntile):
                t0 = ti * 128
                lt = lgt[ti % 2]
                kb.dma(lt[:], self.lg_all[:, t0:t0 + 128], reads=[self.lg_all], writes=[lt])
                A = self.bank[ti % 2]
                kb.mm(A, A[:, 0:16], lt, lt[:], comb, comb[:])
                kb.act(S, S[:], A, A[:, 0:16], AF.Sigmoid)
                kb.tt(sel, sel[:], S, S[:], rb, rb[:], ALU.add)
                sv = sel[:].rearrange("p (g k) -> p g k", k=4)
                pairs = ((0, 1), (0, 2), (0, 3), (1, 2), (1, 3), (2, 3))
                for pi_, (a, b_) in enumerate(pairs):
                    kb.tt(pr, pr[:, pi_, :], sel, sv[:, :, a], sel, sv[:, :, b_], ALU.add)
                kb.tt(gs, gs[:], pr, pr[:, 0, :], pr, pr[:, 1, :], ALU.max)
                for pi_ in range(2, 6):
                    kb.tt(gs, gs[:], gs, gs[:], pr, pr[:, pi_, :], ALU.max)
                kb.op("dve", lambda e: e.tensor_reduce(out=sc1[:, 0:1], in_=gs[:], axis=AX.X, op=ALU.max), reads=[gs], writes=[sc1])
                kb.ts(eq, eq[:], gs, gs[:], sc1[:, 0:1], None, ALU.is_equal, extra=[sc1])
                kb.ts(eq, eq[:], eq, eq[:], -1.0, BIG, ALU.add, ALU.mult)
                smv = selm[:].rearrange("p (g k) -> p g k", k=4)
                for k in range(4):
                    kb.tt(selm, smv[:, :, k], sel, sv[:, :, k], eq, eq[:], ALU.add)
                kb.op("dve", lambda e: e.tensor_reduce(out=sc1[:, 1:2], in_=selm[:], axis=AX.X, op=ALU.max), reads=[selm], writes=[sc1])
                kb.ts(is1, is1[:], selm, selm[:], sc1[:, 1:2], None, ALU.is_equal, extra=[sc1])
                kb.stt(selm, selm[:], is1, is1[:], -BIG, selm, selm[:], ALU.mult, ALU.add)
                kb.op("dve", lambda e: e.tensor_reduce(out=sc1[:, 2:3], in_=selm[:], axis=AX.X, op=ALU.max), reads=[selm], writes=[sc1])
                kb.ts(is2, is2[:], selm, selm[:], sc1[:, 2:3], None, ALU.is_equal, extra=[sc1])
                kb.tt(is1, is1[:], is1, is1[:], is2, is2[:], ALU.add)
                kb.tt(is1, is1[:], is1, is1[:], S, S[:], ALU.mult)
                kb.op("dve", lambda e: e.tensor_reduce(out=sc1[:, 3:4], in_=is1[:], axis=AX.X, op=ALU.add), reads=[is1], writes=[sc1])
                kb.op("dve", lambda e: e.reciprocal(out=sc1[:, 4:5], in_=sc1[:, 3:4]), reads=[sc1], writes=[sc1])
                kb.ts(G, G[:], is1, is1[:], sc1[:, 4:5], None, ALU.mult, extra=[sc1])
                B = self.bank[2 + ti % 2]
                kb.op("pe", lambda e: e.transpose(B[0:16, 0:128], G[:], self.ident[:]), reads=[G, self.ident], writes=[B])
                gt_ = gtt[(ti // 4) % 2]
                kb.copy(gt_, gt_[:, (ti % 4) * 128:(ti % 4 + 1) * 128], B, B[0:16, 0:128])
                if ti % 4 == 3 or ti == ntile - 1:
                    g0 = (ti // 4) * 512; gn = t0 + 128 - g0
                    kb.dma(self.GTd[:, g0:g0 + gn], gt_[:, :gn], reads=[gt_], writes=[self.GTd])
        with kb.scope():
            w1 = [kb.sb([128, 8, 512], BF16, "w1") for _ in range(4)]
            w3 = [kb.sb([128, 8, 512], BF16, "w3") for _ in range(4)]
            w2 = [kb.sb([128, 4, 1024], BF16, "w2") for _ in range(4)]
            stg = [kb.sb([128, 1024], F32, "estg") for _ in range(2)]
            si = 0
            for i in range(4):
                for (src, dstt) in ((e1, w1[i]), (e3, w3[i]), (e2, w2[i])):
                    dflat = dstt[:].rearrange("p a b -> p (a b)")
                    for hh in range(4):
                        st = stg[si % 2]
                        kb.dma(st[:], src[i, :, hh * 1024:(hh + 1) * 1024], reads=[src], writes=[st])
                        kb.copy(dstt, dflat[:, hh * 1024:(hh + 1) * 1024], st, st[:], e=("dve", "pool")[si % 2])
                        si += 1
            esel = kb.sb([16, 4, 128], F32, "esel2")
            kb.dma(esel[:].rearrange("p a b -> p (a b)"), seli[:], reads=[seli], writes=[esel])
            h2 = [kb.sb([128, 8, 512], BF16, "eh2") for _ in range(2)]
            gt = [kb.sb([16, 512], F32, "egt") for _ in range(2)]
            hp = [kb.sb([128, 4, 512], BF16, "ehp") for _ in range(4)]
            sl = kb.sb([128, 512], F32, "esl"); tq = kb.sb([128, 512], F32, "etq")
            yo = [kb.sb([128, 512], F32, "eyo") for _ in range(2)]
            for gi, (s, n, isc) in enumerate(self.tgroups()):
                h = h2[gi % 2]; g_ = gt[gi % 2]
                ht, hap = self.h2T_all.cols(s, n)
                kb.dma(h[:, :, :n], hap.rearrange("(k p) n -> p k n", p=128), reads=[ht], writes=[h])
                kb.dma(g_[:, :n], self.GTd[:, s:s + n], reads=[self.GTd], writes=[g_])
                for i in range(4):
                    Gb = self.bank[5]
                    kb.mm(Gb, Gb[:, :n], esel, esel[:, i, :], g_, g_[:, :n])
                    for fc in range(4):
                        A = self.bank[(fc % 2) * 2]; B = self.bank[(fc % 2) * 2 + 1]
                        for k in range(8):
                            kb.mm(A, A[:, :n], w1[i], w1[i][:, k, fc * 128:(fc + 1) * 128], h, h[:, k, :n], start=(k == 0), stop=(k == 7))
                        for k in range(8):
                            kb.mm(B, B[:, :n], w3[i], w3[i][:, k, fc * 128:(fc + 1) * 128], h, h[:, k, :n], start=(k == 0), stop=(k == 7))
                        kb.act(sl, sl[:, :n], A, A[:, :n], AF.Silu)
                        kb.tt(tq, tq[:, :n], sl, sl[:, :n], B, B[:, :n], ALU.mult)
                        kb.tt(hp[i], hp[i][:, fc, :n], tq, tq[:, :n], Gb, Gb[:, :n], ALU.mult)
                tp, tr, off = self.ypcols(s, n)
                for c in range(8):
                    Y = self.bank[c % 2]
                    for i in range(4):
                        for fc in range(4):
                            kb.mm(Y, Y[:, :n], w2[i], w2[i][:, fc, c * 128:(c + 1) * 128], hp[i], hp[i][:, fc, :n],
                                  start=(i == 0 and fc == 0), stop=(i == 3 and fc == 3))
                    y_ = yo[c % 2]
                    kb.copy(y_, y_[:, :n], Y, Y[:, :n], e=("dve", "act")[c % 2])
                    kb.dma(tp[c * 128:(c + 1) * 128, off:off + n], y_[:, :n], reads=[y_], writes=[tp])
            for c0, cn, tp, tr in self.ypc:
                kb._deps("pool", [tp], [tr])
                ins = self.nc.gpsimd.collective_compute("ReduceScatter", ALU.add, replica_groups=QUADS,
                                                        ins=[tp.h.ap().opt()], outs=[tr.h.ap().opt()])
                kb.cc_cnt += 1; ins.then_inc(kb.cc); kb._mark((kb.cc, kb.cc_cnt), [tp], [tr])
        with kb.scope():
            lnt = kb.sb([128, 2, 4], F32, "lnt3")
            kb.dma(lnt[:], lnp[:], reads=[lnp], writes=[lnt])
            pre = [kb.sb([128, 2, 512], F32, "epre") for _ in range(2)]
            xts = [kb.sb([128, 2, 512], F32, "ext") for _ in range(2)]
            tmp = kb.sb([128, 512], F32, "etmp")
            for gi, (s, n, isc) in enumerate(self.tgroups()):
                xt = xts[gi % 2]; pr_ = pre[gi % 2]
                v = 1 if isc else 0
                tp, tr, off = self.ypcols(s, n)
                kb.dma(pr_[:, :, :n], tr[:, off:off + n].rearrange("(j p) n -> p j n", p=128), reads=[tr], writes=[pr_])
                kb.dma(xt[:, :, :n], self.x1T[:, :, s:s + n].rearrange("j p n -> p j n"), reads=[self.x1T], writes=[xt])
                for j in range(2):
                    kb.act(tmp, tmp[:, :n], pr_, pr_[:, j, :n], AF.Identity, extra=[modv], scale=modv[:, 2 * 5 + j, v:v + 1])
                    kb.stt(pr_, pr_[:, j, :n], xt, xt[:, j, :n], DN_ALPHA, tmp, tmp[:, :n], ALU.mult, ALU.add)
                self.ln_stats(pr_, s, n)
                kb.dma(self.preT[:, :, s:s + n].rearrange("j p n -> p j n"), pr_[:, :, :n], reads=[pr_], writes=[self.preT])
            self.ln_gather()
            for gi, (s, n, isc) in enumerate(self.tgroups()):
                pr_ = pre[gi % 2]; xo = xts[gi % 2]
                kb.dma(pr_[:, :, :n], self.preT[:, :, s:s + n].rearrange("j p n -> p j n"), reads=[self.preT], writes=[pr_])
                p1, p2 = self.ln_coef(s, n)
                for j in range(2):
                    kb.tt(tmp, tmp[:, :n], pr_, pr_[:, j, :n], p1, p1[:, :n], ALU.mult)
                    kb.tt(tmp, tmp[:, :n], tmp, tmp[:, :n], p2, p2[:, :n], ALU.add)
                    kb.act(xo, xo[:, j, :n], tmp, tmp[:, :n], AF.Identity, extra=[lnt], scale=lnt[:, j, 2:3], bias=lnt[:, j, 3:4])
                kb.dma(self.x2T[:, :, s:s + n].rearrange("j p n -> p j n"), xo[:, :, :n], reads=[xo], writes=[self.x2T])


    def phase_gdn(self, l):
        kb = self.kb; nt = self.ntok; nl = self.nl
        gcv = self.inp(f"gconv{l}", [128, 9]); gpar = self.inp(f"gpar{l}", [1, 8]); gnw = self.inp(f"gnorm{l}", [1, 128])
        cpk = self.inp("c_gdn", [64, 7 * 64])
        bo = self.inp("c_bones", [128, 128])
        GQT = kb.dram([128, nt], F32, "GQT"); GKT = kb.dram([128, nt], F32, "GKT")
        GKM = kb.dram([nt, 128], F32, "GKM"); GVM = kb.dram([nt, 128], F32, "GVM"); GG = kb.dram([nt, 8], F32, "GG")
        OD = [kb.dram([nt, 128], F32, f"OD{d}") for d in range(2)]
        with kb.scope():
            cw = kb.sb([128, 9], F32, "cw"); kb.dma(cw[:], gcv[:], reads=[gcv], writes=[cw])
            bones = kb.sb([128, 128], F32, "bones"); kb.dma(bones[:], bo[:], reads=[bo], writes=[bones])
            par = kb.sb([128, 8], F32, "gpar"); kb.dma(par[:], bcast_ap(gpar, 8), reads=[gpar], writes=[par])
            kb.act(par, par[:, 0:4], par, par[:, 0:4], AF.Exp)
            kb.ts(par, par[:, 0:4], par, par[:, 0:4], -1.0, None, ALU.mult)
            xin = [kb.sb([128, 514], F32, "gx") for _ in range(2)]
            u = kb.sb([128, 512], F32, "gu"); sq = kb.sb([128, 512], F32, "gsq"); rs = kb.sb([128, 512], F32, "grs")
            uo = [kb.sb([128, 512], F32, "guo") for _ in range(2)]
            tmo = [kb.sb([128, 128], F32, "gtm") for _ in range(2)]
            gab = kb.sb([128, 8], F32, "gab"); gt = kb.sb([128, 8], F32, "ggt")
            it = 0
            for gi, (s, n, isc) in enumerate(self.tgroups()):
                seg0, seg1 = (nl, nt) if isc else (0, nl)
                for part in range(3):
                    x = xin[it % 2]; it += 1
                    lo = 1 if s == seg0 else 0; hi = 1 if s + n == seg1 else 0
                    if lo:
                        kb.memset(x, x[:, 0:1], 0.0)
                    if hi:
                        kb.memset(x, x[:, n + 1:n + 2], 0.0)
                    kb.dma(x[:, lo:n + 2 - hi], self.GP[part, :, s - 1 + lo:s + n + 1 - hi], reads=[self.GP], writes=[x])
                    kb.ts(u, u[:, :n], x, x[:, 0:n], cw[:, 3 * part:3 * part + 1], None, ALU.mult, extra=[cw])
                    kb.stt(u, u[:, :n], x, x[:, 1:n + 1], cw[:, 3 * part + 1:3 * part + 2], u, u[:, :n], ALU.mult, ALU.add, extra=[cw])
                    kb.stt(u, u[:, :n], x, x[:, 2:n + 2], cw[:, 3 * part + 2:3 * part + 3], u, u[:, :n], ALU.mult, ALU.add, extra=[cw])
                    o = uo[part % 2]
                    if part < 2:
                        kb.act(u, u[:, :n], u, u[:, :n], AF.Silu)
                        kb.tt(sq, sq[:, :n], u, u[:, :n], u, u[:, :n], ALU.mult)
                        A = self.bank[part]
                        kb.mm(A, A[:, :n], bones, bones[:], sq, sq[:, :n])
                        kb.ts(rs, rs[:, :n], A, A[:, :n], RMS_EPS, None, ALU.add)
                        kb.act(rs, rs[:, :n], rs, rs[:, :n], AF.Sqrt)
                        kb.op("dve", lambda e: e.reciprocal(out=rs[:, :n], in_=rs[:, :n]), reads=[rs], writes=[rs])
                        kb.stt(o, o[:, :n], u, u[:, :n], 0.125 if part == 0 else 1.0, rs, rs[:, :n], ALU.mult, ALU.mult)
                        dst = GQT if part == 0 else GKT
                        kb.dma(dst[:, s:s + n], o[:, :n], reads=[o], writes=[dst])
                    else:
                        kb.act(o, o[:, :n], u, u[:, :n], AF.Silu)
                    if part >= 1:
                        dstm = GKM if part == 1 else GVM
                        for tt_ in range(n // 128):
                            B = self.bank[2 + tt_ % 2]
                            kb.op("pe", lambda e: e.transpose(B[:, 0:128], o[:, tt_ * 128:(tt_ + 1) * 128], self.ident[:]), reads=[o, self.ident], writes=[B])
                            tm = tmo[tt_ % 2]
                            kb.copy(tm, tm[:], B, B[:, 0:128])
                            kb.dma(dstm[s + tt_ * 128:s + (tt_ + 1) * 128, :], tm[:], reads=[tm], writes=[dstm])
                for tt_ in range(n // 128):
                    t0 = s + tt_ * 128
                    kb.dma(gab[:], self.GAB[t0:t0 + 128, :], reads=[self.GAB], writes=[gab])
                    kb.tt(gt, gt[:, 0:4], gab, gab[:, 0:4], par, par[:, 4:8], ALU.add)
                    kb.act(gt, gt[:, 0:4], gt, gt[:, 0:4], AF.Exp)
                    kb.ts(gt, gt[:, 0:4], gt, gt[:, 0:4], 1.0, None, ALU.add)
                    kb.act(gt, gt[:, 0:4], gt, gt[:, 0:4], AF.Ln)
                    kb.tt(gt, gt[:, 0:4], gt, gt[:, 0:4], par, par[:, 0:4], ALU.mult)
                    kb.act(gt, gt[:, 4:8], gab, gab[:, 4:8], AF.Sigmoid)
                    kb.dma(GG[t0:t0 + 128, :], gt[:], reads=[gt], writes=[GG])
        with kb.scope():
            cp = kb.sb([64, 7, 64], F32, "cp"); kb.dma(cp[:].rearrange("p a b -> p (a b)"), cpk[:], reads=[cpk], writes=[cp])
            MU, ML, I64, ONES, MUS, MLS = (cp[:, i, :] for i in range(6))
            I4 = kb.sb([64, 4, 64], F32, "I4")
            for u_ in range(4):
                kb.copy(I4, I4[:, u_, :], cp, I64)
            S = kb.sb([64, 4, 64], F32, "gS"); kb.memset(S, S[:], 0.0)
            R2 = nl // 64
            seq0 = [nl + 64 * j for j in range(4)] + [64 * j for j in range(R2)]
            seq1 = [nl + 64 * j for j in range(3, -1, -1)] + [64 * j for j in range(R2 - 1, -1, -1)]

            def T4(name):
                return kb.sb([64, 4, 64], F32, name)
            qT = [[kb.sb([64, 64], F32, "cq") for _ in range(4)] for _ in range(2)]
            kT = [[kb.sb([64, 64], F32, "ck") for _ in range(4)] for _ in range(2)]
            kM = [[kb.sb([64, 128], F32, "ckm") for _ in range(2)] for _ in range(2)]
            vM = [[kb.sb([64, 128], F32, "cvm") for _ in range(2)] for _ in range(2)]
            gg = [[kb.sb([64, 8], F32, "cgg") for _ in range(2)] for _ in range(2)]
            Gbc = T4("Gbc"); Bbc = T4("Bbc"); gcc = kb.sb([64, 4], F32, "gcc"); gcr = T4("gcr"); nD = T4("nD")
            e1 = T4("e1"); e2 = T4("e2"); dec = T4("dec"); decT = T4("decT"); brow = T4("brow")
            X = [T4("X0"), T4("X1")]; XT = [T4("XT0"), T4("XT1")]; TT = T4("TT"); inT = T4("inT")
            kbg = T4("kbg"); vb = T4("vb"); kdec = T4("kdec"); wT = T4("wT"); uval = T4("uval"); qdT = T4("qdT")
            egr = T4("egr"); sc4 = kb.sb([64, 16], F32, "sc4"); vnew = T4("vnew"); osb = [T4("osb0"), T4("osb1")]
            for step in range(len(seq0)):
                pp = step % 2
                toks = (seq0[step], seq1[step])
                for d in range(2):
                    t0 = toks[d]
                    for h in range(2):
                        kb.dma(qT[pp][d * 2 + h][:], GQT[64 * h:64 * h + 64, t0:t0 + 64], reads=[GQT], writes=[qT[pp][d * 2 + h]])
                        kb.dma(kT[pp][d * 2 + h][:], GKT[64 * h:64 * h + 64, t0:t0 + 64], reads=[GKT], writes=[kT[pp][d * 2 + h]])
                    kb.dma(kM[pp][d][:], GKM[t0:t0 + 64, :], reads=[GKM], writes=[kM[pp][d]])
                    kb.dma(vM[pp][d][:], GVM[t0:t0 + 64, :], reads=[GVM], writes=[vM[pp][d]])
                    kb.dma(gg[pp][d][:], GG[t0:t0 + 64, :], reads=[GG], writes=[gg[pp][d]])
                units = [(d, h) for d in range(2) for h in range(2)]
                b0, b1, b2, b3 = self.bank[0], self.bank[1], self.bank[2], self.bank[3]

                def b4(bk):
                    return bk[0:64, 0:256].rearrange("p (a b) -> p a b", a=4)
                for u_, (d, h) in enumerate(units):
                    g_ = gg[pp][d]
                    kb.ts(Gbc, Gbc[:, u_, :], cp, ONES, g_[:, d * 2 + h:d * 2 + h + 1], None, ALU.mult, extra=[g_])
                    kb.ts(Bbc, Bbc[:, u_, :], cp, ONES, g_[:, 4 + d * 2 + h:4 + d * 2 + h + 1], None, ALU.mult, extra=[g_])
                for u_, (d, h) in enumerate(units):
                    Ud = MU if d == 0 else ML
                    g_ = gg[pp][d]
                    kb.mm(b0, b0[0:64, u_:u_ + 1], cp, Ud, g_, g_[:, d * 2 + h:d * 2 + h + 1])
                    kb.mm(b1, b4(b1)[:, u_, :], Gbc, Gbc[:, u_, :], cp, Ud)
                    kb.mm(b2, b4(b2)[:, u_, :], Bbc, Bbc[:, u_, :], cp, I64)
                kb.copy(gcc, gcc[:], b0, b0[0:64, 0:4])
                kb.copy(gcr, gcr[:], b1, b4(b1))
                kb.copy(brow, brow[:], b2, b4(b2), e="act")
                for u_ in range(4):
                    kb.ts(nD, nD[:, u_, :], gcr, gcr[:, u_, :], gcc[:, u_:u_ + 1], None, ALU.subtract, extra=[gcc])
                kb.ts(e1, e1[:], nD, nD[:], -1.0, 0.0, ALU.mult, ALU.min)
                kb.act(e1, e1[:], e1, e1[:], AF.Exp)
                kb.ts(e2, e2[:], nD, nD[:], 0.0, None, ALU.min)
                kb.act(e2, e2[:], e2, e2[:], AF.Exp)
                kb.act(egr, egr[:], gcr, gcr[:], AF.Exp)
                for u_, (d, h) in enumerate(units):
                    kb.mm(b0, b4(b0)[:, u_, :], kT[pp][u_], kT[pp][u_][:], kT[pp][u_], kT[pp][u_][:])
                    kb.mm(b3, b4(b3)[:, u_, :], kT[pp][u_], kT[pp][u_][:], qT[pp][u_], qT[pp][u_][:])
                for u_, (d, h) in enumerate(units):
                    Ms, MsT, MT = (MLS, MUS, MU) if d == 0 else (MUS, MLS, ML)
                    g_ = gg[pp][d]
                    kb.tt(dec, dec[:, u_, :], e1, e1[:, u_, :], cp, Ms, ALU.mult)
                    kb.tt(decT, decT[:, u_, :], e2, e2[:, u_, :], cp, MsT, ALU.mult)
                    kb.tt(e2, e2[:, u_, :], e2, e2[:, u_, :], cp, MT, ALU.mult)
                kb.tt(X[0], X[0][:], b0, b4(b0), dec, dec[:], ALU.mult)
                kb.tt(XT[0], XT[0][:], b0, b4(b0), decT, decT[:], ALU.mult)
                kb.tt(XT[0], XT[0][:], XT[0], XT[0][:], brow, brow[:], ALU.mult)
                for u_, (d, h) in enumerate(units):
                    g_ = gg[pp][d]
                    kb.ts(X[0], X[0][:, u_, :], X[0], X[0][:, u_, :], g_[:, 4 + d * 2 + h:4 + d * 2 + h + 1], None, ALU.mult, extra=[g_])
                kb.tt(inT, inT[:], b3, b4(b3), e2, e2[:], ALU.mult)
                kb.tt(TT, TT[:], I4, I4[:], XT[0], XT[0][:], ALU.subtract)
                cur = 0
                for lev in range(5):
                    nx = 1 - cur
                    for u_ in range(4):
                        kb.mm(b0, b4(b0)[:, u_, :], XT[cur], XT[cur][:, u_, :], X[cur], X[cur][:, u_, :])
                        kb.mm(b1, b4(b1)[:, u_, :], X[cur], X[cur][:, u_, :], XT[cur], XT[cur][:, u_, :])
                    kb.copy(X[nx], X[nx][:], b0, b4(b0))
                    kb.copy(XT[nx], XT[nx][:], b1, b4(b1), e="act")
                    for u_ in range(4):
                        kb.mm(b2, b4(b2)[:, u_, :], X[nx], X[nx][:, u_, :], TT, TT[:, u_, :])
                    kb.tt(TT, TT[:], TT, TT[:], b2, b4(b2), ALU.add)
                    cur = nx
                kb.act(sc4, sc4[:, 0:4], gcc, gcc[:], AF.Exp)
                for u_, (d, h) in enumerate(units):
                    g_ = gg[pp][d]
                    last = 63 if d == 0 else 0
                    kb.tt(sc4, sc4[:, 4 + u_:5 + u_], sc4, sc4[:, u_:u_ + 1], g_, g_[:, 4 + d * 2 + h:4 + d * 2 + h + 1], ALU.mult)
                    kb.tt(sc4, sc4[:, 8 + u_:9 + u_], gcr, gcr[:, u_, last:last + 1], gcc, gcc[:, u_:u_ + 1], ALU.subtract)
                kb.act(sc4, sc4[:, 8:12], sc4, sc4[:, 8:12], AF.Exp)
                for u_, (d, h) in enumerate(units):
                    g_ = gg[pp][d]
                    hs_ = slice(64 * h, 64 * h + 64)
                    kb.ts(kbg, kbg[:, u_, :], kM[pp][d], kM[pp][d][:, hs_], sc4[:, 4 + u_:5 + u_], None, ALU.mult, extra=[sc4])
                    kb.ts(kdec, kdec[:, u_, :], kM[pp][d], kM[pp][d][:, hs_], sc4[:, 8 + u_:9 + u_], None, ALU.mult, extra=[sc4])
                    kb.ts(vb, vb[:, u_, :], vM[pp][d], vM[pp][d][:, hs_], g_[:, 4 + d * 2 + h:4 + d * 2 + h + 1], None, ALU.mult, extra=[g_])
                    kb.tt(qdT, qdT[:, u_, :], qT[pp][u_], qT[pp][u_][:], egr, egr[:, u_, :], ALU.mult)
                for u_ in range(4):
                    kb.mm(b0, b4(b0)[:, u_, :], kbg, kbg[:, u_, :], TT, TT[:, u_, :])
                    kb.mm(b1, b4(b1)[:, u_, :], TT, TT[:, u_, :], vb, vb[:, u_, :])
                kb.copy(wT, wT[:], b0, b4(b0))
                kb.copy(uval, uval[:], b1, b4(b1), e="act")
                for u_ in range(4):
                    kb.mm(b2, b4(b2)[:, u_, :], wT, wT[:, u_, :], S, S[:, u_, :])
                kb.tt(vnew, vnew[:], uval, uval[:], b2, b4(b2), ALU.subtract)
                for u_ in range(4):
                    kb.mm(b3, b4(b3)[:, u_, :], qdT, qdT[:, u_, :], S, S[:, u_, :], start=True, stop=False)
                    kb.mm(b3, b4(b3)[:, u_, :], inT, inT[:, u_, :], vnew, vnew[:, u_, :], start=False, stop=True)
                    kb.mm(b0, b4(b0)[:, u_, :], kdec, kdec[:, u_, :], vnew, vnew[:, u_, :])
                ob_ = osb[pp]
                kb.copy(ob_, ob_[:], b3, b4(b3), e="act")
                for u_, (d, h) in enumerate(units):
                    last = 63 if d == 0 else 0
                    kb.stt(S, S[:, u_, :], S, S[:, u_, :], egr[:, u_, last:last + 1], b0, b4(b0)[:, u_, :], ALU.mult, ALU.add, extra=[egr])
                for d in range(2):
                    kb.dma(OD[d][toks[d]:toks[d] + 64, :].rearrange("p (h v) -> p h v", h=2), ob_[:, 2 * d:2 * d + 2, :], reads=[ob_], writes=[OD[d]])
        with kb.scope():
            gnb = kb.sb([128, 128], F32, "gnb"); kb.dma(gnb[:], bcast_ap(gnw, 128), reads=[gnw], writes=[gnb])
            o0 = [kb.sb([128, 128], F32, "po0") for _ in range(2)]; o1 = [kb.sb([128, 128], F32, "po1") for _ in range(2)]
            zt = [kb.sb([128, 128], F32, "pz") for _ in range(2)]; sq = kb.sb([128, 128], F32, "psq"); ss = kb.sb([128, 4], F32, "pss")
            y16 = [kb.sb([128, 128], BF16, "py") for _ in range(2)]; stg = [kb.sb([128, 128], BF16, "pstg") for _ in range(2)]
            for ti in range(nt // 128):
                t0 = ti * 128; a = o0[ti % 2]; b_ = o1[ti % 2]; z = zt[ti % 2]
                kb.dma(a[:], OD[0][t0:t0 + 128, :], reads=[OD[0]], writes=[a])
                kb.dma(b_[:], OD[1][t0:t0 + 128, :], reads=[OD[1]], writes=[b_])
                kb.dma(z[:], self.GZ[t0:t0 + 128, :], reads=[self.GZ], writes=[z])
                kb.tt(a, a[:], a, a[:], b_, b_[:], ALU.add)
                kb.tt(sq, sq[:], a, a[:], a, a[:], ALU.mult)
                kb.op("dve", lambda e: e.tensor_reduce(out=ss[:, 0:2], in_=sq[:].rearrange("p (h v) -> p h v", h=2), axis=AX.X, op=ALU.add), reads=[sq], writes=[ss])
                kb.ts(ss, ss[:, 0:2], ss, ss[:, 0:2], 1.0 / 64, RMS_EPS, ALU.mult, ALU.add)
                kb.act(ss, ss[:, 0:2], ss, ss[:, 0:2], AF.Sqrt)
                kb.op("dve", lambda e: e.reciprocal(out=ss[:, 2:4], in_=ss[:, 0:2]), reads=[ss], writes=[ss])
                kb.act(z, z[:], z, z[:], AF.Silu)
                for h in range(2):
                    hs_ = slice(64 * h, 64 * h + 64)
                    kb.stt(a, a[:, hs_], a, a[:, hs_], ss[:, 2 + h:3 + h], gnb, gnb[:, hs_], ALU.mult, ALU.mult, extra=[ss])
                y = y16[ti % 2]
                kb.tt(y, y[:], a, a[:], z, z[:], ALU.mult)
                self.store_T(y, y[:], 128, 128, t0, self.bank[4 + ti % 2], stg[ti % 2])


    def phase_hyena(self, l):
        kb = self.kb; nt = self.ntok; nl = self.nl
        hcv = self.inp(f"hconv{l}", [128, 9]); hw1 = self.inp(f"hw1_{l}", [33, 64]); hw2 = self.inp(f"hw2_{l}", [64, 64])
        hw3 = self.inp(f"hw3_{l}", [64, 4 * 128]); hpr = self.inp(f"hpar{l}", [64, 3]); hdl = self.inp(f"hdel{l}", [128, 6])
        jmi = self.inp("c_jm", [128, 128])
        segs = [(0, nl), (nl, NC)]
        consts = {L: (self.inp(f"c_hyF{L}", [33, 2 * L]), self.inp(f"c_hyT{L}", [1, 2 * L])) for _, L in segs}
        HV = kb.dram([3, 128, nt], F32, "HV")
        TA = {L: [kb.dram([128, 2 * L + 254], BF16, f"TA{L}_{o}") for o in range(2)] for _, L in segs}
        TWO_PI = 2.0 * math.pi
        with kb.scope():
            cw = kb.sb([128, 9], F32, "hcw"); kb.dma(cw[:], hcv[:], reads=[hcv], writes=[cw])
            xin = [kb.sb([128, 514], F32, "hx") for _ in range(2)]
            uo = [kb.sb([128, 512], F32, "hu") for _ in range(2)]
            it = 0
            for gi, (s, n, isc) in enumerate(self.tgroups()):
                seg0, seg1 = (nl, nt) if isc else (0, nl)
                for part in range(3):
                    x = xin[it % 2]; u = uo[it % 2]; it += 1
                    lo = 1 if s == seg0 else 0; hi = 1 if s + n == seg1 else 0
                    if lo:
                        kb.memset(x, x[:, 0:1], 0.0)
                    if hi:
                        kb.memset(x, x[:, n + 1:n + 2], 0.0)
                    kb.dma(x[:, lo:n + 2 - hi], self.HY[part, :, s - 1 + lo:s + n + 1 - hi], reads=[self.HY], writes=[x])
                    kb.ts(u, u[:, :n], x, x[:, 0:n], cw[:, 3 * part:3 * part + 1], None, ALU.mult, extra=[cw])
                    kb.stt(u, u[:, :n], x, x[:, 1:n + 1], cw[:, 3 * part + 1:3 * part + 2], u, u[:, :n], ALU.mult, ALU.add, extra=[cw])
                    kb.stt(u, u[:, :n], x, x[:, 2:n + 2], cw[:, 3 * part + 2:3 * part + 3], u, u[:, :n], ALU.mult, ALU.add, extra=[cw])
                    kb.dma(HV[part, :, s:s + n], u[:, :n], reads=[u], writes=[HV])
            w1 = kb.sb([33, 64], F32, "hw1"); w2 = kb.sb([64, 64], F32, "hw2"); w3 = kb.sb([64, 4, 128], F32, "hw3")
            pr = kb.sb([64, 8], F32, "hpr"); dl = kb.sb([128, 6], F32, "hdl")
            kb.dma(w1[:], hw1[:], reads=[hw1], writes=[w1]); kb.dma(w2[:], hw2[:], reads=[hw2], writes=[w2])
            kb.dma(w3[:].rearrange("p a b -> p (a b)"), hw3[:], reads=[hw3], writes=[w3])
            kb.dma(pr[:, 0:3], hpr[:], reads=[hpr], writes=[pr]); kb.dma(dl[:], hdl[:], reads=[hdl], writes=[dl])
            for i_ in range(2):
                kb.tt(pr, pr[:, 3 + i_:4 + i_], pr, pr[:, 0:1], pr, pr[:, 1 + i_:2 + i_], ALU.mult)
            kb.act(dl, dl[:, 0:4], dl, dl[:, 0:4], AF.Abs)
            kb.ts(dl, dl[:, 0:4], dl, dl[:, 0:4], -1.0, None, ALU.mult)
            nacc = kb.sb([128, 8], F32, "hnacc"); kb.memset(nacc, nacc[:], 0.0)
            ft = [kb.sb([33, 512], F32, "hft") for _ in range(2)]; tl = [kb.sb([128, 512], F32, "htl") for _ in range(2)]
            a1 = kb.sb([64, 512], F32, "ha1"); a2 = kb.sb([64, 512], F32, "ha2"); kk = kb.sb([64, 512], F32, "hkk")
            win = kb.sb([128, 512], F32, "hwin"); hh = kb.sb([128, 512], F32, "hhh"); hab = kb.sb([128, 512], F32, "hab")
            hbf = [kb.sb([128, 512], BF16, "hbf") for _ in range(2)]; red = kb.sb([128, 2], F32, "hred")
            zpad = kb.sb([128, 128], BF16, "hzp"); kb.memset(zpad, zpad[:], 0.0)
            for si_, (_, L) in enumerate(segs):
                Fc, Tc = consts[L]
                for o in range(2):
                    kb.dma(TA[L][o][:, 0:127], zpad[:, 0:127], reads=[zpad], writes=[TA[L][o]])
                    kb.dma(TA[L][o][:, 2 * L + 126:2 * L + 254], zpad[:, 0:128], reads=[zpad], writes=[TA[L][o]])
                gidx = 0
                for dd in (1, 0):
                    for (j0, n) in groups_of(L, 512):
                        f = ft[gidx % 2]; t_ = tl[gidx % 2]; gidx += 1
                        kb.dma(f[:, :n], Fc[:, dd * L + j0:dd * L + j0 + n], reads=[Fc], writes=[f])
                        kb.dma(t_[:, :n], bcast_ap(Tc, n, offset=dd * L + j0), reads=[Tc], writes=[t_])
                        A = self.bank[0]; B = self.bank[1]
                        kb.mm(A, A[0:64, :n], w1, w1[:], f, f[:, :n])
                        kb.act(a1, a1[:, :n], A, A[0:64, :n], AF.Identity, extra=[pr], scale=pr[:, 0:1], bias=pr[:, 3:4])
                        kb.ts(kk, kk[:, :n], a1, a1[:, :n], 1.0 / TWO_PI, 12582912.0, ALU.mult, ALU.add)
                        kb.ts(kk, kk[:, :n], kk, kk[:, :n], -12582912.0, None, ALU.add)
                        kb.stt(a1, a1[:, :n], kk, kk[:, :n], -TWO_PI, a1, a1[:, :n], ALU.mult, ALU.add)
                        kb.act(a1, a1[:, :n], a1, a1[:, :n], AF.Sin)
                        kb.mm(B, B[0:64, :n], w2, w2[:], a1, a1[:, :n])
                        kb.act(a2, a2[:, :n], B, B[0:64, :n], AF.Identity, extra=[pr], scale=pr[:, 0:1], bias=pr[:, 4:5])
                        kb.ts(kk, kk[:, :n], a2, a2[:, :n], 1.0 / TWO_PI, 12582912.0, ALU.mult, ALU.add)
                        kb.ts(kk, kk[:, :n], kk, kk[:, :n], -12582912.0, None, ALU.add)
                        kb.stt(a2, a2[:, :n], kk, kk[:, :n], -TWO_PI, a2, a2[:, :n], ALU.mult, ALU.add)
                        kb.act(a2, a2[:, :n], a2, a2[:, :n], AF.Sin)
                        for o in range(2):
                            C = self.bank[2 + o]
                            kb.mm(C, C[:, :n], w3, w3[:, dd * 2 + o, :], a2, a2[:, :n])
                            kb.act(win, win[:, :n], t_, t_[:, :n], AF.Exp, extra=[dl], scale=dl[:, dd * 2 + o:dd * 2 + o + 1])
                            kb.stt(hh, hh[:, :n], win, win[:, :n], 0.05, C, C[:, :n], ALU.add, ALU.mult)
                            if dd == 1 and j0 + n == L:
                                kb.memset(hh, hh[:, n - 1:n], 0.0, e="dve")
                            kb.act(hab, hab[:, :n], hh, hh[:, :n], AF.Abs)
                            kb.op("dve", lambda e: e.tensor_reduce(out=red[:, 0:1], in_=hab[:, :n], axis=AX.X, op=ALU.add), reads=[hab], writes=[red])
                            kb.tt(nacc, nacc[:, 2 * si_ + o:2 * si_ + o + 1], nacc, nacc[:, 2 * si_ + o:2 * si_ + o + 1], red, red[:, 0:1], ALU.add)
                            hb_ = hbf[o]
                            kb.copy(hb_, hb_[:, :n], hh, hh[:, :n], e="pool")
                            base = (127 if dd == 1 else L + 126) + j0
                            nn = n - 1 if (dd == 1 and j0 + n == L) else n
                            kb.dma(TA[L][o][:, base:base + nn], hb_[:, :nn], reads=[hb_], writes=[TA[L][o]])
            kb.op("dve", lambda e: e.reciprocal(out=nacc[:, 4:8], in_=nacc[:, 0:4]), reads=[nacc], writes=[nacc])
            nrm = kb.sb([128, 4], F32, "hnrm_keep") if False else None
            self.h_nrm = kb.dram([128, 4], F32, "h_nrm"); self.h_sk = kb.dram([128, 2], F32, "h_sk")
            kb.dma(self.h_nrm[:], nacc[:, 4:8], reads=[nacc], writes=[self.h_nrm])
            kb.dma(self.h_sk[:], dl[:, 4:6], reads=[dl], writes=[self.h_sk])
        ZC = kb.dram([128, nt], F32, "HZC")
        for si_, (s0, L) in enumerate(segs):
            nb = L // 128; pad = nb - 1; W = (2 * nb - 1) * 128
            with kb.scope():
                jm = kb.sb([128, 128], BF16, "jm"); jmf = kb.sb([128, 128], F32, "jmf")
                kb.dma(jmf[:], jmi[:], reads=[jmi], writes=[jmf]); kb.copy(jm, jm[:], jmf, jmf[:])
                nr = kb.sb([128, 4], F32, "hnr"); sk = kb.sb([128, 2], F32, "hsk")
                kb.dma(nr[:], self.h_nrm[:], reads=[self.h_nrm], writes=[nr]); kb.dma(sk[:], self.h_sk[:], reads=[self.h_sk], writes=[sk])
                zp = kb.sb([128, nb + 2 * pad, 128], BF16, "zp")
                if pad:
                    kb.memset(zp, zp[:, 0:pad, :], 0.0); kb.memset(zp, zp[:, pad + nb:, :], 0.0)
                yT = kb.sb([128, nb, 128], F32, "yT")
                HP = min(2 * nb - 1, 64)
                Hs = [kb.sb([128, HP * 128], BF16, "Hs") for _ in range(2)]
                hi_ = 0
                zf = [kb.sb([128, 128], F32, "zf") for _ in range(2)]; zb = [kb.sb([128, 128], BF16, "zb") for _ in range(2)]
                zt = [kb.sb([128, 128], BF16, "zt") for _ in range(2)]
                g_ = [kb.sb([128, 128], F32, "hg") for _ in range(2)]; yo = [kb.sb([128, 128], F32, "hyo") for _ in range(2)]
                y16 = [kb.sb([128, 128], BF16, "hy16") for _ in range(2)]
                for o in range(2):
                    src = HV if o == 0 else ZC
                    for J in range(nb):
                        a = zf[J % 2]; b_ = zb[J % 2]; c_ = zt[J % 2]
                        if o == 0:
                            kb.dma(a[:], HV[0, :, s0 + J * 128:s0 + (J + 1) * 128], reads=[HV], writes=[a])
                        else:
                            kb.dma(a[:], ZC[:, s0 + J * 128:s0 + (J + 1) * 128], reads=[ZC], writes=[a])
                        kb.copy(b_, b_[:], a, a[:])
                        P1 = self.bank[J % 2]
                        pv = P1[:].bitcast(BF16)
                        kb.op("pe", lambda e: e.transpose(pv[:, 0:128], b_[:], self.identb[:]), reads=[b_, self.identb], writes=[P1])
                        kb.copy(c_, c_[:], P1, pv[:, 0:128], e="act")
                        P2 = self.bank[2 + J % 2]
                        kb.mm(P2, P2[:, 0:128], jm, jm[:], c_, c_[:])
                        kb.copy(zp, zp[:, pad + J, :], P2, P2[:, 0:128])
                    for c in range(128):
                        Y = self.bank[4 + c % 4]
                        for p0, pn in groups_of(2 * nb - 1, HP):
                            H = Hs[hi_ % 2]; hi_ += 1
                            hsrc = bass.AP(tensor=TA[L][o].h, offset=c * (2 * L + 254) + 127 + p0 * 128, ap=[[1, 128], [1, pn * 128]])
                            kb.dma(H[:, 0:pn * 128], hsrc, reads=[TA[L][o]], writes=[H])
                            for dq in range(pn):
                                dp = p0 + dq
                                kb.mm(Y, Y[:, 0:nb], H, H[:, dq * 128:(dq + 1) * 128], zp, zp[:, 2 * pad - dp:2 * pad - dp + nb, c],
                                      start=(dp == 0), stop=(dp == 2 * nb - 2))
                        kb.copy(yT, yT[:, :, c], Y, Y[:, 0:nb], e=("dve", "act")[c % 2])
                    for I in range(nb):
                        P1 = self.bank[I % 2]
                        kb.op("pe", lambda e: e.transpose(P1[:, 0:128], yT[:, I, :], self.ident[:]), reads=[yT, self.ident], writes=[P1])
                        a = zf[I % 2]; gt = g_[I % 2]; y_ = yo[I % 2]
                        c0 = s0 + I * 128
                        if o == 0:
                            kb.dma(a[:], HV[0, :, c0:c0 + 128], reads=[HV], writes=[a])
                        else:
                            kb.dma(a[:], ZC[:, c0:c0 + 128], reads=[ZC], writes=[a])
                        kb.dma(gt[:], HV[1 + o, :, c0:c0 + 128], reads=[HV], writes=[gt])
                        kb.ts(y_, y_[:], P1, P1[:, 0:128], nr[:, 2 * si_ + o:2 * si_ + o + 1], None, ALU.mult, extra=[nr])
                        kb.stt(y_, y_[:], a, a[:], sk[:, o:o + 1], y_, y_[:], ALU.mult, ALU.add, extra=[sk])
                        if o == 0:
                            kb.tt(y_, y_[:], y_, y_[:], gt, gt[:], ALU.mult)
                            kb.dma(ZC[:, c0:c0 + 128], y_[:], reads=[y_], writes=[ZC])
                        else:
                            yb = y16[I % 2]
                            kb.tt(yb, yb[:], y_, y_[:], gt, gt[:], ALU.mult)
                            yt, yap = self.YT_my.cols(c0, 128)
                            kb.dma(yap[0:128, :], yb[:], reads=[yb], writes=[yt])


def build_full(depth=DEPTH, nl=NL):
    P = Prog(nl, depth, ())
    kb = P.kb
    P.setup_consts()
    P.setup_mod()
    P.ln_alloc()
    P.alloc_A()
    P.alloc_mix()
    P.alloc_tail()
    P.alloc_moe()
    xT = P.inp("xT", [2, 128, P.ntok])
    x_in = xT
    for l in range(depth):
        lam_init = 0.8 - 0.6 * math.exp(-0.3 * l)
        P.phase_A(l, x_in, 0, 1, P.hT_my, P.hT_all)
        P.phase_B(l)
        P.phase_hyena(l)
        P.phase_gdn(l)
        P.phase_diff(l, lam_init)
        P.phase_na(l)
        for (c0, cn, tm), (_, _, ta) in zip(P.YT_my.ch, P.YT_allc.ch):
            kb.allgather(tm, ta, QUADS)
        P.phase_merge(l, x_in)
        P.phase_moe(l)
        x_in = P.x2T
    o = P.out("o_x", [2, 128, nl])
    for j in range(2):
        kb.dma(o[j, :, :], P.x2T[j, :, 0:nl], reads=[P.x2T], writes=[o])
    kb.finish([o])
    return P


def core_bg(r): return r // 4, r % 4
def prep_common(inp, r, nl, l_list=(0,)):
    b, g = core_bg(r)
    m = {}
    sel = np.zeros((8, 2), np.float32); sel[0::2, 0] = 1; sel[1::2, 1] = 1
    m["c_sel8"] = sel
    cc = np.stack([inp["c"][b], inp["c_ctx"]], -1)
    m["cT"] = np.ascontiguousarray(cc.reshape(8, 128, 2).transpose(1, 0, 2))
    cols = np.concatenate([w * 1024 + 256 * g + np.arange(256) for w in range(6)])
    for l in l_list:
        wm = inp["w_mod"][l][:, cols]
        m[f"wmod{l}"] = np.ascontiguousarray(wm.reshape(8, 128, 1536).transpose(1, 0, 2))
        m[f"bmod{l}"] = np.ascontiguousarray(inp["b_mod"][l][cols].reshape(12, 128).T)
    xs = np.concatenate([inp["x"][b, :nl], inp["ctx"][b]], 0)
    m["xT"] = np.ascontiguousarray(xs[:, 256 * g:256 * g + 256].T.reshape(2, 128, nl + 256))
    return m

SPLIT = [1536, 1536, 512, 16, 16, 1536, 1536, 4096]
OFF = np.concatenate([[0], np.cumsum(SPLIT)])
def wg_cols(g):
    o_hy, o_gq, o_gz, o_ga, o_gb, o_d, o_n, o_gate = OFF[:8]
    c = []
    for part in range(3): c.append(o_hy + part * 512 + 128 * g + np.arange(128))
    for part in range(3): c.append(o_gq + part * 512 + 128 * g + np.arange(128))
    c.append(o_gz + 128 * g + np.arange(128))
    for part in range(3): c.append(o_d + part * 512 + 128 * g + np.arange(128))
    for part in range(3): c.append(o_n + part * 512 + 128 * g + np.arange(128))
    ab = []
    for base in (o_ga, o_gb):
        for d in range(2):
            for h in range(2): ab.append(base + d * 8 + 2 * g + h)
    c.append(np.array(ab))
    return np.concatenate(c)
def rope_tables(nl):
    t = np.arange(nl); row = (t // 64).astype(np.float32); col = (t % 64).astype(np.float32)
    inv = (10000.0 ** (-np.arange(16, dtype=np.float32) / 16)).astype(np.float32)
    ar = row[:, None] * inv; ac = col[:, None] * inv
    cosd = np.ones((64, nl + 256), np.float32); sind = np.zeros((64, nl + 256), np.float32)
    for hh, ang in enumerate((ar, ac)):
        for t2 in range(2):
            cosd[hh * 32 + t2 * 16: hh * 32 + t2 * 16 + 16, :nl] = np.cos(ang).T
            sind[hh * 32 + t2 * 16: hh * 32 + t2 * 16 + 16, :nl] = np.sin(ang).T
    return np.concatenate([cosd, cosd], 0), np.concatenate([sind, sind], 0)
def na_consts():
    cols = np.arange(64)
    cstart = np.clip(cols - 8, 0, 48)
    col_ok = (cols[None, :] >= cstart[:, None]) & (cols[None, :] < cstart[:, None] + 16)
    dc = np.clip(cols[None, :] - cols[:, None], -15, 15) + 15
    return col_ok.T.astype(np.float32).copy(), dc
def prep_mix(inp, r, nl, l):
    b, g = core_bg(r); m = {}
    w = inp["w_in"][l][:, wg_cols(g)]
    m[f"wg{l}"] = np.ascontiguousarray(w.reshape(8, 128, -1).transpose(1, 0, 2))
    c, s_ = rope_tables(nl); m["c_cosT"] = c; m["c_sinT"] = s_
    m["c_ident"] = np.eye(128, dtype=np.float32)
    m[f"dlam{l}"] = inp["diff_lam"][l]
    m[f"dnorm{l}"] = inp["diff_norm"][l][None, :]
    mask, dc = na_consts(); m["c_namask"] = mask
    rp = inp["na_rpb"][l][2 * g:2 * g + 2]
    bias = rp[:, :, dc]
    m[f"nabias{l}"] = np.ascontiguousarray(bias.transpose(3, 0, 1, 2).reshape(64, -1))
    return m

def prep_tail(inp, r, nl, l, ref=None):
    import ml_dtypes
    b, g = core_bg(r); m = {}
    og = OFF[7]
    gc = np.concatenate([og + mm * 1024 + 256 * g + np.arange(256) for mm in range(4)])
    wgate = inp["w_in"][l][:, gc]
    m[f"wgate{l}"] = np.ascontiguousarray(wgate.reshape(8, 128, 1024).transpose(1, 0, 2))
    bp = inp["branch_proj"][l][:, :, 256 * g:256 * g + 256]
    bp = bp.reshape(4, 4, 128, 256).transpose(2, 1, 0, 3)
    m[f"bp{l}"] = np.ascontiguousarray(bp.reshape(128, 16, 256))
    wo = inp["w_out"][l][:, 256 * g:256 * g + 256]
    m[f"wout{l}"] = np.ascontiguousarray(wo.reshape(8, 128, 256).transpose(1, 0, 2))
    f = 256 * g + np.arange(256)
    lnp = np.stack([inp["ln_g"][l, 0][f], inp["ln_b"][l, 0][f], inp["ln_g"][l, 1][f], inp["ln_b"][l, 1][f]], -1)
    m[f"lnp{l}"] = np.ascontiguousarray(lnp.reshape(2, 128, 4).transpose(1, 0, 2))
    if ref is not None:
        ys = []
        for g2 in range(4):
            for nm in ("ya", "yb", "yc", "yd"):
                full = np.concatenate([ref[nm + "_x"][b, :nl], ref[nm + "_c"][b]], 0)
                ys.append(full[:, 128 * g2:128 * g2 + 128].T)
        m["yt_ref"] = np.concatenate(ys, 0).astype(ml_dtypes.bfloat16)
    return m

def prep_moe(inp, r, nl, l, ref=None):
    b, g = core_bg(r); m = {}
    f = 256 * g + np.arange(256)
    m["rw"] = np.ascontiguousarray(inp["router_w"][f].reshape(2, 128, 16).transpose(1, 0, 2))
    m["rb"] = inp["router_b"][None, :]
    comb = np.zeros((64, 16), np.float32)
    for rr in range(4):
        comb[rr * 16 + np.arange(16), np.arange(16)] = 1
    m["c_comb"] = comb
    es = np.zeros((16, 4, 128), np.float32)
    for i in range(4): es[4 * g + i, i, :] = 1
    m["c_esel"] = es.reshape(16, 512)
    E = [4 * g + i for i in range(4)]
    m[f"ew1_{l}"] = np.ascontiguousarray(inp["exp_w1"][l][E].reshape(4, 8, 128, 512).transpose(0, 2, 1, 3).reshape(4, 128, 4096))
    m[f"ew3_{l}"] = np.ascontiguousarray(inp["exp_w3"][l][E].reshape(4, 8, 128, 512).transpose(0, 2, 1, 3).reshape(4, 128, 4096))
    m[f"ew2_{l}"] = np.ascontiguousarray(inp["exp_w2"][l][E].reshape(4, 4, 128, 1024).transpose(0, 2, 1, 3).reshape(4, 128, 4096))
    m["c_ident"] = np.eye(128, dtype=np.float32)
    if ref is not None:
        x1 = np.concatenate([ref['x1'][b, :nl], ref['xc1'][b]], 0)[:, f].T
        m["x1_ref"] = np.ascontiguousarray(x1.reshape(2, 128, -1))
    return m

def prep_gdn(inp, r, nl, l):
    b, g = core_bg(r); m = {}
    gc = np.concatenate([inp["gdn_conv"][l][part * 512 + 128 * g + np.arange(128)] for part in range(3)], 1)
    m[f"gconv{l}"] = np.ascontiguousarray(gc)
    al = [inp["gdn_a_log"][l][d, 2 * g + h] for d in range(2) for h in range(2)]
    dtb = [inp["gdn_dt_bias"][l][d, 2 * g + h] for d in range(2) for h in range(2)]
    m[f"gpar{l}"] = np.array([al + dtb], np.float32)
    m[f"gnorm{l}"] = np.concatenate([inp["gdn_norm"][l]] * 2)[None, :].astype(np.float32)
    p = np.arange(64)
    MU = (p[:, None] <= p[None, :]).astype(np.float32); ML = (p[:, None] >= p[None, :]).astype(np.float32)
    I = np.eye(64, dtype=np.float32); ON = np.ones((64, 64), np.float32)
    m["c_gdn"] = np.ascontiguousarray(np.stack([MU, ML, I, ON, MU - I, ML - I, ON], 1).reshape(64, 7 * 64))
    bo = np.zeros((128, 128), np.float32); bo[:64, :64] = 1; bo[64:, 64:] = 1
    m["c_bones"] = bo
    return m

def hy_feats(L):
    f32 = np.float32
    t = np.linspace(0.0, 1.0, L, dtype=f32)[:, None]
    ang = (f32(2.0 * np.pi) * np.arange(L, dtype=f32)[:, None] / f32(L)).astype(f32)
    bands = np.linspace(1e-4, 15, 16, dtype=f32)[None, :]
    feats = np.concatenate([t, np.cos(bands * ang), -np.sin(bands * ang)], -1).astype(f32)
    F = np.concatenate([feats.T, feats[::-1].T], 1)
    T = np.concatenate([t[:, 0], t[::-1, 0]])[None, :]
    return np.ascontiguousarray(F), np.ascontiguousarray(T.astype(f32))
def prep_hy(inp, r, nl, l):
    b, g = core_bg(r); m = {}
    ch = 128 * g + np.arange(128)
    m[f"hconv{l}"] = np.ascontiguousarray(np.concatenate([inp["hy_conv"][l][part * 512 + ch] for part in range(3)], 1))
    m[f"hw1_{l}"] = inp["hy_w1"][l]; m[f"hw2_{l}"] = inp["hy_w2"][l]
    m[f"hw3_{l}"] = np.ascontiguousarray(np.concatenate([inp["hy_w3"][l][:, dd * 1024 + o * 512 + ch] for dd in range(2) for o in range(2)], 1))
    m[f"hpar{l}"] = np.ascontiguousarray(np.stack([inp["hy_freq"][l], inp["hy_b1"][l], inp["hy_b2"][l]], 1))
    dl = [inp["hy_deltas"][l][dd, o, ch] for dd in range(2) for o in range(2)] + [inp["hy_skip"][l][o, ch] for o in range(2)]
    m[f"hdel{l}"] = np.ascontiguousarray(np.stack(dl, 1))
    m["c_jm"] = np.eye(128, dtype=np.float32)[::-1].copy()
    for L in (nl, 256):
        F, T = hy_feats(L); m[f"c_hyF{L}"] = F; m[f"c_hyT{L}"] = T
    return m


def kernel(**inputs):
    inp = {k: np.asarray(v) for k, v in inputs.items()}
    depth = DEPTH
    P = build_full(depth, NL)
    maps = []
    for r in range(8):
        m = prep_common(inp, r, NL, l_list=tuple(range(depth)))
        for l in range(depth):
            m.update(prep_mix(inp, r, NL, l)); m.update(prep_gdn(inp, r, NL, l)); m.update(prep_hy(inp, r, NL, l))
            m.update(prep_tail(inp, r, NL, l)); m.update(prep_moe(inp, r, NL, l))
        missing = sorted(set(P.inputs) - set(m))
        assert not missing, missing
        maps.append({k: np.ascontiguousarray(v) for k, v in m.items() if k in P.inputs})
    res = run_bass_kernel_spmd(P.nc, maps, core_ids=list(range(8)))
    out = np.zeros((2, NL, D), np.float32)
    for r in range(8):
        b, g = r // 4, r % 4
        o = np.asarray(res.results[r]["o_x"])
        out[b, :, 256 * g:256 * g + 256] = o.reshape(256, NL).T
    return out
```

```python
import math
from contextlib import ExitStack
import numpy as np
import concourse.bass as bass
import concourse.mybir as mybir
from concourse.bass_utils import run_bass_kernel_spmd

F32 = mybir.dt.float32
BF16 = mybir.dt.bfloat16
AF = mybir.ActivationFunctionType
ALU = mybir.AluOpType
AX = mybir.AxisListType

D = 1024
NL = 8192
NC = 256
NTOK = NL + NC
DEPTH = 4
GRID_W = 64
LN_EPS = 1e-5
RMS_EPS = 1e-6
DN_ALPHA = (2 * DEPTH) ** 0.25
QUADS = [[0, 1, 2, 3], [4, 5, 6, 7]]
NWG = 13 * 128 + 8


class T:
    __slots__ = ("h", "w", "r", "name", "excl")

    def __init__(self, h, name=None, excl=False):
        self.h = h; self.w = None; self.r = {}; self.name = name; self.excl = excl

    def __getitem__(self, idx):
        return self.h[idx]


class KB:
    def __init__(self, nc, n_lanes=24):
        self.nc = nc
        self.eng = {"pe": nc.tensor, "act": nc.scalar, "dve": nc.vector, "pool": nc.gpsimd, "sp": nc.sync}
        self.sem = {k: nc.alloc_semaphore("s_" + k) for k in self.eng}
        self.cnt = {k: 0 for k in self.eng}
        self.lanes = [nc.alloc_semaphore(f"lane{i}") for i in range(n_lanes)]
        self.lane_cnt = [0] * n_lanes
        self.lane_rr = 0
        self.cc = nc.alloc_semaphore("cc")
        self.cc_cnt = 0
        self.waited = {}
        self.nid = 0
        self.dq = 0
        self.stack = None

    def sb(self, shape, dtype, name="sb"):
        self.nid += 1
        nm = f"{name}_{self.nid}"
        if self.stack is not None:
            return T(self.stack.enter_context(self.nc.sbuf_tensor(nm, list(shape), dtype)), nm)
        return T(self.nc.alloc_sbuf_tensor(nm, list(shape), dtype), nm)

    def barrier(self):
        targets = [(self.sem[k], self.cnt[k]) for k in self.eng]
        targets += [(self.lanes[i], 16 * c) for i, c in enumerate(self.lane_cnt)]
        targets.append((self.cc, self.cc_cnt))
        for e in self.eng:
            for sm, v in targets:
                if v:
                    self._wait(e, sm, v)

    def scope(self):
        kb = self

        class _S:
            def __enter__(s2):
                s2.prev = kb.stack
                kb.stack = ExitStack()
                kb.stack.__enter__()
                return s2

            def __exit__(s2, *a):
                kb.barrier()
                kb.stack.__exit__(None, None, None)
                kb.stack = s2.prev
                return False
        return _S()

    def ps(self, shape, dtype=F32, name="ps"):
        self.nid += 1
        nm = f"{name}_{self.nid}"
        return T(self.nc.alloc_psum_tensor(nm, list(shape), dtype), nm, excl=True)

    def dram(self, shape, dtype, name="dr", kind="Internal"):
        self.nid += 1
        nm = name if kind != "Internal" else f"{name}_{self.nid}"
        return T(self.nc.dram_tensor(nm, list(shape), dtype, kind=kind), nm)

    def _wait(self, e, sem, val):
        key = (e, id(sem))
        if self.waited.get(key, 0) >= val:
            return
        self.waited[key] = val
        self.eng[e].wait_ge(sem, val)

    def _deps(self, e, reads, writes, skip_same=False):
        deps = {}

        def add(d):
            if d is None:
                return
            s, v = d
            if deps.get(id(s), (None, 0))[1] < v:
                deps[id(s)] = (s, v)
        for t in reads:
            add(t.w)
            if t.excl:
                for s_v in t.r.values():
                    add(s_v)
        for t in writes:
            add(t.w)
            for s_v in t.r.values():
                add(s_v)
        for s, v in deps.values():
            if skip_same and s is self.sem.get(e):
                continue
            self._wait(e, s, v)

    def _mark(self, me, reads, writes):
        s, v = me
        for t in reads:
            t.r[id(s)] = me
        for t in writes:
            t.w = me; t.r = {}

    def op(self, e, fn, reads=(), writes=()):
        self._deps(e, reads, writes, skip_same=(e == "pe"))
        ins = fn(self.eng[e])
        self.cnt[e] += 1
        ins.then_inc(self.sem[e], 1)
        self._mark((self.sem[e], self.cnt[e]), reads, writes)
        return ins

    def dma(self, out, in_, reads=(), writes=(), q=None, **kw):
        if q is None:
            q = ("sp", "act", "pool")[self.dq % 2]
            self.dq += 1
        self._deps(q, reads, writes)
        li = self.lane_rr; self.lane_rr = (self.lane_rr + 1) % len(self.lanes)
        ls = self.lanes[li]
        if self.lane_cnt[li]:
            self._wait(q, ls, 16 * self.lane_cnt[li])
        ins = self.eng[q].dma_start(out=out, in_=in_, **kw)
        self.lane_cnt[li] += 1
        ins.then_inc(ls, 16)
        self._mark((ls, 16 * self.lane_cnt[li]), reads, writes)
        return ins

    def allgather(self, src, dst, groups):
        self._deps("pool", [src], [dst])
        ins = self.nc.gpsimd.collective_compute("AllGather", ALU.bypass, replica_groups=groups,
                                                ins=[src.h.ap().opt()], outs=[dst.h.ap().opt()])
        self.cc_cnt += 1
        ins.then_inc(self.cc)
        self._mark((self.cc, self.cc_cnt), [src], [dst])

    def finish(self, tiles, e="sp"):
        for t in tiles:
            if t.w is not None:
                self._wait(e, t.w[0], t.w[1])

    def mm(self, out_t, out_ap, lhsT_t, lhsT_ap, rhs_t, rhs_ap, start=True, stop=True, skip=False):
        return self.op("pe", lambda e: e.matmul(out_ap, lhsT_ap, rhs_ap, start=start, stop=stop, skip_group_check=skip),
                       reads=[lhsT_t, rhs_t], writes=[out_t])

    def act(self, out_t, out_ap, in_t, in_ap, func, extra=(), **kw):
        return self.op("act", lambda e: e.activation(out=out_ap, in_=in_ap, func=func, **kw),
                       reads=[in_t, *extra], writes=[out_t])

    def tt(self, out_t, out_ap, a_t, a_ap, b_t, b_ap, op, e="dve"):
        return self.op(e, lambda en: en.tensor_tensor(out=out_ap, in0=a_ap, in1=b_ap, op=op),
                       reads=[a_t, b_t], writes=[out_t])

    def ts(self, out_t, out_ap, a_t, a_ap, s1, s2, op0, op1=None, extra=(), e="dve"):
        if op1 is None:
            return self.op(e, lambda en: en.tensor_scalar(out=out_ap, in0=a_ap, scalar1=s1, scalar2=None, op0=op0),
                           reads=[a_t, *extra], writes=[out_t])
        return self.op(e, lambda en: en.tensor_scalar(out=out_ap, in0=a_ap, scalar1=s1, scalar2=s2, op0=op0, op1=op1),
                       reads=[a_t, *extra], writes=[out_t])

    def stt(self, out_t, out_ap, a_t, a_ap, s, b_t, b_ap, op0, op1, extra=()):
        return self.op("dve", lambda en: en.scalar_tensor_tensor(out=out_ap, in0=a_ap, scalar=s, in1=b_ap, op0=op0, op1=op1),
                       reads=[a_t, b_t, *extra], writes=[out_t])

    def copy(self, out_t, out_ap, in_t, in_ap, e="dve"):
        if e == "act":
            return self.op("act", lambda en: en.copy(out=out_ap, in_=in_ap), reads=[in_t], writes=[out_t])
        return self.op(e, lambda en: en.tensor_copy(out=out_ap, in_=in_ap), reads=[in_t], writes=[out_t])

    def memset(self, t, ap, val, e="pool"):
        return self.op(e, lambda en: en.memset(ap, val), reads=[], writes=[t])


def bcast_ap(t, n, offset=0, parts=128):
    return bass.AP(tensor=t.h, offset=offset, ap=[[0, parts], [1, n]])


CHUNK = 2048


class CD:
    def __init__(self, kb, rows, ntok, dtype, name, chunk=CHUNK):
        self.ch = [(c0, cn, kb.dram([rows, cn], dtype, f"{name}{i}")) for i, (c0, cn) in enumerate(groups_of(ntok, chunk))]

    def cols(self, s, n):
        for c0, cn, t in self.ch:
            if c0 <= s and s + n <= c0 + cn:
                return t, t[:, s - c0:s - c0 + n]
        raise ValueError((s, n))


def groups_of(n, g):
    return [(i, min(g, n - i)) for i in range(0, n, g)]


class Prog:
    def __init__(self, nl=NL, depth=DEPTH, debug=None):
        self.nl = nl; self.ntok = nl + NC; self.depth = depth
        self.debug = debug or []
        self.nc = bass.Bass("TRN2", target_bir_lowering=False)
        self.kb = KB(self.nc)
        self.inputs = {}
        self.outs = {}

    def inp(self, name, shape, dtype=F32):
        if name in self.inputs:
            return self.inputs[name]
        t = self.kb.dram(shape, dtype, name, kind="ExternalInput")
        self.inputs[name] = t
        return t

    def out(self, name, shape, dtype=F32):
        t = self.kb.dram(shape, dtype, name, kind="ExternalOutput")
        self.outs[name] = t
        return t

    def tgroups(self, gs=512):
        res = [(s, n, False) for s, n in groups_of(self.nl, gs)]
        res += [(self.nl + s, n, True) for s, n in groups_of(NC, gs)]
        return res

    def setup_consts(self):
        kb = self.kb
        self.ones_col = kb.sb([128, 1], F32, "ones_col")
        kb.memset(self.ones_col, self.ones_col[:], 1.0)
        self.ones_row = kb.sb([1, 128], F32, "ones_row")
        kb.memset(self.ones_row, self.ones_row[:], 1.0)
        sel = self.inp("c_sel8", [8, 2])
        self.sel8 = kb.sb([8, 2], F32, "sel8")
        kb.dma(self.sel8[:], sel[:], reads=[sel], writes=[self.sel8])
        self.bank = [kb.ps([128, 512], F32, f"bank{i}") for i in range(8)]

    def phase_mod(self, l):
        kb = self.kb
        wm = self.inp(f"wmod{l}", [128, 8, 12 * 128])
        bm = self.inp(f"bmod{l}", [128, 12])
        modv = self.modv[l]
        bsb = self.bm_stage
        kb.dma(bsb[:], bm[:], reads=[bm], writes=[bsb])
        ps = self.bank[0]
        for k in range(8):
            wt = self.wm_stage[k % 2]
            kb.dma(wt[:], wm[:, k, :], reads=[wm], writes=[wt])
            for c in range(12):
                kb.mm(ps, ps[:, 2 * c:2 * c + 2], wt, wt[:, c * 128:(c + 1) * 128], self.csil, self.csil[:, k, :],
                      start=(k == 0 and c == 0), stop=(k == 7), skip=True)
        for v in range(2):
            kb.tt(modv, modv[:, :, v], ps, ps[:, 0:24].rearrange("p (c v) -> p c v", v=2)[:, :, v], bsb, bsb[:], ALU.add)
        for which in (1, 4):
            kb.ts(modv, modv[:, 2 * which:2 * which + 2, :], modv, modv[:, 2 * which:2 * which + 2, :], 1.0, None, ALU.add)
        return modv

    def setup_mod(self):
        kb = self.kb
        cT = self.inp("cT", [128, 8, 2])
        self.csil = kb.sb([128, 8, 2], F32, "csil")
        craw = kb.sb([128, 8, 2], F32, "craw")
        kb.dma(craw[:], cT[:], reads=[cT], writes=[craw])
        kb.act(self.csil, self.csil[:], craw, craw[:], AF.Silu)
        self.modv = [kb.sb([128, 12, 2], F32, f"modv{l}") for l in range(self.depth)]
        with kb.scope():
            self.wm_stage = [kb.sb([128, 12 * 128], F32, f"wmst{i}") for i in range(2)]
            self.bm_stage = kb.sb([128, 12], F32, "bmst")
            for l in range(self.depth):
                self.phase_mod(l)

    def ln_alloc(self):
        kb = self.kb
        self.st_in = kb.dram([2, self.ntok], F32, "st_in")
        self.st_all = kb.dram([8, self.ntok], F32, "st_all")
        self.ln_sq = kb.sb([128, 512], F32, "ln_sq")
        self.ln_st = kb.sb([1, 2, 512], F32, "ln_st")
        self.ln_sa = kb.sb([8, 512], F32, "ln_sa")
        self.ln_v = [kb.sb([1, 512], F32, f"ln_v{i}") for i in range(4)]

    def ln_stats(self, xt, s, n):
        kb = self.kb
        p1, p2 = self.bank[6], self.bank[7]
        for j in range(2):
            kb.mm(p1, p1[0:1, :n], self.ones_col, self.ones_col[:], xt, xt[:, j, :n], start=(j == 0), stop=(j == 1))
        for j in range(2):
            kb.act(self.ln_sq, self.ln_sq[:, :n], xt, xt[:, j, :n], AF.Square)
            kb.mm(p2, p2[0:1, :n], self.ones_col, self.ones_col[:], self.ln_sq, self.ln_sq[:, :n], start=(j == 0), stop=(j == 1))
        kb.copy(self.ln_st, self.ln_st[:, 0, :n], p1, p1[0:1, :n])
        kb.copy(self.ln_st, self.ln_st[:, 1, :n], p2, p2[0:1, :n], e="act")
        kb.dma(self.st_in[:, s:s + n].rearrange("(o r) n -> o r n", o=1), self.ln_st[:, :, :n], reads=[self.ln_st], writes=[self.st_in])

    def ln_gather(self):
        self.kb.allgather(self.st_in, self.st_all, QUADS)

    def ln_coef(self, s, n):
        kb = self.kb
        p1, p2 = self.bank[6], self.bank[7]
        kb.dma(self.ln_sa[:, :n], self.st_all[:, s:s + n], reads=[self.st_all], writes=[self.ln_sa])
        kb.mm(p1, p1[0:1, :n], self.sel8, self.sel8[:, 0:1], self.ln_sa, self.ln_sa[:, :n])
        kb.mm(p2, p2[0:1, :n], self.sel8, self.sel8[:, 1:2], self.ln_sa, self.ln_sa[:, :n])
        m, v, r, q = self.ln_v
        kb.ts(m, m[:, :n], p1, p1[0:1, :n], 1.0 / D, None, ALU.mult)
        kb.tt(v, v[:, :n], m, m[:, :n], m, m[:, :n], ALU.mult)
        kb.stt(v, v[:, :n], p2, p2[0:1, :n], 1.0 / D, v, v[:, :n], ALU.mult, ALU.subtract)
        kb.ts(v, v[:, :n], v, v[:, :n], LN_EPS, None, ALU.add)
        kb.act(v, v[:, :n], v, v[:, :n], AF.Sqrt)
        kb.op('dve', lambda en: en.reciprocal(out=r[:, :n], in_=v[:, :n]), reads=[v], writes=[r])
        kb.stt(q, q[:, :n], m, m[:, :n], -1.0, r, r[:, :n], ALU.mult, ALU.mult)
        kb.mm(p1, p1[:, :n], self.ones_row, self.ones_row[:], r, r[:, :n])
        kb.mm(p2, p2[:, :n], self.ones_row, self.ones_row[:], q, q[:, :n])
        return p1, p2

    def phase_A(self, l, xT, which_shift, which_scale, hT_my, hT_all, h32=None):
        kb = self.kb
        modv = self.modv[l]
        for (s, n, isc) in self.tgroups():
            xt = self.xa[0]
            kb.dma(xt[:, :, :n], xT[:, :, s:s + n].rearrange("j p n -> p j n"), reads=[xT], writes=[xt])
            self.ln_stats(xt, s, n)
        self.ln_gather()
        for gi, (s, n, isc) in enumerate(self.tgroups()):
            xt = self.xa[gi % 2]
            kb.dma(xt[:, :, :n], xT[:, :, s:s + n].rearrange("j p n -> p j n"), reads=[xT], writes=[xt])
            p1, p2 = self.ln_coef(s, n)
            hb = self.hb[gi % 2]
            v = 1 if isc else 0
            for j in range(2):
                tmp = self.xtmp
                kb.tt(tmp, tmp[:, :n], xt, xt[:, j, :n], p1, p1[:, :n], ALU.mult)
                kb.tt(tmp, tmp[:, :n], tmp, tmp[:, :n], p2, p2[:, :n], ALU.add)
                kb.act(hb, hb[:, j, :n], tmp, tmp[:, :n], AF.Identity, extra=[modv],
                       scale=modv[:, 2 * which_scale + j, v:v + 1], bias=modv[:, 2 * which_shift + j, v:v + 1])
                if h32 is not None:
                    kb.act(h32[1], h32[1][:, j, :n], tmp, tmp[:, :n], AF.Identity, extra=[modv],
                           scale=modv[:, 2 * which_scale + j, v:v + 1], bias=modv[:, 2 * which_shift + j, v:v + 1])
            ht, hap = hT_my.cols(s, n)
            kb.dma(hap.rearrange("(j p) n -> p j n", p=128), hb[:, :, :n], reads=[hb], writes=[ht])
            if h32 is not None:
                kb.dma(h32[0][:, :, s:s + n].rearrange("j p n -> p j n"), h32[1][:, :, :n], reads=[h32[1]], writes=[h32[0]])
        for (c0, cn, tm), (_, _, ta) in zip(hT_my.ch, hT_all.ch):
            kb.allgather(tm, ta, QUADS)

    def alloc_A(self):
        kb = self.kb
        self.xa = [kb.sb([128, 2, 512], F32, f"xa{i}") for i in range(2)]
        self.hb = [kb.sb([128, 2, 512], BF16, f"hb{i}") for i in range(2)]
        self.xtmp = kb.sb([128, 512], F32, "xtmp")
        self.hT_my = CD(kb, 256, self.ntok, BF16, "hT_my")
        self.hT_all = CD(kb, 1024, self.ntok, BF16, "hT_all")


    def alloc_mix(self):
        kb = self.kb; nt = self.ntok
        self.HY = kb.dram([3, 128, nt], F32, "HY")
        self.GP = kb.dram([3, 128, nt], F32, "GP")
        self.GZ = kb.dram([nt, 128], F32, "GZ")
        self.GAB = kb.dram([nt, 8], F32, "GAB")
        self.DQ = kb.dram([128, nt], BF16, "DQ"); self.DK = kb.dram([128, nt], BF16, "DK")
        self.DV = kb.dram([nt, 128], BF16, "DV")
        self.NQ = kb.dram([128, nt], BF16, "NQ"); self.NK = kb.dram([128, nt], BF16, "NK")
        self.NV = kb.dram([nt, 128], BF16, "NV")
        self.YT_my = CD(kb, 512, nt, BF16, "YT_my", chunk=1024)
        self.YT_allc = CD(kb, 2048, nt, BF16, "YT_all", chunk=1024)
        self.cosT = self.inp("c_cosT", [128, nt]); self.sinT = self.inp("c_sinT", [128, nt])
        self.alloc_mix_ident()

    def alloc_mix_ident(self):
        kb = self.kb
        if hasattr(self, "ident"):
            return
        identb = self.inp("c_ident", [128, 128])
        self.ident = kb.sb([128, 128], F32, "ident")
        kb.dma(self.ident[:], identb[:], reads=[identb], writes=[self.ident])
        self.identb = kb.sb([128, 128], BF16, "identb")
        kb.copy(self.identb, self.identb[:], self.ident, self.ident[:])

    def phase_B(self, l):
        kb = self.kb
        wg = self.inp(f"wg{l}", [128, 8, NWG])
        with kb.scope():
            wgb = kb.sb([128, 8, NWG + 256], BF16, "wgb")
            stg = [kb.sb([128, NWG], F32, "wgst") for _ in range(2)]
            for k in range(8):
                st = stg[k % 2]
                kb.dma(st[:], wg[:, k, :], reads=[wg], writes=[st])
                kb.copy(wgb, wgb[:, k, 0:NWG], st, st[:], e=("dve", "pool")[k % 2])
            for k in range(8):
                for si, src in enumerate((7, 8)):
                    sv = wgb[:, k, src * 128:(src + 1) * 128].rearrange("p (a t s) -> p a t s", a=4, t=2, s=16)
                    dv = wgb[:, k, NWG + si * 128:NWG + (si + 1) * 128].rearrange("p (a t s) -> p a t s", a=4, t=2, s=16)
                    kb.ts(wgb, dv[:, :, 0, :], wgb, sv[:, :, 1, :], -1.0, None, ALU.mult)
                    kb.copy(wgb, dv[:, :, 1, :], wgb, sv[:, :, 0, :])
            hsb = [kb.sb([128, 8, 512], BF16, "hsb") for _ in range(2)]
            cst = kb.sb([128, 512], F32, "cst"); snt = kb.sb([128, 512], F32, "snt")
            evf = [kb.sb([128, 512], F32, "evf") for _ in range(2)]
            t1 = kb.sb([128, 512], F32, "t1"); t2 = kb.sb([128, 512], F32, "t2")
            ob = [kb.sb([128, 512], BF16, "ob") for _ in range(2)]
            tmz = [kb.sb([128, 128], F32, "tmz") for _ in range(2)]
            tmv = [kb.sb([128, 2, 128], BF16, "tmv") for _ in range(2)]
            tmg = [kb.sb([128, 8], F32, "tmg") for _ in range(2)]
            hT_all = self.hT_all
            import os
            bstop = int(os.environ.get("BSTOP", "9"))
            for gi, (s, n, isc) in enumerate(self.tgroups()):
                if bstop == 0:
                    break
                hs = hsb[gi % 2]
                ht, hap = hT_all.cols(s, n)
                kb.dma(hs[:, :, :n], hap.rearrange("(k p) n -> p k n", p=128), reads=[ht], writes=[hs])
                kb.dma(cst[:, :n], self.cosT[:, s:s + n], reads=[self.cosT], writes=[cst])
                kb.dma(snt[:, :n], self.sinT[:, s:s + n], reads=[self.sinT], writes=[snt])

                def fm(cb, bk):
                    for k in range(8):
                        kb.mm(bk, bk[:, :n], wgb, wgb[:, k, cb:cb + 128], hs, hs[:, k, :n], start=(k == 0), stop=(k == 7))
                for ci in range(6):
                    if bstop == 5:
                        break
                    bk = self.bank[ci % 4]; fm(ci * 128, bk)
                    ev = evf[ci % 2]
                    kb.copy(ev, ev[:, :n], bk, bk[:, :n], e=("dve", "act")[ci % 2])
                    dst = self.HY if ci < 3 else self.GP
                    kb.dma(dst[ci % 3, :, s:s + n], ev[:, :n], reads=[ev], writes=[dst])
                if bstop == 1:
                    continue
                for qi, (cb, rb, dst) in enumerate(()) if bstop == 5 else []:
                    pass
                for qi, (cb, rb, dst) in enumerate(((7 * 128, NWG, self.DQ), (8 * 128, NWG + 128, self.DK)) if bstop != 5 else ()):
                    b0, b1 = self.bank[0 + 2 * qi], self.bank[1 + 2 * qi]
                    fm(cb, b0); fm(rb, b1)
                    kb.tt(t1, t1[:, :n], b0, b0[:, :n], cst, cst[:, :n], ALU.mult)
                    kb.tt(t2, t2[:, :n], b1, b1[:, :n], snt, snt[:, :n], ALU.mult)
                    o = ob[qi]
                    kb.tt(o, o[:, :n], t1, t1[:, :n], t2, t2[:, :n], ALU.add, e="pool")
                    kb.dma(dst[:, s:s + n], o[:, :n], reads=[o], writes=[dst])
                if bstop == 2:
                    continue
                for qi, (cb, dst) in enumerate(((10 * 128, self.NQ), (11 * 128, self.NK)) if bstop != 5 else ()):
                    bk = self.bank[qi]; fm(cb, bk)
                    o = ob[qi]
                    kb.copy(o, o[:, :n], bk, bk[:, :n], e=("dve", "act")[qi])
                    kb.dma(dst[:, s:s + n], o[:, :n], reads=[o], writes=[dst])
                if bstop == 3:
                    continue
                for tt_ in range(n // 128):
                    bk = self.bank[int(os.environ.get("TMB", "4")) + tt_ % 2]
                    tsl = slice(tt_ * 128, (tt_ + 1) * 128)
                    for (cb, w, off) in ((6 * 128, 128, 0), (9 * 128, 128, 128), (12 * 128, 128, 256), (13 * 128, 8, 384))[:int(os.environ.get('TMN', '4'))]:
                        for k in range(8):
                            if os.environ.get("TMX", "") == "nomm":
                                break
                            kb.mm(bk, bk[:, off:off + w], hs, hs[:, k, tsl], wgb, wgb[:, k, cb:cb + w], start=(k == 0), stop=(k == 7))
                    if os.environ.get("TMX", "") == "nocopy":
                        continue
                    tok0 = s + tt_ * 128
                    z = tmz[tt_ % 2]; v = tmv[tt_ % 2]; g_ = tmg[tt_ % 2]
                    tmd = int(os.environ.get("TMD", "15"))
                    kb.copy(z, z[:], bk, bk[:, 0:128], e=os.environ.get("TME", "act"))
                    if tmd & 1:
                        kb.dma(self.GZ[tok0:tok0 + 128, :], z[:], reads=[z], writes=[self.GZ])
                    kb.copy(v, v[:, 0, :], bk, bk[:, 128:256])
                    kb.copy(v, v[:, 1, :], bk, bk[:, 256:384])
                    if tmd & 2:
                        kb.dma(self.DV[tok0:tok0 + 128, :], v[:, 0, :], reads=[v], writes=[self.DV])
                    if tmd & 4:
                        kb.dma(self.NV[tok0:tok0 + 128, :], v[:, 1, :], reads=[v], writes=[self.NV])
                    kb.copy(g_, g_[:], bk, bk[:, 384:392], e=os.environ.get("TME", "act"))
                    if tmd & 8:
                        kb.dma(self.GAB[tok0:tok0 + 128, :], g_[:], reads=[g_], writes=[self.GAB])

    def store_T(self, src_t, src_ap, nq, row0, tok0, bank, stage):
        kb = self.kb
        pb = bank
        pv = pb[:].bitcast(BF16)
        kb.op("pe", lambda e: e.transpose(pv[:, 0:nq], src_ap, self.identb[0:nq, 0:nq]), reads=[src_t, self.identb], writes=[pb])
        kb.copy(stage, stage[:, 0:nq], pb, pv[:, 0:nq])
        yt, yap = self.YT_my.cols(tok0, nq)
        kb.dma(yap[row0:row0 + 128, :], stage[:, 0:nq], reads=[stage], writes=[yt])

    def phase_diff(self, l, lam_init):
        kb = self.kb; nt = self.ntok; ntile = nt // 128
        dl = self.inp(f"dlam{l}", [4, 64]); dn = self.inp(f"dnorm{l}", [1, 128])
        with kb.scope():
            kT = kb.sb([128, nt], BF16, "kT")
            kb.dma(kT[:], self.DK[:], reads=[self.DK], writes=[kT])
            vA = kb.sb([128, ntile, 130], BF16, "vA")
            kb.memset(vA, vA[:, :, 128:130], 1.0)
            for t0, tn in groups_of(ntile, 8):
                kb.dma(vA[:, t0:t0 + tn, 0:128], self.DV[t0 * 128:(t0 + tn) * 128, :].rearrange("(t p) c -> p t c", p=128), reads=[self.DV], writes=[vA])
            lm = kb.sb([128, 4, 64], F32, "lm")
            kb.dma(lm[:].rearrange("p a b -> p (a b)"), bcast_ap(dl, 256), reads=[dl], writes=[lm])
            lp = kb.sb([128, 2, 64], F32, "lp"); ls = kb.sb([128, 4], F32, "ls")
            kb.tt(lp, lp[:, 0, :], lm, lm[:, 0, :], lm, lm[:, 1, :], ALU.mult)
            kb.tt(lp, lp[:, 1, :], lm, lm[:, 2, :], lm, lm[:, 3, :], ALU.mult)
            kb.op("dve", lambda e: e.tensor_reduce(out=ls[:, 0:2], in_=lp[:], axis=AX.X, op=ALU.add), reads=[lp], writes=[ls])
            kb.act(ls, ls[:, 0:2], ls, ls[:, 0:2], AF.Exp)
            kb.tt(ls, ls[:, 2:3], ls, ls[:, 0:1], ls, ls[:, 1:2], ALU.subtract)
            kb.ts(ls, ls[:, 3:4], ls, ls[:, 2:3], lam_init, -1.0, ALU.add, ALU.mult)
            nw = kb.sb([128, 128], F32, "nw")
            kb.dma(nw[:], bcast_ap(dn, 128), reads=[dn], writes=[nw])
            kb.ts(nw, nw[:], nw, nw[:], 1.0 - lam_init, None, ALU.mult)
            qsb = [kb.sb([128, 512], BF16, "qsb") for _ in range(2)]
            pT = [kb.sb([128, 512], BF16, "pT") for _ in range(4)]
            rr = kb.sb([128, 4], F32, "rr"); of = kb.sb([128, 128], F32, "of"); osq = kb.sb([128, 128], F32, "osq")
            ob16 = [kb.sb([128, 128], BF16, "ob16") for _ in range(2)]
            stg = [kb.sb([128, 128], BF16, "stg") for _ in range(2)]
            sc = 64 ** -0.5
            pi = 0
            import os
            dstop = int(os.environ.get("DSTOP", "9"))
            for gi, (s, n, isc) in enumerate(self.tgroups()):
                if dstop == 0:
                    break
                q = qsb[gi % 2]
                kb.dma(q[:, :n], self.DQ[:, s:s + n], reads=[self.DQ], writes=[q])
                ktiles = list(range(self.nl // 128, ntile)) if isc else list(range(ntile))
                nqt = n // 128
                first = {}
                for ki, kt in enumerate(ktiles):
                    for c in range(2):
                        sb_ = self.bank[4 + (pi % 4)]
                        kb.mm(sb_, sb_[:, :n], kT, kT[c * 64:(c + 1) * 64, kt * 128:(kt + 1) * 128], q, q[c * 64:(c + 1) * 64, :n])
                        p = pT[pi % 4]; pi += 1
                        kb.act(p, p[:, :n], sb_, sb_[:, :n], AF.Exp, scale=sc)
                        for qt in range(nqt):
                            ab = self.bank[c * 2 + qt // 2]; off = (qt % 2) * 160
                            st = id(ab) not in first
                            first[id(ab)] = 1
                            kb.mm(ab, ab[:, off:off + 129], p, p[:, qt * 128:(qt + 1) * 128], vA, vA[:, kt, 0:129],
                                  start=st, stop=(ki == len(ktiles) - 1), skip=True)
                for qt in range(nqt):
                    if dstop == 1:
                        break
                    a0 = self.bank[0 + qt // 2]; a1 = self.bank[2 + qt // 2]; off = (qt % 2) * 160
                    kb.op("dve", lambda e: e.reciprocal(out=rr[:, 0:1], in_=a0[:, off + 128:off + 129]), reads=[a0], writes=[rr])
                    kb.op("dve", lambda e: e.reciprocal(out=rr[:, 1:2], in_=a1[:, off + 128:off + 129]), reads=[a1], writes=[rr])
                    kb.tt(rr, rr[:, 1:2], rr, rr[:, 1:2], ls, ls[:, 3:4], ALU.mult)
                    kb.ts(of, of[:], a0, a0[:, off:off + 128], rr[:, 0:1], None, ALU.mult, extra=[rr])
                    kb.stt(of, of[:], a1, a1[:, off:off + 128], rr[:, 1:2], of, of[:], ALU.mult, ALU.add, extra=[rr])
                    kb.op("act", lambda e: e.activation(out=osq[:], in_=of[:], func=AF.Square, accum_out=rr[:, 2:3]), reads=[of], writes=[osq, rr])
                    kb.ts(rr, rr[:, 2:3], rr, rr[:, 2:3], 1.0 / 128, RMS_EPS, ALU.mult, ALU.add)
                    kb.act(rr, rr[:, 2:3], rr, rr[:, 2:3], AF.Sqrt)
                    kb.op("dve", lambda e: e.reciprocal(out=rr[:, 3:4], in_=rr[:, 2:3]), reads=[rr], writes=[rr])
                    o16 = ob16[qt % 2]
                    kb.stt(o16, o16[:], of, of[:], rr[:, 3:4], nw, nw[:], ALU.mult, ALU.mult, extra=[rr])
                    if dstop == 2:
                        continue
                    self.store_T(o16, o16[:], 128, 256, s + qt * 128, self.bank[4 + qt % 2], stg[qt % 2])


    def phase_na(self, l):
        kb = self.kb; nt = self.ntok; nl = self.nl; R = nl // 64
        nb = self.inp(f"nabias{l}", [64, 2 * 15 * 64])
        nm = self.inp("c_namask", [64, 64])
        sc = 64 ** -0.5
        with kb.scope():
            kT = kb.sb([128, nt], BF16, "nkT"); qT = kb.sb([128, nt], BF16, "nqT")
            kb.dma(kT[:], self.NK[:], reads=[self.NK], writes=[kT])
            kb.dma(qT[:], self.NQ[:], reads=[self.NQ], writes=[qT])
            v64 = kb.sb([64, R, 2, 65], BF16, "v64")
            kb.memset(v64, v64[:, :, :, 64:65], 1.0)
            for r0, rn in groups_of(R, 16):
                for hh in range(2):
                    kb.dma(v64[:, r0:r0 + rn, hh, 0:64],
                           self.NV[r0 * 64:(r0 + rn) * 64, hh * 64:(hh + 1) * 64].rearrange("(r p) c -> p r c", p=64),
                           reads=[self.NV], writes=[v64])
            vcx = kb.sb([128, 2, 2, 65], BF16, "vcx")
            kb.memset(vcx, vcx[:, :, :, 64:65], 1.0)
            for hh in range(2):
                kb.dma(vcx[:, :, hh, 0:64], self.NV[nl:nt, hh * 64:(hh + 1) * 64].rearrange("(t p) c -> p t c", p=128), reads=[self.NV], writes=[vcx])
            EB = kb.sb([64, 2, 15, 64], F32, "EB"); msk = kb.sb([64, 64], F32, "msk")
            kb.dma(EB[:].rearrange("p a b c -> p (a b c)"), nb[:], reads=[nb], writes=[EB])
            kb.dma(msk[:], nm[:], reads=[nm], writes=[msk])
            kb.act(EB, EB[:].rearrange("p a b c -> p (a b c)"), EB, EB[:].rearrange("p a b c -> p (a b c)"), AF.Exp)
            for hh in range(2):
                for dr in range(15):
                    kb.tt(EB, EB[:, hh, dr, :], EB, EB[:, hh, dr, :], msk, msk[:], ALU.mult)
            pwf = [kb.sb([64, 512], F32, "pwf") for _ in range(2)]
            pwb = [kb.sb([64, 512], BF16, "pwb") for _ in range(2)]
            pcb = [kb.sb([128, 256], BF16, "pcb") for _ in range(2)]
            rr = kb.sb([128, 4], F32, "nrr")
            o16 = [kb.sb([128, 128], BF16, "no16") for _ in range(2)]
            stg = [kb.sb([128, 128], BF16, "nstg") for _ in range(2)]
            it = 0
            for r in range(R):
                rs = min(max(r - 4, 0), R - 8)
                dr0 = rs - r + 7
                o = o16[r % 2]
                for hh in range(2):
                    hp = slice(hh * 64, (hh + 1) * 64)
                    sbk = self.bank[4 + it % 2]; sck = self.bank[6 + it % 2]; ab = self.bank[it % 2]
                    qv = qT[hp, r * 64:(r + 1) * 64]
                    for j in range(8):
                        kb.mm(sbk, sbk[0:64, j * 64:(j + 1) * 64], kT, kT[hp, (rs + j) * 64:(rs + j + 1) * 64], qT, qv)
                    for t in range(2):
                        kb.mm(sck, sck[:, t * 64:(t + 1) * 64], kT, kT[hp, nl + t * 128:nl + (t + 1) * 128], qT, qv)
                    pf = pwf[it % 2]; pb = pwb[it % 2]; pc = pcb[it % 2]
                    kb.act(pf, pf[:], sbk, sbk[0:64, :], AF.Exp, scale=sc)
                    kb.tt(pb, pb[:], pf, pf[:], EB, EB[:, hh, dr0:dr0 + 8, :].rearrange("p a b -> p (a b)"), ALU.mult)
                    kb.act(pc, pc[:, 0:128], sck, sck[:, 0:128], AF.Exp, scale=sc)
                    for j in range(8):
                        kb.mm(ab, ab[0:64, 0:65], pb, pb[:, j * 64:(j + 1) * 64], v64, v64[:, rs + j, hh, :], start=(j == 0), stop=False)
                    for t in range(2):
                        kb.mm(ab, ab[0:64, 0:65], pc, pc[:, t * 64:(t + 1) * 64], vcx, vcx[:, t, hh, :], start=False, stop=(t == 1))
                    kb.op("dve", lambda e: e.reciprocal(out=rr[0:64, hh:hh + 1], in_=ab[0:64, 64:65]), reads=[ab], writes=[rr])
                    kb.ts(o, o[0:64, hp], ab, ab[0:64, 0:64], rr[0:64, hh:hh + 1], None, ALU.mult, extra=[rr])
                    it += 1
                self.store_T(o, o[0:64, :], 64, 384, r * 64, self.bank[2 + r % 2], stg[r % 2])
            for qt in range(2):
                o = o16[qt]
                for hh in range(2):
                    hp = slice(hh * 64, (hh + 1) * 64)
                    sck = self.bank[6 + it % 2]; ab = self.bank[it % 2]; pc = pcb[it % 2]
                    for t in range(2):
                        kb.mm(sck, sck[:, t * 128:(t + 1) * 128], kT, kT[hp, nl + t * 128:nl + (t + 1) * 128],
                              qT, qT[hp, nl + qt * 128:nl + (qt + 1) * 128])
                    kb.act(pc, pc[:, 0:256], sck, sck[:, 0:256], AF.Exp, scale=sc)
                    for t in range(2):
                        kb.mm(ab, ab[:, 0:65], pc, pc[:, t * 128:(t + 1) * 128], vcx, vcx[:, t, hh, :], start=(t == 0), stop=(t == 1))
                    kb.op("dve", lambda e: e.reciprocal(out=rr[:, hh:hh + 1], in_=ab[:, 64:65]), reads=[ab], writes=[rr])
                    kb.ts(o, o[:, hp], ab, ab[:, 0:64], rr[:, hh:hh + 1], None, ALU.mult, extra=[rr])
                    it += 1
                self.store_T(o, o[:], 128, 384, nl + qt * 128, self.bank[2 + qt], stg[qt])


    def alloc_tail(self):
        kb = self.kb; nt = self.ntok
        if not hasattr(self, "YT_allc"):
            self.YT_allc = CD(kb, 2048, nt, BF16, "YT_all", chunk=1024)
        self.mT_my = CD(kb, 256, nt, BF16, "mT_my"); self.mT_all = CD(kb, 1024, nt, BF16, "mT_all")
        self.preT = kb.dram([2, 128, nt], F32, "preT")
        self.x1T = kb.dram([2, 128, nt], F32, "x1T")

    def phase_merge(self, l, xT):
        kb = self.kb
        wgi = self.inp(f"wgate{l}", [128, 8, 1024]); bpi = self.inp(f"bp{l}", [128, 16, 256])
        woi = self.inp(f"wout{l}", [128, 8, 256]); lnp = self.inp(f"lnp{l}", [128, 2, 4])
        modv = self.modv[l]
        with kb.scope():
            wgt = kb.sb([128, 8, 1024], BF16, "wgt"); bpt = kb.sb([128, 16, 256], BF16, "bpt")
            wot = kb.sb([128, 8, 256], BF16, "wot"); lnt = kb.sb([128, 2, 4], F32, "lnt")
            stg = [kb.sb([128, 1024], F32, "mstg") for _ in range(2)]
            si = 0
            for k in range(8):
                st = stg[si % 2]; si += 1
                kb.dma(st[:], wgi[:, k, :], reads=[wgi], writes=[st])
                kb.copy(wgt, wgt[:, k, :], st, st[:], e=("dve", "pool")[k % 2])
            for q4 in range(4):
                st = stg[si % 2]; si += 1
                kb.dma(st[:].rearrange("p (a b) -> p a b", a=4), bpi[:, 4 * q4:4 * q4 + 4, :], reads=[bpi], writes=[st])
                kb.copy(bpt, bpt[:, 4 * q4:4 * q4 + 4, :].rearrange("p a b -> p (a b)"), st, st[:], e=("dve", "pool")[q4 % 2])
            for k2 in range(2):
                st = stg[si % 2]; si += 1
                kb.dma(st[:].rearrange("p (a b) -> p a b", a=4), woi[:, 4 * k2:4 * k2 + 4, :], reads=[woi], writes=[st])
                kb.copy(wot, wot[:, 4 * k2:4 * k2 + 4, :].rearrange("p a b -> p (a b)"), st, st[:], e=("dve", "pool")[k2 % 2])
            kb.dma(lnt[:], lnp[:], reads=[lnp], writes=[lnt])
            hsb = [kb.sb([128, 8, 512], BF16, "mhs") for _ in range(2)]
            ysb = [kb.sb([128, 16, 512], BF16, "mys") for _ in range(2)]
            sg = kb.sb([128, 512], F32, "msg"); tmp = kb.sb([128, 512], F32, "mtmp"); acc = kb.sb([128, 512], F32, "macc")
            mb = [kb.sb([128, 2, 512], BF16, "mmb") for _ in range(2)]
            it = 0
            for gi, (s, n, isc) in enumerate(self.tgroups()):
                hs = hsb[gi % 2]; ys = ysb[gi % 2]; mo = mb[gi % 2]
                ht, hap = self.hT_all.cols(s, n)
                kb.dma(hs[:, :, :n], hap.rearrange("(k p) n -> p k n", p=128), reads=[ht], writes=[hs])
                yt, yap = self.YT_allc.cols(s, n)
                for hh in range(2):
                    kb.dma(ys[:, 8 * hh:8 * hh + 8, :n], yap[1024 * hh:1024 * (hh + 1), :].rearrange("(q p) n -> p q n", p=128), reads=[yt], writes=[ys])
                for j in range(2):
                    for m in range(4):
                        A = self.bank[(2 * it) % 6]; B = self.bank[(2 * it + 1) % 6]; it += 1
                        cb = m * 256 + j * 128
                        for k in range(8):
                            kb.mm(A, A[:, :n], wgt, wgt[:, k, cb:cb + 128], hs, hs[:, k, :n], start=(k == 0), stop=(k == 7))
                        for g2 in range(4):
                            kb.mm(B, B[:, :n], bpt, bpt[:, g2 * 4 + m, j * 128:(j + 1) * 128], ys, ys[:, g2 * 4 + m, :n], start=(g2 == 0), stop=(g2 == 3))
                        kb.act(sg, sg[:, :n], A, A[:, :n], AF.Sigmoid)
                        if m == 0:
                            kb.tt(acc, acc[:, :n], sg, sg[:, :n], B, B[:, :n], ALU.mult)
                        else:
                            kb.tt(tmp, tmp[:, :n], sg, sg[:, :n], B, B[:, :n], ALU.mult)
                            if m < 3:
                                kb.tt(acc, acc[:, :n], acc, acc[:, :n], tmp, tmp[:, :n], ALU.add, e="pool")
                            else:
                                kb.tt(mo, mo[:, j, :n], acc, acc[:, :n], tmp, tmp[:, :n], ALU.add, e="pool")
                mt, map_ = self.mT_my.cols(s, n)
                kb.dma(map_.rearrange("(j p) n -> p j n", p=128), mo[:, :, :n], reads=[mo], writes=[mt])
            for (c0, cn, tm), (_, _, ta) in zip(self.mT_my.ch, self.mT_all.ch):
                kb.allgather(tm, ta, QUADS)
            pre = [kb.sb([128, 2, 512], F32, "mpre") for _ in range(2)]
            xts = [kb.sb([128, 2, 512], F32, "mxt") for _ in range(2)]
            for gi, (s, n, isc) in enumerate(self.tgroups()):
                ms = hsb[gi % 2]; xt = xts[gi % 2]; pr = pre[gi % 2]
                v = 1 if isc else 0
                mt, map_ = self.mT_all.cols(s, n)
                kb.dma(ms[:, :, :n], map_.rearrange("(k p) n -> p k n", p=128), reads=[mt], writes=[ms])
                kb.dma(xt[:, :, :n], xT[:, :, s:s + n].rearrange("j p n -> p j n"), reads=[xT], writes=[xt])
                for j in range(2):
                    A = self.bank[j]
                    for k in range(8):
                        kb.mm(A, A[:, :n], wot, wot[:, k, j * 128:(j + 1) * 128], ms, ms[:, k, :n], start=(k == 0), stop=(k == 7))
                    kb.act(tmp, tmp[:, :n], A, A[:, :n], AF.Identity, extra=[modv], scale=modv[:, 2 * 2 + j, v:v + 1])
                    kb.stt(pr, pr[:, j, :n], xt, xt[:, j, :n], DN_ALPHA, tmp, tmp[:, :n], ALU.mult, ALU.add)
                self.ln_stats(pr, s, n)
                kb.dma(self.preT[:, :, s:s + n].rearrange("j p n -> p j n"), pr[:, :, :n], reads=[pr], writes=[self.preT])
            self.ln_gather()
            for gi, (s, n, isc) in enumerate(self.tgroups()):
                pr = pre[gi % 2]; xo = xts[gi % 2]
                kb.dma(pr[:, :, :n], self.preT[:, :, s:s + n].rearrange("j p n -> p j n"), reads=[self.preT], writes=[pr])
                p1, p2 = self.ln_coef(s, n)
                for j in range(2):
                    kb.tt(tmp, tmp[:, :n], pr, pr[:, j, :n], p1, p1[:, :n], ALU.mult)
                    kb.tt(tmp, tmp[:, :n], tmp, tmp[:, :n], p2, p2[:, :n], ALU.add)
                    kb.act(xo, xo[:, j, :n], tmp, tmp[:, :n], AF.Identity, extra=[lnt], scale=lnt[:, j, 0:1], bias=lnt[:, j, 1:2])
                kb.dma(self.x1T[:, :, s:s + n].rearrange("j p n -> p j n"), xo[:, :, :n], reads=[xo], writes=[self.x1T])


    def alloc_moe(self):
        kb = self.kb; nt = self.ntok
        self.h2T_my = CD(kb, 256, nt, BF16, "h2T_my"); self.h2T_all = CD(kb, 1024, nt, BF16, "h2T_all")
        self.h2f = kb.dram([2, 128, nt], F32, "h2f")
        self.h32t = kb.sb([128, 2, 512], F32, "h32t")
        self.lg_my = kb.dram([16, nt], F32, "lg_my"); self.lg_all = kb.dram([64, nt], F32, "lg_all")
        self.GTd = kb.dram([16, nt], F32, "GTd")
        self.ypc = [(c0, cn, kb.dram([1024, cn], F32, f"ypc{i}"), kb.dram([256, cn], F32, f"yrc{i}"))
                    for i, (c0, cn) in enumerate(groups_of(nt, 1024))]
        self.x2T = kb.dram([2, 128, nt], F32, "x2T")

    def ypcols(self, s, n):
        for c0, cn, tp, tr in self.ypc:
            if c0 <= s and s + n <= c0 + cn:
                return tp, tr, s - c0
        raise ValueError((s, n))

    def phase_moe(self, l):
        kb = self.kb; nt = self.ntok
        modv = self.modv[l]
        rwi = self.inp("rw", [128, 2, 16]); rbi = self.inp("rb", [1, 16]); cmb = self.inp("c_comb", [64, 16])
        seli = self.inp("c_esel", [16, 4 * 128]); lnp = self.inp(f"lnp{l}", [128, 2, 4]) if f"lnp{l}" not in self.inputs else self.inputs[f"lnp{l}"]
        e1 = self.inp(f"ew1_{l}", [4, 128, 8 * 512]); e3 = self.inp(f"ew3_{l}", [4, 128, 8 * 512]); e2 = self.inp(f"ew2_{l}", [4, 128, 4 * 1024])
        self.phase_A(l, self.x1T, 3, 4, self.h2T_my, self.h2T_all, h32=(self.h2f, self.h32t))
        BIG = 1.0e9
        with kb.scope():
            rw = kb.sb([128, 2, 16], F32, "rw"); rb = kb.sb([128, 16], F32, "rb"); comb = kb.sb([64, 16], F32, "comb")
            esel = kb.sb([16, 4, 128], F32, "esel"); lnt = kb.sb([128, 2, 4], F32, "lnt2")
            kb.dma(rw[:], rwi[:], reads=[rwi], writes=[rw])
            kb.dma(rb[:], bcast_ap(rbi, 16), reads=[rbi], writes=[rb])
            kb.dma(comb[:], cmb[:], reads=[cmb], writes=[comb])
            kb.dma(esel[:].rearrange("p a b -> p (a b)"), seli[:], reads=[seli], writes=[esel])
            kb.dma(lnt[:], lnp[:], reads=[lnp], writes=[lnt])
            hf = [kb.sb([128, 2, 512], F32, "hf") for _ in range(2)]
            lgs = [kb.sb([16, 512], F32, "lgs") for _ in range(2)]
            for gi, (s, n, isc) in enumerate(self.tgroups()):
                h = hf[gi % 2]; lg = lgs[gi % 2]
                kb.dma(h[:, :, :n], self.h2f[:, :, s:s + n].rearrange("j p n -> p j n"), reads=[self.h2f], writes=[h])
                A = self.bank[gi % 2]
                for j in range(2):
                    kb.mm(A, A[0:16, :n], rw, rw[:, j, :], h, h[:, j, :n], start=(j == 0), stop=(j == 1))
                kb.copy(lg, lg[:, :n], A, A[0:16, :n])
                kb.dma(self.lg_my[:, s:s + n], lg[:, :n], reads=[lg], writes=[self.lg_my])
            kb.allgather(self.lg_my, self.lg_all, QUADS)
            lgt = [kb.sb([64, 128], F32, "lgt") for _ in range(2)]
            S = kb.sb([128, 16], F32, "rS"); sel = kb.sb([128, 16], F32, "rsel"); selm = kb.sb([128, 16], F32, "rselm")
            pr = kb.sb([128, 6, 4], F32, "rpr"); gs = kb.sb([128, 4], F32, "rgs"); sc1 = kb.sb([128, 8], F32, "rsc")
            eq = kb.sb([128, 4], F32, "req"); is1 = kb.sb([128, 16], F32, "ris1"); is2 = kb.sb([128, 16], F32, "ris2")
            G = kb.sb([128, 16], F32, "rG"); gtt = [kb.sb([16, 512], F32, "gtt") for _ in range(2)]
            ntile = nt // 128
            for ti in range(ntile):
                t0 = ti * 128
                lt = lgt[ti % 2]
                kb.dma(lt[:], self.lg_all[:, t0:t0 + 128], reads=[self.lg_all], writes=[lt])
                A = self.bank[ti % 2]
                kb.mm(A, A[:, 0:16], lt, lt[:], comb, comb[:])
                kb.act(S, S[:], A, A[:, 0:16], AF.Sigmoid)
                kb.tt(sel, sel[:], S, S[:], rb, rb[:], ALU.add)
                sv = sel[:].rearrange("p (g k) -> p g k", k=4)
                pairs = ((0, 1), (0, 2), (0, 3), (1, 2), (1, 3), (2, 3))
                for pi_, (a, b_) in enumerate(pairs):
                    kb.tt(pr, pr[:, pi_, :], sel, sv[:, :, a], sel, sv[:, :, b_], ALU.add)
                kb.tt(gs, gs[:], pr, pr[:, 0, :], pr, pr[:, 1, :], ALU.max)
                for pi_ in range(2, 6):
                    kb.tt(gs, gs[:], gs, gs[:], pr, pr[:, pi_, :], ALU.max)
                kb.op("dve", lambda e: e.tensor_reduce(out=sc1[:, 0:1], in_=gs[:], axis=AX.X, op=ALU.max), reads=[gs], writes=[sc1])
                kb.ts(eq, eq[:], gs, gs[:], sc1[:, 0:1], None, ALU.is_equal, extra=[sc1])
                kb.ts(eq, eq[:], eq, eq[:], -1.0, BIG, ALU.add, ALU.mult)
                smv = selm[:].rearrange("p (g k) -> p g k", k=4)
                for k in range(4):
                    kb.tt(selm, smv[:, :, k], sel, sv[:, :, k], eq, eq[:], ALU.add)
                kb.op("dve", lambda e: e.tensor_reduce(out=sc1[:, 1:2], in_=selm[:], axis=AX.X, op=ALU.max), reads=[selm], writes=[sc1])
                kb.ts(is1, is1[:], selm, selm[:], sc1[:, 1:2], None, ALU.is_equal, extra=[sc1])
                kb.stt(selm, selm[:], is1, is1[:], -BIG, selm, selm[:], ALU.mult, ALU.add)
                kb.op("dve", lambda e: e.tensor_reduce(out=sc1[:, 2:3], in_=selm[:], axis=AX.X, op=ALU.max), reads=[selm], writes=[sc1])
                kb.ts(is2, is2[:], selm, selm[:], sc1[:, 2:3], None, ALU.is_equal, extra=[sc1])
                kb.tt(is1, is1[:], is1, is1[:], is2, is2[:], ALU.add)
                kb.tt(is1, is1[:], is1, is1[:], S, S[:], ALU.mult)
                kb.op("dve", lambda e: e.tensor_reduce(out=sc1[:, 3:4], in_=is1[:], axis=AX.X, op=ALU.add), reads=[is1], writes=[sc1])
                kb.op("dve", lambda e: e.reciprocal(out=sc1[:, 4:5], in_=sc1[:, 3:4]), reads=[sc1], writes=[sc1])
                kb.ts(G, G[:], is1, is1[:], sc1[:, 4:5], None, ALU.mult, extra=[sc1])
                B = self.bank[2 + ti % 2]
                kb.op("pe", lambda e: e.transpose(B[0:16, 0:128], G[:], self.ident[:]), reads=[G, self.ident], writes=[B])
                gt_ = gtt[(ti // 4) % 2]
                kb.copy(gt_, gt_[:, (ti % 4) * 128:(ti % 4 + 1) * 128], B, B[0:16, 0:128])
                if ti % 4 == 3 or ti == ntile - 1:
                    g0 = (ti // 4) * 512; gn = t0 + 128 - g0
                    kb.dma(self.GTd[:, g0:g0 + gn], gt_[:, :gn], reads=[gt_], writes=[self.GTd])
        with kb.scope():
            w1 = [kb.sb([128, 8, 512], BF16, "w1") for _ in range(4)]
            w3 = [kb.sb([128, 8, 512], BF16, "w3") for _ in range(4)]
            w2 = [kb.sb([128, 4, 1024], BF16, "w2") for _ in range(4)]
            stg = [kb.sb([128, 1024], F32, "estg") for _ in range(2)]
            si = 0
            for i in range(4):
                for (src, dstt) in ((e1, w1[i]), (e3, w3[i]), (e2, w2[i])):
                    dflat = dstt[:].rearrange("p a b -> p (a b)")
                    for hh in range(4):
                        st = stg[si % 2]
                        kb.dma(st[:], src[i, :, hh * 1024:(hh + 1) * 1024], reads=[src], writes=[st])
                        kb.copy(dstt, dflat[:, hh * 1024:(hh + 1) * 1024], st, st[:], e=("dve", "pool")[si % 2])
                        si += 1
            esel = kb.sb([16, 4, 128], F32, "esel2")
            kb.dma(esel[:].rearrange("p a b -> p (a b)"), seli[:], reads=[seli], writes=[esel])
            h2 = [kb.sb([128, 8, 512], BF16, "eh2") for _ in range(2)]
            gt = [kb.sb([16, 512], F32, "egt") for _ in range(2)]
            hp = [kb.sb([128, 4, 512], BF16, "ehp") for _ in range(4)]
            sl = kb.sb([128, 512], F32, "esl"); tq = kb.sb([128, 512], F32, "etq")
            yo = [kb.sb([128, 512], F32, "eyo") for _ in range(2)]
            for gi, (s, n, isc) in enumerate(self.tgroups()):
                h = h2[gi % 2]; g_ = gt[gi % 2]
                ht, hap = self.h2T_all.cols(s, n)
                kb.dma(h[:, :, :n], hap.rearrange("(k p) n -> p k n", p=128), reads=[ht], writes=[h])
                kb.dma(g_[:, :n], self.GTd[:, s:s + n], reads=[self.GTd], writes=[g_])
                for i in range(4):
                    Gb = self.bank[5]
                    kb.mm(Gb, Gb[:, :n], esel, esel[:, i, :], g_, g_[:, :n])
                    for fc in range(4):
                        A = self.bank[(fc % 2) * 2]; B = self.bank[(fc % 2) * 2 + 1]
                        for k in range(8):
                            kb.mm(A, A[:, :n], w1[i], w1[i][:, k, fc * 128:(fc + 1) * 128], h, h[:, k, :n], start=(k == 0), stop=(k == 7))
                        for k in range(8):
                            kb.mm(B, B[:, :n], w3[i], w3[i][:, k, fc * 128:(fc + 1) * 128], h, h[:, k, :n], start=(k == 0), stop=(k == 7))
                        kb.act(sl, sl[:, :n], A, A[:, :n], AF.Silu)
                        kb.tt(tq, tq[:, :n], sl, sl[:, :n], B, B[:, :n], ALU.mult)
                        kb.tt(hp[i], hp[i][:, fc, :n], tq, tq[:, :n], Gb, Gb[:, :n], ALU.mult)
                tp, tr, off = self.ypcols(s, n)
                for c in range(8):
                    Y = self.bank[c % 2]
                    for i in range(4):
                        for fc in range(4):
                            kb.mm(Y, Y[:, :n], w2[i], w2[i][:, fc, c * 128:(c + 1) * 128], hp[i], hp[i][:, fc, :n],
                                  start=(i == 0 and fc == 0), stop=(i == 3 and fc == 3))
                    y_ = yo[c % 2]
                    kb.copy(y_, y_[:, :n], Y, Y[:, :n], e=("dve", "act")[c % 2])
                    kb.dma(tp[c * 128:(c + 1) * 128, off:off + n], y_[:, :n], reads=[y_], writes=[tp])
            for c0, cn, tp, tr in self.ypc:
                kb._deps("pool", [tp], [tr])
                ins = self.nc.gpsimd.collective_compute("ReduceScatter", ALU.add, replica_groups=QUADS,
                                                        ins=[tp.h.ap().opt()], outs=[tr.h.ap().opt()])
                kb.cc_cnt += 1; ins.then_inc(kb.cc); kb._mark((kb.cc, kb.cc_cnt), [tp], [tr])
        with kb.scope():
            lnt = kb.sb([128, 2, 4], F32, "lnt3")
            kb.dma(lnt[:], lnp[:], reads=[lnp], writes=[lnt])
            pre = [kb.sb([128, 2, 512], F32, "epre") for _ in range(2)]
            xts = [kb.sb([128, 2, 512], F32, "ext") for _ in range(2)]
            tmp = kb.sb([128, 512], F32, "etmp")
            for gi, (s, n, isc) in enumerate(self.tgroups()):
                xt = xts[gi % 2]; pr_ = pre[gi % 2]
                v = 1 if isc else 0
                tp, tr, off = self.ypcols(s, n)
                kb.dma(pr_[:, :, :n], tr[:, off:off + n].rearrange("(j p) n -> p j n", p=128), reads=[tr], writes=[pr_])
                kb.dma(xt[:, :, :n], self.x1T[:, :, s:s + n].rearrange("j p n -> p j n"), reads=[self.x1T], writes=[xt])
                for j in range(2):
                    kb.act(tmp, tmp[:, :n], pr_, pr_[:, j, :n], AF.Identity, extra=[modv], scale=modv[:, 2 * 5 + j, v:v + 1])
                    kb.stt(pr_, pr_[:, j, :n], xt, xt[:, j, :n], DN_ALPHA, tmp, tmp[:, :n], ALU.mult, ALU.add)
                self.ln_stats(pr_, s, n)
                kb.dma(self.preT[:, :, s:s + n].rearrange("j p n -> p j n"), pr_[:, :, :n], reads=[pr_], writes=[self.preT])
            self.ln_gather()
            for gi, (s, n, isc) in enumerate(self.tgroups()):
                pr_ = pre[gi % 2]; xo = xts[gi % 2]
                kb.dma(pr_[:, :, :n], self.preT[:, :, s:s + n].rearrange("j p n -> p j n"), reads=[self.preT], writes=[pr_])
                p1, p2 = self.ln_coef(s, n)
                for j in range(2):
                    kb.tt(tmp, tmp[:, :n], pr_, pr_[:, j, :n], p1, p1[:, :n], ALU.mult)
                    kb.tt(tmp, tmp[:, :n], tmp, tmp[:, :n], p2, p2[:, :n], ALU.add)
                    kb.act(xo, xo[:, j, :n], tmp, tmp[:, :n], AF.Identity, extra=[lnt], scale=lnt[:, j, 2:3], bias=lnt[:, j, 3:4])
                kb.dma(self.x2T[:, :, s:s + n].rearrange("j p n -> p j n"), xo[:, :, :n], reads=[xo], writes=[self.x2T])


    def phase_gdn(self, l):
        kb = self.kb; nt = self.ntok; nl = self.nl
        gcv = self.inp(f"gconv{l}", [128, 9]); gpar = self.inp(f"gpar{l}", [1, 8]); gnw = self.inp(f"gnorm{l}", [1, 128])
        cpk = self.inp("c_gdn", [64, 7 * 64])
        bo = self.inp("c_bones", [128, 128])
        GQT = kb.dram([128, nt], F32, "GQT"); GKT = kb.dram([128, nt], F32, "GKT")
        GKM = kb.dram([nt, 128], F32, "GKM"); GVM = kb.dram([nt, 128], F32, "GVM"); GG = kb.dram([nt, 8], F32, "GG")
        OD = [kb.dram([nt, 128], F32, f"OD{d}") for d in range(2)]
        with kb.scope():
            cw = kb.sb([128, 9], F32, "cw"); kb.dma(cw[:], gcv[:], reads=[gcv], writes=[cw])
            bones = kb.sb([128, 128], F32, "bones"); kb.dma(bones[:], bo[:], reads=[bo], writes=[bones])
            par = kb.sb([128, 8], F32, "gpar"); kb.dma(par[:], bcast_ap(gpar, 8), reads=[gpar], writes=[par])
            kb.act(par, par[:, 0:4], par, par[:, 0:4], AF.Exp)
            kb.ts(par, par[:, 0:4], par, par[:, 0:4], -1.0, None, ALU.mult)
            xin = [kb.sb([128, 514], F32, "gx") for _ in range(2)]
            u = kb.sb([128, 512], F32, "gu"); sq = kb.sb([128, 512], F32, "gsq"); rs = kb.sb([128, 512], F32, "grs")
            uo = [kb.sb([128, 512], F32, "guo") for _ in range(2)]
            uo16 = [kb.sb([128, 512], BF16, "guo16") for _ in range(2)]
            tmo = [kb.sb([128, 128], F32, "gtm") for _ in range(2)]
            gab = kb.sb([128, 8], F32, "gab"); gt = kb.sb([128, 8], F32, "ggt")
            it = 0
            for gi, (s, n, isc) in enumerate(self.tgroups()):
                seg0, seg1 = (nl, nt) if isc else (0, nl)
                for part in range(3):
                    x = xin[it % 2]; it += 1
                    lo = 1 if s == seg0 else 0; hi = 1 if s + n == seg1 else 0
                    if lo:
                        kb.memset(x, x[:, 0:1], 0.0)
                    if hi:
                        kb.memset(x, x[:, n + 1:n + 2], 0.0)
                    kb.dma(x[:, lo:n + 2 - hi], self.GP[part, :, s - 1 + lo:s + n + 1 - hi], reads=[self.GP], writes=[x])
                    kb.ts(u, u[:, :n], x, x[:, 0:n], cw[:, 3 * part:3 * part + 1], None, ALU.mult, extra=[cw])
                    kb.stt(u, u[:, :n], x, x[:, 1:n + 1], cw[:, 3 * part + 1:3 * part + 2], u, u[:, :n], ALU.mult, ALU.add, extra=[cw])
                    kb.stt(u, u[:, :n], x, x[:, 2:n + 2], cw[:, 3 * part + 2:3 * part + 3], u, u[:, :n], ALU.mult, ALU.add, extra=[cw])
                    o = uo[part % 2]
                    if part < 2:
                        kb.act(u, u[:, :n], u, u[:, :n], AF.Silu)
                        kb.tt(sq, sq[:, :n], u, u[:, :n], u, u[:, :n], ALU.mult)
                        A = self.bank[part]
                        kb.mm(A, A[:, :n], bones, bones[:], sq, sq[:, :n])
                        kb.ts(rs, rs[:, :n], A, A[:, :n], RMS_EPS, None, ALU.add)
                        kb.act(rs, rs[:, :n], rs, rs[:, :n], AF.Sqrt)
                        kb.op("dve", lambda e: e.reciprocal(out=rs[:, :n], in_=rs[:, :n]), reads=[rs], writes=[rs])
                        kb.stt(o, o[:, :n], u, u[:, :n], 0.125 if part == 0 else 1.0, rs, rs[:, :n], ALU.mult, ALU.mult)
                        dst = GQT if part == 0 else GKT
                        kb.dma(dst[:, s:s + n], o[:, :n], reads=[o], writes=[dst])
                    else:
                        kb.act(o, o[:, :n], u, u[:, :n], AF.Silu)
                    if part >= 1:
                        dstm = GKM if part == 1 else GVM
                        for tt_ in range(n // 128):
                            B = self.bank[2 + tt_ % 2]
                            kb.op("pe", lambda e: e.transpose(B[:, 0:128], o[:, tt_ * 128:(tt_ + 1) * 128], self.ident[:]), reads=[o, self.ident], writes=[B])
                            tm = tmo[tt_ % 2]
                            kb.copy(tm, tm[:], B, B[:, 0:128])
                            kb.dma(dstm[s + tt_ * 128:s + (tt_ + 1) * 128, :], tm[:], reads=[tm], writes=[dstm])
                for tt_ in range(n // 128):
                    t0 = s + tt_ * 128
                    kb.dma(gab[:], self.GAB[t0:t0 + 128, :], reads=[self.GAB], writes=[gab])
                    kb.tt(gt, gt[:, 0:4], gab, gab[:, 0:4], par, par[:, 4:8], ALU.add)
                    kb.act(gt, gt[:, 0:4], gt, gt[:, 0:4], AF.Exp)
                    kb.ts(gt, gt[:, 0:4], gt, gt[:, 0:4], 1.0, None, ALU.add)
                    kb.act(gt, gt[:, 0:4], gt, gt[:, 0:4], AF.Ln)
                    kb.tt(gt, gt[:, 0:4], gt, gt[:, 0:4], par, par[:, 0:4], ALU.mult)
                    kb.act(gt, gt[:, 4:8], gab, gab[:, 4:8], AF.Sigmoid)
                    kb.dma(GG[t0:t0 + 128, :], gt[:], reads=[gt], writes=[GG])
        with kb.scope():
            cp = kb.sb([64, 7, 64], F32, "cp"); kb.dma(cp[:].rearrange("p a b -> p (a b)"), cpk[:], reads=[cpk], writes=[cp])
            MU, ML, I64, ONES, MUS, MLS = (cp[:, i, :] for i in range(6))
            I4 = kb.sb([64, 4, 64], F32, "I4")
            for u_ in range(4):
                kb.copy(I4, I4[:, u_, :], cp, I64)
            S = kb.sb([64, 4, 64], F32, "gS"); kb.memset(S, S[:], 0.0)
            R2 = nl // 64
            seq0 = [nl + 64 * j for j in range(4)] + [64 * j for j in range(R2)]
            seq1 = [nl + 64 * j for j in range(3, -1, -1)] + [64 * j for j in range(R2 - 1, -1, -1)]
            NU = 8
            units = [(p_, d, h) for p_ in range(2) for d in range(2) for h in range(2)]

            def T8(name, dt=F32):
                return kb.sb([64, NU, 64], dt, name)
            I8 = T8("I8")
            for u_ in range(NU):
                kb.copy(I8, I8[:, u_, :], cp, I64)
            qT = [[kb.sb([64, 64], F32, "cq") for _ in range(NU)] for _ in range(2)]
            kT = [[kb.sb([64, 64], F32, "ck") for _ in range(NU)] for _ in range(2)]
            kM = [[kb.sb([64, 128], F32, "ckm") for _ in range(4)] for _ in range(2)]
            vM = [[kb.sb([64, 128], F32, "cvm") for _ in range(4)] for _ in range(2)]
            gg = [[kb.sb([64, 8], F32, "cgg") for _ in range(4)] for _ in range(2)]
            Gbc = T8("Gbc"); Bbc = T8("Bbc"); gcc = kb.sb([64, NU], F32, "gcc"); gcr = T8("gcr"); nD = T8("nD")
            e1 = T8("e1"); e2 = T8("e2"); dec = T8("dec"); decT = T8("decT"); brow = T8("brow")
            X = [T8("X0"), T8("X1")]; XT = [T8("XT0"), T8("XT1")]; TT = T8("TT"); inT = T8("inT")
            kbg = T8("kbg"); vb = T8("vb"); kdec = T8("kdec"); wT = T8("wT"); uval = T8("uval"); qdT = T8("qdT")
            egr = T8("egr"); sc4 = kb.sb([64, 4 * NU], F32, "sc4"); vnew = T8("vnew"); osb = [T8("osb0"), T8("osb1")]
            nsteps = len(seq0) // 2
            for step in range(nsteps):
                pp = step % 2
                toks = {(p_, d): (seq0, seq1)[d][2 * step + p_] for p_ in range(2) for d in range(2)}
                for p_ in range(2):
                    for d in range(2):
                        t0 = toks[(p_, d)]; pd = p_ * 2 + d
                        for h in range(2):
                            u_ = p_ * 4 + d * 2 + h
                            kb.dma(qT[pp][u_][:], GQT[64 * h:64 * h + 64, t0:t0 + 64], reads=[GQT], writes=[qT[pp][u_]])
                            kb.dma(kT[pp][u_][:], GKT[64 * h:64 * h + 64, t0:t0 + 64], reads=[GKT], writes=[kT[pp][u_]])
                        kb.dma(kM[pp][pd][:], GKM[t0:t0 + 64, :], reads=[GKM], writes=[kM[pp][pd]])
                        kb.dma(vM[pp][pd][:], GVM[t0:t0 + 64, :], reads=[GVM], writes=[vM[pp][pd]])
                        kb.dma(gg[pp][pd][:], GG[t0:t0 + 64, :], reads=[GG], writes=[gg[pp][pd]])
                b0, b1, b2, b3 = self.bank[0], self.bank[1], self.bank[2], self.bank[3]

                def b4(bk):
                    return bk[0:64, 0:512].rearrange("p (a b) -> p a b", a=NU)

                def gcol(u_, off=0):
                    p_, d, h = units[u_]
                    g_ = gg[pp][p_ * 2 + d]
                    c = off + d * 2 + h
                    return g_, g_[:, c:c + 1]
                for u_, (p_, d, h) in enumerate(units):
                    g_, ga = gcol(u_); _, ba = gcol(u_, 4)
                    kb.ts(Gbc, Gbc[:, u_, :], cp, ONES, ga, None, ALU.mult, extra=[g_])
                    kb.ts(Bbc, Bbc[:, u_, :], cp, ONES, ba, None, ALU.mult, extra=[g_])
                for u_, (p_, d, h) in enumerate(units):
                    Ud = MU if d == 0 else ML
                    g_, ga = gcol(u_)
                    kb.mm(b0, b0[0:64, u_:u_ + 1], cp, Ud, g_, ga)
                    kb.mm(b1, b4(b1)[:, u_, :], Gbc, Gbc[:, u_, :], cp, Ud)
                    kb.mm(b2, b4(b2)[:, u_, :], Bbc, Bbc[:, u_, :], cp, I64)
                kb.copy(gcc, gcc[:], b0, b0[0:64, 0:NU])
                kb.copy(gcr, gcr[:], b1, b4(b1))
                kb.copy(brow, brow[:], b2, b4(b2), e="act")
                for u_ in range(NU):
                    kb.ts(nD, nD[:, u_, :], gcr, gcr[:, u_, :], gcc[:, u_:u_ + 1], None, ALU.subtract, extra=[gcc])
                kb.ts(e1, e1[:], nD, nD[:], -1.0, 0.0, ALU.mult, ALU.min)
                kb.act(e1, e1[:], e1, e1[:], AF.Exp)
                kb.ts(e2, e2[:], nD, nD[:], 0.0, None, ALU.min)
                kb.act(e2, e2[:], e2, e2[:], AF.Exp)
                kb.act(egr, egr[:], gcr, gcr[:], AF.Exp)
                for u_ in range(NU):
                    kb.mm(b0, b4(b0)[:, u_, :], kT[pp][u_], kT[pp][u_][:], kT[pp][u_], kT[pp][u_][:])
                    kb.mm(b3, b4(b3)[:, u_, :], kT[pp][u_], kT[pp][u_][:], qT[pp][u_], qT[pp][u_][:])
                for u_, (p_, d, h) in enumerate(units):
                    Ms, MsT, MT = (MLS, MUS, MU) if d == 0 else (MUS, MLS, ML)
                    kb.tt(dec, dec[:, u_, :], e1, e1[:, u_, :], cp, Ms, ALU.mult)
                    kb.tt(decT, decT[:, u_, :], e2, e2[:, u_, :], cp, MsT, ALU.mult)
                    kb.tt(e2, e2[:, u_, :], e2, e2[:, u_, :], cp, MT, ALU.mult)
                kb.tt(X[0], X[0][:], b0, b4(b0), dec, dec[:], ALU.mult)
                kb.tt(XT[0], XT[0][:], b0, b4(b0), decT, decT[:], ALU.mult)
                kb.tt(XT[0], XT[0][:], XT[0], XT[0][:], brow, brow[:], ALU.mult)
                for u_ in range(NU):
                    g_, ba = gcol(u_, 4)
                    kb.ts(X[0], X[0][:, u_, :], X[0], X[0][:, u_, :], ba, None, ALU.mult, extra=[g_])
                kb.tt(inT, inT[:], b3, b4(b3), e2, e2[:], ALU.mult)
                kb.tt(TT, TT[:], I8, I8[:], XT[0], XT[0][:], ALU.subtract)
                cur = 0
                for lev in range(5):
                    nx = 1 - cur
                    for u_ in range(NU):
                        kb.mm(b0, b4(b0)[:, u_, :], XT[cur], XT[cur][:, u_, :], X[cur], X[cur][:, u_, :])
                        kb.mm(b1, b4(b1)[:, u_, :], X[cur], X[cur][:, u_, :], XT[cur], XT[cur][:, u_, :])
                    kb.copy(X[nx], X[nx][:], b0, b4(b0))
                    kb.copy(XT[nx], XT[nx][:], b1, b4(b1), e="act")
                    for u_ in range(NU):
                        kb.mm(b2, b4(b2)[:, u_, :], X[nx], X[nx][:, u_, :], TT, TT[:, u_, :])
                    kb.tt(TT, TT[:], TT, TT[:], b2, b4(b2), ALU.add)
                    cur = nx
                kb.act(sc4, sc4[:, 0:NU], gcc, gcc[:], AF.Exp)
                for u_, (p_, d, h) in enumerate(units):
                    g_, ba = gcol(u_, 4)
                    last = 63 if d == 0 else 0
                    kb.tt(sc4, sc4[:, NU + u_:NU + u_ + 1], sc4, sc4[:, u_:u_ + 1], g_, ba, ALU.mult)
                    kb.tt(sc4, sc4[:, 2 * NU + u_:2 * NU + u_ + 1], gcr, gcr[:, u_, last:last + 1], gcc, gcc[:, u_:u_ + 1], ALU.subtract)
                kb.act(sc4, sc4[:, 2 * NU:3 * NU], sc4, sc4[:, 2 * NU:3 * NU], AF.Exp)
                for u_, (p_, d, h) in enumerate(units):
                    g_, ba = gcol(u_, 4)
                    pd = p_ * 2 + d
                    hs_ = slice(64 * h, 64 * h + 64)
                    kb.ts(kbg, kbg[:, u_, :], kM[pp][pd], kM[pp][pd][:, hs_], sc4[:, NU + u_:NU + u_ + 1], None, ALU.mult, extra=[sc4])
                    kb.ts(kdec, kdec[:, u_, :], kM[pp][pd], kM[pp][pd][:, hs_], sc4[:, 2 * NU + u_:2 * NU + u_ + 1], None, ALU.mult, extra=[sc4])
                    kb.ts(vb, vb[:, u_, :], vM[pp][pd], vM[pp][pd][:, hs_], ba, None, ALU.mult, extra=[g_])
                    kb.tt(qdT, qdT[:, u_, :], qT[pp][u_], qT[pp][u_][:], egr, egr[:, u_, :], ALU.mult)
                for u_ in range(NU):
                    kb.mm(b0, b4(b0)[:, u_, :], kbg, kbg[:, u_, :], TT, TT[:, u_, :])
                    kb.mm(b1, b4(b1)[:, u_, :], TT, TT[:, u_, :], vb, vb[:, u_, :])
                kb.copy(wT, wT[:], b0, b4(b0))
                kb.copy(uval, uval[:], b1, b4(b1), e="act")
                ob_ = osb[pp]
                for p_ in range(2):
                    us = list(range(p_ * 4, p_ * 4 + 4))
                    sl_ = slice(p_ * 4, p_ * 4 + 4)
                    for u_ in us:
                        kb.mm(b2, b4(b2)[:, u_, :], wT, wT[:, u_, :], S, S[:, u_ % 4, :])
                    kb.tt(vnew, vnew[:, sl_, :], uval, uval[:, sl_, :], b2, b4(b2)[:, sl_, :], ALU.subtract)
                    for u_ in us:
                        kb.mm(b3, b4(b3)[:, u_, :], qdT, qdT[:, u_, :], S, S[:, u_ % 4, :], start=True, stop=False)
                        kb.mm(b3, b4(b3)[:, u_, :], inT, inT[:, u_, :], vnew, vnew[:, u_, :], start=False, stop=True)
                        kb.mm(b0, b4(b0)[:, u_, :], kdec, kdec[:, u_, :], vnew, vnew[:, u_, :])
                    kb.copy(ob_, ob_[:, sl_, :], b3, b4(b3)[:, sl_, :], e="act")
                    for u_ in us:
                        _, d, h = units[u_]
                        last = 63 if d == 0 else 0
                        kb.stt(S, S[:, u_ % 4, :], S, S[:, u_ % 4, :], egr[:, u_, last:last + 1], b0, b4(b0)[:, u_, :], ALU.mult, ALU.add, extra=[egr])
                    for d in range(2):
                        t0 = toks[(p_, d)]
                        kb.dma(OD[d][t0:t0 + 64, :].rearrange("p (h v) -> p h v", h=2), ob_[:, p_ * 4 + 2 * d:p_ * 4 + 2 * d + 2, :], reads=[ob_], writes=[OD[d]])
        with kb.scope():
            gnb = kb.sb([128, 128], F32, "gnb"); kb.dma(gnb[:], bcast_ap(gnw, 128), reads=[gnw], writes=[gnb])
            o0 = [kb.sb([128, 128], F32, "po0") for _ in range(2)]; o1 = [kb.sb([128, 128], F32, "po1") for _ in range(2)]
            zt = [kb.sb([128, 128], F32, "pz") for _ in range(2)]; sq = kb.sb([128, 128], F32, "psq"); ss = kb.sb([128, 4], F32, "pss")
            y16 = [kb.sb([128, 128], BF16, "py") for _ in range(2)]; stg = [kb.sb([128, 128], BF16, "pstg") for _ in range(2)]
            for ti in range(nt // 128):
                t0 = ti * 128; a = o0[ti % 2]; b_ = o1[ti % 2]; z = zt[ti % 2]
                kb.dma(a[:], OD[0][t0:t0 + 128, :], reads=[OD[0]], writes=[a])
                kb.dma(b_[:], OD[1][t0:t0 + 128, :], reads=[OD[1]], writes=[b_])
                kb.dma(z[:], self.GZ[t0:t0 + 128, :], reads=[self.GZ], writes=[z])
                kb.tt(a, a[:], a, a[:], b_, b_[:], ALU.add)
                kb.tt(sq, sq[:], a, a[:], a, a[:], ALU.mult)
                kb.op("dve", lambda e: e.tensor_reduce(out=ss[:, 0:2], in_=sq[:].rearrange("p (h v) -> p h v", h=2), axis=AX.X, op=ALU.add), reads=[sq], writes=[ss])
                kb.ts(ss, ss[:, 0:2], ss, ss[:, 0:2], 1.0 / 64, RMS_EPS, ALU.mult, ALU.add)
                kb.act(ss, ss[:, 0:2], ss, ss[:, 0:2], AF.Sqrt)
                kb.op("dve", lambda e: e.reciprocal(out=ss[:, 2:4], in_=ss[:, 0:2]), reads=[ss], writes=[ss])
                kb.act(z, z[:], z, z[:], AF.Silu)
                for h in range(2):
                    hs_ = slice(64 * h, 64 * h + 64)
                    kb.stt(a, a[:, hs_], a, a[:, hs_], ss[:, 2 + h:3 + h], gnb, gnb[:, hs_], ALU.mult, ALU.mult, extra=[ss])
                y = y16[ti % 2]
                kb.tt(y, y[:], a, a[:], z, z[:], ALU.mult)
                self.store_T(y, y[:], 128, 128, t0, self.bank[4 + ti % 2], stg[ti % 2])


    def phase_hyena(self, l):
        kb = self.kb; nt = self.ntok; nl = self.nl
        hcv = self.inp(f"hconv{l}", [128, 9]); hw1 = self.inp(f"hw1_{l}", [33, 64]); hw2 = self.inp(f"hw2_{l}", [64, 64])
        hw3 = self.inp(f"hw3_{l}", [64, 4 * 128]); hpr = self.inp(f"hpar{l}", [64, 3]); hdl = self.inp(f"hdel{l}", [128, 6])
        jmi = self.inp("c_jm", [128, 128])
        segs = [(0, nl), (nl, NC)]
        consts = {L: (self.inp(f"c_hyF{L}", [33, 2 * L]), self.inp(f"c_hyT{L}", [1, 2 * L])) for _, L in segs}
        HV = kb.dram([3, 128, nt], F32, "HV")
        TA = {L: [kb.dram([128, 2 * L + 254], BF16, f"TA{L}_{o}") for o in range(2)] for _, L in segs}
        TWO_PI = 2.0 * math.pi
        with kb.scope():
            cw = kb.sb([128, 9], F32, "hcw"); kb.dma(cw[:], hcv[:], reads=[hcv], writes=[cw])
            xin = [kb.sb([128, 514], F32, "hx") for _ in range(2)]
            uo = [kb.sb([128, 512], F32, "hu") for _ in range(2)]
            it = 0
            for gi, (s, n, isc) in enumerate(self.tgroups()):
                seg0, seg1 = (nl, nt) if isc else (0, nl)
                for part in range(3):
                    x = xin[it % 2]; u = uo[it % 2]; it += 1
                    lo = 1 if s == seg0 else 0; hi = 1 if s + n == seg1 else 0
                    if lo:
                        kb.memset(x, x[:, 0:1], 0.0)
                    if hi:
                        kb.memset(x, x[:, n + 1:n + 2], 0.0)
                    kb.dma(x[:, lo:n + 2 - hi], self.HY[part, :, s - 1 + lo:s + n + 1 - hi], reads=[self.HY], writes=[x])
                    kb.ts(u, u[:, :n], x, x[:, 0:n], cw[:, 3 * part:3 * part + 1], None, ALU.mult, extra=[cw])
                    kb.stt(u, u[:, :n], x, x[:, 1:n + 1], cw[:, 3 * part + 1:3 * part + 2], u, u[:, :n], ALU.mult, ALU.add, extra=[cw])
                    kb.stt(u, u[:, :n], x, x[:, 2:n + 2], cw[:, 3 * part + 2:3 * part + 3], u, u[:, :n], ALU.mult, ALU.add, extra=[cw])
                    kb.dma(HV[part, :, s:s + n], u[:, :n], reads=[u], writes=[HV])
            w1 = kb.sb([33, 64], F32, "hw1"); w2 = kb.sb([64, 64], F32, "hw2"); w3 = kb.sb([64, 4, 128], F32, "hw3")
            pr = kb.sb([64, 8], F32, "hpr"); dl = kb.sb([128, 6], F32, "hdl")
            kb.dma(w1[:], hw1[:], reads=[hw1], writes=[w1]); kb.dma(w2[:], hw2[:], reads=[hw2], writes=[w2])
            kb.dma(w3[:].rearrange("p a b -> p (a b)"), hw3[:], reads=[hw3], writes=[w3])
            kb.dma(pr[:, 0:3], hpr[:], reads=[hpr], writes=[pr]); kb.dma(dl[:], hdl[:], reads=[hdl], writes=[dl])
            for i_ in range(2):
                kb.tt(pr, pr[:, 3 + i_:4 + i_], pr, pr[:, 0:1], pr, pr[:, 1 + i_:2 + i_], ALU.mult)
            kb.act(dl, dl[:, 0:4], dl, dl[:, 0:4], AF.Abs)
            kb.ts(dl, dl[:, 0:4], dl, dl[:, 0:4], -1.0, None, ALU.mult)
            nacc = kb.sb([128, 8], F32, "hnacc"); kb.memset(nacc, nacc[:], 0.0)
            ft = [kb.sb([33, 512], F32, "hft") for _ in range(2)]; tl = [kb.sb([128, 512], F32, "htl") for _ in range(2)]
            a1 = kb.sb([64, 512], F32, "ha1"); a2 = kb.sb([64, 512], F32, "ha2"); kk = kb.sb([64, 512], F32, "hkk")
            win = kb.sb([128, 512], F32, "hwin"); hh = kb.sb([128, 512], F32, "hhh"); hab = kb.sb([128, 512], F32, "hab")
            hbf = [kb.sb([128, 512], BF16, "hbf") for _ in range(2)]; red = kb.sb([128, 2], F32, "hred")
            zpad = kb.sb([128, 128], BF16, "hzp"); kb.memset(zpad, zpad[:], 0.0)
            for si_, (_, L) in enumerate(segs):
                Fc, Tc = consts[L]
                for o in range(2):
                    kb.dma(TA[L][o][:, 0:127], zpad[:, 0:127], reads=[zpad], writes=[TA[L][o]])
                    kb.dma(TA[L][o][:, 2 * L + 126:2 * L + 254], zpad[:, 0:128], reads=[zpad], writes=[TA[L][o]])
                gidx = 0
                for dd in (1, 0):
                    for (j0, n) in groups_of(L, 512):
                        f = ft[gidx % 2]; t_ = tl[gidx % 2]; gidx += 1
                        kb.dma(f[:, :n], Fc[:, dd * L + j0:dd * L + j0 + n], reads=[Fc], writes=[f])
                        kb.dma(t_[:, :n], bcast_ap(Tc, n, offset=dd * L + j0), reads=[Tc], writes=[t_])
                        A = self.bank[0]; B = self.bank[1]
                        kb.mm(A, A[0:64, :n], w1, w1[:], f, f[:, :n])
                        kb.act(a1, a1[:, :n], A, A[0:64, :n], AF.Identity, extra=[pr], scale=pr[:, 0:1], bias=pr[:, 3:4])
                        kb.ts(kk, kk[:, :n], a1, a1[:, :n], 1.0 / TWO_PI, 12582912.0, ALU.mult, ALU.add)
                        kb.ts(kk, kk[:, :n], kk, kk[:, :n], -12582912.0, None, ALU.add)
                        kb.stt(a1, a1[:, :n], kk, kk[:, :n], -TWO_PI, a1, a1[:, :n], ALU.mult, ALU.add)
                        kb.act(a1, a1[:, :n], a1, a1[:, :n], AF.Sin)
                        kb.mm(B, B[0:64, :n], w2, w2[:], a1, a1[:, :n])
                        kb.act(a2, a2[:, :n], B, B[0:64, :n], AF.Identity, extra=[pr], scale=pr[:, 0:1], bias=pr[:, 4:5])
                        kb.ts(kk, kk[:, :n], a2, a2[:, :n], 1.0 / TWO_PI, 12582912.0, ALU.mult, ALU.add)
                        kb.ts(kk, kk[:, :n], kk, kk[:, :n], -12582912.0, None, ALU.add)
                        kb.stt(a2, a2[:, :n], kk, kk[:, :n], -TWO_PI, a2, a2[:, :n], ALU.mult, ALU.add)
                        kb.act(a2, a2[:, :n], a2, a2[:, :n], AF.Sin)
                        for o in range(2):
                            C = self.bank[2 + o]
                            kb.mm(C, C[:, :n], w3, w3[:, dd * 2 + o, :], a2, a2[:, :n])
                            kb.act(win, win[:, :n], t_, t_[:, :n], AF.Exp, extra=[dl], scale=dl[:, dd * 2 + o:dd * 2 + o + 1])
                            kb.stt(hh, hh[:, :n], win, win[:, :n], 0.05, C, C[:, :n], ALU.add, ALU.mult)
                            if dd == 1 and j0 + n == L:
                                kb.memset(hh, hh[:, n - 1:n], 0.0, e="dve")
                            kb.act(hab, hab[:, :n], hh, hh[:, :n], AF.Abs)
                            kb.op("dve", lambda e: e.tensor_reduce(out=red[:, 0:1], in_=hab[:, :n], axis=AX.X, op=ALU.add), reads=[hab], writes=[red])
                            kb.tt(nacc, nacc[:, 2 * si_ + o:2 * si_ + o + 1], nacc, nacc[:, 2 * si_ + o:2 * si_ + o + 1], red, red[:, 0:1], ALU.add)
                            hb_ = hbf[o]
                            kb.copy(hb_, hb_[:, :n], hh, hh[:, :n], e="pool")
                            base = (127 if dd == 1 else L + 126) + j0
                            nn = n - 1 if (dd == 1 and j0 + n == L) else n
                            kb.dma(TA[L][o][:, base:base + nn], hb_[:, :nn], reads=[hb_], writes=[TA[L][o]])
            kb.op("dve", lambda e: e.reciprocal(out=nacc[:, 4:8], in_=nacc[:, 0:4]), reads=[nacc], writes=[nacc])
            nrm = kb.sb([128, 4], F32, "hnrm_keep") if False else None
            self.h_nrm = kb.dram([128, 4], F32, "h_nrm"); self.h_sk = kb.dram([128, 2], F32, "h_sk")
            kb.dma(self.h_nrm[:], nacc[:, 4:8], reads=[nacc], writes=[self.h_nrm])
            kb.dma(self.h_sk[:], dl[:, 4:6], reads=[dl], writes=[self.h_sk])
        ZC = kb.dram([128, nt], F32, "HZC")
        for si_, (s0, L) in enumerate(segs):
            nb = L // 128; pad = nb - 1; W = (2 * nb - 1) * 128
            with kb.scope():
                jm = kb.sb([128, 128], BF16, "jm"); jmf = kb.sb([128, 128], F32, "jmf")
                kb.dma(jmf[:], jmi[:], reads=[jmi], writes=[jmf]); kb.copy(jm, jm[:], jmf, jmf[:])
                nr = kb.sb([128, 4], F32, "hnr"); sk = kb.sb([128, 2], F32, "hsk")
                kb.dma(nr[:], self.h_nrm[:], reads=[self.h_nrm], writes=[nr]); kb.dma(sk[:], self.h_sk[:], reads=[self.h_sk], writes=[sk])
                zp = kb.sb([128, nb + 2 * pad, 128], BF16, "zp")
                if pad:
                    kb.memset(zp, zp[:, 0:pad, :], 0.0); kb.memset(zp, zp[:, pad + nb:, :], 0.0)
                yT = kb.sb([128, nb, 128], F32, "yT")
                HP = min(2 * nb - 1, 64)
                Hs = [kb.sb([128, HP * 128], BF16, "Hs") for _ in range(2)]
                hi_ = 0
                zf = [kb.sb([128, 128], F32, "zf") for _ in range(2)]; zb = [kb.sb([128, 128], BF16, "zb") for _ in range(2)]
                zt = [kb.sb([128, 128], BF16, "zt") for _ in range(2)]
                g_ = [kb.sb([128, 128], F32, "hg") for _ in range(2)]; yo = [kb.sb([128, 128], F32, "hyo") for _ in range(2)]
                y16 = [kb.sb([128, 128], BF16, "hy16") for _ in range(2)]
                for o in range(2):
                    src = HV if o == 0 else ZC
                    for J in range(nb):
                        a = zf[J % 2]; b_ = zb[J % 2]; c_ = zt[J % 2]
                        if o == 0:
                            kb.dma(a[:], HV[0, :, s0 + J * 128:s0 + (J + 1) * 128], reads=[HV], writes=[a])
                        else:
                            kb.dma(a[:], ZC[:, s0 + J * 128:s0 + (J + 1) * 128], reads=[ZC], writes=[a])
                        kb.copy(b_, b_[:], a, a[:])
                        P1 = self.bank[J % 2]
                        pv = P1[:].bitcast(BF16)
                        kb.op("pe", lambda e: e.transpose(pv[:, 0:128], b_[:], self.identb[:]), reads=[b_, self.identb], writes=[P1])
                        kb.copy(c_, c_[:], P1, pv[:, 0:128], e="act")
                        P2 = self.bank[2 + J % 2]
                        kb.mm(P2, P2[:, 0:128], jm, jm[:], c_, c_[:])
                        kb.copy(zp, zp[:, pad + J, :], P2, P2[:, 0:128])
                    for c in range(128):
                        Y = self.bank[4 + c % 4]
                        for p0, pn in groups_of(2 * nb - 1, HP):
                            H = Hs[hi_ % 2]; hi_ += 1
                            hsrc = bass.AP(tensor=TA[L][o].h, offset=c * (2 * L + 254) + 127 + p0 * 128, ap=[[1, 128], [1, pn * 128]])
                            kb.dma(H[:, 0:pn * 128], hsrc, reads=[TA[L][o]], writes=[H])
                            for dq in range(pn):
                                dp = p0 + dq
                                kb.mm(Y, Y[:, 0:nb], H, H[:, dq * 128:(dq + 1) * 128], zp, zp[:, 2 * pad - dp:2 * pad - dp + nb, c],
                                      start=(dp == 0), stop=(dp == 2 * nb - 2))
                        kb.copy(yT, yT[:, :, c], Y, Y[:, 0:nb], e=("dve", "act")[c % 2])
                    for I in range(nb):
                        P1 = self.bank[I % 2]
                        kb.op("pe", lambda e: e.transpose(P1[:, 0:128], yT[:, I, :], self.ident[:]), reads=[yT, self.ident], writes=[P1])
                        a = zf[I % 2]; gt = g_[I % 2]; y_ = yo[I % 2]
                        c0 = s0 + I * 128
                        if o == 0:
                            kb.dma(a[:], HV[0, :, c0:c0 + 128], reads=[HV], writes=[a])
                        else:
                            kb.dma(a[:], ZC[:, c0:c0 + 128], reads=[ZC], writes=[a])
                        kb.dma(gt[:], HV[1 + o, :, c0:c0 + 128], reads=[HV], writes=[gt])
                        kb.ts(y_, y_[:], P1, P1[:, 0:128], nr[:, 2 * si_ + o:2 * si_ + o + 1], None, ALU.mult, extra=[nr])
                        kb.stt(y_, y_[:], a, a[:], sk[:, o:o + 1], y_, y_[:], ALU.mult, ALU.add, extra=[sk])
                        if o == 0:
                            kb.tt(y_, y_[:], y_, y_[:], gt, gt[:], ALU.mult)
                            kb.dma(ZC[:, c0:c0 + 128], y_[:], reads=[y_], writes=[ZC])
                        else:
                            yb = y16[I % 2]
                            kb.tt(yb, yb[:], y_, y_[:], gt, gt[:], ALU.mult)
                            yt, yap = self.YT_my.cols(c0, 128)
                            kb.dma(yap[0:128, :], yb[:], reads=[yb], writes=[yt])


def build_full(depth=DEPTH, nl=NL):
    P = Prog(nl, depth, ())
    kb = P.kb
    P.setup_consts()
    P.setup_mod()
    P.ln_alloc()
    P.alloc_A()
    P.alloc_mix()
    P.alloc_tail()
    P.alloc_moe()
    xT = P.inp("xT", [2, 128, P.ntok])
    x_in = xT
    for l in range(depth):
        lam_init = 0.8 - 0.6 * math.exp(-0.3 * l)
        P.phase_A(l, x_in, 0, 1, P.hT_my, P.hT_all)
        P.phase_B(l)
        P.phase_hyena(l)
        P.phase_gdn(l)
        P.phase_diff(l, lam_init)
        P.phase_na(l)
        for (c0, cn, tm), (_, _, ta) in zip(P.YT_my.ch, P.YT_allc.ch):
            kb.allgather(tm, ta, QUADS)
        P.phase_merge(l, x_in)
        P.phase_moe(l)
        x_in = P.x2T
    o = P.out("o_x", [2, 128, nl])
    for j in range(2):
        kb.dma(o[j, :, :], P.x2T[j, :, 0:nl], reads=[P.x2T], writes=[o])
    kb.finish([o])
    return P


def core_bg(r): return r // 4, r % 4
def prep_common(inp, r, nl, l_list=(0,)):
    b, g = core_bg(r)
    m = {}
    sel = np.zeros((8, 2), np.float32); sel[0::2, 0] = 1; sel[1::2, 1] = 1
    m["c_sel8"] = sel
    cc = np.stack([inp["c"][b], inp["c_ctx"]], -1)
    m["cT"] = np.ascontiguousarray(cc.reshape(8, 128, 2).transpose(1, 0, 2))
    cols = np.concatenate([w * 1024 + 256 * g + np.arange(256) for w in range(6)])
    for l in l_list:
        wm = inp["w_mod"][l][:, cols]
        m[f"wmod{l}"] = np.ascontiguousarray(wm.reshape(8, 128, 1536).transpose(1, 0, 2))
        m[f"bmod{l}"] = np.ascontiguousarray(inp["b_mod"][l][cols].reshape(12, 128).T)
    xs = np.concatenate([inp["x"][b, :nl], inp["ctx"][b]], 0)
    m["xT"] = np.ascontiguousarray(xs[:, 256 * g:256 * g + 256].T.reshape(2, 128, nl + 256))
    return m

SPLIT = [1536, 1536, 512, 16, 16, 1536, 1536, 4096]
OFF = np.concatenate([[0], np.cumsum(SPLIT)])
def wg_cols(g):
    o_hy, o_gq, o_gz, o_ga, o_gb, o_d, o_n, o_gate = OFF[:8]
    c = []
    for part in range(3): c.append(o_hy + part * 512 + 128 * g + np.arange(128))
    for part in range(3): c.append(o_gq + part * 512 + 128 * g + np.arange(128))
    c.append(o_gz + 128 * g + np.arange(128))
    for part in range(3): c.append(o_d + part * 512 + 128 * g + np.arange(128))
    for part in range(3): c.append(o_n + part * 512 + 128 * g + np.arange(128))
    ab = []
    for base in (o_ga, o_gb):
        for d in range(2):
            for h in range(2): ab.append(base + d * 8 + 2 * g + h)
    c.append(np.array(ab))
    return np.concatenate(c)
def rope_tables(nl):
    t = np.arange(nl); row = (t // 64).astype(np.float32); col = (t % 64).astype(np.float32)
    inv = (10000.0 ** (-np.arange(16, dtype=np.float32) / 16)).astype(np.float32)
    ar = row[:, None] * inv; ac = col[:, None] * inv
    cosd = np.ones((64, nl + 256), np.float32); sind = np.zeros((64, nl + 256), np.float32)
    for hh, ang in enumerate((ar, ac)):
        for t2 in range(2):
            cosd[hh * 32 + t2 * 16: hh * 32 + t2 * 16 + 16, :nl] = np.cos(ang).T
            sind[hh * 32 + t2 * 16: hh * 32 + t2 * 16 + 16, :nl] = np.sin(ang).T
    return np.concatenate([cosd, cosd], 0), np.concatenate([sind, sind], 0)
def na_consts():
    cols = np.arange(64)
    cstart = np.clip(cols - 8, 0, 48)
    col_ok = (cols[None, :] >= cstart[:, None]) & (cols[None, :] < cstart[:, None] + 16)
    dc = np.clip(cols[None, :] - cols[:, None], -15, 15) + 15
    return col_ok.T.astype(np.float32).copy(), dc
def prep_mix(inp, r, nl, l):
    b, g = core_bg(r); m = {}
    w = inp["w_in"][l][:, wg_cols(g)]
    m[f"wg{l}"] = np.ascontiguousarray(w.reshape(8, 128, -1).transpose(1, 0, 2))
    c, s_ = rope_tables(nl); m["c_cosT"] = c; m["c_sinT"] = s_
    m["c_ident"] = np.eye(128, dtype=np.float32)
    m[f"dlam{l}"] = inp["diff_lam"][l]
    m[f"dnorm{l}"] = inp["diff_norm"][l][None, :]
    mask, dc = na_consts(); m["c_namask"] = mask
    rp = inp["na_rpb"][l][2 * g:2 * g + 2]
    bias = rp[:, :, dc]
    m[f"nabias{l}"] = np.ascontiguousarray(bias.transpose(3, 0, 1, 2).reshape(64, -1))
    return m

def prep_tail(inp, r, nl, l, ref=None):
    import ml_dtypes
    b, g = core_bg(r); m = {}
    og = OFF[7]
    gc = np.concatenate([og + mm * 1024 + 256 * g + np.arange(256) for mm in range(4)])
    wgate = inp["w_in"][l][:, gc]
    m[f"wgate{l}"] = np.ascontiguousarray(wgate.reshape(8, 128, 1024).transpose(1, 0, 2))
    bp = inp["branch_proj"][l][:, :, 256 * g:256 * g + 256]
    bp = bp.reshape(4, 4, 128, 256).transpose(2, 1, 0, 3)
    m[f"bp{l}"] = np.ascontiguousarray(bp.reshape(128, 16, 256))
    wo = inp["w_out"][l][:, 256 * g:256 * g + 256]
    m[f"wout{l}"] = np.ascontiguousarray(wo.reshape(8, 128, 256).transpose(1, 0, 2))
    f = 256 * g + np.arange(256)
    lnp = np.stack([inp["ln_g"][l, 0][f], inp["ln_b"][l, 0][f], inp["ln_g"][l, 1][f], inp["ln_b"][l, 1][f]], -1)
    m[f"lnp{l}"] = np.ascontiguousarray(lnp.reshape(2, 128, 4).transpose(1, 0, 2))
    if ref is not None:
        ys = []
        for g2 in range(4):
            for nm in ("ya", "yb", "yc", "yd"):
                full = np.concatenate([ref[nm + "_x"][b, :nl], ref[nm + "_c"][b]], 0)
                ys.append(full[:, 128 * g2:128 * g2 + 128].T)
        m["yt_ref"] = np.concatenate(ys, 0).astype(ml_dtypes.bfloat16)
    return m

def prep_moe(inp, r, nl, l, ref=None):
    b, g = core_bg(r); m = {}
    f = 256 * g + np.arange(256)
    m["rw"] = np.ascontiguousarray(inp["router_w"][f].reshape(2, 128, 16).transpose(1, 0, 2))
    m["rb"] = inp["router_b"][None, :]
    comb = np.zeros((64, 16), np.float32)
    for rr in range(4):
        comb[rr * 16 + np.arange(16), np.arange(16)] = 1
    m["c_comb"] = comb
    es = np.zeros((16, 4, 128), np.float32)
    for i in range(4): es[4 * g + i, i, :] = 1
    m["c_esel"] = es.reshape(16, 512)
    E = [4 * g + i for i in range(4)]
    m[f"ew1_{l}"] = np.ascontiguousarray(inp["exp_w1"][l][E].reshape(4, 8, 128, 512).transpose(0, 2, 1, 3).reshape(4, 128, 4096))
    m[f"ew3_{l}"] = np.ascontiguousarray(inp["exp_w3"][l][E].reshape(4, 8, 128, 512).transpose(0, 2, 1, 3).reshape(4, 128, 4096))
    m[f"ew2_{l}"] = np.ascontiguousarray(inp["exp_w2"][l][E].reshape(4, 4, 128, 1024).transpose(0, 2, 1, 3).reshape(4, 128, 4096))
    m["c_ident"] = np.eye(128, dtype=np.float32)
    if ref is not None:
        x1 = np.concatenate([ref['x1'][b, :nl], ref['xc1'][b]], 0)[:, f].T
        m["x1_ref"] = np.ascontiguousarray(x1.reshape(2, 128, -1))
    return m

def prep_gdn(inp, r, nl, l):
    b, g = core_bg(r); m = {}
    gc = np.concatenate([inp["gdn_conv"][l][part * 512 + 128 * g + np.arange(128)] for part in range(3)], 1)
    m[f"gconv{l}"] = np.ascontiguousarray(gc)
    al = [inp["gdn_a_log"][l][d, 2 * g + h] for d in range(2) for h in range(2)]
    dtb = [inp["gdn_dt_bias"][l][d, 2 * g + h] for d in range(2) for h in range(2)]
    m[f"gpar{l}"] = np.array([al + dtb], np.float32)
    m[f"gnorm{l}"] = np.concatenate([inp["gdn_norm"][l]] * 2)[None, :].astype(np.float32)
    p = np.arange(64)
    MU = (p[:, None] <= p[None, :]).astype(np.float32); ML = (p[:, None] >= p[None, :]).astype(np.float32)
    I = np.eye(64, dtype=np.float32); ON = np.ones((64, 64), np.float32)
    m["c_gdn"] = np.ascontiguousarray(np.stack([MU, ML, I, ON, MU - I, ML - I, ON], 1).reshape(64, 7 * 64))
    bo = np.zeros((128, 128), np.float32); bo[:64, :64] = 1; bo[64:, 64:] = 1
    m["c_bones"] = bo
    return m

def hy_feats(L):
    f32 = np.float32
    t = np.linspace(0.0, 1.0, L, dtype=f32)[:, None]
    ang = (f32(2.0 * np.pi) * np.arange(L, dtype=f32)[:, None] / f32(L)).astype(f32)
    bands = np.linspace(1e-4, 15, 16, dtype=f32)[None, :]
    feats = np.concatenate([t, np.cos(bands * ang), -np.sin(bands * ang)], -1).astype(f32)
    F = np.concatenate([feats.T, feats[::-1].T], 1)
    T = np.concatenate([t[:, 0], t[::-1, 0]])[None, :]
    return np.ascontiguousarray(F), np.ascontiguousarray(T.astype(f32))
def prep_hy(inp, r, nl, l):
    b, g = core_bg(r); m = {}
    ch = 128 * g + np.arange(128)
    m[f"hconv{l}"] = np.ascontiguousarray(np.concatenate([inp["hy_conv"][l][part * 512 + ch] for part in range(3)], 1))
    m[f"hw1_{l}"] = inp["hy_w1"][l]; m[f"hw2_{l}"] = inp["hy_w2"][l]
    m[f"hw3_{l}"] = np.ascontiguousarray(np.concatenate([inp["hy_w3"][l][:, dd * 1024 + o * 512 + ch] for dd in range(2) for o in range(2)], 1))
    m[f"hpar{l}"] = np.ascontiguousarray(np.stack([inp["hy_freq"][l], inp["hy_b1"][l], inp["hy_b2"][l]], 1))
    dl = [inp["hy_deltas"][l][dd, o, ch] for dd in range(2) for o in range(2)] + [inp["hy_skip"][l][o, ch] for o in range(2)]
    m[f"hdel{l}"] = np.ascontiguousarray(np.stack(dl, 1))
    m["c_jm"] = np.eye(128, dtype=np.float32)[::-1].copy()
    for L in (nl, 256):
        F, T = hy_feats(L); m[f"c_hyF{L}"] = F; m[f"c_hyT{L}"] = T
    return m


def kernel(**inputs):
    inp = {k: np.asarray(v) for k, v in inputs.items()}
    depth = DEPTH
    P = build_full(depth, NL)
    maps = []
    for r in range(8):
        m = prep_common(inp, r, NL, l_list=tuple(range(depth)))
        for l in range(depth):
            m.update(prep_mix(inp, r, NL, l)); m.update(prep_gdn(inp, r, NL, l)); m.update(prep_hy(inp, r, NL, l))
            m.update(prep_tail(inp, r, NL, l)); m.update(prep_moe(inp, r, NL, l))
        missing = sorted(set(P.inputs) - set(m))
        assert not missing, missing
        maps.append({k: np.ascontiguousarray(v) for k, v in m.items() if k in P.inputs})
    res = run_bass_kernel_spmd(P.nc, maps, core_ids=list(range(8)))
    out = np.zeros((2, NL, D), np.float32)
    for r in range(8):
        b, g = r // 4, r % 4
        o = np.asarray(res.results[r]["o_x"])
        out[b, :, 256 * g:256 * g + 256] = o.reshape(256, NL).T
    return out
```

```python
import math
from contextlib import ExitStack
import numpy as np
import concourse.bass as bass
import concourse.mybir as mybir
from concourse.bass_utils import run_bass_kernel_spmd

F32 = mybir.dt.float32
BF16 = mybir.dt.bfloat16
AF = mybir.ActivationFunctionType
ALU = mybir.AluOpType
AX = mybir.AxisListType

D = 1024
NL = 8192
NC = 256
NTOK = NL + NC
DEPTH = 4
GRID_W = 64
LN_EPS = 1e-5
RMS_EPS = 1e-6
DN_ALPHA = (2 * DEPTH) ** 0.25
QUADS = [[0, 1, 2, 3], [4, 5, 6, 7]]
NWG = 13 * 128 + 8


class T:
    __slots__ = ("h", "w", "r", "name", "excl")

    def __init__(self, h, name=None, excl=False):
        self.h = h; self.w = None; self.r = {}; self.name = name; self.excl = excl

    def __getitem__(self, idx):
        return self.h[idx]


class KB:
    def __init__(self, nc, n_lanes=24):
        self.nc = nc
        self.eng = {"pe": nc.tensor, "act": nc.scalar, "dve": nc.vector, "pool": nc.gpsimd, "sp": nc.sync}
        self.sem = {k: nc.alloc_semaphore("s_" + k) for k in self.eng}
        self.cnt = {k: 0 for k in self.eng}
        self.lanes = [nc.alloc_semaphore(f"lane{i}") for i in range(n_lanes)]
        self.lane_cnt = [0] * n_lanes
        self.lane_rr = 0
        self.cc = nc.alloc_semaphore("cc")
        self.cc_cnt = 0
        self.waited = {}
        self.nid = 0
        self.dq = 0
        self.stack = None

    def sb(self, shape, dtype, name="sb"):
        self.nid += 1
        nm = f"{name}_{self.nid}"
        if self.stack is not None:
            return T(self.stack.enter_context(self.nc.sbuf_tensor(nm, list(shape), dtype)), nm)
        return T(self.nc.alloc_sbuf_tensor(nm, list(shape), dtype), nm)

    def barrier(self):
        targets = [(self.sem[k], self.cnt[k]) for k in self.eng]
        targets += [(self.lanes[i], 16 * c) for i, c in enumerate(self.lane_cnt)]
        targets.append((self.cc, self.cc_cnt))
        for e in self.eng:
            for sm, v in targets:
                if v:
                    self._wait(e, sm, v)

    def scope(self):
        kb = self

        class _S:
            def __enter__(s2):
                s2.prev = kb.stack
                kb.stack = ExitStack()
                kb.stack.__enter__()
                return s2

            def __exit__(s2, *a):
                kb.barrier()
                kb.stack.__exit__(None, None, None)
                kb.stack = s2.prev
                return False
        return _S()

    def ps(self, shape, dtype=F32, name="ps"):
        self.nid += 1
        nm = f"{name}_{self.nid}"
        return T(self.nc.alloc_psum_tensor(nm, list(shape), dtype), nm, excl=True)

    def dram(self, shape, dtype, name="dr", kind="Internal"):
        self.nid += 1
        nm = name if kind != "Internal" else f"{name}_{self.nid}"
        return T(self.nc.dram_tensor(nm, list(shape), dtype, kind=kind), nm)

    def _wait(self, e, sem, val):
        key = (e, id(sem))
        if self.waited.get(key, 0) >= val:
            return
        self.waited[key] = val
        self.eng[e].wait_ge(sem, val)

    def _deps(self, e, reads, writes, skip_same=False):
        deps = {}

        def add(d):
            if d is None:
                return
            s, v = d
            if deps.get(id(s), (None, 0))[1] < v:
                deps[id(s)] = (s, v)
        for t in reads:
            add(t.w)
            if t.excl:
                for s_v in t.r.values():
                    add(s_v)
        for t in writes:
            add(t.w)
            for s_v in t.r.values():
                add(s_v)
        for s, v in deps.values():
            if skip_same and s is self.sem.get(e):
                continue
            self._wait(e, s, v)

    def _mark(self, me, reads, writes):
        s, v = me
        for t in reads:
            t.r[id(s)] = me
        for t in writes:
            t.w = me; t.r = {}

    def op(self, e, fn, reads=(), writes=()):
        self._deps(e, reads, writes, skip_same=(e == "pe"))
        ins = fn(self.eng[e])
        self.cnt[e] += 1
        ins.then_inc(self.sem[e], 1)
        self._mark((self.sem[e], self.cnt[e]), reads, writes)
        return ins

    def dma(self, out, in_, reads=(), writes=(), q=None, **kw):
        if q is None:
            q = ("sp", "act", "pool")[self.dq % 2]
            self.dq += 1
        self._deps(q, reads, writes)
        li = self.lane_rr; self.lane_rr = (self.lane_rr + 1) % len(self.lanes)
        ls = self.lanes[li]
        if self.lane_cnt[li]:
            self._wait(q, ls, 16 * self.lane_cnt[li])
        ins = self.eng[q].dma_start(out=out, in_=in_, **kw)
        self.lane_cnt[li] += 1
        ins.then_inc(ls, 16)
        self._mark((ls, 16 * self.lane_cnt[li]), reads, writes)
        return ins

    def allgather(self, src, dst, groups):
        self._deps("pool", [src], [dst])
        ins = self.nc.gpsimd.collective_compute("AllGather", ALU.bypass, replica_groups=groups,
                                                ins=[src.h.ap().opt()], outs=[dst.h.ap().opt()])
        self.cc_cnt += 1
        ins.then_inc(self.cc)
        self._mark((self.cc, self.cc_cnt), [src], [dst])

    def finish(self, tiles, e="sp"):
        for t in tiles:
            if t.w is not None:
                self._wait(e, t.w[0], t.w[1])

    def mm(self, out_t, out_ap, lhsT_t, lhsT_ap, rhs_t, rhs_ap, start=True, stop=True, skip=False):
        return self.op("pe", lambda e: e.matmul(out_ap, lhsT_ap, rhs_ap, start=start, stop=stop, skip_group_check=skip),
                       reads=[lhsT_t, rhs_t], writes=[out_t])

    def act(self, out_t, out_ap, in_t, in_ap, func, extra=(), **kw):
        return self.op("act", lambda e: e.activation(out=out_ap, in_=in_ap, func=func, **kw),
                       reads=[in_t, *extra], writes=[out_t])

    def tt(self, out_t, out_ap, a_t, a_ap, b_t, b_ap, op, e="dve"):
        return self.op(e, lambda en: en.tensor_tensor(out=out_ap, in0=a_ap, in1=b_ap, op=op),
                       reads=[a_t, b_t], writes=[out_t])

    def ts(self, out_t, out_ap, a_t, a_ap, s1, s2, op0, op1=None, extra=(), e="dve"):
        if op1 is None:
            return self.op(e, lambda en: en.tensor_scalar(out=out_ap, in0=a_ap, scalar1=s1, scalar2=None, op0=op0),
                           reads=[a_t, *extra], writes=[out_t])
        return self.op(e, lambda en: en.tensor_scalar(out=out_ap, in0=a_ap, scalar1=s1, scalar2=s2, op0=op0, op1=op1),
                       reads=[a_t, *extra], writes=[out_t])

    def stt(self, out_t, out_ap, a_t, a_ap, s, b_t, b_ap, op0, op1, extra=()):
        return self.op("dve", lambda en: en.scalar_tensor_tensor(out=out_ap, in0=a_ap, scalar=s, in1=b_ap, op0=op0, op1=op1),
                       reads=[a_t, b_t, *extra], writes=[out_t])

    def copy(self, out_t, out_ap, in_t, in_ap, e="dve"):
        if e == "act":
            return self.op("act", lambda en: en.copy(out=out_ap, in_=in_ap), reads=[in_t], writes=[out_t])
        return self.op(e, lambda en: en.tensor_copy(out=out_ap, in_=in_ap), reads=[in_t], writes=[out_t])

    def memset(self, t, ap, val, e="pool"):
        return self.op(e, lambda en: en.memset(ap, val), reads=[], writes=[t])


def bcast_ap(t, n, offset=0, parts=128):
    return bass.AP(tensor=t.h, offset=offset, ap=[[0, parts], [1, n]])


CHUNK = 2048


class CD:
    def __init__(self, kb, rows, ntok, dtype, name, chunk=CHUNK):
        self.ch = [(c0, cn, kb.dram([rows, cn], dtype, f"{name}{i}")) for i, (c0, cn) in enumerate(groups_of(ntok, chunk))]

    def cols(self, s, n):
        for c0, cn, t in self.ch:
            if c0 <= s and s + n <= c0 + cn:
                return t, t[:, s - c0:s - c0 + n]
        raise ValueError((s, n))


def groups_of(n, g):
    return [(i, min(g, n - i)) for i in range(0, n, g)]


class Prog:
    def __init__(self, nl=NL, depth=DEPTH, debug=None):
        self.nl = nl; self.ntok = nl + NC; self.depth = depth
        self.debug = debug or []
        self.nc = bass.Bass("TRN2", target_bir_lowering=False)
        self.kb = KB(self.nc)
        self.inputs = {}
        self.outs = {}

    def inp(self, name, shape, dtype=F32):
        if name in self.inputs:
            return self.inputs[name]
        t = self.kb.dram(shape, dtype, name, kind="ExternalInput")
        self.inputs[name] = t
        return t

    def out(self, name, shape, dtype=F32):
        t = self.kb.dram(shape, dtype, name, kind="ExternalOutput")
        self.outs[name] = t
        return t

    def tgroups(self, gs=512):
        res = [(s, n, False) for s, n in groups_of(self.nl, gs)]
        res += [(self.nl + s, n, True) for s, n in groups_of(NC, gs)]
        return res

    def setup_consts(self):
        kb = self.kb
        self.ones_col = kb.sb([128, 1], F32, "ones_col")
        kb.memset(self.ones_col, self.ones_col[:], 1.0)
        self.ones_row = kb.sb([1, 128], F32, "ones_row")
        kb.memset(self.ones_row, self.ones_row[:], 1.0)
        sel = self.inp("c_sel8", [8, 2])
        self.sel8 = kb.sb([8, 2], F32, "sel8")
        kb.dma(self.sel8[:], sel[:], reads=[sel], writes=[self.sel8])
        self.bank = [kb.ps([128, 512], F32, f"bank{i}") for i in range(8)]

    def phase_mod(self, l):
        kb = self.kb
        wm = self.inp(f"wmod{l}", [128, 8, 12 * 128])
        bm = self.inp(f"bmod{l}", [128, 12])
        modv = self.modv[l]
        bsb = self.bm_stage
        kb.dma(bsb[:], bm[:], reads=[bm], writes=[bsb])
        ps = self.bank[0]
        for k in range(8):
            wt = self.wm_stage[k % 2]
            kb.dma(wt[:], wm[:, k, :], reads=[wm], writes=[wt])
            for c in range(12):
                kb.mm(ps, ps[:, 2 * c:2 * c + 2], wt, wt[:, c * 128:(c + 1) * 128], self.csil, self.csil[:, k, :],
                      start=(k == 0 and c == 0), stop=(k == 7), skip=True)
        for v in range(2):
            kb.tt(modv, modv[:, :, v], ps, ps[:, 0:24].rearrange("p (c v) -> p c v", v=2)[:, :, v], bsb, bsb[:], ALU.add)
        for which in (1, 4):
            kb.ts(modv, modv[:, 2 * which:2 * which + 2, :], modv, modv[:, 2 * which:2 * which + 2, :], 1.0, None, ALU.add)
        return modv

    def setup_mod(self):
        kb = self.kb
        cT = self.inp("cT", [128, 8, 2])
        self.csil = kb.sb([128, 8, 2], F32, "csil")
        craw = kb.sb([128, 8, 2], F32, "craw")
        kb.dma(craw[:], cT[:], reads=[cT], writes=[craw])
        kb.act(self.csil, self.csil[:], craw, craw[:], AF.Silu)
        self.modv = [kb.sb([128, 12, 2], F32, f"modv{l}") for l in range(self.depth)]
        with kb.scope():
            self.wm_stage = [kb.sb([128, 12 * 128], F32, f"wmst{i}") for i in range(2)]
            self.bm_stage = kb.sb([128, 12], F32, "bmst")
            for l in range(self.depth):
                self.phase_mod(l)

    def ln_alloc(self):
        kb = self.kb
        self.st_in = kb.dram([2, self.ntok], F32, "st_in")
        self.st_all = kb.dram([8, self.ntok], F32, "st_all")
        self.ln_sq = kb.sb([128, 512], F32, "ln_sq")
        self.ln_st = kb.sb([1, 2, 512], F32, "ln_st")
        self.ln_sa = kb.sb([8, 512], F32, "ln_sa")
        self.ln_v = [kb.sb([1, 512], F32, f"ln_v{i}") for i in range(4)]

    def ln_stats(self, xt, s, n):
        kb = self.kb
        p1, p2 = self.bank[6], self.bank[7]
        for j in range(2):
            kb.mm(p1, p1[0:1, :n], self.ones_col, self.ones_col[:], xt, xt[:, j, :n], start=(j == 0), stop=(j == 1))
        for j in range(2):
            kb.act(self.ln_sq, self.ln_sq[:, :n], xt, xt[:, j, :n], AF.Square)
            kb.mm(p2, p2[0:1, :n], self.ones_col, self.ones_col[:], self.ln_sq, self.ln_sq[:, :n], start=(j == 0), stop=(j == 1))
        kb.copy(self.ln_st, self.ln_st[:, 0, :n], p1, p1[0:1, :n])
        kb.copy(self.ln_st, self.ln_st[:, 1, :n], p2, p2[0:1, :n], e="act")
        kb.dma(self.st_in[:, s:s + n].rearrange("(o r) n -> o r n", o=1), self.ln_st[:, :, :n], reads=[self.ln_st], writes=[self.st_in])

    def ln_gather(self):
        self.kb.allgather(self.st_in, self.st_all, QUADS)

    def ln_coef(self, s, n):
        kb = self.kb
        p1, p2 = self.bank[6], self.bank[7]
        kb.dma(self.ln_sa[:, :n], self.st_all[:, s:s + n], reads=[self.st_all], writes=[self.ln_sa])
        kb.mm(p1, p1[0:1, :n], self.sel8, self.sel8[:, 0:1], self.ln_sa, self.ln_sa[:, :n])
        kb.mm(p2, p2[0:1, :n], self.sel8, self.sel8[:, 1:2], self.ln_sa, self.ln_sa[:, :n])
        m, v, r, q = self.ln_v
        kb.ts(m, m[:, :n], p1, p1[0:1, :n], 1.0 / D, None, ALU.mult)
        kb.tt(v, v[:, :n], m, m[:, :n], m, m[:, :n], ALU.mult)
        kb.stt(v, v[:, :n], p2, p2[0:1, :n], 1.0 / D, v, v[:, :n], ALU.mult, ALU.subtract)
        kb.ts(v, v[:, :n], v, v[:, :n], LN_EPS, None, ALU.add)
        kb.act(v, v[:, :n], v, v[:, :n], AF.Sqrt)
        kb.op('dve', lambda en: en.reciprocal(out=r[:, :n], in_=v[:, :n]), reads=[v], writes=[r])
        kb.stt(q, q[:, :n], m, m[:, :n], -1.0, r, r[:, :n], ALU.mult, ALU.mult)
        kb.mm(p1, p1[:, :n], self.ones_row, self.ones_row[:], r, r[:, :n])
        kb.mm(p2, p2[:, :n], self.ones_row, self.ones_row[:], q, q[:, :n])
        return p1, p2

    def phase_A(self, l, xT, which_shift, which_scale, hT_my, hT_all, h32=None):
        kb = self.kb
        modv = self.modv[l]
        for (s, n, isc) in self.tgroups():
            xt = self.xa[0]
            kb.dma(xt[:, :, :n], xT[:, :, s:s + n].rearrange("j p n -> p j n"), reads=[xT], writes=[xt])
            self.ln_stats(xt, s, n)
        self.ln_gather()
        for gi, (s, n, isc) in enumerate(self.tgroups()):
            xt = self.xa[gi % 2]
            kb.dma(xt[:, :, :n], xT[:, :, s:s + n].rearrange("j p n -> p j n"), reads=[xT], writes=[xt])
            p1, p2 = self.ln_coef(s, n)
            hb = self.hb[gi % 2]
            v = 1 if isc else 0
            for j in range(2):
                tmp = self.xtmp
                kb.tt(tmp, tmp[:, :n], xt, xt[:, j, :n], p1, p1[:, :n], ALU.mult)
                kb.tt(tmp, tmp[:, :n], tmp, tmp[:, :n], p2, p2[:, :n], ALU.add)
                kb.act(hb, hb[:, j, :n], tmp, tmp[:, :n], AF.Identity, extra=[modv],
                       scale=modv[:, 2 * which_scale + j, v:v + 1], bias=modv[:, 2 * which_shift + j, v:v + 1])
                if h32 is not None:
                    kb.act(h32[1], h32[1][:, j, :n], tmp, tmp[:, :n], AF.Identity, extra=[modv],
                           scale=modv[:, 2 * which_scale + j, v:v + 1], bias=modv[:, 2 * which_shift + j, v:v + 1])
            ht, hap = hT_my.cols(s, n)
            kb.dma(hap.rearrange("(j p) n -> p j n", p=128), hb[:, :, :n], reads=[hb], writes=[ht])
            if h32 is not None:
                kb.dma(h32[0][:, :, s:s + n].rearrange("j p n -> p j n"), h32[1][:, :, :n], reads=[h32[1]], writes=[h32[0]])
        for (c0, cn, tm), (_, _, ta) in zip(hT_my.ch, hT_all.ch):
            kb.allgather(tm, ta, QUADS)

    def alloc_A(self):
        kb = self.kb
        self.xa = [kb.sb([128, 2, 512], F32, f"xa{i}") for i in range(2)]
        self.hb = [kb.sb([128, 2, 512], BF16, f"hb{i}") for i in range(2)]
        self.xtmp = kb.sb([128, 512], F32, "xtmp")
        self.hT_my = CD(kb, 256, self.ntok, BF16, "hT_my")
        self.hT_all = CD(kb, 1024, self.ntok, BF16, "hT_all")


    def alloc_mix(self):
        kb = self.kb; nt = self.ntok
        self.HY = kb.dram([3, 128, nt], F32, "HY")
        self.GP = kb.dram([3, 128, nt], F32, "GP")
        self.GZ = kb.dram([nt, 128], F32, "GZ")
        self.GAB = kb.dram([nt, 8], F32, "GAB")
        self.DQ = kb.dram([128, nt], BF16, "DQ"); self.DK = kb.dram([128, nt], BF16, "DK")
        self.DV = kb.dram([nt, 128], BF16, "DV")
        self.NQ = kb.dram([128, nt], BF16, "NQ"); self.NK = kb.dram([128, nt], BF16, "NK")
        self.NV = kb.dram([nt, 128], BF16, "NV")
        self.YT_my = CD(kb, 512, nt, BF16, "YT_my", chunk=1024)
        self.YT_allc = CD(kb, 2048, nt, BF16, "YT_all", chunk=1024)
        self.cosT = self.inp("c_cosT", [128, nt]); self.sinT = self.inp("c_sinT", [128, nt])
        self.alloc_mix_ident()

    def alloc_mix_ident(self):
        kb = self.kb
        if hasattr(self, "ident"):
            return
        identb = self.inp("c_ident", [128, 128])
        self.ident = kb.sb([128, 128], F32, "ident")
        kb.dma(self.ident[:], identb[:], reads=[identb], writes=[self.ident])
        self.identb = kb.sb([128, 128], BF16, "identb")
        kb.copy(self.identb, self.identb[:], self.ident, self.ident[:])

    def phase_B(self, l):
        kb = self.kb
        wg = self.inp(f"wg{l}", [128, 8, NWG])
        with kb.scope():
            wgb = kb.sb([128, 8, NWG + 256], BF16, "wgb")
            stg = [kb.sb([128, NWG], F32, "wgst") for _ in range(2)]
            for k in range(8):
                st = stg[k % 2]
                kb.dma(st[:], wg[:, k, :], reads=[wg], writes=[st])
                kb.copy(wgb, wgb[:, k, 0:NWG], st, st[:], e=("dve", "pool")[k % 2])
            for k in range(8):
                for si, src in enumerate((7, 8)):
                    sv = wgb[:, k, src * 128:(src + 1) * 128].rearrange("p (a t s) -> p a t s", a=4, t=2, s=16)
                    dv = wgb[:, k, NWG + si * 128:NWG + (si + 1) * 128].rearrange("p (a t s) -> p a t s", a=4, t=2, s=16)
                    kb.ts(wgb, dv[:, :, 0, :], wgb, sv[:, :, 1, :], -1.0, None, ALU.mult)
                    kb.copy(wgb, dv[:, :, 1, :], wgb, sv[:, :, 0, :])
            hsb = [kb.sb([128, 8, 512], BF16, "hsb") for _ in range(2)]
            cst = kb.sb([128, 512], F32, "cst"); snt = kb.sb([128, 512], F32, "snt")
            evf = [kb.sb([128, 512], F32, "evf") for _ in range(2)]
            t1 = kb.sb([128, 512], F32, "t1"); t2 = kb.sb([128, 512], F32, "t2")
            ob = [kb.sb([128, 512], BF16, "ob") for _ in range(2)]
            tmz = [kb.sb([128, 128], F32, "tmz") for _ in range(2)]
            tmv = [kb.sb([128, 2, 128], BF16, "tmv") for _ in range(2)]
            tmg = [kb.sb([128, 8], F32, "tmg") for _ in range(2)]
            hT_all = self.hT_all
            import os
            bstop = int(os.environ.get("BSTOP", "9"))
            for gi, (s, n, isc) in enumerate(self.tgroups()):
                if bstop == 0:
                    break
                hs = hsb[gi % 2]
                ht, hap = hT_all.cols(s, n)
                kb.dma(hs[:, :, :n], hap.rearrange("(k p) n -> p k n", p=128), reads=[ht], writes=[hs])
                kb.dma(cst[:, :n], self.cosT[:, s:s + n], reads=[self.cosT], writes=[cst])
                kb.dma(snt[:, :n], self.sinT[:, s:s + n], reads=[self.sinT], writes=[snt])

                def fm(cb, bk):
                    for k in range(8):
                        kb.mm(bk, bk[:, :n], wgb, wgb[:, k, cb:cb + 128], hs, hs[:, k, :n], start=(k == 0), stop=(k == 7))
                for ci in range(6):
                    if bstop == 5:
                        break
                    bk = self.bank[ci % 4]; fm(ci * 128, bk)
                    ev = evf[ci % 2]
                    kb.copy(ev, ev[:, :n], bk, bk[:, :n], e=("dve", "act")[ci % 2])
                    dst = self.HY if ci < 3 else self.GP
                    kb.dma(dst[ci % 3, :, s:s + n], ev[:, :n], reads=[ev], writes=[dst])
                if bstop == 1:
                    continue
                for qi, (cb, rb, dst) in enumerate(()) if bstop == 5 else []:
                    pass
                for qi, (cb, rb, dst) in enumerate(((7 * 128, NWG, self.DQ), (8 * 128, NWG + 128, self.DK)) if bstop != 5 else ()):
                    b0, b1 = self.bank[0 + 2 * qi], self.bank[1 + 2 * qi]
                    fm(cb, b0); fm(rb, b1)
                    kb.tt(t1, t1[:, :n], b0, b0[:, :n], cst, cst[:, :n], ALU.mult)
                    kb.tt(t2, t2[:, :n], b1, b1[:, :n], snt, snt[:, :n], ALU.mult)
                    o = ob[qi]
                    kb.tt(o, o[:, :n], t1, t1[:, :n], t2, t2[:, :n], ALU.add, e="pool")
                    kb.dma(dst[:, s:s + n], o[:, :n], reads=[o], writes=[dst])
                if bstop == 2:
                    continue
                for qi, (cb, dst) in enumerate(((10 * 128, self.NQ), (11 * 128, self.NK)) if bstop != 5 else ()):
                    bk = self.bank[qi]; fm(cb, bk)
                    o = ob[qi]
                    kb.copy(o, o[:, :n], bk, bk[:, :n], e=("dve", "act")[qi])
                    kb.dma(dst[:, s:s + n], o[:, :n], reads=[o], writes=[dst])
                if bstop == 3:
                    continue
                for tt_ in range(n // 128):
                    bk = self.bank[int(os.environ.get("TMB", "4")) + tt_ % 2]
                    tsl = slice(tt_ * 128, (tt_ + 1) * 128)
                    for (cb, w, off) in ((6 * 128, 128, 0), (9 * 128, 128, 128), (12 * 128, 128, 256), (13 * 128, 8, 384))[:int(os.environ.get('TMN', '4'))]:
                        for k in range(8):
                            if os.environ.get("TMX", "") == "nomm":
                                break
                            kb.mm(bk, bk[:, off:off + w], hs, hs[:, k, tsl], wgb, wgb[:, k, cb:cb + w], start=(k == 0), stop=(k == 7))
                    if os.environ.get("TMX", "") == "nocopy":
                        continue
                    tok0 = s + tt_ * 128
                    z = tmz[tt_ % 2]; v = tmv[tt_ % 2]; g_ = tmg[tt_ % 2]
                    tmd = int(os.environ.get("TMD", "15"))
                    kb.copy(z, z[:], bk, bk[:, 0:128], e=os.environ.get("TME", "act"))
                    if tmd & 1:
                        kb.dma(self.GZ[tok0:tok0 + 128, :], z[:], reads=[z], writes=[self.GZ])
                    kb.copy(v, v[:, 0, :], bk, bk[:, 128:256])
                    kb.copy(v, v[:, 1, :], bk, bk[:, 256:384])
                    if tmd & 2:
                        kb.dma(self.DV[tok0:tok0 + 128, :], v[:, 0, :], reads=[v], writes=[self.DV])
                    if tmd & 4:
                        kb.dma(self.NV[tok0:tok0 + 128, :], v[:, 1, :], reads=[v], writes=[self.NV])
                    kb.copy(g_, g_[:], bk, bk[:, 384:392], e=os.environ.get("TME", "act"))
                    if tmd & 8:
                        kb.dma(self.GAB[tok0:tok0 + 128, :], g_[:], reads=[g_], writes=[self.GAB])

    def store_T(self, src_t, src_ap, nq, row0, tok0, bank, stage):
        kb = self.kb
        pb = bank
        pv = pb[:].bitcast(BF16)
        kb.op("pe", lambda e: e.transpose(pv[:, 0:nq], src_ap, self.identb[0:nq, 0:nq]), reads=[src_t, self.identb], writes=[pb])
        kb.copy(stage, stage[:, 0:nq], pb, pv[:, 0:nq])
        yt, yap = self.YT_my.cols(tok0, nq)
        kb.dma(yap[row0:row0 + 128, :], stage[:, 0:nq], reads=[stage], writes=[yt])

    def phase_diff(self, l, lam_init):
        kb = self.kb; nt = self.ntok; ntile = nt // 128
        dl = self.inp(f"dlam{l}", [4, 64]); dn = self.inp(f"dnorm{l}", [1, 128])
        with kb.scope():
            kT = kb.sb([128, nt], BF16, "kT")
            kb.dma(kT[:], self.DK[:], reads=[self.DK], writes=[kT])
            vA = kb.sb([128, ntile, 130], BF16, "vA")
            kb.memset(vA, vA[:, :, 128:130], 1.0)
            for t0, tn in groups_of(ntile, 8):
                kb.dma(vA[:, t0:t0 + tn, 0:128], self.DV[t0 * 128:(t0 + tn) * 128, :].rearrange("(t p) c -> p t c", p=128), reads=[self.DV], writes=[vA])
            lm = kb.sb([128, 4, 64], F32, "lm")
            kb.dma(lm[:].rearrange("p a b -> p (a b)"), bcast_ap(dl, 256), reads=[dl], writes=[lm])
            lp = kb.sb([128, 2, 64], F32, "lp"); ls = kb.sb([128, 4], F32, "ls")
            kb.tt(lp, lp[:, 0, :], lm, lm[:, 0, :], lm, lm[:, 1, :], ALU.mult)
            kb.tt(lp, lp[:, 1, :], lm, lm[:, 2, :], lm, lm[:, 3, :], ALU.mult)
            kb.op("dve", lambda e: e.tensor_reduce(out=ls[:, 0:2], in_=lp[:], axis=AX.X, op=ALU.add), reads=[lp], writes=[ls])
            kb.act(ls, ls[:, 0:2], ls, ls[:, 0:2], AF.Exp)
            kb.tt(ls, ls[:, 2:3], ls, ls[:, 0:1], ls, ls[:, 1:2], ALU.subtract)
            kb.ts(ls, ls[:, 3:4], ls, ls[:, 2:3], lam_init, -1.0, ALU.add, ALU.mult)
            nw = kb.sb([128, 128], F32, "nw")
            kb.dma(nw[:], bcast_ap(dn, 128), reads=[dn], writes=[nw])
            kb.ts(nw, nw[:], nw, nw[:], 1.0 - lam_init, None, ALU.mult)
            qsb = [kb.sb([128, 512], BF16, "qsb") for _ in range(2)]
            pT = [kb.sb([128, 512], BF16, "pT") for _ in range(4)]
            rr = kb.sb([128, 4], F32, "rr"); of = kb.sb([128, 128], F32, "of"); osq = kb.sb([128, 128], F32, "osq")
            ob16 = [kb.sb([128, 128], BF16, "ob16") for _ in range(2)]
            stg = [kb.sb([128, 128], BF16, "stg") for _ in range(2)]
            sc = 64 ** -0.5
            pi = 0
            import os
            dstop = int(os.environ.get("DSTOP", "9"))
            for gi, (s, n, isc) in enumerate(self.tgroups()):
                if dstop == 0:
                    break
                q = qsb[gi % 2]
                kb.dma(q[:, :n], self.DQ[:, s:s + n], reads=[self.DQ], writes=[q])
                ktiles = list(range(self.nl // 128, ntile)) if isc else list(range(ntile))
                nqt = n // 128
                first = {}
                for ki, kt in enumerate(ktiles):
                    for c in range(2):
                        sb_ = self.bank[4 + (pi % 4)]
                        kb.mm(sb_, sb_[:, :n], kT, kT[c * 64:(c + 1) * 64, kt * 128:(kt + 1) * 128], q, q[c * 64:(c + 1) * 64, :n])
                        p = pT[pi % 4]; pi += 1
                        kb.act(p, p[:, :n], sb_, sb_[:, :n], AF.Exp, scale=sc)
                        for qt in range(nqt):
                            ab = self.bank[c * 2 + qt // 2]; off = (qt % 2) * 160
                            st = id(ab) not in first
                            first[id(ab)] = 1
                            kb.mm(ab, ab[:, off:off + 129], p, p[:, qt * 128:(qt + 1) * 128], vA, vA[:, kt, 0:129],
                                  start=st, stop=(ki == len(ktiles) - 1), skip=True)
                for qt in range(nqt):
                    if dstop == 1:
                        break
                    a0 = self.bank[0 + qt // 2]; a1 = self.bank[2 + qt // 2]; off = (qt % 2) * 160
                    kb.op("dve", lambda e: e.reciprocal(out=rr[:, 0:1], in_=a0[:, off + 128:off + 129]), reads=[a0], writes=[rr])
                    kb.op("dve", lambda e: e.reciprocal(out=rr[:, 1:2], in_=a1[:, off + 128:off + 129]), reads=[a1], writes=[rr])
                    kb.tt(rr, rr[:, 1:2], rr, rr[:, 1:2], ls, ls[:, 3:4], ALU.mult)
                    kb.ts(of, of[:], a0, a0[:, off:off + 128], rr[:, 0:1], None, ALU.mult, extra=[rr])
                    kb.stt(of, of[:], a1, a1[:, off:off + 128], rr[:, 1:2], of, of[:], ALU.mult, ALU.add, extra=[rr])
                    kb.op("act", lambda e: e.activation(out=osq[:], in_=of[:], func=AF.Square, accum_out=rr[:, 2:3]), reads=[of], writes=[osq, rr])
                    kb.ts(rr, rr[:, 2:3], rr, rr[:, 2:3], 1.0 / 128, RMS_EPS, ALU.mult, ALU.add)
                    kb.act(rr, rr[:, 2:3], rr, rr[:, 2:3], AF.Sqrt)
                    kb.op("dve", lambda e: e.reciprocal(out=rr[:, 3:4], in_=rr[:, 2:3]), reads=[rr], writes=[rr])
                    o16 = ob16[qt % 2]
                    kb.stt(o16, o16[:], of, of[:], rr[:, 3:4], nw, nw[:], ALU.mult, ALU.mult, extra=[rr])
                    if dstop == 2:
                        continue
                    self.store_T(o16, o16[:], 128, 256, s + qt * 128, self.bank[4 + qt % 2], stg[qt % 2])


    def phase_na(self, l):
        kb = self.kb; nt = self.ntok; nl = self.nl; R = nl // 64
        nb = self.inp(f"nabias{l}", [64, 2 * 15 * 64])
        nm = self.inp("c_namask", [64, 64])
        sc = 64 ** -0.5
        with kb.scope():
            kT = kb.sb([128, nt], BF16, "nkT"); qT = kb.sb([128, nt], BF16, "nqT")
            kb.dma(kT[:], self.NK[:], reads=[self.NK], writes=[kT])
            kb.dma(qT[:], self.NQ[:], reads=[self.NQ], writes=[qT])
            v64 = kb.sb([64, R, 2, 65], BF16, "v64")
            kb.memset(v64, v64[:, :, :, 64:65], 1.0)
            for r0, rn in groups_of(R, 16):
                for hh in range(2):
                    kb.dma(v64[:, r0:r0 + rn, hh, 0:64],
                           self.NV[r0 * 64:(r0 + rn) * 64, hh * 64:(hh + 1) * 64].rearrange("(r p) c -> p r c", p=64),
                           reads=[self.NV], writes=[v64])
            vcx = kb.sb([128, 2, 2, 65], BF16, "vcx")
            kb.memset(vcx, vcx[:, :, :, 64:65], 1.0)
            for hh in range(2):
                kb.dma(vcx[:, :, hh, 0:64], self.NV[nl:nt, hh * 64:(hh + 1) * 64].rearrange("(t p) c -> p t c", p=128), reads=[self.NV], writes=[vcx])
            EB = kb.sb([64, 2, 15, 64], F32, "EB"); msk = kb.sb([64, 64], F32, "msk")
            kb.dma(EB[:].rearrange("p a b c -> p (a b c)"), nb[:], reads=[nb], writes=[EB])
            kb.dma(msk[:], nm[:], reads=[nm], writes=[msk])
            kb.act(EB, EB[:].rearrange("p a b c -> p (a b c)"), EB, EB[:].rearrange("p a b c -> p (a b c)"), AF.Exp)
            for hh in range(2):
                for dr in range(15):
                    kb.tt(EB, EB[:, hh, dr, :], EB, EB[:, hh, dr, :], msk, msk[:], ALU.mult)
            pwf = [kb.sb([64, 512], F32, "pwf") for _ in range(2)]
            pwb = [kb.sb([64, 512], BF16, "pwb") for _ in range(2)]
            pcb = [kb.sb([128, 256], BF16, "pcb") for _ in range(2)]
            rr = kb.sb([128, 4], F32, "nrr")
            o16 = [kb.sb([128, 128], BF16, "no16") for _ in range(2)]
            stg = [kb.sb([128, 128], BF16, "nstg") for _ in range(2)]
            it = 0
            for r in range(R):
                rs = min(max(r - 4, 0), R - 8)
                dr0 = rs - r + 7
                o = o16[r % 2]
                for hh in range(2):
                    hp = slice(hh * 64, (hh + 1) * 64)
                    sbk = self.bank[4 + it % 2]; sck = self.bank[6 + it % 2]; ab = self.bank[it % 2]
                    qv = qT[hp, r * 64:(r + 1) * 64]
                    for j in range(8):
                        kb.mm(sbk, sbk[0:64, j * 64:(j + 1) * 64], kT, kT[hp, (rs + j) * 64:(rs + j + 1) * 64], qT, qv)
                    for t in range(2):
                        kb.mm(sck, sck[:, t * 64:(t + 1) * 64], kT, kT[hp, nl + t * 128:nl + (t + 1) * 128], qT, qv)
                    pf = pwf[it % 2]; pb = pwb[it % 2]; pc = pcb[it % 2]
                    kb.act(pf, pf[:], sbk, sbk[0:64, :], AF.Exp, scale=sc)
                    kb.tt(pb, pb[:], pf, pf[:], EB, EB[:, hh, dr0:dr0 + 8, :].rearrange("p a b -> p (a b)"), ALU.mult)
                    kb.act(pc, pc[:, 0:128], sck, sck[:, 0:128], AF.Exp, scale=sc)
                    for j in range(8):
                        kb.mm(ab, ab[0:64, 0:65], pb, pb[:, j * 64:(j + 1) * 64], v64, v64[:, rs + j, hh, :], start=(j == 0), stop=False)
                    for t in range(2):
                        kb.mm(ab, ab[0:64, 0:65], pc, pc[:, t * 64:(t + 1) * 64], vcx, vcx[:, t, hh, :], start=False, stop=(t == 1))
                    kb.op("dve", lambda e: e.reciprocal(out=rr[0:64, hh:hh + 1], in_=ab[0:64, 64:65]), reads=[ab], writes=[rr])
                    kb.ts(o, o[0:64, hp], ab, ab[0:64, 0:64], rr[0:64, hh:hh + 1], None, ALU.mult, extra=[rr])
                    it += 1
                self.store_T(o, o[0:64, :], 64, 384, r * 64, self.bank[2 + r % 2], stg[r % 2])
            for qt in range(2):
                o = o16[qt]
                for hh in range(2):
                    hp = slice(hh * 64, (hh + 1) * 64)
                    sck = self.bank[6 + it % 2]; ab = self.bank[it % 2]; pc = pcb[it % 2]
                    for t in range(2):
                        kb.mm(sck, sck[:, t * 128:(t + 1) * 128], kT, kT[hp, nl + t * 128:nl + (t + 1) * 128],
                              qT, qT[hp, nl + qt * 128:nl + (qt + 1) * 128])
                    kb.act(pc, pc[:, 0:256], sck, sck[:, 0:256], AF.Exp, scale=sc)
                    for t in range(2):
                        kb.mm(ab, ab[:, 0:65], pc, pc[:, t * 128:(t + 1) * 128], vcx, vcx[:, t, hh, :], start=(t == 0), stop=(t == 1))
                    kb.op("dve", lambda e: e.reciprocal(out=rr[:, hh:hh + 1], in_=ab[:, 64:65]), reads=[ab], writes=[rr])
                    kb.ts(o, o[:, hp], ab, ab[:, 0:64], rr[:, hh:hh + 1], None, ALU.mult, extra=[rr])
                    it += 1
                self.store_T(o, o[:], 128, 384, nl + qt * 128, self.bank[2 + qt], stg[qt])


    def alloc_tail(self):
        kb = self.kb; nt = self.ntok
        if not hasattr(self, "YT_allc"):
            self.YT_allc = CD(kb, 2048, nt, BF16, "YT_all", chunk=1024)
        self.mT_my = CD(kb, 256, nt, BF16, "mT_my"); self.mT_all = CD(kb, 1024, nt, BF16, "mT_all")
        self.preT = kb.dram([2, 128, nt], F32, "preT")
        self.x1T = kb.dram([2, 128, nt], F32, "x1T")

    def phase_merge(self, l, xT):
        kb = self.kb
        wgi = self.inp(f"wgate{l}", [128, 8, 1024]); bpi = self.inp(f"bp{l}", [128, 16, 256])
        woi = self.inp(f"wout{l}", [128, 8, 256]); lnp = self.inp(f"lnp{l}", [128, 2, 4])
        modv = self.modv[l]
        with kb.scope():
            wgt = kb.sb([128, 8, 1024], BF16, "wgt"); bpt = kb.sb([128, 16, 256], BF16, "bpt")
            wot = kb.sb([128, 8, 256], BF16, "wot"); lnt = kb.sb([128, 2, 4], F32, "lnt")
            stg = [kb.sb([128, 1024], F32, "mstg") for _ in range(2)]
            si = 0
            for k in range(8):
                st = stg[si % 2]; si += 1
                kb.dma(st[:], wgi[:, k, :], reads=[wgi], writes=[st])
                kb.copy(wgt, wgt[:, k, :], st, st[:], e=("dve", "pool")[k % 2])
            for q4 in range(4):
                st = stg[si % 2]; si += 1
                kb.dma(st[:].rearrange("p (a b) -> p a b", a=4), bpi[:, 4 * q4:4 * q4 + 4, :], reads=[bpi], writes=[st])
                kb.copy(bpt, bpt[:, 4 * q4:4 * q4 + 4, :].rearrange("p a b -> p (a b)"), st, st[:], e=("dve", "pool")[q4 % 2])
            for k2 in range(2):
                st = stg[si % 2]; si += 1
                kb.dma(st[:].rearrange("p (a b) -> p a b", a=4), woi[:, 4 * k2:4 * k2 + 4, :], reads=[woi], writes=[st])
                kb.copy(wot, wot[:, 4 * k2:4 * k2 + 4, :].rearrange("p a b -> p (a b)"), st, st[:], e=("dve", "pool")[k2 % 2])
            kb.dma(lnt[:], lnp[:], reads=[lnp], writes=[lnt])
            hsb = [kb.sb([128, 8, 512], BF16, "mhs") for _ in range(2)]
            ysb = [kb.sb([128, 16, 512], BF16, "mys") for _ in range(2)]
            sg = kb.sb([128, 512], F32, "msg"); tmp = kb.sb([128, 512], F32, "mtmp"); acc = kb.sb([128, 512], F32, "macc")
            mb = [kb.sb([128, 2, 512], BF16, "mmb") for _ in range(2)]
            it = 0
            for gi, (s, n, isc) in enumerate(self.tgroups()):
                hs = hsb[gi % 2]; ys = ysb[gi % 2]; mo = mb[gi % 2]
                ht, hap = self.hT_all.cols(s, n)
                kb.dma(hs[:, :, :n], hap.rearrange("(k p) n -> p k n", p=128), reads=[ht], writes=[hs])
                yt, yap = self.YT_allc.cols(s, n)
                for hh in range(2):
                    kb.dma(ys[:, 8 * hh:8 * hh + 8, :n], yap[1024 * hh:1024 * (hh + 1), :].rearrange("(q p) n -> p q n", p=128), reads=[yt], writes=[ys])
                for j in range(2):
                    for m in range(4):
                        A = self.bank[(2 * it) % 6]; B = self.bank[(2 * it + 1) % 6]; it += 1
                        cb = m * 256 + j * 128
                        for k in range(8):
                            kb.mm(A, A[:, :n], wgt, wgt[:, k, cb:cb + 128], hs, hs[:, k, :n], start=(k == 0), stop=(k == 7))
                        for g2 in range(4):
                            kb.mm(B, B[:, :n], bpt, bpt[:, g2 * 4 + m, j * 128:(j + 1) * 128], ys, ys[:, g2 * 4 + m, :n], start=(g2 == 0), stop=(g2 == 3))
                        kb.act(sg, sg[:, :n], A, A[:, :n], AF.Sigmoid)
                        if m == 0:
                            kb.tt(acc, acc[:, :n], sg, sg[:, :n], B, B[:, :n], ALU.mult)
                        else:
                            kb.tt(tmp, tmp[:, :n], sg, sg[:, :n], B, B[:, :n], ALU.mult)
                            if m < 3:
                                kb.tt(acc, acc[:, :n], acc, acc[:, :n], tmp, tmp[:, :n], ALU.add, e="pool")
                            else:
                                kb.tt(mo, mo[:, j, :n], acc, acc[:, :n], tmp, tmp[:, :n], ALU.add, e="pool")
                mt, map_ = self.mT_my.cols(s, n)
                kb.dma(map_.rearrange("(j p) n -> p j n", p=128), mo[:, :, :n], reads=[mo], writes=[mt])
            for (c0, cn, tm), (_, _, ta) in zip(self.mT_my.ch, self.mT_all.ch):
                kb.allgather(tm, ta, QUADS)
            pre = [kb.sb([128, 2, 512], F32, "mpre") for _ in range(2)]
            xts = [kb.sb([128, 2, 512], F32, "mxt") for _ in range(2)]
            for gi, (s, n, isc) in enumerate(self.tgroups()):
                ms = hsb[gi % 2]; xt = xts[gi % 2]; pr = pre[gi % 2]
                v = 1 if isc else 0
                mt, map_ = self.mT_all.cols(s, n)
                kb.dma(ms[:, :, :n], map_.rearrange("(k p) n -> p k n", p=128), reads=[mt], writes=[ms])
                kb.dma(xt[:, :, :n], xT[:, :, s:s + n].rearrange("j p n -> p j n"), reads=[xT], writes=[xt])
                for j in range(2):
                    A = self.bank[j]
                    for k in range(8):
                        kb.mm(A, A[:, :n], wot, wot[:, k, j * 128:(j + 1) * 128], ms, ms[:, k, :n], start=(k == 0), stop=(k == 7))
                    kb.act(tmp, tmp[:, :n], A, A[:, :n], AF.Identity, extra=[modv], scale=modv[:, 2 * 2 + j, v:v + 1])
                    kb.stt(pr, pr[:, j, :n], xt, xt[:, j, :n], DN_ALPHA, tmp, tmp[:, :n], ALU.mult, ALU.add)
                self.ln_stats(pr, s, n)
                kb.dma(self.preT[:, :, s:s + n].rearrange("j p n -> p j n"), pr[:, :, :n], reads=[pr], writes=[self.preT])
            self.ln_gather()
            for gi, (s, n, isc) in enumerate(self.tgroups()):
                pr = pre[gi % 2]; xo = xts[gi % 2]
                kb.dma(pr[:, :, :n], self.preT[:, :, s:s + n].rearrange("j p n -> p j n"), reads=[self.preT], writes=[pr])
                p1, p2 = self.ln_coef(s, n)
                for j in range(2):
                    kb.tt(tmp, tmp[:, :n], pr, pr[:, j, :n], p1, p1[:, :n], ALU.mult)
                    kb.tt(tmp, tmp[:, :n], tmp, tmp[:, :n], p2, p2[:, :n], ALU.add)
                    kb.act(xo, xo[:, j, :n], tmp, tmp[:, :n], AF.Identity, extra=[lnt], scale=lnt[:, j, 0:1], bias=lnt[:, j, 1:2])
                kb.dma(self.x1T[:, :, s:s + n].rearrange("j p n -> p j n"), xo[:, :, :n], reads=[xo], writes=[self.x1T])


    def alloc_moe(self):
        kb = self.kb; nt = self.ntok
        self.h2T_my = CD(kb, 256, nt, BF16, "h2T_my"); self.h2T_all = CD(kb, 1024, nt, BF16, "h2T_all")
        self.h2f = kb.dram([2, 128, nt], F32, "h2f")
        self.h32t = kb.sb([128, 2, 512], F32, "h32t")
        self.lg_my = kb.dram([16, nt], F32, "lg_my"); self.lg_all = kb.dram([64, nt], F32, "lg_all")
        self.GTd = kb.dram([16, nt], F32, "GTd")
        self.ypc = [(c0, cn, kb.dram([1024, cn], F32, f"ypc{i}"), kb.dram([256, cn], F32, f"yrc{i}"))
                    for i, (c0, cn) in enumerate(groups_of(nt, 1024))]
        self.x2T = kb.dram([2, 128, nt], F32, "x2T")

    def ypcols(self, s, n):
        for c0, cn, tp, tr in self.ypc:
            if c0 <= s and s + n <= c0 + cn:
                return tp, tr, s - c0
        raise ValueError((s, n))

    def phase_moe(self, l):
        kb = self.kb; nt = self.ntok
        modv = self.modv[l]
        rwi = self.inp("rw", [128, 2, 16]); rbi = self.inp("rb", [1, 16]); cmb = self.inp("c_comb", [64, 16])
        seli = self.inp("c_esel", [16, 4 * 128]); lnp = self.inp(f"lnp{l}", [128, 2, 4]) if f"lnp{l}" not in self.inputs else self.inputs[f"lnp{l}"]
        e1 = self.inp(f"ew1_{l}", [4, 128, 8 * 512]); e3 = self.inp(f"ew3_{l}", [4, 128, 8 * 512]); e2 = self.inp(f"ew2_{l}", [4, 128, 4 * 1024])
        self.phase_A(l, self.x1T, 3, 4, self.h2T_my, self.h2T_all, h32=(self.h2f, self.h32t))
        BIG = 1.0e9
        with kb.scope():
            rw = kb.sb([128, 2, 16], F32, "rw"); rb = kb.sb([128, 16], F32, "rb"); comb = kb.sb([64, 16], F32, "comb")
            esel = kb.sb([16, 4, 128], F32, "esel"); lnt = kb.sb([128, 2, 4], F32, "lnt2")
            kb.dma(rw[:], rwi[:], reads=[rwi], writes=[rw])
            kb.dma(rb[:], bcast_ap(rbi, 16), reads=[rbi], writes=[rb])
            kb.dma(comb[:], cmb[:], reads=[cmb], writes=[comb])
            kb.dma(esel[:].rearrange("p a b -> p (a b)"), seli[:], reads=[seli], writes=[esel])
            kb.dma(lnt[:], lnp[:], reads=[lnp], writes=[lnt])
            hf = [kb.sb([128, 2, 512], F32, "hf") for _ in range(2)]
            lgs = [kb.sb([16, 512], F32, "lgs") for _ in range(2)]
            for gi, (s, n, isc) in enumerate(self.tgroups()):
                h = hf[gi % 2]; lg = lgs[gi % 2]
                kb.dma(h[:, :, :n], self.h2f[:, :, s:s + n].rearrange("j p n -> p j n"), reads=[self.h2f], writes=[h])
                A = self.bank[gi % 2]
                for j in range(2):
                    kb.mm(A, A[0:16, :n], rw, rw[:, j, :], h, h[:, j, :n], start=(j == 0), stop=(j == 1))
                kb.copy(lg, lg[:, :n], A, A[0:16, :n])
                kb.dma(self.lg_my[:, s:s + n], lg[:, :n], reads=[lg], writes=[self.lg_my])
            kb.allgather(self.lg_my, self.lg_all, QUADS)
            lgt = [kb.sb([64, 128], F32, "lgt") for _ in range(2)]
            S = kb.sb([128, 16], F32, "rS"); sel = kb.sb([128, 16], F32, "rsel"); selm = kb.sb([128, 16], F32, "rselm")
            pr = kb.sb([128, 6, 4], F32, "rpr"); gs = kb.sb([128, 4], F32, "rgs"); sc1 = kb.sb([128, 8], F32, "rsc")
            eq = kb.sb([128, 4], F32, "req"); is1 = kb.sb([128, 16], F32, "ris1"); is2 = kb.sb([128, 16], F32, "ris2")
            G = kb.sb([128, 16], F32, "rG"); gtt = [kb.sb([16, 512], F32, "gtt") for _ in range(2)]
            ntile = nt // 128
            for ti in range(ntile):
                t0 = ti * 128
                lt = lgt[ti % 2]
                kb.dma(lt[:], self.lg_all[:, t0:t0 + 128], reads=[self.lg_all], writes=[lt])
                A = self.bank[ti % 2]
                kb.mm(A, A[:, 0:16], lt, lt[:], comb, comb[:])
                kb.act(S, S[:], A, A[:, 0:16], AF.Sigmoid)
                kb.tt(sel, sel[:], S, S[:], rb, rb[:], ALU.add)
                sv = sel[:].rearrange("p (g k) -> p g k", k=4)
                pairs = ((0, 1), (0, 2), (0, 3), (1, 2), (1, 3), (2, 3))
                for pi_, (a, b_) in enumerate(pairs):
                    kb.tt(pr, pr[:, pi_, :], sel, sv[:, :, a], sel, sv[:, :, b_], ALU.add)
                kb.tt(gs, gs[:], pr, pr[:, 0, :], pr, pr[:, 1, :], ALU.max)
                for pi_ in range(2, 6):
                    kb.tt(gs, gs[:], gs, gs[:], pr, pr[:, pi_, :], ALU.max)
                kb.op("dve", lambda e: e.tensor_reduce(out=sc1[:, 0:1], in_=gs[:], axis=AX.X, op=ALU.max), reads=[gs], writes=[sc1])
                kb.ts(eq, eq[:], gs, gs[:], sc1[:, 0:1], None, ALU.is_equal, extra=[sc1])
                kb.ts(eq, eq[:], eq, eq[:], -1.0, BIG, ALU.add, ALU.mult)
                smv = selm[:].rearrange("p (g k) -> p g k", k=4)
                for k in range(4):
                    kb.tt(selm, smv[:, :, k], sel, sv[:, :, k], eq, eq[:], ALU.add)
                kb.op("dve", lambda e: e.tensor_reduce(out=sc1[:, 1:2], in_=selm[:], axis=AX.X, op=ALU.max), reads=[selm], writes=[sc1])
                kb.ts(is1, is1[:], selm, selm[:], sc1[:, 1:2], None, ALU.is_equal, extra=[sc1])
                kb.stt(selm, selm[:], is1, is1[:], -BIG, selm, selm[:], ALU.mult, ALU.add)
                kb.op("dve", lambda e: e.tensor_reduce(out=sc1[:, 2:3], in_=selm[:], axis=AX.X, op=ALU.max), reads=[selm], writes=[sc1])
                kb.ts(is2, is2[:], selm, selm[:], sc1[:, 2:3], None, ALU.is_equal, extra=[sc1])
                kb.tt(is1, is1[:], is1, is1[:], is2, is2[:], ALU.add)
                kb.tt(is1, is1[:], is1, is1[:], S, S[:], ALU.mult)
                kb.op("dve", lambda e: e.tensor_reduce(out=sc1[:, 3:4], in_=is1[:], axis=AX.X, op=ALU.add), reads=[is1], writes=[sc1])
                kb.op("dve", lambda e: e.reciprocal(out=sc1[:, 4:5], in_=sc1[:, 3:4]), reads=[sc1], writes=[sc1])
                kb.ts(G, G[:], is1, is1[:], sc1[:, 4:5], None, ALU.mult, extra=[sc1])
                B = self.bank[2 + ti % 2]
                kb.op("pe", lambda e: e.transpose(B[0:16, 0:128], G[:], self.ident[:]), reads=[G, self.ident], writes=[B])
                gt_ = gtt[(ti // 4) % 2]
                kb.copy(gt_, gt_[:, (ti % 4) * 128:(ti % 4 + 1) * 128], B, B[0:16, 0:128])
                if ti % 4 == 3 or ti == ntile - 1:
                    g0 = (ti // 4) * 512; gn = t0 + 128 - g0
                    kb.dma(self.GTd[:, g0:g0 + gn], gt_[:, :gn], reads=[gt_], writes=[self.GTd])
        with kb.scope():
            w1 = [kb.sb([128, 8, 512], BF16, "w1") for _ in range(4)]
            w3 = [kb.sb([128, 8, 512], BF16, "w3") for _ in range(4)]
            w2 = [kb.sb([128, 4, 1024], BF16, "w2") for _ in range(4)]
            stg = [kb.sb([128, 1024], F32, "estg") for _ in range(2)]
            si = 0
            for i in range(4):
                for (src, dstt) in ((e1, w1[i]), (e3, w3[i]), (e2, w2[i])):
                    dflat = dstt[:].rearrange("p a b -> p (a b)")
                    for hh in range(4):
                        st = stg[si % 2]
                        kb.dma(st[:], src[i, :, hh * 1024:(hh + 1) * 1024], reads=[src], writes=[st])
                        kb.copy(dstt, dflat[:, hh * 1024:(hh + 1) * 1024], st, st[:], e=("dve", "pool")[si % 2])
                        si += 1
            esel = kb.sb([16, 4, 128], F32, "esel2")
            kb.dma(esel[:].rearrange("p a b -> p (a b)"), seli[:], reads=[seli], writes=[esel])
            h2 = [kb.sb([128, 8, 512], BF16, "eh2") for _ in range(2)]
            gt = [kb.sb([16, 512], F32, "egt") for _ in range(2)]
            hp = [kb.sb([128, 4, 512], BF16, "ehp") for _ in range(4)]
            sl = kb.sb([128, 512], F32, "esl"); tq = kb.sb([128, 512], F32, "etq")
            yo = [kb.sb([128, 512], F32, "eyo") for _ in range(2)]
            for gi, (s, n, isc) in enumerate(self.tgroups()):
                h = h2[gi % 2]; g_ = gt[gi % 2]
                ht, hap = self.h2T_all.cols(s, n)
                kb.dma(h[:, :, :n], hap.rearrange("(k p) n -> p k n", p=128), reads=[ht], writes=[h])
                kb.dma(g_[:, :n], self.GTd[:, s:s + n], reads=[self.GTd], writes=[g_])
                for i in range(4):
                    Gb = self.bank[5]
                    kb.mm(Gb, Gb[:, :n], esel, esel[:, i, :], g_, g_[:, :n])
                    for fc in range(4):
                        A = self.bank[(fc % 2) * 2]; B = self.bank[(fc % 2) * 2 + 1]
                        for k in range(8):
                            kb.mm(A, A[:, :n], w1[i], w1[i][:, k, fc * 128:(fc + 1) * 128], h, h[:, k, :n], start=(k == 0), stop=(k == 7))
                        for k in range(8):
                            kb.mm(B, B[:, :n], w3[i], w3[i][:, k, fc * 128:(fc + 1) * 128], h, h[:, k, :n], start=(k == 0), stop=(k == 7))
                        kb.act(sl, sl[:, :n], A, A[:, :n], AF.Silu)
                        kb.tt(tq, tq[:, :n], sl, sl[:, :n], B, B[:, :n], ALU.mult)
                        kb.tt(hp[i], hp[i][:, fc, :n], tq, tq[:, :n], Gb, Gb[:, :n], ALU.mult)
                tp, tr, off = self.ypcols(s, n)
                for c in range(8):
                    Y = self.bank[c % 2]
                    for i in range(4):
                        for fc in range(4):
                            kb.mm(Y, Y[:, :n], w2[i], w2[i][:, fc, c * 128:(c + 1) * 128], hp[i], hp[i][:, fc, :n],
                                  start=(i == 0 and fc == 0), stop=(i == 3 and fc == 3))
                    y_ = yo[c % 2]
                    kb.copy(y_, y_[:, :n], Y, Y[:, :n], e=("dve", "act")[c % 2])
                    kb.dma(tp[c * 128:(c + 1) * 128, off:off + n], y_[:, :n], reads=[y_], writes=[tp])
            for c0, cn, tp, tr in self.ypc:
                kb._deps("pool", [tp], [tr])
                ins = self.nc.gpsimd.collective_compute("ReduceScatter", ALU.add, replica_groups=QUADS,
                                                        ins=[tp.h.ap().opt()], outs=[tr.h.ap().opt()])
                kb.cc_cnt += 1; ins.then_inc(kb.cc); kb._mark((kb.cc, kb.cc_cnt), [tp], [tr])
        with kb.scope():
            lnt = kb.sb([128, 2, 4], F32, "lnt3")
            kb.dma(lnt[:], lnp[:], reads=[lnp], writes=[lnt])
            pre = [kb.sb([128, 2, 512], F32, "epre") for _ in range(2)]
            xts = [kb.sb([128, 2, 512], F32, "ext") for _ in range(2)]
            tmp = kb.sb([128, 512], F32, "etmp")
            for gi, (s, n, isc) in enumerate(self.tgroups()):
                xt = xts[gi % 2]; pr_ = pre[gi % 2]
                v = 1 if isc else 0
                tp, tr, off = self.ypcols(s, n)
                kb.dma(pr_[:, :, :n], tr[:, off:off + n].rearrange("(j p) n -> p j n", p=128), reads=[tr], writes=[pr_])
                kb.dma(xt[:, :, :n], self.x1T[:, :, s:s + n].rearrange("j p n -> p j n"), reads=[self.x1T], writes=[xt])
                for j in range(2):
                    kb.act(tmp, tmp[:, :n], pr_, pr_[:, j, :n], AF.Identity, extra=[modv], scale=modv[:, 2 * 5 + j, v:v + 1])
                    kb.stt(pr_, pr_[:, j, :n], xt, xt[:, j, :n], DN_ALPHA, tmp, tmp[:, :n], ALU.mult, ALU.add)
                self.ln_stats(pr_, s, n)
                kb.dma(self.preT[:, :, s:s + n].rearrange("j p n -> p j n"), pr_[:, :, :n], reads=[pr_], writes=[self.preT])
            self.ln_gather()
            for gi, (s, n, isc) in enumerate(self.tgroups()):
                pr_ = pre[gi % 2]; xo = xts[gi % 2]
                kb.dma(pr_[:, :, :n], self.preT[:, :, s:s + n].rearrange("j p n -> p j n"), reads=[self.preT], writes=[pr_])
                p1, p2 = self.ln_coef(s, n)
                for j in range(2):
                    kb.tt(tmp, tmp[:, :n], pr_, pr_[:, j, :n], p1, p1[:, :n], ALU.mult)
                    kb.tt(tmp, tmp[:, :n], tmp, tmp[:, :n], p2, p2[:, :n], ALU.add)
                    kb.act(xo, xo[:, j, :n], tmp, tmp[:, :n], AF.Identity, extra=[lnt], scale=lnt[:, j, 2:3], bias=lnt[:, j, 3:4])
                kb.dma(self.x2T[:, :, s:s + n].rearrange("j p n -> p j n"), xo[:, :, :n], reads=[xo], writes=[self.x2T])


    def phase_gdn(self, l):
        kb = self.kb; nt = self.ntok; nl = self.nl
        gcv = self.inp(f"gconv{l}", [128, 9]); gpar = self.inp(f"gpar{l}", [1, 8]); gnw = self.inp(f"gnorm{l}", [1, 128])
        cpk = self.inp("c_gdn", [64, 7 * 64])
        bo = self.inp("c_bones", [128, 128])
        GQT = kb.dram([128, nt], F32, "GQT"); GKT = kb.dram([128, nt], F32, "GKT")
        GKM = kb.dram([nt, 128], F32, "GKM"); GVM = kb.dram([nt, 128], F32, "GVM"); GG = kb.dram([nt, 8], F32, "GG")
        OD = [kb.dram([nt, 128], F32, f"OD{d}") for d in range(2)]
        with kb.scope():
            cw = kb.sb([128, 9], F32, "cw"); kb.dma(cw[:], gcv[:], reads=[gcv], writes=[cw])
            bones = kb.sb([128, 128], F32, "bones"); kb.dma(bones[:], bo[:], reads=[bo], writes=[bones])
            par = kb.sb([128, 8], F32, "gpar"); kb.dma(par[:], bcast_ap(gpar, 8), reads=[gpar], writes=[par])
            kb.act(par, par[:, 0:4], par, par[:, 0:4], AF.Exp)
            kb.ts(par, par[:, 0:4], par, par[:, 0:4], -1.0, None, ALU.mult)
            xin = [kb.sb([128, 514], F32, "gx") for _ in range(2)]
            u = kb.sb([128, 512], F32, "gu"); sq = kb.sb([128, 512], F32, "gsq"); rs = kb.sb([128, 512], F32, "grs")
            uo = [kb.sb([128, 512], F32, "guo") for _ in range(2)]
            uo16 = [kb.sb([128, 512], BF16, "guo16") for _ in range(2)]
            tmo = [kb.sb([128, 128], F32, "gtm") for _ in range(2)]
            gab = kb.sb([128, 8], F32, "gab"); gt = kb.sb([128, 8], F32, "ggt")
            it = 0
            for gi, (s, n, isc) in enumerate(self.tgroups()):
                seg0, seg1 = (nl, nt) if isc else (0, nl)
                for part in range(3):
                    x = xin[it % 2]; it += 1
                    lo = 1 if s == seg0 else 0; hi = 1 if s + n == seg1 else 0
                    if lo:
                        kb.memset(x, x[:, 0:1], 0.0)
                    if hi:
                        kb.memset(x, x[:, n + 1:n + 2], 0.0)
                    kb.dma(x[:, lo:n + 2 - hi], self.GP[part, :, s - 1 + lo:s + n + 1 - hi], reads=[self.GP], writes=[x])
                    kb.ts(u, u[:, :n], x, x[:, 0:n], cw[:, 3 * part:3 * part + 1], None, ALU.mult, extra=[cw])
                    kb.stt(u, u[:, :n], x, x[:, 1:n + 1], cw[:, 3 * part + 1:3 * part + 2], u, u[:, :n], ALU.mult, ALU.add, extra=[cw])
                    kb.stt(u, u[:, :n], x, x[:, 2:n + 2], cw[:, 3 * part + 2:3 * part + 3], u, u[:, :n], ALU.mult, ALU.add, extra=[cw])
                    o = uo[part % 2]
                    if part < 2:
                        kb.act(u, u[:, :n], u, u[:, :n], AF.Silu)
                        kb.tt(sq, sq[:, :n], u, u[:, :n], u, u[:, :n], ALU.mult)
                        A = self.bank[part]
                        kb.mm(A, A[:, :n], bones, bones[:], sq, sq[:, :n])
                        kb.ts(rs, rs[:, :n], A, A[:, :n], RMS_EPS, None, ALU.add)
                        kb.act(rs, rs[:, :n], rs, rs[:, :n], AF.Sqrt)
                        kb.op("dve", lambda e: e.reciprocal(out=rs[:, :n], in_=rs[:, :n]), reads=[rs], writes=[rs])
                        kb.stt(o, o[:, :n], u, u[:, :n], 0.125 if part == 0 else 1.0, rs, rs[:, :n], ALU.mult, ALU.mult)
                        dst = GQT if part == 0 else GKT
                        kb.dma(dst[:, s:s + n], o[:, :n], reads=[o], writes=[dst])
                    else:
                        kb.act(o, o[:, :n], u, u[:, :n], AF.Silu)
                    if part >= 1:
                        dstm = GKM if part == 1 else GVM
                        for tt_ in range(n // 128):
                            B = self.bank[2 + tt_ % 2]
                            kb.op("pe", lambda e: e.transpose(B[:, 0:128], o[:, tt_ * 128:(tt_ + 1) * 128], self.ident[:]), reads=[o, self.ident], writes=[B])
                            tm = tmo[tt_ % 2]
                            kb.copy(tm, tm[:], B, B[:, 0:128])
                            kb.dma(dstm[s + tt_ * 128:s + (tt_ + 1) * 128, :], tm[:], reads=[tm], writes=[dstm])
                for tt_ in range(n // 128):
                    t0 = s + tt_ * 128
                    kb.dma(gab[:], self.GAB[t0:t0 + 128, :], reads=[self.GAB], writes=[gab])
                    kb.tt(gt, gt[:, 0:4], gab, gab[:, 0:4], par, par[:, 4:8], ALU.add)
                    kb.act(gt, gt[:, 0:4], gt, gt[:, 0:4], AF.Exp)
                    kb.ts(gt, gt[:, 0:4], gt, gt[:, 0:4], 1.0, None, ALU.add)
                    kb.act(gt, gt[:, 0:4], gt, gt[:, 0:4], AF.Ln)
                    kb.tt(gt, gt[:, 0:4], gt, gt[:, 0:4], par, par[:, 0:4], ALU.mult)
                    kb.act(gt, gt[:, 4:8], gab, gab[:, 4:8], AF.Sigmoid)
                    kb.dma(GG[t0:t0 + 128, :], gt[:], reads=[gt], writes=[GG])
        with kb.scope():
            cp = kb.sb([64, 7, 64], F32, "cp"); kb.dma(cp[:].rearrange("p a b -> p (a b)"), cpk[:], reads=[cpk], writes=[cp])
            MU, ML, I64, ONES, MUS, MLS = (cp[:, i, :] for i in range(6))
            I4 = kb.sb([64, 4, 64], F32, "I4")
            for u_ in range(4):
                kb.copy(I4, I4[:, u_, :], cp, I64)
            S = kb.sb([64, 4, 64], F32, "gS"); kb.memset(S, S[:], 0.0)
            R2 = nl // 64
            seq0 = [nl + 64 * j for j in range(4)] + [64 * j for j in range(R2)]
            seq1 = [nl + 64 * j for j in range(3, -1, -1)] + [64 * j for j in range(R2 - 1, -1, -1)]
            NU = 8
            units = [(p_, d, h) for p_ in range(2) for d in range(2) for h in range(2)]

            def T8(name, dt=F32):
                return kb.sb([64, NU, 64], dt, name)
            I8 = T8("I8")
            for u_ in range(NU):
                kb.copy(I8, I8[:, u_, :], cp, I64)
            qT = [[kb.sb([64, 64], F32, "cq") for _ in range(NU)] for _ in range(2)]
            kT = [[kb.sb([64, 64], F32, "ck") for _ in range(NU)] for _ in range(2)]
            kM = [[kb.sb([64, 128], F32, "ckm") for _ in range(4)] for _ in range(2)]
            vM = [[kb.sb([64, 128], F32, "cvm") for _ in range(4)] for _ in range(2)]
            gg = [[kb.sb([64, 8], F32, "cgg") for _ in range(4)] for _ in range(2)]
            Gbc = T8("Gbc"); Bbc = T8("Bbc"); gcc = kb.sb([64, NU], F32, "gcc"); gcr = T8("gcr"); nD = T8("nD")
            e1 = T8("e1"); e2 = T8("e2"); dec = T8("dec"); decT = T8("decT"); brow = T8("brow")
            X = [T8("X0"), T8("X1")]; XT = [T8("XT0"), T8("XT1")]; TT = T8("TT"); inT = T8("inT")
            kbg = T8("kbg"); vb = T8("vb"); kdec = T8("kdec"); wT = T8("wT"); uval = T8("uval"); qdT = T8("qdT")
            egr = T8("egr"); sc4 = kb.sb([64, 4 * NU], F32, "sc4"); vnew = T8("vnew"); osb = [T8("osb0"), T8("osb1")]
            nsteps = len(seq0) // 2
            for step in range(nsteps):
                pp = step % 2
                toks = {(p_, d): (seq0, seq1)[d][2 * step + p_] for p_ in range(2) for d in range(2)}
                for p_ in range(2):
                    for d in range(2):
                        t0 = toks[(p_, d)]; pd = p_ * 2 + d
                        for h in range(2):
                            u_ = p_ * 4 + d * 2 + h
                            kb.dma(qT[pp][u_][:], GQT[64 * h:64 * h + 64, t0:t0 + 64], reads=[GQT], writes=[qT[pp][u_]])
                            kb.dma(kT[pp][u_][:], GKT[64 * h:64 * h + 64, t0:t0 + 64], reads=[GKT], writes=[kT[pp][u_]])
                        kb.dma(kM[pp][pd][:], GKM[t0:t0 + 64, :], reads=[GKM], writes=[kM[pp][pd]])
                        kb.dma(vM[pp][pd][:], GVM[t0:t0 + 64, :], reads=[GVM], writes=[vM[pp][pd]])
                        kb.dma(gg[pp][pd][:], GG[t0:t0 + 64, :], reads=[GG], writes=[gg[pp][pd]])
                b0, b1, b2, b3 = self.bank[0], self.bank[1], self.bank[2], self.bank[3]

                def b4(bk):
                    return bk[0:64, 0:512].rearrange("p (a b) -> p a b", a=NU)

                def gcol(u_, off=0):
                    p_, d, h = units[u_]
                    g_ = gg[pp][p_ * 2 + d]
                    c = off + d * 2 + h
                    return g_, g_[:, c:c + 1]
                for u_, (p_, d, h) in enumerate(units):
                    g_, ga = gcol(u_); _, ba = gcol(u_, 4)
                    kb.ts(Gbc, Gbc[:, u_, :], cp, ONES, ga, None, ALU.mult, extra=[g_])
                    kb.ts(Bbc, Bbc[:, u_, :], cp, ONES, ba, None, ALU.mult, extra=[g_])
                for u_, (p_, d, h) in enumerate(units):
                    Ud = MU if d == 0 else ML
                    g_, ga = gcol(u_)
                    kb.mm(b0, b0[0:64, u_:u_ + 1], cp, Ud, g_, ga)
                    kb.mm(b1, b4(b1)[:, u_, :], Gbc, Gbc[:, u_, :], cp, Ud)
                    kb.mm(b2, b4(b2)[:, u_, :], Bbc, Bbc[:, u_, :], cp, I64)
                kb.copy(gcc, gcc[:], b0, b0[0:64, 0:NU])
                kb.copy(gcr, gcr[:], b1, b4(b1))
                kb.copy(brow, brow[:], b2, b4(b2), e="act")
                for u_ in range(NU):
                    kb.ts(nD, nD[:, u_, :], gcr, gcr[:, u_, :], gcc[:, u_:u_ + 1], None, ALU.subtract, extra=[gcc])
                kb.ts(e1, e1[:], nD, nD[:], -1.0, 0.0, ALU.mult, ALU.min)
                kb.act(e1, e1[:], e1, e1[:], AF.Exp)
                kb.ts(e2, e2[:], nD, nD[:], 0.0, None, ALU.min)
                kb.act(e2, e2[:], e2, e2[:], AF.Exp)
                kb.act(egr, egr[:], gcr, gcr[:], AF.Exp)
                for u_ in range(NU):
                    kb.mm(b0, b4(b0)[:, u_, :], kT[pp][u_], kT[pp][u_][:], kT[pp][u_], kT[pp][u_][:])
                    kb.mm(b3, b4(b3)[:, u_, :], kT[pp][u_], kT[pp][u_][:], qT[pp][u_], qT[pp][u_][:])
                for u_, (p_, d, h) in enumerate(units):
                    Ms, MsT, MT = (MLS, MUS, MU) if d == 0 else (MUS, MLS, ML)
                    kb.tt(dec, dec[:, u_, :], e1, e1[:, u_, :], cp, Ms, ALU.mult)
                    kb.tt(decT, decT[:, u_, :], e2, e2[:, u_, :], cp, MsT, ALU.mult)
                    kb.tt(e2, e2[:, u_, :], e2, e2[:, u_, :], cp, MT, ALU.mult)
                kb.tt(X[0], X[0][:], b0, b4(b0), dec, dec[:], ALU.mult)
                kb.tt(XT[0], XT[0][:], b0, b4(b0), decT, decT[:], ALU.mult)
                kb.tt(XT[0], XT[0][:], XT[0], XT[0][:], brow, brow[:], ALU.mult)
                for u_ in range(NU):
                    g_, ba = gcol(u_, 4)
                    kb.ts(X[0], X[0][:, u_, :], X[0], X[0][:, u_, :], ba, None, ALU.mult, extra=[g_])
                kb.tt(inT, inT[:], b3, b4(b3), e2, e2[:], ALU.mult)
                kb.tt(TT, TT[:], I8, I8[:], XT[0], XT[0][:], ALU.subtract)
                cur = 0
                for lev in range(5):
                    nx = 1 - cur
                    for u_ in range(NU):
                        kb.mm(b0, b4(b0)[:, u_, :], XT[cur], XT[cur][:, u_, :], X[cur], X[cur][:, u_, :])
                        kb.mm(b1, b4(b1)[:, u_, :], X[cur], X[cur][:, u_, :], XT[cur], XT[cur][:, u_, :])
                    kb.copy(X[nx], X[nx][:], b0, b4(b0))
                    kb.copy(XT[nx], XT[nx][:], b1, b4(b1), e="act")
                    for u_ in range(NU):
                        kb.mm(b2, b4(b2)[:, u_, :], X[nx], X[nx][:, u_, :], TT, TT[:, u_, :])
                    kb.tt(TT, TT[:], TT, TT[:], b2, b4(b2), ALU.add)
                    cur = nx
                kb.act(sc4, sc4[:, 0:NU], gcc, gcc[:], AF.Exp)
                for u_, (p_, d, h) in enumerate(units):
                    g_, ba = gcol(u_, 4)
                    last = 63 if d == 0 else 0
                    kb.tt(sc4, sc4[:, NU + u_:NU + u_ + 1], sc4, sc4[:, u_:u_ + 1], g_, ba, ALU.mult)
                    kb.tt(sc4, sc4[:, 2 * NU + u_:2 * NU + u_ + 1], gcr, gcr[:, u_, last:last + 1], gcc, gcc[:, u_:u_ + 1], ALU.subtract)
                kb.act(sc4, sc4[:, 2 * NU:3 * NU], sc4, sc4[:, 2 * NU:3 * NU], AF.Exp)
                for u_, (p_, d, h) in enumerate(units):
                    g_, ba = gcol(u_, 4)
                    pd = p_ * 2 + d
                    hs_ = slice(64 * h, 64 * h + 64)
                    kb.ts(kbg, kbg[:, u_, :], kM[pp][pd], kM[pp][pd][:, hs_], sc4[:, NU + u_:NU + u_ + 1], None, ALU.mult, extra=[sc4])
                    kb.ts(kdec, kdec[:, u_, :], kM[pp][pd], kM[pp][pd][:, hs_], sc4[:, 2 * NU + u_:2 * NU + u_ + 1], None, ALU.mult, extra=[sc4])
                    kb.ts(vb, vb[:, u_, :], vM[pp][pd], vM[pp][pd][:, hs_], ba, None, ALU.mult, extra=[g_])
                    kb.tt(qdT, qdT[:, u_, :], qT[pp][u_], qT[pp][u_][:], egr, egr[:, u_, :], ALU.mult)
                for u_ in range(NU):
                    kb.mm(b0, b4(b0)[:, u_, :], kbg, kbg[:, u_, :], TT, TT[:, u_, :])
                    kb.mm(b1, b4(b1)[:, u_, :], TT, TT[:, u_, :], vb, vb[:, u_, :])
                kb.copy(wT, wT[:], b0, b4(b0))
                kb.copy(uval, uval[:], b1, b4(b1), e="act")
                ob_ = osb[pp]
                for p_ in range(2):
                    us = list(range(p_ * 4, p_ * 4 + 4))
                    sl_ = slice(p_ * 4, p_ * 4 + 4)
                    for u_ in us:
                        kb.mm(b2, b4(b2)[:, u_, :], wT, wT[:, u_, :], S, S[:, u_ % 4, :])
                    kb.tt(vnew, vnew[:, sl_, :], uval, uval[:, sl_, :], b2, b4(b2)[:, sl_, :], ALU.subtract)
                    for u_ in us:
                        kb.mm(b3, b4(b3)[:, u_, :], qdT, qdT[:, u_, :], S, S[:, u_ % 4, :], start=True, stop=False)
                        kb.mm(b3, b4(b3)[:, u_, :], inT, inT[:, u_, :], vnew, vnew[:, u_, :], start=False, stop=True)
                        kb.mm(b0, b4(b0)[:, u_, :], kdec, kdec[:, u_, :], vnew, vnew[:, u_, :])
                    kb.copy(ob_, ob_[:, sl_, :], b3, b4(b3)[:, sl_, :], e="act")
                    for u_ in us:
                        _, d, h = units[u_]
                        last = 63 if d == 0 else 0
                        kb.stt(S, S[:, u_ % 4, :], S, S[:, u_ % 4, :], egr[:, u_, last:last + 1], b0, b4(b0)[:, u_, :], ALU.mult, ALU.add, extra=[egr])
                    for d in range(2):
                        t0 = toks[(p_, d)]
                        kb.dma(OD[d][t0:t0 + 64, :].rearrange("p (h v) -> p h v", h=2), ob_[:, p_ * 4 + 2 * d:p_ * 4 + 2 * d + 2, :], reads=[ob_], writes=[OD[d]])
        with kb.scope():
            gnb = kb.sb([128, 128], F32, "gnb"); kb.dma(gnb[:], bcast_ap(gnw, 128), reads=[gnw], writes=[gnb])
            o0 = [kb.sb([128, 128], F32, "po0") for _ in range(2)]; o1 = [kb.sb([128, 128], F32, "po1") for _ in range(2)]
            zt = [kb.sb([128, 128], F32, "pz") for _ in range(2)]; sq = kb.sb([128, 128], F32, "psq"); ss = kb.sb([128, 4], F32, "pss")
            y16 = [kb.sb([128, 128], BF16, "py") for _ in range(2)]; stg = [kb.sb([128, 128], BF16, "pstg") for _ in range(2)]
            for ti in range(nt // 128):
                t0 = ti * 128; a = o0[ti % 2]; b_ = o1[ti % 2]; z = zt[ti % 2]
                kb.dma(a[:], OD[0][t0:t0 + 128, :], reads=[OD[0]], writes=[a])
                kb.dma(b_[:], OD[1][t0:t0 + 128, :], reads=[OD[1]], writes=[b_])
                kb.dma(z[:], self.GZ[t0:t0 + 128, :], reads=[self.GZ], writes=[z])
                kb.tt(a, a[:], a, a[:], b_, b_[:], ALU.add)
                kb.tt(sq, sq[:], a, a[:], a, a[:], ALU.mult)
                kb.op("dve", lambda e: e.tensor_reduce(out=ss[:, 0:2], in_=sq[:].rearrange("p (h v) -> p h v", h=2), axis=AX.X, op=ALU.add), reads=[sq], writes=[ss])
                kb.ts(ss, ss[:, 0:2], ss, ss[:, 0:2], 1.0 / 64, RMS_EPS, ALU.mult, ALU.add)
                kb.act(ss, ss[:, 0:2], ss, ss[:, 0:2], AF.Sqrt)
                kb.op("dve", lambda e: e.reciprocal(out=ss[:, 2:4], in_=ss[:, 0:2]), reads=[ss], writes=[ss])
                kb.act(z, z[:], z, z[:], AF.Silu)
                for h in range(2):
                    hs_ = slice(64 * h, 64 * h + 64)
                    kb.stt(a, a[:, hs_], a, a[:, hs_], ss[:, 2 + h:3 + h], gnb, gnb[:, hs_], ALU.mult, ALU.mult, extra=[ss])
                y = y16[ti % 2]
                kb.tt(y, y[:], a, a[:], z, z[:], ALU.mult)
                self.store_T(y, y[:], 128, 128, t0, self.bank[4 + ti % 2], stg[ti % 2])


    def phase_hyena(self, l):
        kb = self.kb; nt = self.ntok; nl = self.nl
        hcv = self.inp(f"hconv{l}", [128, 9]); hw1 = self.inp(f"hw1_{l}", [33, 64]); hw2 = self.inp(f"hw2_{l}", [64, 64])
        hw3 = self.inp(f"hw3_{l}", [64, 4 * 128]); hpr = self.inp(f"hpar{l}", [64, 3]); hdl = self.inp(f"hdel{l}", [128, 6])
        jmi = self.inp("c_jm", [128, 128])
        segs = [(0, nl), (nl, NC)]
        consts = {L: (self.inp(f"c_hyF{L}", [33, 2 * L]), self.inp(f"c_hyT{L}", [1, 2 * L])) for _, L in segs}
        HV = kb.dram([3, 128, nt], F32, "HV")
        TA = {L: [kb.dram([128, 2 * L + 254], BF16, f"TA{L}_{o}") for o in range(2)] for _, L in segs}
        TWO_PI = 2.0 * math.pi
        with kb.scope():
            cw = kb.sb([128, 9], F32, "hcw"); kb.dma(cw[:], hcv[:], reads=[hcv], writes=[cw])
            xin = [kb.sb([128, 514], F32, "hx") for _ in range(2)]
            uo = [kb.sb([128, 512], F32, "hu") for _ in range(2)]
            it = 0
            for gi, (s, n, isc) in enumerate(self.tgroups()):
                seg0, seg1 = (nl, nt) if isc else (0, nl)
                for part in range(3):
                    x = xin[it % 2]; u = uo[it % 2]; it += 1
                    lo = 1 if s == seg0 else 0; hi = 1 if s + n == seg1 else 0
                    if lo:
                        kb.memset(x, x[:, 0:1], 0.0)
                    if hi:
                        kb.memset(x, x[:, n + 1:n + 2], 0.0)
                    kb.dma(x[:, lo:n + 2 - hi], self.HY[part, :, s - 1 + lo:s + n + 1 - hi], reads=[self.HY], writes=[x])
                    kb.ts(u, u[:, :n], x, x[:, 0:n], cw[:, 3 * part:3 * part + 1], None, ALU.mult, extra=[cw])
                    kb.stt(u, u[:, :n], x, x[:, 1:n + 1], cw[:, 3 * part + 1:3 * part + 2], u, u[:, :n], ALU.mult, ALU.add, extra=[cw])
                    kb.stt(u, u[:, :n], x, x[:, 2:n + 2], cw[:, 3 * part + 2:3 * part + 3], u, u[:, :n], ALU.mult, ALU.add, extra=[cw])
                    kb.dma(HV[part, :, s:s + n], u[:, :n], reads=[u], writes=[HV])
            w1 = kb.sb([33, 64], F32, "hw1"); w2 = kb.sb([64, 64], F32, "hw2"); w3 = kb.sb([64, 4, 128], F32, "hw3")
            pr = kb.sb([64, 8], F32, "hpr"); dl = kb.sb([128, 6], F32, "hdl")
            kb.dma(w1[:], hw1[:], reads=[hw1], writes=[w1]); kb.dma(w2[:], hw2[:], reads=[hw2], writes=[w2])
            kb.dma(w3[:].rearrange("p a b -> p (a b)"), hw3[:], reads=[hw3], writes=[w3])
            kb.dma(pr[:, 0:3], hpr[:], reads=[hpr], writes=[pr]); kb.dma(dl[:], hdl[:], reads=[hdl], writes=[dl])
            for i_ in range(2):
                kb.tt(pr, pr[:, 3 + i_:4 + i_], pr, pr[:, 0:1], pr, pr[:, 1 + i_:2 + i_], ALU.mult)
            kb.act(dl, dl[:, 0:4], dl, dl[:, 0:4], AF.Abs)
            kb.ts(dl, dl[:, 0:4], dl, dl[:, 0:4], -1.0, None, ALU.mult)
            nacc = kb.sb([128, 8], F32, "hnacc"); kb.memset(nacc, nacc[:], 0.0)
            ft = [kb.sb([33, 512], F32, "hft") for _ in range(2)]; tl = [kb.sb([128, 512], F32, "htl") for _ in range(2)]
            a1 = kb.sb([64, 512], F32, "ha1"); a2 = kb.sb([64, 512], F32, "ha2"); kk = kb.sb([64, 512], F32, "hkk")
            win = kb.sb([128, 512], F32, "hwin"); hh = kb.sb([128, 512], F32, "hhh"); hab = kb.sb([128, 512], F32, "hab")
            hbf = [kb.sb([128, 512], BF16, "hbf") for _ in range(2)]; red = kb.sb([128, 2], F32, "hred")
            zpad = kb.sb([128, 128], BF16, "hzp"); kb.memset(zpad, zpad[:], 0.0)
            for si_, (_, L) in enumerate(segs):
                Fc, Tc = consts[L]
                for o in range(2):
                    kb.dma(TA[L][o][:, 0:127], zpad[:, 0:127], reads=[zpad], writes=[TA[L][o]])
                    kb.dma(TA[L][o][:, 2 * L + 126:2 * L + 254], zpad[:, 0:128], reads=[zpad], writes=[TA[L][o]])
                gidx = 0
                for dd in (1, 0):
                    for (j0, n) in groups_of(L, 512):
                        f = ft[gidx % 2]; t_ = tl[gidx % 2]; gidx += 1
                        kb.dma(f[:, :n], Fc[:, dd * L + j0:dd * L + j0 + n], reads=[Fc], writes=[f])
                        kb.dma(t_[:, :n], bcast_ap(Tc, n, offset=dd * L + j0), reads=[Tc], writes=[t_])
                        A = self.bank[0]; B = self.bank[1]
                        kb.mm(A, A[0:64, :n], w1, w1[:], f, f[:, :n])
                        kb.act(a1, a1[:, :n], A, A[0:64, :n], AF.Identity, extra=[pr], scale=pr[:, 0:1], bias=pr[:, 3:4])
                        kb.ts(kk, kk[:, :n], a1, a1[:, :n], 1.0 / TWO_PI, 12582912.0, ALU.mult, ALU.add)
                        kb.ts(kk, kk[:, :n], kk, kk[:, :n], -12582912.0, None, ALU.add)
                        kb.stt(a1, a1[:, :n], kk, kk[:, :n], -TWO_PI, a1, a1[:, :n], ALU.mult, ALU.add)
                        kb.act(a1, a1[:, :n], a1, a1[:, :n], AF.Sin)
                        kb.mm(B, B[0:64, :n], w2, w2[:], a1, a1[:, :n])
                        kb.act(a2, a2[:, :n], B, B[0:64, :n], AF.Identity, extra=[pr], scale=pr[:, 0:1], bias=pr[:, 4:5])
                        kb.ts(kk, kk[:, :n], a2, a2[:, :n], 1.0 / TWO_PI, 12582912.0, ALU.mult, ALU.add)
                        kb.ts(kk, kk[:, :n], kk, kk[:, :n], -12582912.0, None, ALU.add)
                        kb.stt(a2, a2[:, :n], kk, kk[:, :n], -TWO_PI, a2, a2[:, :n], ALU.mult, ALU.add)
                        kb.act(a2, a2[:, :n], a2, a2[:, :n], AF.Sin)
                        for o in range(2):
                            C = self.bank[2 + o]
                            kb.mm(C, C[:, :n], w3, w3[:, dd * 2 + o, :], a2, a2[:, :n])
                            kb.act(win, win[:, :n], t_, t_[:, :n], AF.Exp, extra=[dl], scale=dl[:, dd * 2 + o:dd * 2 + o + 1])
                            kb.stt(hh, hh[:, :n], win, win[:, :n], 0.05, C, C[:, :n], ALU.add, ALU.mult)
                            if dd == 1 and j0 + n == L:
                                kb.memset(hh, hh[:, n - 1:n], 0.0, e="dve")
                            kb.act(hab, hab[:, :n], hh, hh[:, :n], AF.Abs)
                            kb.op("dve", lambda e: e.tensor_reduce(out=red[:, 0:1], in_=hab[:, :n], axis=AX.X, op=ALU.add), reads=[hab], writes=[red])
                            kb.tt(nacc, nacc[:, 2 * si_ + o:2 * si_ + o + 1], nacc, nacc[:, 2 * si_ + o:2 * si_ + o + 1], red, red[:, 0:1], ALU.add)
                            hb_ = hbf[o]
                            kb.copy(hb_, hb_[:, :n], hh, hh[:, :n], e="pool")
                            base = (127 if dd == 1 else L + 126) + j0
                            nn = n - 1 if (dd == 1 and j0 + n == L) else n
                            kb.dma(TA[L][o][:, base:base + nn], hb_[:, :nn], reads=[hb_], writes=[TA[L][o]])
            kb.op("dve", lambda e: e.reciprocal(out=nacc[:, 4:8], in_=nacc[:, 0:4]), reads=[nacc], writes=[nacc])
            nrm = kb.sb([128, 4], F32, "hnrm_keep") if False else None
            self.h_nrm = kb.dram([128, 4], F32, "h_nrm"); self.h_sk = kb.dram([128, 2], F32, "h_sk")
            kb.dma(self.h_nrm[:], nacc[:, 4:8], reads=[nacc], writes=[self.h_nrm])
            kb.dma(self.h_sk[:], dl[:, 4:6], reads=[dl], writes=[self.h_sk])
        ZC = kb.dram([128, nt], F32, "HZC")
        for si_, (s0, L) in enumerate(segs):
            nb = L // 128; pad = nb - 1; W = (2 * nb - 1) * 128
            with kb.scope():
                jm = kb.sb([128, 128], BF16, "jm"); jmf = kb.sb([128, 128], F32, "jmf")
                kb.dma(jmf[:], jmi[:], reads=[jmi], writes=[jmf]); kb.copy(jm, jm[:], jmf, jmf[:])
                nr = kb.sb([128, 4], F32, "hnr"); sk = kb.sb([128, 2], F32, "hsk")
                kb.dma(nr[:], self.h_nrm[:], reads=[self.h_nrm], writes=[nr]); kb.dma(sk[:], self.h_sk[:], reads=[self.h_sk], writes=[sk])
                zp = kb.sb([128, nb + 2 * pad, 128], BF16, "zp")
                if pad:
                    kb.memset(zp, zp[:, 0:pad, :], 0.0); kb.memset(zp, zp[:, pad + nb:, :], 0.0)
                yT = kb.sb([128, nb, 128], F32, "yT")
                HP = min(2 * nb - 1, 32)
                Hs = [kb.sb([128, HP * 128], BF16, "Hs") for _ in range(4)]
                hi_ = 0
                zf = [kb.sb([128, 128], F32, "zf") for _ in range(2)]; zb = [kb.sb([128, 128], BF16, "zb") for _ in range(2)]
                zt = [kb.sb([128, 128], BF16, "zt") for _ in range(2)]
                g_ = [kb.sb([128, 128], F32, "hg") for _ in range(2)]; yo = [kb.sb([128, 128], F32, "hyo") for _ in range(2)]
                y16 = [kb.sb([128, 128], BF16, "hy16") for _ in range(2)]
                for o in range(2):
                    src = HV if o == 0 else ZC
                    for J in range(nb):
                        a = zf[J % 2]; b_ = zb[J % 2]; c_ = zt[J % 2]
                        if o == 0:
                            kb.dma(a[:], HV[0, :, s0 + J * 128:s0 + (J + 1) * 128], reads=[HV], writes=[a])
                        else:
                            kb.dma(a[:], ZC[:, s0 + J * 128:s0 + (J + 1) * 128], reads=[ZC], writes=[a])
                        kb.copy(b_, b_[:], a, a[:])
                        P1 = self.bank[J % 2]
                        pv = P1[:].bitcast(BF16)
                        kb.op("pe", lambda e: e.transpose(pv[:, 0:128], b_[:], self.identb[:]), reads=[b_, self.identb], writes=[P1])
                        kb.copy(c_, c_[:], P1, pv[:, 0:128], e="act")
                        P2 = self.bank[2 + J % 2]
                        kb.mm(P2, P2[:, 0:128], jm, jm[:], c_, c_[:])
                        kb.copy(zp, zp[:, pad + J, :], P2, P2[:, 0:128])
                    for c in range(128):
                        Y = self.bank[4 + c % 4]
                        for p0, pn in groups_of(2 * nb - 1, HP):
                            H = Hs[hi_ % 4]; hi_ += 1
                            hsrc = bass.AP(tensor=TA[L][o].h, offset=c * (2 * L + 254) + 127 + p0 * 128, ap=[[1, 128], [1, pn * 128]])
                            kb.dma(H[:, 0:pn * 128], hsrc, reads=[TA[L][o]], writes=[H])
                            for dq in range(pn):
                                dp = p0 + dq
                                kb.mm(Y, Y[:, 0:nb], H, H[:, dq * 128:(dq + 1) * 128], zp, zp[:, 2 * pad - dp:2 * pad - dp + nb, c],
                                      start=(dp == 0), stop=(dp == 2 * nb - 2))
                        kb.copy(yT, yT[:, :, c], Y, Y[:, 0:nb], e=("dve", "act")[c % 2])
                    for I in range(nb):
                        P1 = self.bank[I % 2]
                        kb.op("pe", lambda e: e.transpose(P1[:, 0:128], yT[:, I, :], self.ident[:]), reads=[yT, self.ident], writes=[P1])
                        a = zf[I % 2]; gt = g_[I % 2]; y_ = yo[I % 2]
                        c0 = s0 + I * 128
                        if o == 0:
                            kb.dma(a[:], HV[0, :, c0:c0 + 128], reads=[HV], writes=[a])
                        else:
                            kb.dma(a[:], ZC[:, c0:c0 + 128], reads=[ZC], writes=[a])
                        kb.dma(gt[:], HV[1 + o, :, c0:c0 + 128], reads=[HV], writes=[gt])
                        kb.ts(y_, y_[:], P1, P1[:, 0:128], nr[:, 2 * si_ + o:2 * si_ + o + 1], None, ALU.mult, extra=[nr])
                        kb.stt(y_, y_[:], a, a[:], sk[:, o:o + 1], y_, y_[:], ALU.mult, ALU.add, extra=[sk])
                        if o == 0:
                            kb.tt(y_, y_[:], y_, y_[:], gt, gt[:], ALU.mult)
                            kb.dma(ZC[:, c0:c0 + 128], y_[:], reads=[y_], writes=[ZC])
                        else:
                            yb = y16[I % 2]
                            kb.tt(yb, yb[:], y_, y_[:], gt, gt[:], ALU.mult)
                            yt, yap = self.YT_my.cols(c0, 128)
                            kb.dma(yap[0:128, :], yb[:], reads=[yb], writes=[yt])


def build_full(depth=DEPTH, nl=NL):
    P = Prog(nl, depth, ())
    kb = P.kb
    P.setup_consts()
    P.setup_mod()
    P.ln_alloc()
    P.alloc_A()
    P.alloc_mix()
    P.alloc_tail()
    P.alloc_moe()
    xT = P.inp("xT", [2, 128, P.ntok])
    x_in = xT
    for l in range(depth):
        lam_init = 0.8 - 0.6 * math.exp(-0.3 * l)
        P.phase_A(l, x_in, 0, 1, P.hT_my, P.hT_all)
        P.phase_B(l)
        P.phase_hyena(l)
        P.phase_gdn(l)
        P.phase_diff(l, lam_init)
        P.phase_na(l)
        for (c0, cn, tm), (_, _, ta) in zip(P.YT_my.ch, P.YT_allc.ch):
            kb.allgather(tm, ta, QUADS)
        P.phase_merge(l, x_in)
        P.phase_moe(l)
        x_in = P.x2T
    o = P.out("o_x", [2, 128, nl])
    for j in range(2):
        kb.dma(o[j, :, :], P.x2T[j, :, 0:nl], reads=[P.x2T], writes=[o])
    kb.finish([o])
    return P


def core_bg(r): return r // 4, r % 4
def prep_common(inp, r, nl, l_list=(0,)):
    b, g = core_bg(r)
    m = {}
    sel = np.zeros((8, 2), np.float32); sel[0::2, 0] = 1; sel[1::2, 1] = 1
    m["c_sel8"] = sel
    cc = np.stack([inp["c"][b], inp["c_ctx"]], -1)
    m["cT"] = np.ascontiguousarray(cc.reshape(8, 128, 2).transpose(1, 0, 2))
    cols = np.concatenate([w * 1024 + 256 * g + np.arange(256) for w in range(6)])
    for l in l_list:
        wm = inp["w_mod"][l][:, cols]
        m[f"wmod{l}"] = np.ascontiguousarray(wm.reshape(8, 128, 1536).transpose(1, 0, 2))
        m[f"bmod{l}"] = np.ascontiguousarray(inp["b_mod"][l][cols].reshape(12, 128).T)
    xs = np.concatenate([inp["x"][b, :nl], inp["ctx"][b]], 0)
    m["xT"] = np.ascontiguousarray(xs[:, 256 * g:256 * g + 256].T.reshape(2, 128, nl + 256))
    return m

SPLIT = [1536, 1536, 512, 16, 16, 1536, 1536, 4096]
OFF = np.concatenate([[0], np.cumsum(SPLIT)])
def wg_cols(g):
    o_hy, o_gq, o_gz, o_ga, o_gb, o_d, o_n, o_gate = OFF[:8]
    c = []
    for part in range(3): c.append(o_hy + part * 512 + 128 * g + np.arange(128))
    for part in range(3): c.append(o_gq + part * 512 + 128 * g + np.arange(128))
    c.append(o_gz + 128 * g + np.arange(128))
    for part in range(3): c.append(o_d + part * 512 + 128 * g + np.arange(128))
    for part in range(3): c.append(o_n + part * 512 + 128 * g + np.arange(128))
    ab = []
    for base in (o_ga, o_gb):
        for d in range(2):
            for h in range(2): ab.append(base + d * 8 + 2 * g + h)
    c.append(np.array(ab))
    return np.concatenate(c)
def rope_tables(nl):
    t = np.arange(nl); row = (t // 64).astype(np.float32); col = (t % 64).astype(np.float32)
    inv = (10000.0 ** (-np.arange(16, dtype=np.float32) / 16)).astype(np.float32)
    ar = row[:, None] * inv; ac = col[:, None] * inv
    cosd = np.ones((64, nl + 256), np.float32); sind = np.zeros((64, nl + 256), np.float32)
    for hh, ang in enumerate((ar, ac)):
        for t2 in range(2):
            cosd[hh * 32 + t2 * 16: hh * 32 + t2 * 16 + 16, :nl] = np.cos(ang).T
            sind[hh * 32 + t2 * 16: hh * 32 + t2 * 16 + 16, :nl] = np.sin(ang).T
    return np.concatenate([cosd, cosd], 0), np.concatenate([sind, sind], 0)
def na_consts():
    cols = np.arange(64)
    cstart = np.clip(cols - 8, 0, 48)
    col_ok = (cols[None, :] >= cstart[:, None]) & (cols[None, :] < cstart[:, None] + 16)
    dc = np.clip(cols[None, :] - cols[:, None], -15, 15) + 15
    return col_ok.T.astype(np.float32).copy(), dc
def prep_mix(inp, r, nl, l):
    b, g = core_bg(r); m = {}
    w = inp["w_in"][l][:, wg_cols(g)]
    m[f"wg{l}"] = np.ascontiguousarray(w.reshape(8, 128, -1).transpose(1, 0, 2))
    c, s_ = rope_tables(nl); m["c_cosT"] = c; m["c_sinT"] = s_
    m["c_ident"] = np.eye(128, dtype=np.float32)
    m[f"dlam{l}"] = inp["diff_lam"][l]
    m[f"dnorm{l}"] = inp["diff_norm"][l][None, :]
    mask, dc = na_consts(); m["c_namask"] = mask
    rp = inp["na_rpb"][l][2 * g:2 * g + 2]
    bias = rp[:, :, dc]
    m[f"nabias{l}"] = np.ascontiguousarray(bias.transpose(3, 0, 1, 2).reshape(64, -1))
    return m

def prep_tail(inp, r, nl, l, ref=None):
    import ml_dtypes
    b, g = core_bg(r); m = {}
    og = OFF[7]
    gc = np.concatenate([og + mm * 1024 + 256 * g + np.arange(256) for mm in range(4)])
    wgate = inp["w_in"][l][:, gc]
    m[f"wgate{l}"] = np.ascontiguousarray(wgate.reshape(8, 128, 1024).transpose(1, 0, 2))
    bp = inp["branch_proj"][l][:, :, 256 * g:256 * g + 256]
    bp = bp.reshape(4, 4, 128, 256).transpose(2, 1, 0, 3)
    m[f"bp{l}"] = np.ascontiguousarray(bp.reshape(128, 16, 256))
    wo = inp["w_out"][l][:, 256 * g:256 * g + 256]
    m[f"wout{l}"] = np.ascontiguousarray(wo.reshape(8, 128, 256).transpose(1, 0, 2))
    f = 256 * g + np.arange(256)
    lnp = np.stack([inp["ln_g"][l, 0][f], inp["ln_b"][l, 0][f], inp["ln_g"][l, 1][f], inp["ln_b"][l, 1][f]], -1)
    m[f"lnp{l}"] = np.ascontiguousarray(lnp.reshape(2, 128, 4).transpose(1, 0, 2))
    if ref is not None:
        ys = []
        for g2 in range(4):
            for nm in ("ya", "yb", "yc", "yd"):
                full = np.concatenate([ref[nm + "_x"][b, :nl], ref[nm + "_c"][b]], 0)
                ys.append(full[:, 128 * g2:128 * g2 + 128].T)
        m["yt_ref"] = np.concatenate(ys, 0).astype(ml_dtypes.bfloat16)
    return m

def prep_moe(inp, r, nl, l, ref=None):
    b, g = core_bg(r); m = {}
    f = 256 * g + np.arange(256)
    m["rw"] = np.ascontiguousarray(inp["router_w"][f].reshape(2, 128, 16).transpose(1, 0, 2))
    m["rb"] = inp["router_b"][None, :]
    comb = np.zeros((64, 16), np.float32)
    for rr in range(4):
        comb[rr * 16 + np.arange(16), np.arange(16)] = 1
    m["c_comb"] = comb
    es = np.zeros((16, 4, 128), np.float32)
    for i in range(4): es[4 * g + i, i, :] = 1
    m["c_esel"] = es.reshape(16, 512)
    E = [4 * g + i for i in range(4)]
    m[f"ew1_{l}"] = np.ascontiguousarray(inp["exp_w1"][l][E].reshape(4, 8, 128, 512).transpose(0, 2, 1, 3).reshape(4, 128, 4096))
    m[f"ew3_{l}"] = np.ascontiguousarray(inp["exp_w3"][l][E].reshape(4, 8, 128, 512).transpose(0, 2, 1, 3).reshape(4, 128, 4096))
    m[f"ew2_{l}"] = np.ascontiguousarray(inp["exp_w2"][l][E].reshape(4, 4, 128, 1024).transpose(0, 2, 1, 3).reshape(4, 128, 4096))
    m["c_ident"] = np.eye(128, dtype=np.float32)
    if ref is not None:
        x1 = np.concatenate([ref['x1'][b, :nl], ref['xc1'][b]], 0)[:, f].T
        m["x1_ref"] = np.ascontiguousarray(x1.reshape(2, 128, -1))
    return m

def prep_gdn(inp, r, nl, l):
    b, g = core_bg(r); m = {}
    gc = np.concatenate([inp["gdn_conv"][l][part * 512 + 128 * g + np.arange(128)] for part in range(3)], 1)
    m[f"gconv{l}"] = np.ascontiguousarray(gc)
    al = [inp["gdn_a_log"][l][d, 2 * g + h] for d in range(2) for h in range(2)]
    dtb = [inp["gdn_dt_bias"][l][d, 2 * g + h] for d in range(2) for h in range(2)]
    m[f"gpar{l}"] = np.array([al + dtb], np.float32)
    m[f"gnorm{l}"] = np.concatenate([inp["gdn_norm"][l]] * 2)[None, :].astype(np.float32)
    p = np.arange(64)
    MU = (p[:, None] <= p[None, :]).astype(np.float32); ML = (p[:, None] >= p[None, :]).astype(np.float32)
    I = np.eye(64, dtype=np.float32); ON = np.ones((64, 64), np.float32)
    m["c_gdn"] = np.ascontiguousarray(np.stack([MU, ML, I, ON, MU - I, ML - I, ON], 1).reshape(64, 7 * 64))
    bo = np.zeros((128, 128), np.float32); bo[:64, :64] = 1; bo[64:, 64:] = 1
    m["c_bones"] = bo
    return m

def hy_feats(L):
    f32 = np.float32
    t = np.linspace(0.0, 1.0, L, dtype=f32)[:, None]
    ang = (f32(2.0 * np.pi) * np.arange(L, dtype=f32)[:, None] / f32(L)).astype(f32)
    bands = np.linspace(1e-4, 15, 16, dtype=f32)[None, :]
    feats = np.concatenate([t, np.cos(bands * ang), -np.sin(bands * ang)], -1).astype(f32)
    F = np.concatenate([feats.T, feats[::-1].T], 1)
    T = np.concatenate([t[:, 0], t[::-1, 0]])[None, :]
    return np.ascontiguousarray(F), np.ascontiguousarray(T.astype(f32))
def prep_hy(inp, r, nl, l):
    b, g = core_bg(r); m = {}
    ch = 128 * g + np.arange(128)
    m[f"hconv{l}"] = np.ascontiguousarray(np.concatenate([inp["hy_conv"][l][part * 512 + ch] for part in range(3)], 1))
    m[f"hw1_{l}"] = inp["hy_w1"][l]; m[f"hw2_{l}"] = inp["hy_w2"][l]
    m[f"hw3_{l}"] = np.ascontiguousarray(np.concatenate([inp["hy_w3"][l][:, dd * 1024 + o * 512 + ch] for dd in range(2) for o in range(2)], 1))
    m[f"hpar{l}"] = np.ascontiguousarray(np.stack([inp["hy_freq"][l], inp["hy_b1"][l], inp["hy_b2"][l]], 1))
    dl = [inp["hy_deltas"][l][dd, o, ch] for dd in range(2) for o in range(2)] + [inp["hy_skip"][l][o, ch] for o in range(2)]
    m[f"hdel{l}"] = np.ascontiguousarray(np.stack(dl, 1))
    m["c_jm"] = np.eye(128, dtype=np.float32)[::-1].copy()
    for L in (nl, 256):
        F, T = hy_feats(L); m[f"c_hyF{L}"] = F; m[f"c_hyT{L}"] = T
    return m


def kernel(**inputs):
    inp = {k: np.asarray(v) for k, v in inputs.items()}
    depth = DEPTH
    P = build_full(depth, NL)
    maps = []
    for r in range(8):
        m = prep_common(inp, r, NL, l_list=tuple(range(depth)))
        for l in range(depth):
            m.update(prep_mix(inp, r, NL, l)); m.update(prep_gdn(inp, r, NL, l)); m.update(prep_hy(inp, r, NL, l))
            m.update(prep_tail(inp, r, NL, l)); m.update(prep_moe(inp, r, NL, l))
        missing = sorted(set(P.inputs) - set(m))
        assert not missing, missing
        maps.append({k: np.ascontiguousarray(v) for k, v in m.items() if k in P.inputs})
    res = run_bass_kernel_spmd(P.nc, maps, core_ids=list(range(8)))
    out = np.zeros((2, NL, D), np.float32)
    for r in range(8):
        b, g = r // 4, r % 4
        o = np.asarray(res.results[r]["o_x"])
        out[b, :, 256 * g:256 * g + 256] = o.reshape(256, NL).T
    return out
```

```python
import math
from contextlib import ExitStack
import numpy as np
import concourse.bass as bass
import concourse.mybir as mybir
from concourse.bass_utils import run_bass_kernel_spmd

F32 = mybir.dt.float32
BF16 = mybir.dt.bfloat16
AF = mybir.ActivationFunctionType
ALU = mybir.AluOpType
AX = mybir.AxisListType

D = 1024
NL = 8192
NC = 256
NTOK = NL + NC
DEPTH = 4
GRID_W = 64
LN_EPS = 1e-5
RMS_EPS = 1e-6
DN_ALPHA = (2 * DEPTH) ** 0.25
QUADS = [[0, 1, 2, 3], [4, 5, 6, 7]]
NWG = 13 * 128 + 8


class T:
    __slots__ = ("h", "w", "r", "name", "excl")

    def __init__(self, h, name=None, excl=False):
        self.h = h; self.w = None; self.r = {}; self.name = name; self.excl = excl

    def __getitem__(self, idx):
        return self.h[idx]


class KB:
    def __init__(self, nc, n_lanes=24):
        self.nc = nc
        self.eng = {"pe": nc.tensor, "act": nc.scalar, "dve": nc.vector, "pool": nc.gpsimd, "sp": nc.sync}
        self.sem = {k: nc.alloc_semaphore("s_" + k) for k in self.eng}
        self.cnt = {k: 0 for k in self.eng}
        self.lanes = [nc.alloc_semaphore(f"lane{i}") for i in range(n_lanes)]
        self.lane_cnt = [0] * n_lanes
        self.lane_rr = 0
        self.cc = nc.alloc_semaphore("cc")
        self.cc_cnt = 0
        self.waited = {}
        self.nid = 0
        self.dq = 0
        self.stack = None

    def sb(self, shape, dtype, name="sb"):
        self.nid += 1
        nm = f"{name}_{self.nid}"
        if self.stack is not None:
            return T(self.stack.enter_context(self.nc.sbuf_tensor(nm, list(shape), dtype)), nm)
        return T(self.nc.alloc_sbuf_tensor(nm, list(shape), dtype), nm)

    def barrier(self):
        targets = [(self.sem[k], self.cnt[k]) for k in self.eng]
        targets += [(self.lanes[i], 16 * c) for i, c in enumerate(self.lane_cnt)]
        targets.append((self.cc, self.cc_cnt))
        for e in self.eng:
            for sm, v in targets:
                if v:
                    self._wait(e, sm, v)

    def scope(self):
        kb = self

        class _S:
            def __enter__(s2):
                s2.prev = kb.stack
                kb.stack = ExitStack()
                kb.stack.__enter__()
                return s2

            def __exit__(s2, *a):
                kb.barrier()
                kb.stack.__exit__(None, None, None)
                kb.stack = s2.prev
                return False
        return _S()

    def ps(self, shape, dtype=F32, name="ps"):
        self.nid += 1
        nm = f"{name}_{self.nid}"
        return T(self.nc.alloc_psum_tensor(nm, list(shape), dtype), nm, excl=True)

    def dram(self, shape, dtype, name="dr", kind="Internal"):
        self.nid += 1
        nm = name if kind != "Internal" else f"{name}_{self.nid}"
        return T(self.nc.dram_tensor(nm, list(shape), dtype, kind=kind), nm)

    def _wait(self, e, sem, val):
        key = (e, id(sem))
        if self.waited.get(key, 0) >= val:
            return
        self.waited[key] = val
        self.eng[e].wait_ge(sem, val)

    def _deps(self, e, reads, writes, skip_same=False):
        deps = {}

        def add(d):
            if d is None:
                return
            s, v = d
            if deps.get(id(s), (None, 0))[1] < v:
                deps[id(s)] = (s, v)
        for t in reads:
            add(t.w)
            if t.excl:
                for s_v in t.r.values():
                    add(s_v)
        for t in writes:
            add(t.w)
            for s_v in t.r.values():
                add(s_v)
        for s, v in deps.values():
            if skip_same and s is self.sem.get(e):
                continue
            self._wait(e, s, v)

    def _mark(self, me, reads, writes):
        s, v = me
        for t in reads:
            t.r[id(s)] = me
        for t in writes:
            t.w = me; t.r = {}

    def op(self, e, fn, reads=(), writes=()):
        self._deps(e, reads, writes, skip_same=(e == "pe"))
        ins = fn(self.eng[e])
        self.cnt[e] += 1
        ins.then_inc(self.sem[e], 1)
        self._mark((self.sem[e], self.cnt[e]), reads, writes)
        return ins

    def dma(self, out, in_, reads=(), writes=(), q=None, **kw):
        if q is None:
            q = ("sp", "act", "pool")[self.dq % 2]
            self.dq += 1
        self._deps(q, reads, writes)
        li = self.lane_rr; self.lane_rr = (self.lane_rr + 1) % len(self.lanes)
        ls = self.lanes[li]
        if self.lane_cnt[li]:
            self._wait(q, ls, 16 * self.lane_cnt[li])
        ins = self.eng[q].dma_start(out=out, in_=in_, **kw)
        self.lane_cnt[li] += 1
        ins.then_inc(ls, 16)
        self._mark((ls, 16 * self.lane_cnt[li]), reads, writes)
        return ins

    def allgather(self, src, dst, groups):
        self._deps("pool", [src], [dst])
        ins = self.nc.gpsimd.collective_compute("AllGather", ALU.bypass, replica_groups=groups,
                                                ins=[src.h.ap().opt()], outs=[dst.h.ap().opt()])
        self.cc_cnt += 1
        ins.then_inc(self.cc)
        self._mark((self.cc, self.cc_cnt), [src], [dst])

    def finish(self, tiles, e="sp"):
        for t in tiles:
            if t.w is not None:
                self._wait(e, t.w[0], t.w[1])

    def mm(self, out_t, out_ap, lhsT_t, lhsT_ap, rhs_t, rhs_ap, start=True, stop=True, skip=False):
        return self.op("pe", lambda e: e.matmul(out_ap, lhsT_ap, rhs_ap, start=start, stop=stop, skip_group_check=skip),
                       reads=[lhsT_t, rhs_t], writes=[out_t])

    def act(self, out_t, out_ap, in_t, in_ap, func, extra=(), **kw):
        return self.op("act", lambda e: e.activation(out=out_ap, in_=in_ap, func=func, **kw),
                       reads=[in_t, *extra], writes=[out_t])

    def tt(self, out_t, out_ap, a_t, a_ap, b_t, b_ap, op, e="dve"):
        return self.op(e, lambda en: en.tensor_tensor(out=out_ap, in0=a_ap, in1=b_ap, op=op),
                       reads=[a_t, b_t], writes=[out_t])

    def ts(self, out_t, out_ap, a_t, a_ap, s1, s2, op0, op1=None, extra=(), e="dve"):
        if op1 is None:
            return self.op(e, lambda en: en.tensor_scalar(out=out_ap, in0=a_ap, scalar1=s1, scalar2=None, op0=op0),
                           reads=[a_t, *extra], writes=[out_t])
        return self.op(e, lambda en: en.tensor_scalar(out=out_ap, in0=a_ap, scalar1=s1, scalar2=s2, op0=op0, op1=op1),
                       reads=[a_t, *extra], writes=[out_t])

    def stt(self, out_t, out_ap, a_t, a_ap, s, b_t, b_ap, op0, op1, extra=()):
        return self.op("dve", lambda en: en.scalar_tensor_tensor(out=out_ap, in0=a_ap, scalar=s, in1=b_ap, op0=op0, op1=op1),
                       reads=[a_t, b_t, *extra], writes=[out_t])

    def copy(self, out_t, out_ap, in_t, in_ap, e="dve"):
        if e == "act":
            return self.op("act", lambda en: en.copy(out=out_ap, in_=in_ap), reads=[in_t], writes=[out_t])
        return self.op(e, lambda en: en.tensor_copy(out=out_ap, in_=in_ap), reads=[in_t], writes=[out_t])

    def memset(self, t, ap, val, e="pool"):
        return self.op(e, lambda en: en.memset(ap, val), reads=[], writes=[t])


def bcast_ap(t, n, offset=0, parts=128):
    return bass.AP(tensor=t.h, offset=offset, ap=[[0, parts], [1, n]])


CHUNK = 2048


class CD:
    def __init__(self, kb, rows, ntok, dtype, name, chunk=CHUNK):
        self.ch = [(c0, cn, kb.dram([rows, cn], dtype, f"{name}{i}")) for i, (c0, cn) in enumerate(groups_of(ntok, chunk))]

    def cols(self, s, n):
        for c0, cn, t in self.ch:
            if c0 <= s and s + n <= c0 + cn:
                return t, t[:, s - c0:s - c0 + n]
        raise ValueError((s, n))


def groups_of(n, g):
    return [(i, min(g, n - i)) for i in range(0, n, g)]


class Prog:
    def __init__(self, nl=NL, depth=DEPTH, debug=None):
        self.nl = nl; self.ntok = nl + NC; self.depth = depth
        self.debug = debug or []
        self.nc = bass.Bass("TRN2", target_bir_lowering=False)
        self.kb = KB(self.nc)
        self.inputs = {}
        self.outs = {}

    def inp(self, name, shape, dtype=F32):
        if name in self.inputs:
            return self.inputs[name]
        t = self.kb.dram(shape, dtype, name, kind="ExternalInput")
        self.inputs[name] = t
        return t

    def out(self, name, shape, dtype=F32):
        t = self.kb.dram(shape, dtype, name, kind="ExternalOutput")
        self.outs[name] = t
        return t

    def tgroups(self, gs=512):
        res = [(s, n, False) for s, n in groups_of(self.nl, gs)]
        res += [(self.nl + s, n, True) for s, n in groups_of(NC, gs)]
        return res

    def setup_consts(self):
        kb = self.kb
        self.ones_col = kb.sb([128, 1], F32, "ones_col")
        kb.memset(self.ones_col, self.ones_col[:], 1.0)
        self.ones_row = kb.sb([1, 128], F32, "ones_row")
        kb.memset(self.ones_row, self.ones_row[:], 1.0)
        sel = self.inp("c_sel8", [8, 2])
        self.sel8 = kb.sb([8, 2], F32, "sel8")
        kb.dma(self.sel8[:], sel[:], reads=[sel], writes=[self.sel8])
        self.bank = [kb.ps([128, 512], F32, f"bank{i}") for i in range(8)]

    def phase_mod(self, l):
        kb = self.kb
        wm = self.inp(f"wmod{l}", [128, 8, 12 * 128])
        bm = self.inp(f"bmod{l}", [128, 12])
        modv = self.modv[l]
        bsb = self.bm_stage
        kb.dma(bsb[:], bm[:], reads=[bm], writes=[bsb])
        ps = self.bank[0]
        for k in range(8):
            wt = self.wm_stage[k % 2]
            kb.dma(wt[:], wm[:, k, :], reads=[wm], writes=[wt])
            for c in range(12):
                kb.mm(ps, ps[:, 2 * c:2 * c + 2], wt, wt[:, c * 128:(c + 1) * 128], self.csil, self.csil[:, k, :],
                      start=(k == 0 and c == 0), stop=(k == 7), skip=True)
        for v in range(2):
            kb.tt(modv, modv[:, :, v], ps, ps[:, 0:24].rearrange("p (c v) -> p c v", v=2)[:, :, v], bsb, bsb[:], ALU.add)
        for which in (1, 4):
            kb.ts(modv, modv[:, 2 * which:2 * which + 2, :], modv, modv[:, 2 * which:2 * which + 2, :], 1.0, None, ALU.add)
        return modv

    def setup_mod(self):
        kb = self.kb
        cT = self.inp("cT", [128, 8, 2])
        self.csil = kb.sb([128, 8, 2], F32, "csil")
        craw = kb.sb([128, 8, 2], F32, "craw")
        kb.dma(craw[:], cT[:], reads=[cT], writes=[craw])
        kb.act(self.csil, self.csil[:], craw, craw[:], AF.Silu)
        self.modv = [kb.sb([128, 12, 2], F32, f"modv{l}") for l in range(self.depth)]
        with kb.scope():
            self.wm_stage = [kb.sb([128, 12 * 128], F32, f"wmst{i}") for i in range(2)]
            self.bm_stage = kb.sb([128, 12], F32, "bmst")
            for l in range(self.depth):
                self.phase_mod(l)

    def ln_alloc(self):
        kb = self.kb
        self.st_in = kb.dram([2, self.ntok], F32, "st_in")
        self.st_all = kb.dram([8, self.ntok], F32, "st_all")
        self.ln_sq = kb.sb([128, 512], F32, "ln_sq")
        self.ln_st = kb.sb([1, 2, 512], F32, "ln_st")
        self.ln_sa = kb.sb([8, 512], F32, "ln_sa")
        self.ln_v = [kb.sb([1, 512], F32, f"ln_v{i}") for i in range(4)]

    def ln_stats(self, xt, s, n):
        kb = self.kb
        p1, p2 = self.bank[6], self.bank[7]
        for j in range(2):
            kb.mm(p1, p1[0:1, :n], self.ones_col, self.ones_col[:], xt, xt[:, j, :n], start=(j == 0), stop=(j == 1))
        for j in range(2):
            kb.act(self.ln_sq, self.ln_sq[:, :n], xt, xt[:, j, :n], AF.Square)
            kb.mm(p2, p2[0:1, :n], self.ones_col, self.ones_col[:], self.ln_sq, self.ln_sq[:, :n], start=(j == 0), stop=(j == 1))
        kb.copy(self.ln_st, self.ln_st[:, 0, :n], p1, p1[0:1, :n])
        kb.copy(self.ln_st, self.ln_st[:, 1, :n], p2, p2[0:1, :n], e="act")
        kb.dma(self.st_in[:, s:s + n].rearrange("(o r) n -> o r n", o=1), self.ln_st[:, :, :n], reads=[self.ln_st], writes=[self.st_in])

    def ln_gather(self):
        self.kb.allgather(self.st_in, self.st_all, QUADS)

    def ln_coef(self, s, n):
        kb = self.kb
        p1, p2 = self.bank[6], self.bank[7]
        kb.dma(self.ln_sa[:, :n], self.st_all[:, s:s + n], reads=[self.st_all], writes=[self.ln_sa])
        kb.mm(p1, p1[0:1, :n], self.sel8, self.sel8[:, 0:1], self.ln_sa, self.ln_sa[:, :n])
        kb.mm(p2, p2[0:1, :n], self.sel8, self.sel8[:, 1:2], self.ln_sa, self.ln_sa[:, :n])
        m, v, r, q = self.ln_v
        kb.ts(m, m[:, :n], p1, p1[0:1, :n], 1.0 / D, None, ALU.mult)
        kb.tt(v, v[:, :n], m, m[:, :n], m, m[:, :n], ALU.mult)
        kb.stt(v, v[:, :n], p2, p2[0:1, :n], 1.0 / D, v, v[:, :n], ALU.mult, ALU.subtract)
        kb.ts(v, v[:, :n], v, v[:, :n], LN_EPS, None, ALU.add)
        kb.act(v, v[:, :n], v, v[:, :n], AF.Sqrt)
        kb.op('dve', lambda en: en.reciprocal(out=r[:, :n], in_=v[:, :n]), reads=[v], writes=[r])
        kb.stt(q, q[:, :n], m, m[:, :n], -1.0, r, r[:, :n], ALU.mult, ALU.mult)
        kb.mm(p1, p1[:, :n], self.ones_row, self.ones_row[:], r, r[:, :n])
        kb.mm(p2, p2[:, :n], self.ones_row, self.ones_row[:], q, q[:, :n])
        return p1, p2

    def phase_A(self, l, xT, which_shift, which_scale, hT_my, hT_all, h32=None):
        kb = self.kb
        modv = self.modv[l]
        for (s, n, isc) in self.tgroups():
            xt = self.xa[0]
            kb.dma(xt[:, :, :n], xT[:, :, s:s + n].rearrange("j p n -> p j n"), reads=[xT], writes=[xt])
            self.ln_stats(xt, s, n)
        self.ln_gather()
        for gi, (s, n, isc) in enumerate(self.tgroups()):
            xt = self.xa[gi % 2]
            kb.dma(xt[:, :, :n], xT[:, :, s:s + n].rearrange("j p n -> p j n"), reads=[xT], writes=[xt])
            p1, p2 = self.ln_coef(s, n)
            hb = self.hb[gi % 2]
            v = 1 if isc else 0
            for j in range(2):
                tmp = self.xtmp
                kb.tt(tmp, tmp[:, :n], xt, xt[:, j, :n], p1, p1[:, :n], ALU.mult)
                kb.tt(tmp, tmp[:, :n], tmp, tmp[:, :n], p2, p2[:, :n], ALU.add)
                kb.act(hb, hb[:, j, :n], tmp, tmp[:, :n], AF.Identity, extra=[modv],
                       scale=modv[:, 2 * which_scale + j, v:v + 1], bias=modv[:, 2 * which_shift + j, v:v + 1])
                if h32 is not None:
                    kb.act(h32[1], h32[1][:, j, :n], tmp, tmp[:, :n], AF.Identity, extra=[modv],
                           scale=modv[:, 2 * which_scale + j, v:v + 1], bias=modv[:, 2 * which_shift + j, v:v + 1])
            ht, hap = hT_my.cols(s, n)
            kb.dma(hap.rearrange("(j p) n -> p j n", p=128), hb[:, :, :n], reads=[hb], writes=[ht])
            if h32 is not None:
                kb.dma(h32[0][:, :, s:s + n].rearrange("j p n -> p j n"), h32[1][:, :, :n], reads=[h32[1]], writes=[h32[0]])
        for (c0, cn, tm), (_, _, ta) in zip(hT_my.ch, hT_all.ch):
            kb.allgather(tm, ta, QUADS)

    def alloc_A(self):
        kb = self.kb
        self.xa = [kb.sb([128, 2, 512], F32, f"xa{i}") for i in range(2)]
        self.hb = [kb.sb([128, 2, 512], BF16, f"hb{i}") for i in range(2)]
        self.xtmp = kb.sb([128, 512], F32, "xtmp")
        self.hT_my = CD(kb, 256, self.ntok, BF16, "hT_my")
        self.hT_all = CD(kb, 1024, self.ntok, BF16, "hT_all")


    def alloc_mix(self):
        kb = self.kb; nt = self.ntok
        self.HY = kb.dram([3, 128, nt], F32, "HY")
        self.GP = kb.dram([3, 128, nt], F32, "GP")
        self.GZ = kb.dram([nt, 128], F32, "GZ")
        self.GAB = kb.dram([nt, 8], F32, "GAB")
        self.DQ = kb.dram([128, nt], BF16, "DQ"); self.DK = kb.dram([128, nt], BF16, "DK")
        self.DV = kb.dram([nt, 128], BF16, "DV")
        self.NQ = kb.dram([128, nt], BF16, "NQ"); self.NK = kb.dram([128, nt], BF16, "NK")
        self.NV = kb.dram([nt, 128], BF16, "NV")
        self.YT_my = CD(kb, 512, nt, BF16, "YT_my", chunk=1024)
        self.YT_allc = CD(kb, 2048, nt, BF16, "YT_all", chunk=1024)
        self.cosT = self.inp("c_cosT", [128, nt]); self.sinT = self.inp("c_sinT", [128, nt])
        self.alloc_mix_ident()

    def alloc_mix_ident(self):
        kb = self.kb
        if hasattr(self, "ident"):
            return
        identb = self.inp("c_ident", [128, 128])
        self.ident = kb.sb([128, 128], F32, "ident")
        kb.dma(self.ident[:], identb[:], reads=[identb], writes=[self.ident])
        self.identb = kb.sb([128, 128], BF16, "identb")
        kb.copy(self.identb, self.identb[:], self.ident, self.ident[:])

    def phase_B(self, l):
        kb = self.kb
        wg = self.inp(f"wg{l}", [128, 8, NWG])
        with kb.scope():
            wgb = kb.sb([128, 8, NWG + 256], BF16, "wgb")
            stg = [kb.sb([128, NWG], F32, "wgst") for _ in range(2)]
            for k in range(8):
                st = stg[k % 2]
                kb.dma(st[:], wg[:, k, :], reads=[wg], writes=[st])
                kb.copy(wgb, wgb[:, k, 0:NWG], st, st[:], e=("dve", "pool")[k % 2])
            for k in range(8):
                for si, src in enumerate((7, 8)):
                    sv = wgb[:, k, src * 128:(src + 1) * 128].rearrange("p (a t s) -> p a t s", a=4, t=2, s=16)
                    dv = wgb[:, k, NWG + si * 128:NWG + (si + 1) * 128].rearrange("p (a t s) -> p a t s", a=4, t=2, s=16)
                    kb.ts(wgb, dv[:, :, 0, :], wgb, sv[:, :, 1, :], -1.0, None, ALU.mult)
                    kb.copy(wgb, dv[:, :, 1, :], wgb, sv[:, :, 0, :])
            hsb = [kb.sb([128, 8, 512], BF16, "hsb") for _ in range(2)]
            cst = kb.sb([128, 512], F32, "cst"); snt = kb.sb([128, 512], F32, "snt")
            evf = [kb.sb([128, 512], F32, "evf") for _ in range(2)]
            t1 = kb.sb([128, 512], F32, "t1"); t2 = kb.sb([128, 512], F32, "t2")
            ob = [kb.sb([128, 512], BF16, "ob") for _ in range(2)]
            tmz = [kb.sb([128, 128], F32, "tmz") for _ in range(2)]
            tmv = [kb.sb([128, 2, 128], BF16, "tmv") for _ in range(2)]
            tmg = [kb.sb([128, 8], F32, "tmg") for _ in range(2)]
            hT_all = self.hT_all
            import os
            bstop = int(os.environ.get("BSTOP", "9"))
            for gi, (s, n, isc) in enumerate(self.tgroups()):
                if bstop == 0:
                    break
                hs = hsb[gi % 2]
                ht, hap = hT_all.cols(s, n)
                kb.dma(hs[:, :, :n], hap.rearrange("(k p) n -> p k n", p=128), reads=[ht], writes=[hs])
                kb.dma(cst[:, :n], self.cosT[:, s:s + n], reads=[self.cosT], writes=[cst])
                kb.dma(snt[:, :n], self.sinT[:, s:s + n], reads=[self.sinT], writes=[snt])

                def fm(cb, bk):
                    for k in range(8):
                        kb.mm(bk, bk[:, :n], wgb, wgb[:, k, cb:cb + 128], hs, hs[:, k, :n], start=(k == 0), stop=(k == 7))
                for ci in range(6):
                    if bstop == 5:
                        break
                    bk = self.bank[ci % 4]; fm(ci * 128, bk)
                    ev = evf[ci % 2]
                    kb.copy(ev, ev[:, :n], bk, bk[:, :n], e=("dve", "act")[ci % 2])
                    dst = self.HY if ci < 3 else self.GP
                    kb.dma(dst[ci % 3, :, s:s + n], ev[:, :n], reads=[ev], writes=[dst])
                if bstop == 1:
                    continue
                for qi, (cb, rb, dst) in enumerate(()) if bstop == 5 else []:
                    pass
                for qi, (cb, rb, dst) in enumerate(((7 * 128, NWG, self.DQ), (8 * 128, NWG + 128, self.DK)) if bstop != 5 else ()):
                    b0, b1 = self.bank[0 + 2 * qi], self.bank[1 + 2 * qi]
                    fm(cb, b0); fm(rb, b1)
                    kb.tt(t1, t1[:, :n], b0, b0[:, :n], cst, cst[:, :n], ALU.mult)
                    kb.tt(t2, t2[:, :n], b1, b1[:, :n], snt, snt[:, :n], ALU.mult)
                    o = ob[qi]
                    kb.tt(o, o[:, :n], t1, t1[:, :n], t2, t2[:, :n], ALU.add, e="pool")
                    kb.dma(dst[:, s:s + n], o[:, :n], reads=[o], writes=[dst])
                if bstop == 2:
                    continue
                for qi, (cb, dst) in enumerate(((10 * 128, self.NQ), (11 * 128, self.NK)) if bstop != 5 else ()):
                    bk = self.bank[qi]; fm(cb, bk)
                    o = ob[qi]
                    kb.copy(o, o[:, :n], bk, bk[:, :n], e=("dve", "act")[qi])
                    kb.dma(dst[:, s:s + n], o[:, :n], reads=[o], writes=[dst])
                if bstop == 3:
                    continue
                for tt_ in range(n // 128):
                    bk = self.bank[int(os.environ.get("TMB", "4")) + tt_ % 2]
                    tsl = slice(tt_ * 128, (tt_ + 1) * 128)
                    for (cb, w, off) in ((6 * 128, 128, 0), (9 * 128, 128, 128), (12 * 128, 128, 256), (13 * 128, 8, 384))[:int(os.environ.get('TMN', '4'))]:
                        for k in range(8):
                            if os.environ.get("TMX", "") == "nomm":
                                break
                            kb.mm(bk, bk[:, off:off + w], hs, hs[:, k, tsl], wgb, wgb[:, k, cb:cb + w], start=(k == 0), stop=(k == 7))
                    if os.environ.get("TMX", "") == "nocopy":
                        continue
                    tok0 = s + tt_ * 128
                    z = tmz[tt_ % 2]; v = tmv[tt_ % 2]; g_ = tmg[tt_ % 2]
                    tmd = int(os.environ.get("TMD", "15"))
                    kb.copy(z, z[:], bk, bk[:, 0:128], e=os.environ.get("TME", "act"))
                    if tmd & 1:
                        kb.dma(self.GZ[tok0:tok0 + 128, :], z[:], reads=[z], writes=[self.GZ])
                    kb.copy(v, v[:, 0, :], bk, bk[:, 128:256])
                    kb.copy(v, v[:, 1, :], bk, bk[:, 256:384])
                    if tmd & 2:
                        kb.dma(self.DV[tok0:tok0 + 128, :], v[:, 0, :], reads=[v], writes=[self.DV])
                    if tmd & 4:
                        kb.dma(self.NV[tok0:tok0 + 128, :], v[:, 1, :], reads=[v], writes=[self.NV])
                    kb.copy(g_, g_[:], bk, bk[:, 384:392], e=os.environ.get("TME", "act"))
                    if tmd & 8:
                        kb.dma(self.GAB[tok0:tok0 + 128, :], g_[:], reads=[g_], writes=[self.GAB])

    def store_T(self, src_t, src_ap, nq, row0, tok0, bank, stage):
        kb = self.kb
        pb = bank
        pv = pb[:].bitcast(BF16)
        kb.op("pe", lambda e: e.transpose(pv[:, 0:nq], src_ap, self.identb[0:nq, 0:nq]), reads=[src_t, self.identb], writes=[pb])
        kb.copy(stage, stage[:, 0:nq], pb, pv[:, 0:nq])
        yt, yap = self.YT_my.cols(tok0, nq)
        kb.dma(yap[row0:row0 + 128, :], stage[:, 0:nq], reads=[stage], writes=[yt])

    def phase_diff(self, l, lam_init):
        kb = self.kb; nt = self.ntok; ntile = nt // 128
        dl = self.inp(f"dlam{l}", [4, 64]); dn = self.inp(f"dnorm{l}", [1, 128])
        with kb.scope():
            kT = kb.sb([128, nt], BF16, "kT")
            kb.dma(kT[:], self.DK[:], reads=[self.DK], writes=[kT])
            vA = kb.sb([128, ntile, 130], BF16, "vA")
            kb.memset(vA, vA[:, :, 128:130], 1.0)
            for t0, tn in groups_of(ntile, 8):
                kb.dma(vA[:, t0:t0 + tn, 0:128], self.DV[t0 * 128:(t0 + tn) * 128, :].rearrange("(t p) c -> p t c", p=128), reads=[self.DV], writes=[vA])
            lm = kb.sb([128, 4, 64], F32, "lm")
            kb.dma(lm[:].rearrange("p a b -> p (a b)"), bcast_ap(dl, 256), reads=[dl], writes=[lm])
            lp = kb.sb([128, 2, 64], F32, "lp"); ls = kb.sb([128, 4], F32, "ls")
            kb.tt(lp, lp[:, 0, :], lm, lm[:, 0, :], lm, lm[:, 1, :], ALU.mult)
            kb.tt(lp, lp[:, 1, :], lm, lm[:, 2, :], lm, lm[:, 3, :], ALU.mult)
            kb.op("dve", lambda e: e.tensor_reduce(out=ls[:, 0:2], in_=lp[:], axis=AX.X, op=ALU.add), reads=[lp], writes=[ls])
            kb.act(ls, ls[:, 0:2], ls, ls[:, 0:2], AF.Exp)
            kb.tt(ls, ls[:, 2:3], ls, ls[:, 0:1], ls, ls[:, 1:2], ALU.subtract)
            kb.ts(ls, ls[:, 3:4], ls, ls[:, 2:3], lam_init, -1.0, ALU.add, ALU.mult)
            nw = kb.sb([128, 128], F32, "nw")
            kb.dma(nw[:], bcast_ap(dn, 128), reads=[dn], writes=[nw])
            kb.ts(nw, nw[:], nw, nw[:], 1.0 - lam_init, None, ALU.mult)
            qsb = [kb.sb([128, 512], BF16, "qsb") for _ in range(2)]
            pT = [kb.sb([128, 512], BF16, "pT") for _ in range(4)]
            rr = kb.sb([128, 4], F32, "rr"); of = kb.sb([128, 128], F32, "of"); osq = kb.sb([128, 128], F32, "osq")
            ob16 = [kb.sb([128, 128], BF16, "ob16") for _ in range(2)]
            stg = [kb.sb([128, 128], BF16, "stg") for _ in range(2)]
            sc = 64 ** -0.5
            pi = 0
            import os
            dstop = int(os.environ.get("DSTOP", "9"))
            for gi, (s, n, isc) in enumerate(self.tgroups()):
                if dstop == 0:
                    break
                q = qsb[gi % 2]
                kb.dma(q[:, :n], self.DQ[:, s:s + n], reads=[self.DQ], writes=[q])
                ktiles = list(range(self.nl // 128, ntile)) if isc else list(range(ntile))
                nqt = n // 128
                first = {}
                for ki, kt in enumerate(ktiles):
                    for c in range(2):
                        sb_ = self.bank[4 + (pi % 4)]
                        kb.mm(sb_, sb_[:, :n], kT, kT[c * 64:(c + 1) * 64, kt * 128:(kt + 1) * 128], q, q[c * 64:(c + 1) * 64, :n])
                        p = pT[pi % 4]; pi += 1
                        kb.act(p, p[:, :n], sb_, sb_[:, :n], AF.Exp, scale=sc)
                        for qt in range(nqt):
                            ab = self.bank[c * 2 + qt // 2]; off = (qt % 2) * 160
                            st = id(ab) not in first
                            first[id(ab)] = 1
                            kb.mm(ab, ab[:, off:off + 129], p, p[:, qt * 128:(qt + 1) * 128], vA, vA[:, kt, 0:129],
                                  start=st, stop=(ki == len(ktiles) - 1), skip=True)
                for qt in range(nqt):
                    if dstop == 1:
                        break
                    a0 = self.bank[0 + qt // 2]; a1 = self.bank[2 + qt // 2]; off = (qt % 2) * 160
                    kb.op("dve", lambda e: e.reciprocal(out=rr[:, 0:1], in_=a0[:, off + 128:off + 129]), reads=[a0], writes=[rr])
                    kb.op("dve", lambda e: e.reciprocal(out=rr[:, 1:2], in_=a1[:, off + 128:off + 129]), reads=[a1], writes=[rr])
                    kb.tt(rr, rr[:, 1:2], rr, rr[:, 1:2], ls, ls[:, 3:4], ALU.mult)
                    kb.ts(of, of[:], a0, a0[:, off:off + 128], rr[:, 0:1], None, ALU.mult, extra=[rr])
                    kb.stt(of, of[:], a1, a1[:, off:off + 128], rr[:, 1:2], of, of[:], ALU.mult, ALU.add, extra=[rr])
                    kb.op("act", lambda e: e.activation(out=osq[:], in_=of[:], func=AF.Square, accum_out=rr[:, 2:3]), reads=[of], writes=[osq, rr])
                    kb.ts(rr, rr[:, 2:3], rr, rr[:, 2:3], 1.0 / 128, RMS_EPS, ALU.mult, ALU.add)
                    kb.act(rr, rr[:, 2:3], rr, rr[:, 2:3], AF.Sqrt)
                    kb.op("dve", lambda e: e.reciprocal(out=rr[:, 3:4], in_=rr[:, 2:3]), reads=[rr], writes=[rr])
                    o16 = ob16[qt % 2]
                    kb.stt(o16, o16[:], of, of[:], rr[:, 3:4], nw, nw[:], ALU.mult, ALU.mult, extra=[rr])
                    if dstop == 2:
                        continue
                    self.store_T(o16, o16[:], 128, 256, s + qt * 128, self.bank[4 + qt % 2], stg[qt % 2])


    def phase_na(self, l):
        kb = self.kb; nt = self.ntok; nl = self.nl; R = nl // 64
        nb = self.inp(f"nabias{l}", [64, 2 * 15 * 64])
        nm = self.inp("c_namask", [64, 64])
        sc = 64 ** -0.5
        with kb.scope():
            kT = kb.sb([128, nt], BF16, "nkT"); qT = kb.sb([128, nt], BF16, "nqT")
            kb.dma(kT[:], self.NK[:], reads=[self.NK], writes=[kT])
            kb.dma(qT[:], self.NQ[:], reads=[self.NQ], writes=[qT])
            v64 = kb.sb([64, R, 2, 65], BF16, "v64")
            kb.memset(v64, v64[:, :, :, 64:65], 1.0)
            for r0, rn in groups_of(R, 16):
                for hh in range(2):
                    kb.dma(v64[:, r0:r0 + rn, hh, 0:64],
                           self.NV[r0 * 64:(r0 + rn) * 64, hh * 64:(hh + 1) * 64].rearrange("(r p) c -> p r c", p=64),
                           reads=[self.NV], writes=[v64])
            vcx = kb.sb([128, 2, 2, 65], BF16, "vcx")
            kb.memset(vcx, vcx[:, :, :, 64:65], 1.0)
            for hh in range(2):
                kb.dma(vcx[:, :, hh, 0:64], self.NV[nl:nt, hh * 64:(hh + 1) * 64].rearrange("(t p) c -> p t c", p=128), reads=[self.NV], writes=[vcx])
            EB = kb.sb([64, 2, 15, 64], F32, "EB"); msk = kb.sb([64, 64], F32, "msk")
            kb.dma(EB[:].rearrange("p a b c -> p (a b c)"), nb[:], reads=[nb], writes=[EB])
            kb.dma(msk[:], nm[:], reads=[nm], writes=[msk])
            kb.act(EB, EB[:].rearrange("p a b c -> p (a b c)"), EB, EB[:].rearrange("p a b c -> p (a b c)"), AF.Exp)
            for hh in range(2):
                for dr in range(15):
                    kb.tt(EB, EB[:, hh, dr, :], EB, EB[:, hh, dr, :], msk, msk[:], ALU.mult)
            pwf = [kb.sb([64, 512], F32, "pwf") for _ in range(2)]
            pwb = [kb.sb([64, 512], BF16, "pwb") for _ in range(2)]
            pcb = [kb.sb([128, 256], BF16, "pcb") for _ in range(2)]
            rr = kb.sb([128, 4], F32, "nrr")
            o16 = [kb.sb([128, 128], BF16, "no16") for _ in range(2)]
            stg = [kb.sb([128, 128], BF16, "nstg") for _ in range(2)]
            it = 0
            for r in range(R):
                rs = min(max(r - 4, 0), R - 8)
                dr0 = rs - r + 7
                o = o16[r % 2]
                for hh in range(2):
                    hp = slice(hh * 64, (hh + 1) * 64)
                    sbk = self.bank[4 + it % 2]; sck = self.bank[6 + it % 2]; ab = self.bank[it % 2]
                    qv = qT[hp, r * 64:(r + 1) * 64]
                    for j in range(8):
                        kb.mm(sbk, sbk[0:64, j * 64:(j + 1) * 64], kT, kT[hp, (rs + j) * 64:(rs + j + 1) * 64], qT, qv)
                    for t in range(2):
                        kb.mm(sck, sck[:, t * 64:(t + 1) * 64], kT, kT[hp, nl + t * 128:nl + (t + 1) * 128], qT, qv)
                    pf = pwf[it % 2]; pb = pwb[it % 2]; pc = pcb[it % 2]
                    kb.act(pf, pf[:], sbk, sbk[0:64, :], AF.Exp, scale=sc)
                    kb.tt(pb, pb[:], pf, pf[:], EB, EB[:, hh, dr0:dr0 + 8, :].rearrange("p a b -> p (a b)"), ALU.mult)
                    kb.act(pc, pc[:, 0:128], sck, sck[:, 0:128], AF.Exp, scale=sc)
                    for j in range(8):
                        kb.mm(ab, ab[0:64, 0:65], pb, pb[:, j * 64:(j + 1) * 64], v64, v64[:, rs + j, hh, :], start=(j == 0), stop=False)
                    for t in range(2):
                        kb.mm(ab, ab[0:64, 0:65], pc, pc[:, t * 64:(t + 1) * 64], vcx, vcx[:, t, hh, :], start=False, stop=(t == 1))
                    kb.op("dve", lambda e: e.reciprocal(out=rr[0:64, hh:hh + 1], in_=ab[0:64, 64:65]), reads=[ab], writes=[rr])
                    kb.ts(o, o[0:64, hp], ab, ab[0:64, 0:64], rr[0:64, hh:hh + 1], None, ALU.mult, extra=[rr])
                    it += 1
                self.store_T(o, o[0:64, :], 64, 384, r * 64, self.bank[2 + r % 2], stg[r % 2])
            for qt in range(2):
                o = o16[qt]
                for hh in range(2):
                    hp = slice(hh * 64, (hh + 1) * 64)
                    sck = self.bank[6 + it % 2]; ab = self.bank[it % 2]; pc = pcb[it % 2]
                    for t in range(2):
                        kb.mm(sck, sck[:, t * 128:(t + 1) * 128], kT, kT[hp, nl + t * 128:nl + (t + 1) * 128],
                              qT, qT[hp, nl + qt * 128:nl + (qt + 1) * 128])
                    kb.act(pc, pc[:, 0:256], sck, sck[:, 0:256], AF.Exp, scale=sc)
                    for t in range(2):
                        kb.mm(ab, ab[:, 0:65], pc, pc[:, t * 128:(t + 1) * 128], vcx, vcx[:, t, hh, :], start=(t == 0), stop=(t == 1))
                    kb.op("dve", lambda e: e.reciprocal(out=rr[:, hh:hh + 1], in_=ab[:, 64:65]), reads=[ab], writes=[rr])
                    kb.ts(o, o[:, hp], ab, ab[:, 0:64], rr[:, hh:hh + 1], None, ALU.mult, extra=[rr])
                    it += 1
                self.store_T(o, o[:], 128, 384, nl + qt * 128, self.bank[2 + qt], stg[qt])


    def alloc_tail(self):
        kb = self.kb; nt = self.ntok
        if not hasattr(self, "YT_allc"):
            self.YT_allc = CD(kb, 2048, nt, BF16, "YT_all", chunk=1024)
        self.mT_my = CD(kb, 256, nt, BF16, "mT_my"); self.mT_all = CD(kb, 1024, nt, BF16, "mT_all")
        self.preT = kb.dram([2, 128, nt], F32, "preT")
        self.x1T = kb.dram([2, 128, nt], F32, "x1T")

    def phase_merge(self, l, xT):
        kb = self.kb
        wgi = self.inp(f"wgate{l}", [128, 8, 1024]); bpi = self.inp(f"bp{l}", [128, 16, 256])
        woi = self.inp(f"wout{l}", [128, 8, 256]); lnp = self.inp(f"lnp{l}", [128, 2, 4])
        modv = self.modv[l]
        with kb.scope():
            wgt = kb.sb([128, 8, 1024], BF16, "wgt"); bpt = kb.sb([128, 16, 256], BF16, "bpt")
            wot = kb.sb([128, 8, 256], BF16, "wot"); lnt = kb.sb([128, 2, 4], F32, "lnt")
            stg = [kb.sb([128, 1024], F32, "mstg") for _ in range(2)]
            si = 0
            for k in range(8):
                st = stg[si % 2]; si += 1
                kb.dma(st[:], wgi[:, k, :], reads=[wgi], writes=[st])
                kb.copy(wgt, wgt[:, k, :], st, st[:], e=("dve", "pool")[k % 2])
            for q4 in range(4):
                st = stg[si % 2]; si += 1
                kb.dma(st[:].rearrange("p (a b) -> p a b", a=4), bpi[:, 4 * q4:4 * q4 + 4, :], reads=[bpi], writes=[st])
                kb.copy(bpt, bpt[:, 4 * q4:4 * q4 + 4, :].rearrange("p a b -> p (a b)"), st, st[:], e=("dve", "pool")[q4 % 2])
            for k2 in range(2):
                st = stg[si % 2]; si += 1
                kb.dma(st[:].rearrange("p (a b) -> p a b", a=4), woi[:, 4 * k2:4 * k2 + 4, :], reads=[woi], writes=[st])
                kb.copy(wot, wot[:, 4 * k2:4 * k2 + 4, :].rearrange("p a b -> p (a b)"), st, st[:], e=("dve", "pool")[k2 % 2])
            kb.dma(lnt[:], lnp[:], reads=[lnp], writes=[lnt])
            hsb = [kb.sb([128, 8, 512], BF16, "mhs") for _ in range(2)]
            ysb = [kb.sb([128, 16, 512], BF16, "mys") for _ in range(2)]
            sg = kb.sb([128, 512], F32, "msg"); tmp = kb.sb([128, 512], F32, "mtmp"); acc = kb.sb([128, 512], F32, "macc")
            mb = [kb.sb([128, 2, 512], BF16, "mmb") for _ in range(2)]
            it = 0
            for gi, (s, n, isc) in enumerate(self.tgroups()):
                hs = hsb[gi % 2]; ys = ysb[gi % 2]; mo = mb[gi % 2]
                ht, hap = self.hT_all.cols(s, n)
                kb.dma(hs[:, :, :n], hap.rearrange("(k p) n -> p k n", p=128), reads=[ht], writes=[hs])
                yt, yap = self.YT_allc.cols(s, n)
                for hh in range(2):
                    kb.dma(ys[:, 8 * hh:8 * hh + 8, :n], yap[1024 * hh:1024 * (hh + 1), :].rearrange("(q p) n -> p q n", p=128), reads=[yt], writes=[ys])
                for j in range(2):
                    for m in range(4):
                        A = self.bank[(2 * it) % 6]; B = self.bank[(2 * it + 1) % 6]; it += 1
                        cb = m * 256 + j * 128
                        for k in range(8):
                            kb.mm(A, A[:, :n], wgt, wgt[:, k, cb:cb + 128], hs, hs[:, k, :n], start=(k == 0), stop=(k == 7))
                        for g2 in range(4):
                            kb.mm(B, B[:, :n], bpt, bpt[:, g2 * 4 + m, j * 128:(j + 1) * 128], ys, ys[:, g2 * 4 + m, :n], start=(g2 == 0), stop=(g2 == 3))
                        kb.act(sg, sg[:, :n], A, A[:, :n], AF.Sigmoid)
                        if m == 0:
                            kb.tt(acc, acc[:, :n], sg, sg[:, :n], B, B[:, :n], ALU.mult)
                        else:
                            kb.tt(tmp, tmp[:, :n], sg, sg[:, :n], B, B[:, :n], ALU.mult)
                            if m < 3:
                                kb.tt(acc, acc[:, :n], acc, acc[:, :n], tmp, tmp[:, :n], ALU.add, e="pool")
                            else:
                                kb.tt(mo, mo[:, j, :n], acc, acc[:, :n], tmp, tmp[:, :n], ALU.add, e="pool")
                mt, map_ = self.mT_my.cols(s, n)
                kb.dma(map_.rearrange("(j p) n -> p j n", p=128), mo[:, :, :n], reads=[mo], writes=[mt])
            for (c0, cn, tm), (_, _, ta) in zip(self.mT_my.ch, self.mT_all.ch):
                kb.allgather(tm, ta, QUADS)
            pre = [kb.sb([128, 2, 512], F32, "mpre") for _ in range(2)]
            xts = [kb.sb([128, 2, 512], F32, "mxt") for _ in range(2)]
            for gi, (s, n, isc) in enumerate(self.tgroups()):
                ms = hsb[gi % 2]; xt = xts[gi % 2]; pr = pre[gi % 2]
                v = 1 if isc else 0
                mt, map_ = self.mT_all.cols(s, n)
                kb.dma(ms[:, :, :n], map_.rearrange("(k p) n -> p k n", p=128), reads=[mt], writes=[ms])
                kb.dma(xt[:, :, :n], xT[:, :, s:s + n].rearrange("j p n -> p j n"), reads=[xT], writes=[xt])
                for j in range(2):
                    A = self.bank[j]
                    for k in range(8):
                        kb.mm(A, A[:, :n], wot, wot[:, k, j * 128:(j + 1) * 128], ms, ms[:, k, :n], start=(k == 0), stop=(k == 7))
                    kb.act(tmp, tmp[:, :n], A, A[:, :n], AF.Identity, extra=[modv], scale=modv[:, 2 * 2 + j, v:v + 1])
                    kb.stt(pr, pr[:, j, :n], xt, xt[:, j, :n], DN_ALPHA, tmp, tmp[:, :n], ALU.mult, ALU.add)
                self.ln_stats(pr, s, n)
                kb.dma(self.preT[:, :, s:s + n].rearrange("j p n -> p j n"), pr[:, :, :n], reads=[pr], writes=[self.preT])
            self.ln_gather()
            for gi, (s, n, isc) in enumerate(self.tgroups()):
                pr = pre[gi % 2]; xo = xts[gi % 2]
                kb.dma(pr[:, :, :n], self.preT[:, :, s:s + n].rearrange("j p n -> p j n"), reads=[self.preT], writes=[pr])
                p1, p2 = self.ln_coef(s, n)
                for j in range(2):
                    kb.tt(tmp, tmp[:, :n], pr, pr[:, j, :n], p1, p1[:, :n], ALU.mult)
                    kb.tt(tmp, tmp[:, :n], tmp, tmp[:, :n], p2, p2[:, :n], ALU.add)
                    kb.act(xo, xo[:, j, :n], tmp, tmp[:, :n], AF.Identity, extra=[lnt], scale=lnt[:, j, 0:1], bias=lnt[:, j, 1:2])
                kb.dma(self.x1T[:, :, s:s + n].rearrange("j p n -> p j n"), xo[:, :, :n], reads=[xo], writes=[self.x1T])


    def alloc_moe(self):
        kb = self.kb; nt = self.ntok
        self.h2T_my = CD(kb, 256, nt, BF16, "h2T_my"); self.h2T_all = CD(kb, 1024, nt, BF16, "h2T_all")
        self.h2f = kb.dram([2, 128, nt], F32, "h2f")
        self.h32t = kb.sb([128, 2, 512], F32, "h32t")
        self.lg_my = kb.dram([16, nt], F32, "lg_my"); self.lg_all = kb.dram([64, nt], F32, "lg_all")
        self.GTd = kb.dram([16, nt], F32, "GTd")
        self.ypc = [(c0, cn, kb.dram([1024, cn], F32, f"ypc{i}"), kb.dram([256, cn], F32, f"yrc{i}"))
                    for i, (c0, cn) in enumerate(groups_of(nt, 1024))]
        self.x2T = kb.dram([2, 128, nt], F32, "x2T")

    def ypcols(self, s, n):
        for c0, cn, tp, tr in self.ypc:
            if c0 <= s and s + n <= c0 + cn:
                return tp, tr, s - c0
        raise ValueError((s, n))

    def phase_moe(self, l):
        kb = self.kb; nt = self.ntok
        modv = self.modv[l]
        rwi = self.inp("rw", [128, 2, 16]); rbi = self.inp("rb", [1, 16]); cmb = self.inp("c_comb", [64, 16])
        seli = self.inp("c_esel", [16, 4 * 128]); lnp = self.inp(f"lnp{l}", [128, 2, 4]) if f"lnp{l}" not in self.inputs else self.inputs[f"lnp{l}"]
        e1 = self.inp(f"ew1_{l}", [4, 128, 8 * 512]); e3 = self.inp(f"ew3_{l}", [4, 128, 8 * 512]); e2 = self.inp(f"ew2_{l}", [4, 128, 4 * 1024])
        self.phase_A(l, self.x1T, 3, 4, self.h2T_my, self.h2T_all, h32=(self.h2f, self.h32t))
        BIG = 1.0e9
        with kb.scope():
            rw = kb.sb([128, 2, 16], F32, "rw"); rb = kb.sb([128, 16], F32, "rb"); comb = kb.sb([64, 16], F32, "comb")
            esel = kb.sb([16, 4, 128], F32, "esel"); lnt = kb.sb([128, 2, 4], F32, "lnt2")
            kb.dma(rw[:], rwi[:], reads=[rwi], writes=[rw])
            kb.dma(rb[:], bcast_ap(rbi, 16), reads=[rbi], writes=[rb])
            kb.dma(comb[:], cmb[:], reads=[cmb], writes=[comb])
            kb.dma(esel[:].rearrange("p a b -> p (a b)"), seli[:], reads=[seli], writes=[esel])
            kb.dma(lnt[:], lnp[:], reads=[lnp], writes=[lnt])
            hf = [kb.sb([128, 2, 512], F32, "hf") for _ in range(2)]
            lgs = [kb.sb([16, 512], F32, "lgs") for _ in range(2)]
            for gi, (s, n, isc) in enumerate(self.tgroups()):
                h = hf[gi % 2]; lg = lgs[gi % 2]
                kb.dma(h[:, :, :n], self.h2f[:, :, s:s + n].rearrange("j p n -> p j n"), reads=[self.h2f], writes=[h])
                A = self.bank[gi % 2]
                for j in range(2):
                    kb.mm(A, A[0:16, :n], rw, rw[:, j, :], h, h[:, j, :n], start=(j == 0), stop=(j == 1))
                kb.copy(lg, lg[:, :n], A, A[0:16, :n])
                kb.dma(self.lg_my[:, s:s + n], lg[:, :n], reads=[lg], writes=[self.lg_my])
            kb.allgather(self.lg_my, self.lg_all, QUADS)
            lgt = [kb.sb([64, 128], F32, "lgt") for _ in range(2)]
            S = kb.sb([128, 16], F32, "rS"); sel = kb.sb([128, 16], F32, "rsel"); selm = kb.sb([128, 16], F32, "rselm")
            pr = kb.sb([128, 6, 4], F32, "rpr"); gs = kb.sb([128, 4], F32, "rgs"); sc1 = kb.sb([128, 8], F32, "rsc")
            eq = kb.sb([128, 4], F32, "req"); is1 = kb.sb([128, 16], F32, "ris1"); is2 = kb.sb([128, 16], F32, "ris2")
            G = kb.sb([128, 16], F32, "rG"); gtt = [kb.sb([16, 512], F32, "gtt") for _ in range(2)]
            ntile = nt // 128
            for ti in range(ntile):
                t0 = ti * 128
                lt = lgt[ti % 2]
                kb.dma(lt[:], self.lg_all[:, t0:t0 + 128], reads=[self.lg_all], writes=[lt])
                A = self.bank[ti % 2]
                kb.mm(A, A[:, 0:16], lt, lt[:], comb, comb[:])
                kb.act(S, S[:], A, A[:, 0:16], AF.Sigmoid)
                kb.tt(sel, sel[:], S, S[:], rb, rb[:], ALU.add)
                sv = sel[:].rearrange("p (g k) -> p g k", k=4)
                pairs = ((0, 1), (0, 2), (0, 3), (1, 2), (1, 3), (2, 3))
                for pi_, (a, b_) in enumerate(pairs):
                    kb.tt(pr, pr[:, pi_, :], sel, sv[:, :, a], sel, sv[:, :, b_], ALU.add)
                kb.tt(gs, gs[:], pr, pr[:, 0, :], pr, pr[:, 1, :], ALU.max)
                for pi_ in range(2, 6):
                    kb.tt(gs, gs[:], gs, gs[:], pr, pr[:, pi_, :], ALU.max)
                kb.op("dve", lambda e: e.tensor_reduce(out=sc1[:, 0:1], in_=gs[:], axis=AX.X, op=ALU.max), reads=[gs], writes=[sc1])
                kb.ts(eq, eq[:], gs, gs[:], sc1[:, 0:1], None, ALU.is_equal, extra=[sc1])
                kb.ts(eq, eq[:], eq, eq[:], -1.0, BIG, ALU.add, ALU.mult)
                smv = selm[:].rearrange("p (g k) -> p g k", k=4)
                for k in range(4):
                    kb.tt(selm, smv[:, :, k], sel, sv[:, :, k], eq, eq[:], ALU.add)
                kb.op("dve", lambda e: e.tensor_reduce(out=sc1[:, 1:2], in_=selm[:], axis=AX.X, op=ALU.max), reads=[selm], writes=[sc1])
                kb.ts(is1, is1[:], selm, selm[:], sc1[:, 1:2], None, ALU.is_equal, extra=[sc1])
                kb.stt(selm, selm[:], is1, is1[:], -BIG, selm, selm[:], ALU.mult, ALU.add)
                kb.op("dve", lambda e: e.tensor_reduce(out=sc1[:, 2:3], in_=selm[:], axis=AX.X, op=ALU.max), reads=[selm], writes=[sc1])
                kb.ts(is2, is2[:], selm, selm[:], sc1[:, 2:3], None, ALU.is_equal, extra=[sc1])
                kb.tt(is1, is1[:], is1, is1[:], is2, is2[:], ALU.add)
                kb.tt(is1, is1[:], is1, is1[:], S, S[:], ALU.mult)
                kb.op("dve", lambda e: e.tensor_reduce(out=sc1[:, 3:4], in_=is1[:], axis=AX.X, op=ALU.add), reads=[is1], writes=[sc1])
                kb.op("dve", lambda e: e.reciprocal(out=sc1[:, 4:5], in_=sc1[:, 3:4]), reads=[sc1], writes=[sc1])
                kb.ts(G, G[:], is1, is1[:], sc1[:, 4:5], None, ALU.mult, extra=[sc1])
                B = self.bank[2 + ti % 2]
                kb.op("pe", lambda e: e.transpose(B[0:16, 0:128], G[:], self.ident[:]), reads=[G, self.ident], writes=[B])
                gt_ = gtt[(ti // 4) % 2]
                kb.copy(gt_, gt_[:, (ti % 4) * 128:(ti % 4 + 1) * 128], B, B[0:16, 0:128])
                if ti % 4 == 3 or ti == ntile - 1:
                    g0 = (ti // 4) * 512; gn = t0 + 128 - g0
                    kb.dma(self.GTd[:, g0:g0 + gn], gt_[:, :gn], reads=[gt_], writes=[self.GTd])
        with kb.scope():
            w1 = [kb.sb([128, 8, 512], BF16, "w1") for _ in range(4)]
            w3 = [kb.sb([128, 8, 512], BF16, "w3") for _ in range(4)]
            w2 = [kb.sb([128, 4, 1024], BF16, "w2") for _ in range(4)]
            stg = [kb.sb([128, 1024], F32, "estg") for _ in range(2)]
            si = 0
            for i in range(4):
                for (src, dstt) in ((e1, w1[i]), (e3, w3[i]), (e2, w2[i])):
                    dflat = dstt[:].rearrange("p a b -> p (a b)")
                    for hh in range(4):
                        st = stg[si % 2]
                        kb.dma(st[:], src[i, :, hh * 1024:(hh + 1) * 1024], reads=[src], writes=[st])
                        kb.copy(dstt, dflat[:, hh * 1024:(hh + 1) * 1024], st, st[:], e=("dve", "pool")[si % 2])
                        si += 1
            esel = kb.sb([16, 4, 128], F32, "esel2")
            kb.dma(esel[:].rearrange("p a b -> p (a b)"), seli[:], reads=[seli], writes=[esel])
            h2 = [kb.sb([128, 8, 512], BF16, "eh2") for _ in range(2)]
            gt = [kb.sb([16, 512], F32, "egt") for _ in range(2)]
            hp = [kb.sb([128, 4, 512], BF16, "ehp") for _ in range(4)]
            sl = kb.sb([128, 512], F32, "esl"); tq = kb.sb([128, 512], F32, "etq")
            yo = [kb.sb([128, 512], F32, "eyo") for _ in range(2)]
            for gi, (s, n, isc) in enumerate(self.tgroups()):
                h = h2[gi % 2]; g_ = gt[gi % 2]
                ht, hap = self.h2T_all.cols(s, n)
                kb.dma(h[:, :, :n], hap.rearrange("(k p) n -> p k n", p=128), reads=[ht], writes=[h])
                kb.dma(g_[:, :n], self.GTd[:, s:s + n], reads=[self.GTd], writes=[g_])
                for i in range(4):
                    Gb = self.bank[5]
                    kb.mm(Gb, Gb[:, :n], esel, esel[:, i, :], g_, g_[:, :n])
                    for fc in range(4):
                        A = self.bank[(fc % 2) * 2]; B = self.bank[(fc % 2) * 2 + 1]
                        for k in range(8):
                            kb.mm(A, A[:, :n], w1[i], w1[i][:, k, fc * 128:(fc + 1) * 128], h, h[:, k, :n], start=(k == 0), stop=(k == 7))
                        for k in range(8):
                            kb.mm(B, B[:, :n], w3[i], w3[i][:, k, fc * 128:(fc + 1) * 128], h, h[:, k, :n], start=(k == 0), stop=(k == 7))
                        kb.act(sl, sl[:, :n], A, A[:, :n], AF.Silu)
                        kb.tt(tq, tq[:, :n], sl, sl[:, :n], B, B[:, :n], ALU.mult)
                        kb.tt(hp[i], hp[i][:, fc, :n], tq, tq[:, :n], Gb, Gb[:, :n], ALU.mult)
                tp, tr, off = self.ypcols(s, n)
                for c in range(8):
                    Y = self.bank[c % 2]
                    for i in range(4):
                        for fc in range(4):
                            kb.mm(Y, Y[:, :n], w2[i], w2[i][:, fc, c * 128:(c + 1) * 128], hp[i], hp[i][:, fc, :n],
                                  start=(i == 0 and fc == 0), stop=(i == 3 and fc == 3))
                    y_ = yo[c % 2]
                    kb.copy(y_, y_[:, :n], Y, Y[:, :n], e=("dve", "act")[c % 2])
                    kb.dma(tp[c * 128:(c + 1) * 128, off:off + n], y_[:, :n], reads=[y_], writes=[tp])
            for c0, cn, tp, tr in self.ypc:
                kb._deps("pool", [tp], [tr])
                ins = self.nc.gpsimd.collective_compute("ReduceScatter", ALU.add, replica_groups=QUADS,
                                                        ins=[tp.h.ap().opt()], outs=[tr.h.ap().opt()])
                kb.cc_cnt += 1; ins.then_inc(kb.cc); kb._mark((kb.cc, kb.cc_cnt), [tp], [tr])
        with kb.scope():
            lnt = kb.sb([128, 2, 4], F32, "lnt3")
            kb.dma(lnt[:], lnp[:], reads=[lnp], writes=[lnt])
            pre = [kb.sb([128, 2, 512], F32, "epre") for _ in range(2)]
            xts = [kb.sb([128, 2, 512], F32, "ext") for _ in range(2)]
            tmp = kb.sb([128, 512], F32, "etmp")
            for gi, (s, n, isc) in enumerate(self.tgroups()):
                xt = xts[gi % 2]; pr_ = pre[gi % 2]
                v = 1 if isc else 0
                tp, tr, off = self.ypcols(s, n)
                kb.dma(pr_[:, :, :n], tr[:, off:off + n].rearrange("(j p) n -> p j n", p=128), reads=[tr], writes=[pr_])
                kb.dma(xt[:, :, :n], self.x1T[:, :, s:s + n].rearrange("j p n -> p j n"), reads=[self.x1T], writes=[xt])
                for j in range(2):
                    kb.act(tmp, tmp[:, :n], pr_, pr_[:, j, :n], AF.Identity, extra=[modv], scale=modv[:, 2 * 5 + j, v:v + 1])
                    kb.stt(pr_, pr_[:, j, :n], xt, xt[:, j, :n], DN_ALPHA, tmp, tmp[:, :n], ALU.mult, ALU.add)
                self.ln_stats(pr_, s, n)
                kb.dma(self.preT[:, :, s:s + n].rearrange("j p n -> p j n"), pr_[:, :, :n], reads=[pr_], writes=[self.preT])
            self.ln_gather()
            for gi, (s, n, isc) in enumerate(self.tgroups()):
                pr_ = pre[gi % 2]; xo = xts[gi % 2]
                kb.dma(pr_[:, :, :n], self.preT[:, :, s:s + n].rearrange("j p n -> p j n"), reads=[self.preT], writes=[pr_])
                p1, p2 = self.ln_coef(s, n)
                for j in range(2):
                    kb.tt(tmp, tmp[:, :n], pr_, pr_[:, j, :n], p1, p1[:, :n], ALU.mult)
                    kb.tt(tmp, tmp[:, :n], tmp, tmp[:, :n], p2, p2[:, :n], ALU.add)
                    kb.act(xo, xo[:, j, :n], tmp, tmp[:, :n], AF.Identity, extra=[lnt], scale=lnt[:, j, 2:3], bias=lnt[:, j, 3:4])
                kb.dma(self.x2T[:, :, s:s + n].rearrange("j p n -> p j n"), xo[:, :, :n], reads=[xo], writes=[self.x2T])


    def phase_gdn(self, l):
        kb = self.kb; nt = self.ntok; nl = self.nl
        gcv = self.inp(f"gconv{l}", [128, 9]); gpar = self.inp(f"gpar{l}", [1, 8]); gnw = self.inp(f"gnorm{l}", [1, 128])
        cpk = self.inp("c_gdn", [64, 7 * 64])
        bo = self.inp("c_bones", [128, 128])
        GQT = kb.dram([128, nt], F32, "GQT"); GKT = kb.dram([128, nt], F32, "GKT")
        GKM = kb.dram([nt, 128], F32, "GKM"); GVM = kb.dram([nt, 128], F32, "GVM"); GG = kb.dram([nt, 8], F32, "GG")
        OD = [kb.dram([nt, 128], F32, f"OD{d}") for d in range(2)]
        with kb.scope():
            cw = kb.sb([128, 9], F32, "cw"); kb.dma(cw[:], gcv[:], reads=[gcv], writes=[cw])
            bones = kb.sb([128, 128], F32, "bones"); kb.dma(bones[:], bo[:], reads=[bo], writes=[bones])
            par = kb.sb([128, 8], F32, "gpar"); kb.dma(par[:], bcast_ap(gpar, 8), reads=[gpar], writes=[par])
            kb.act(par, par[:, 0:4], par, par[:, 0:4], AF.Exp)
            kb.ts(par, par[:, 0:4], par, par[:, 0:4], -1.0, None, ALU.mult)
            xin = [kb.sb([128, 514], F32, "gx") for _ in range(2)]
            u = kb.sb([128, 512], F32, "gu"); sq = kb.sb([128, 512], F32, "gsq"); rs = kb.sb([128, 512], F32, "grs")
            uo = [kb.sb([128, 512], F32, "guo") for _ in range(2)]
            uo16 = [kb.sb([128, 512], BF16, "guo16") for _ in range(2)]
            tmo = [kb.sb([128, 128], F32, "gtm") for _ in range(2)]
            gab = kb.sb([128, 8], F32, "gab"); gt = kb.sb([128, 8], F32, "ggt")
            it = 0
            for gi, (s, n, isc) in enumerate(self.tgroups()):
                seg0, seg1 = (nl, nt) if isc else (0, nl)
                for part in range(3):
                    x = xin[it % 2]; it += 1
                    lo = 1 if s == seg0 else 0; hi = 1 if s + n == seg1 else 0
                    if lo:
                        kb.memset(x, x[:, 0:1], 0.0)
                    if hi:
                        kb.memset(x, x[:, n + 1:n + 2], 0.0)
                    kb.dma(x[:, lo:n + 2 - hi], self.GP[part, :, s - 1 + lo:s + n + 1 - hi], reads=[self.GP], writes=[x])
                    kb.ts(u, u[:, :n], x, x[:, 0:n], cw[:, 3 * part:3 * part + 1], None, ALU.mult, extra=[cw])
                    kb.stt(u, u[:, :n], x, x[:, 1:n + 1], cw[:, 3 * part + 1:3 * part + 2], u, u[:, :n], ALU.mult, ALU.add, extra=[cw])
                    kb.stt(u, u[:, :n], x, x[:, 2:n + 2], cw[:, 3 * part + 2:3 * part + 3], u, u[:, :n], ALU.mult, ALU.add, extra=[cw])
                    o = uo[part % 2]
                    if part < 2:
                        kb.act(u, u[:, :n], u, u[:, :n], AF.Silu)
                        kb.tt(sq, sq[:, :n], u, u[:, :n], u, u[:, :n], ALU.mult)
                        A = self.bank[part]
                        kb.mm(A, A[:, :n], bones, bones[:], sq, sq[:, :n])
                        kb.ts(rs, rs[:, :n], A, A[:, :n], RMS_EPS, None, ALU.add)
                        kb.act(rs, rs[:, :n], rs, rs[:, :n], AF.Sqrt)
                        kb.op("dve", lambda e: e.reciprocal(out=rs[:, :n], in_=rs[:, :n]), reads=[rs], writes=[rs])
                        kb.stt(o, o[:, :n], u, u[:, :n], 0.125 if part == 0 else 1.0, rs, rs[:, :n], ALU.mult, ALU.mult)
                        dst = GQT if part == 0 else GKT
                        kb.dma(dst[:, s:s + n], o[:, :n], reads=[o], writes=[dst])
                    else:
                        kb.act(o, o[:, :n], u, u[:, :n], AF.Silu)
                    if part >= 1:
                        dstm = GKM if part == 1 else GVM
                        for tt_ in range(n // 128):
                            B = self.bank[2 + tt_ % 2]
                            kb.op("pe", lambda e: e.transpose(B[:, 0:128], o[:, tt_ * 128:(tt_ + 1) * 128], self.ident[:]), reads=[o, self.ident], writes=[B])
                            tm = tmo[tt_ % 2]
                            kb.copy(tm, tm[:], B, B[:, 0:128])
                            kb.dma(dstm[s + tt_ * 128:s + (tt_ + 1) * 128, :], tm[:], reads=[tm], writes=[dstm])
                for tt_ in range(n // 128):
                    t0 = s + tt_ * 128
                    kb.dma(gab[:], self.GAB[t0:t0 + 128, :], reads=[self.GAB], writes=[gab])
                    kb.tt(gt, gt[:, 0:4], gab, gab[:, 0:4], par, par[:, 4:8], ALU.add)
                    kb.act(gt, gt[:, 0:4], gt, gt[:, 0:4], AF.Exp)
                    kb.ts(gt, gt[:, 0:4], gt, gt[:, 0:4], 1.0, None, ALU.add)
                    kb.act(gt, gt[:, 0:4], gt, gt[:, 0:4], AF.Ln)
                    kb.tt(gt, gt[:, 0:4], gt, gt[:, 0:4], par, par[:, 0:4], ALU.mult)
                    kb.act(gt, gt[:, 4:8], gab, gab[:, 4:8], AF.Sigmoid)
                    kb.dma(GG[t0:t0 + 128, :], gt[:], reads=[gt], writes=[GG])
        with kb.scope():
            cp = kb.sb([64, 7, 64], F32, "cp"); kb.dma(cp[:].rearrange("p a b -> p (a b)"), cpk[:], reads=[cpk], writes=[cp])
            MU, ML, I64, ONES, MUS, MLS = (cp[:, i, :] for i in range(6))
            I4 = kb.sb([64, 4, 64], F32, "I4")
            for u_ in range(4):
                kb.copy(I4, I4[:, u_, :], cp, I64)
            S = kb.sb([64, 4, 64], F32, "gS"); kb.memset(S, S[:], 0.0)
            R2 = nl // 64
            seq0 = [nl + 64 * j for j in range(4)] + [64 * j for j in range(R2)]
            seq1 = [nl + 64 * j for j in range(3, -1, -1)] + [64 * j for j in range(R2 - 1, -1, -1)]
            NU = 8
            units = [(p_, d, h) for p_ in range(2) for d in range(2) for h in range(2)]

            def T8(name, dt=F32):
                return kb.sb([64, NU, 64], dt, name)
            I8 = T8("I8")
            for u_ in range(NU):
                kb.copy(I8, I8[:, u_, :], cp, I64)
            qT = [[kb.sb([64, 64], F32, "cq") for _ in range(NU)] for _ in range(2)]
            kT = [[kb.sb([64, 64], F32, "ck") for _ in range(NU)] for _ in range(2)]
            kM = [[kb.sb([64, 128], F32, "ckm") for _ in range(4)] for _ in range(2)]
            vM = [[kb.sb([64, 128], F32, "cvm") for _ in range(4)] for _ in range(2)]
            gg = [[kb.sb([64, 8], F32, "cgg") for _ in range(4)] for _ in range(2)]
            Gbc = T8("Gbc"); Bbc = T8("Bbc"); gcc = kb.sb([64, NU], F32, "gcc"); gcr = T8("gcr"); nD = T8("nD")
            e1 = T8("e1"); e2 = T8("e2"); dec = T8("dec"); decT = T8("decT"); brow = T8("brow")
            X = [T8("X0"), T8("X1")]; XT = [T8("XT0"), T8("XT1")]; TT = T8("TT"); inT = T8("inT")
            kbg = T8("kbg"); vb = T8("vb"); kdec = T8("kdec"); wT = T8("wT"); uval = T8("uval"); qdT = T8("qdT")
            egr = T8("egr"); sc4 = kb.sb([64, 4 * NU], F32, "sc4"); vnew = T8("vnew"); osb = [T8("osb0"), T8("osb1")]
            nsteps = len(seq0) // 2
            for step in range(nsteps):
                pp = step % 2
                toks = {(p_, d): (seq0, seq1)[d][2 * step + p_] for p_ in range(2) for d in range(2)}
                for p_ in range(2):
                    for d in range(2):
                        t0 = toks[(p_, d)]; pd = p_ * 2 + d
                        for h in range(2):
                            u_ = p_ * 4 + d * 2 + h
                            kb.dma(qT[pp][u_][:], GQT[64 * h:64 * h + 64, t0:t0 + 64], reads=[GQT], writes=[qT[pp][u_]])
                            kb.dma(kT[pp][u_][:], GKT[64 * h:64 * h + 64, t0:t0 + 64], reads=[GKT], writes=[kT[pp][u_]])
                        kb.dma(kM[pp][pd][:], GKM[t0:t0 + 64, :], reads=[GKM], writes=[kM[pp][pd]])
                        kb.dma(vM[pp][pd][:], GVM[t0:t0 + 64, :], reads=[GVM], writes=[vM[pp][pd]])
                        kb.dma(gg[pp][pd][:], GG[t0:t0 + 64, :], reads=[GG], writes=[gg[pp][pd]])
                b0, b1, b2, b3 = self.bank[0], self.bank[1], self.bank[2], self.bank[3]

                def b4(bk):
                    return bk[0:64, 0:512].rearrange("p (a b) -> p a b", a=NU)

                def gcol(u_, off=0):
                    p_, d, h = units[u_]
                    g_ = gg[pp][p_ * 2 + d]
                    c = off + d * 2 + h
                    return g_, g_[:, c:c + 1]
                for u_, (p_, d, h) in enumerate(units):
                    g_, ga = gcol(u_); _, ba = gcol(u_, 4)
                    kb.ts(Gbc, Gbc[:, u_, :], cp, ONES, ga, None, ALU.mult, extra=[g_])
                    kb.ts(Bbc, Bbc[:, u_, :], cp, ONES, ba, None, ALU.mult, extra=[g_])
                for u_, (p_, d, h) in enumerate(units):
                    Ud = MU if d == 0 else ML
                    g_, ga = gcol(u_)
                    kb.mm(b0, b0[0:64, u_:u_ + 1], cp, Ud, g_, ga)
                    kb.mm(b1, b4(b1)[:, u_, :], Gbc, Gbc[:, u_, :], cp, Ud)
                    kb.mm(b2, b4(b2)[:, u_, :], Bbc, Bbc[:, u_, :], cp, I64)
                kb.copy(gcc, gcc[:], b0, b0[0:64, 0:NU])
                kb.copy(gcr, gcr[:], b1, b4(b1))
                kb.copy(brow, brow[:], b2, b4(b2), e="act")
                for u_ in range(NU):
                    kb.ts(nD, nD[:, u_, :], gcr, gcr[:, u_, :], gcc[:, u_:u_ + 1], None, ALU.subtract, extra=[gcc])
                kb.ts(e1, e1[:], nD, nD[:], -1.0, 0.0, ALU.mult, ALU.min)
                kb.act(e1, e1[:], e1, e1[:], AF.Exp)
                kb.ts(e2, e2[:], nD, nD[:], 0.0, None, ALU.min)
                kb.act(e2, e2[:], e2, e2[:], AF.Exp)
                kb.act(egr, egr[:], gcr, gcr[:], AF.Exp)
                for u_ in range(NU):
                    kb.mm(b0, b4(b0)[:, u_, :], kT[pp][u_], kT[pp][u_][:], kT[pp][u_], kT[pp][u_][:])
                    kb.mm(b3, b4(b3)[:, u_, :], kT[pp][u_], kT[pp][u_][:], qT[pp][u_], qT[pp][u_][:])
                for u_, (p_, d, h) in enumerate(units):
                    Ms, MsT, MT = (MLS, MUS, MU) if d == 0 else (MUS, MLS, ML)
                    kb.tt(dec, dec[:, u_, :], e1, e1[:, u_, :], cp, Ms, ALU.mult)
                    kb.tt(decT, decT[:, u_, :], e2, e2[:, u_, :], cp, MsT, ALU.mult)
                    kb.tt(e2, e2[:, u_, :], e2, e2[:, u_, :], cp, MT, ALU.mult)
                kb.tt(X[0], X[0][:], b0, b4(b0), dec, dec[:], ALU.mult)
                kb.tt(XT[0], XT[0][:], b0, b4(b0), decT, decT[:], ALU.mult)
                kb.tt(XT[0], XT[0][:], XT[0], XT[0][:], brow, brow[:], ALU.mult)
                for u_ in range(NU):
                    g_, ba = gcol(u_, 4)
                    kb.ts(X[0], X[0][:, u_, :], X[0], X[0][:, u_, :], ba, None, ALU.mult, extra=[g_])
                kb.tt(inT, inT[:], b3, b4(b3), e2, e2[:], ALU.mult)
                kb.tt(TT, TT[:], I8, I8[:], XT[0], XT[0][:], ALU.subtract)
                cur = 0
                for lev in range(5):
                    nx = 1 - cur
                    for u_ in range(NU):
                        kb.mm(b0, b4(b0)[:, u_, :], XT[cur], XT[cur][:, u_, :], X[cur], X[cur][:, u_, :])
                        kb.mm(b1, b4(b1)[:, u_, :], X[cur], X[cur][:, u_, :], XT[cur], XT[cur][:, u_, :])
                    kb.copy(X[nx], X[nx][:], b0, b4(b0))
                    kb.copy(XT[nx], XT[nx][:], b1, b4(b1), e="act")
                    for u_ in range(NU):
                        kb.mm(b2, b4(b2)[:, u_, :], X[nx], X[nx][:, u_, :], TT, TT[:, u_, :])
                    kb.tt(TT, TT[:], TT, TT[:], b2, b4(b2), ALU.add)
                    cur = nx
                kb.act(sc4, sc4[:, 0:NU], gcc, gcc[:], AF.Exp)
                for u_, (p_, d, h) in enumerate(units):
                    g_, ba = gcol(u_, 4)
                    last = 63 if d == 0 else 0
                    kb.tt(sc4, sc4[:, NU + u_:NU + u_ + 1], sc4, sc4[:, u_:u_ + 1], g_, ba, ALU.mult)
                    kb.tt(sc4, sc4[:, 2 * NU + u_:2 * NU + u_ + 1], gcr, gcr[:, u_, last:last + 1], gcc, gcc[:, u_:u_ + 1], ALU.subtract)
                kb.act(sc4, sc4[:, 2 * NU:3 * NU], sc4, sc4[:, 2 * NU:3 * NU], AF.Exp)
                for u_, (p_, d, h) in enumerate(units):
                    g_, ba = gcol(u_, 4)
                    pd = p_ * 2 + d
                    hs_ = slice(64 * h, 64 * h + 64)
                    kb.ts(kbg, kbg[:, u_, :], kM[pp][pd], kM[pp][pd][:, hs_], sc4[:, NU + u_:NU + u_ + 1], None, ALU.mult, extra=[sc4])
                    kb.ts(kdec, kdec[:, u_, :], kM[pp][pd], kM[pp][pd][:, hs_], sc4[:, 2 * NU + u_:2 * NU + u_ + 1], None, ALU.mult, extra=[sc4])
                    kb.ts(vb, vb[:, u_, :], vM[pp][pd], vM[pp][pd][:, hs_], ba, None, ALU.mult, extra=[g_])
                    kb.tt(qdT, qdT[:, u_, :], qT[pp][u_], qT[pp][u_][:], egr, egr[:, u_, :], ALU.mult)
                for u_ in range(NU):
                    kb.mm(b0, b4(b0)[:, u_, :], kbg, kbg[:, u_, :], TT, TT[:, u_, :])
                    kb.mm(b1, b4(b1)[:, u_, :], TT, TT[:, u_, :], vb, vb[:, u_, :])
                kb.copy(wT, wT[:], b0, b4(b0))
                kb.copy(uval, uval[:], b1, b4(b1), e="act")
                ob_ = osb[pp]
                for p_ in range(2):
                    us = list(range(p_ * 4, p_ * 4 + 4))
                    sl_ = slice(p_ * 4, p_ * 4 + 4)
                    for u_ in us:
                        kb.mm(b2, b4(b2)[:, u_, :], wT, wT[:, u_, :], S, S[:, u_ % 4, :])
                    kb.tt(vnew, vnew[:, sl_, :], uval, uval[:, sl_, :], b2, b4(b2)[:, sl_, :], ALU.subtract)
                    for u_ in us:
                        kb.mm(b3, b4(b3)[:, u_, :], qdT, qdT[:, u_, :], S, S[:, u_ % 4, :], start=True, stop=False)
                        kb.mm(b3, b4(b3)[:, u_, :], inT, inT[:, u_, :], vnew, vnew[:, u_, :], start=False, stop=True)
                        kb.mm(b0, b4(b0)[:, u_, :], kdec, kdec[:, u_, :], vnew, vnew[:, u_, :])
                    kb.copy(ob_, ob_[:, sl_, :], b3, b4(b3)[:, sl_, :], e="act")
                    for u_ in us:
                        _, d, h = units[u_]
                        last = 63 if d == 0 else 0
                        kb.stt(S, S[:, u_ % 4, :], S, S[:, u_ % 4, :], egr[:, u_, last:last + 1], b0, b4(b0)[:, u_, :], ALU.mult, ALU.add, extra=[egr])
                    for d in range(2):
                        t0 = toks[(p_, d)]
                        kb.dma(OD[d][t0:t0 + 64, :].rearrange("p (h v) -> p h v", h=2), ob_[:, p_ * 4 + 2 * d:p_ * 4 + 2 * d + 2, :], reads=[ob_], writes=[OD[d]])
        with kb.scope():
            gnb = kb.sb([128, 128], F32, "gnb"); kb.dma(gnb[:], bcast_ap(gnw, 128), reads=[gnw], writes=[gnb])
            o0 = [kb.sb([128, 128], F32, "po0") for _ in range(2)]; o1 = [kb.sb([128, 128], F32, "po1") for _ in range(2)]
            zt = [kb.sb([128, 128], F32, "pz") for _ in range(2)]; sq = kb.sb([128, 128], F32, "psq"); ss = kb.sb([128, 4], F32, "pss")
            y16 = [kb.sb([128, 128], BF16, "py") for _ in range(2)]; stg = [kb.sb([128, 128], BF16, "pstg") for _ in range(2)]
            for ti in range(nt // 128):
                t0 = ti * 128; a = o0[ti % 2]; b_ = o1[ti % 2]; z = zt[ti % 2]
                kb.dma(a[:], OD[0][t0:t0 + 128, :], reads=[OD[0]], writes=[a])
                kb.dma(b_[:], OD[1][t0:t0 + 128, :], reads=[OD[1]], writes=[b_])
                kb.dma(z[:], self.GZ[t0:t0 + 128, :], reads=[self.GZ], writes=[z])
                kb.tt(a, a[:], a, a[:], b_, b_[:], ALU.add)
                kb.tt(sq, sq[:], a, a[:], a, a[:], ALU.mult)
                kb.op("dve", lambda e: e.tensor_reduce(out=ss[:, 0:2], in_=sq[:].rearrange("p (h v) -> p h v", h=2), axis=AX.X, op=ALU.add), reads=[sq], writes=[ss])
                kb.ts(ss, ss[:, 0:2], ss, ss[:, 0:2], 1.0 / 64, RMS_EPS, ALU.mult, ALU.add)
                kb.act(ss, ss[:, 0:2], ss, ss[:, 0:2], AF.Sqrt)
                kb.op("dve", lambda e: e.reciprocal(out=ss[:, 2:4], in_=ss[:, 0:2]), reads=[ss], writes=[ss])
                kb.act(z, z[:], z, z[:], AF.Silu)
                for h in range(2):
                    hs_ = slice(64 * h, 64 * h + 64)
                    kb.stt(a, a[:, hs_], a, a[:, hs_], ss[:, 2 + h:3 + h], gnb, gnb[:, hs_], ALU.mult, ALU.mult, extra=[ss])
                y = y16[ti % 2]
                kb.tt(y, y[:], a, a[:], z, z[:], ALU.mult)
                self.store_T(y, y[:], 128, 128, t0, self.bank[4 + ti % 2], stg[ti % 2])


    def phase_hyena(self, l):
        kb = self.kb; nt = self.ntok; nl = self.nl
        hcv = self.inp(f"hconv{l}", [128, 9]); hw1 = self.inp(f"hw1_{l}", [33, 64]); hw2 = self.inp(f"hw2_{l}", [64, 64])
        hw3 = self.inp(f"hw3_{l}", [64, 4 * 128]); hpr = self.inp(f"hpar{l}", [64, 3]); hdl = self.inp(f"hdel{l}", [128, 6])
        jmi = self.inp("c_jm", [128, 128])
        segs = [(0, nl), (nl, NC)]
        consts = {L: (self.inp(f"c_hyF{L}", [33, 2 * L]), self.inp(f"c_hyT{L}", [1, 2 * L])) for _, L in segs}
        HV = kb.dram([3, 128, nt], F32, "HV")
        TA = {L: [kb.dram([128, 2 * L + 254], BF16, f"TA{L}_{o}") for o in range(2)] for _, L in segs}
        TWO_PI = 2.0 * math.pi
        with kb.scope():
            cw = kb.sb([128, 9], F32, "hcw"); kb.dma(cw[:], hcv[:], reads=[hcv], writes=[cw])
            xin = [kb.sb([128, 514], F32, "hx") for _ in range(2)]
            uo = [kb.sb([128, 512], F32, "hu") for _ in range(2)]
            it = 0
            for gi, (s, n, isc) in enumerate(self.tgroups()):
                seg0, seg1 = (nl, nt) if isc else (0, nl)
                for part in range(3):
                    x = xin[it % 2]; u = uo[it % 2]; it += 1
                    lo = 1 if s == seg0 else 0; hi = 1 if s + n == seg1 else 0
                    if lo:
                        kb.memset(x, x[:, 0:1], 0.0)
                    if hi:
                        kb.memset(x, x[:, n + 1:n + 2], 0.0)
                    kb.dma(x[:, lo:n + 2 - hi], self.HY[part, :, s - 1 + lo:s + n + 1 - hi], reads=[self.HY], writes=[x])
                    kb.ts(u, u[:, :n], x, x[:, 0:n], cw[:, 3 * part:3 * part + 1], None, ALU.mult, extra=[cw])
                    kb.stt(u, u[:, :n], x, x[:, 1:n + 1], cw[:, 3 * part + 1:3 * part + 2], u, u[:, :n], ALU.mult, ALU.add, extra=[cw])
                    kb.stt(u, u[:, :n], x, x[:, 2:n + 2], cw[:, 3 * part + 2:3 * part + 3], u, u[:, :n], ALU.mult, ALU.add, extra=[cw])
                    kb.dma(HV[part, :, s:s + n], u[:, :n], reads=[u], writes=[HV])
            w1 = kb.sb([33, 64], F32, "hw1"); w2 = kb.sb([64, 64], F32, "hw2"); w3 = kb.sb([64, 4, 128], F32, "hw3")
            pr = kb.sb([64, 8], F32, "hpr"); dl = kb.sb([128, 6], F32, "hdl")
            kb.dma(w1[:], hw1[:], reads=[hw1], writes=[w1]); kb.dma(w2[:], hw2[:], reads=[hw2], writes=[w2])
            kb.dma(w3[:].rearrange("p a b -> p (a b)"), hw3[:], reads=[hw3], writes=[w3])
            kb.dma(pr[:, 0:3], hpr[:], reads=[hpr], writes=[pr]); kb.dma(dl[:], hdl[:], reads=[hdl], writes=[dl])
            for i_ in range(2):
                kb.tt(pr, pr[:, 3 + i_:4 + i_], pr, pr[:, 0:1], pr, pr[:, 1 + i_:2 + i_], ALU.mult)
            kb.act(dl, dl[:, 0:4], dl, dl[:, 0:4], AF.Abs)
            kb.ts(dl, dl[:, 0:4], dl, dl[:, 0:4], -1.0, None, ALU.mult)
            nacc = kb.sb([128, 8], F32, "hnacc"); kb.memset(nacc, nacc[:], 0.0)
            ft = [kb.sb([33, 512], F32, "hft") for _ in range(2)]; tl = [kb.sb([128, 512], F32, "htl") for _ in range(2)]
            a1 = kb.sb([64, 512], F32, "ha1"); a2 = kb.sb([64, 512], F32, "ha2"); kk = kb.sb([64, 512], F32, "hkk")
            win = kb.sb([128, 512], F32, "hwin"); hh = kb.sb([128, 512], F32, "hhh"); hab = kb.sb([128, 512], F32, "hab")
            hbf = [kb.sb([128, 512], BF16, "hbf") for _ in range(2)]; red = kb.sb([128, 2], F32, "hred")
            zpad = kb.sb([128, 128], BF16, "hzp"); kb.memset(zpad, zpad[:], 0.0)
            for si_, (_, L) in enumerate(segs):
                Fc, Tc = consts[L]
                for o in range(2):
                    kb.dma(TA[L][o][:, 0:127], zpad[:, 0:127], reads=[zpad], writes=[TA[L][o]])
                    kb.dma(TA[L][o][:, 2 * L + 126:2 * L + 254], zpad[:, 0:128], reads=[zpad], writes=[TA[L][o]])
                gidx = 0
                for dd in (1, 0):
                    for (j0, n) in groups_of(L, 512):
                        f = ft[gidx % 2]; t_ = tl[gidx % 2]; gidx += 1
                        kb.dma(f[:, :n], Fc[:, dd * L + j0:dd * L + j0 + n], reads=[Fc], writes=[f])
                        kb.dma(t_[:, :n], bcast_ap(Tc, n, offset=dd * L + j0), reads=[Tc], writes=[t_])
                        A = self.bank[0]; B = self.bank[1]
                        kb.mm(A, A[0:64, :n], w1, w1[:], f, f[:, :n])
                        kb.act(a1, a1[:, :n], A, A[0:64, :n], AF.Identity, extra=[pr], scale=pr[:, 0:1], bias=pr[:, 3:4])
                        kb.ts(kk, kk[:, :n], a1, a1[:, :n], 1.0 / TWO_PI, 12582912.0, ALU.mult, ALU.add)
                        kb.ts(kk, kk[:, :n], kk, kk[:, :n], -12582912.0, None, ALU.add)
                        kb.stt(a1, a1[:, :n], kk, kk[:, :n], -TWO_PI, a1, a1[:, :n], ALU.mult, ALU.add)
                        kb.act(a1, a1[:, :n], a1, a1[:, :n], AF.Sin)
                        kb.mm(B, B[0:64, :n], w2, w2[:], a1, a1[:, :n])
                        kb.act(a2, a2[:, :n], B, B[0:64, :n], AF.Identity, extra=[pr], scale=pr[:, 0:1], bias=pr[:, 4:5])
                        kb.ts(kk, kk[:, :n], a2, a2[:, :n], 1.0 / TWO_PI, 12582912.0, ALU.mult, ALU.add)
                        kb.ts(kk, kk[:, :n], kk, kk[:, :n], -12582912.0, None, ALU.add)
                        kb.stt(a2, a2[:, :n], kk, kk[:, :n], -TWO_PI, a2, a2[:, :n], ALU.mult, ALU.add)
                        kb.act(a2, a2[:, :n], a2, a2[:, :n], AF.Sin)
                        for o in range(2):
                            C = self.bank[2 + o]
                            kb.mm(C, C[:, :n], w3, w3[:, dd * 2 + o, :], a2, a2[:, :n])
                            kb.act(win, win[:, :n], t_, t_[:, :n], AF.Exp, extra=[dl], scale=dl[:, dd * 2 + o:dd * 2 + o + 1])
                            kb.stt(hh, hh[:, :n], win, win[:, :n], 0.05, C, C[:, :n], ALU.add, ALU.mult)
                            if dd == 1 and j0 + n == L:
                                kb.memset(hh, hh[:, n - 1:n], 0.0, e="dve")
                            kb.act(hab, hab[:, :n], hh, hh[:, :n], AF.Abs)
                            kb.op("dve", lambda e: e.tensor_reduce(out=red[:, 0:1], in_=hab[:, :n], axis=AX.X, op=ALU.add), reads=[hab], writes=[red])
                            kb.tt(nacc, nacc[:, 2 * si_ + o:2 * si_ + o + 1], nacc, nacc[:, 2 * si_ + o:2 * si_ + o + 1], red, red[:, 0:1], ALU.add)
                            hb_ = hbf[o]
                            kb.copy(hb_, hb_[:, :n], hh, hh[:, :n], e="pool")
                            base = (127 if dd == 1 else L + 126) + j0
                            nn = n - 1 if (dd == 1 and j0 + n == L) else n
                            kb.dma(TA[L][o][:, base:base + nn], hb_[:, :nn], reads=[hb_], writes=[TA[L][o]])
            kb.op("dve", lambda e: e.reciprocal(out=nacc[:, 4:8], in_=nacc[:, 0:4]), reads=[nacc], writes=[nacc])
            nrm = kb.sb([128, 4], F32, "hnrm_keep") if False else None
            self.h_nrm = kb.dram([128, 4], F32, "h_nrm"); self.h_sk = kb.dram([128, 2], F32, "h_sk")
            kb.dma(self.h_nrm[:], nacc[:, 4:8], reads=[nacc], writes=[self.h_nrm])
            kb.dma(self.h_sk[:], dl[:, 4:6], reads=[dl], writes=[self.h_sk])
        ZC = kb.dram([128, nt], F32, "HZC")
        for si_, (s0, L) in enumerate(segs):
            nb = L // 128; pad = nb - 1; W = (2 * nb - 1) * 128
            with kb.scope():
                jm = kb.sb([128, 128], BF16, "jm"); jmf = kb.sb([128, 128], F32, "jmf")
                kb.dma(jmf[:], jmi[:], reads=[jmi], writes=[jmf]); kb.copy(jm, jm[:], jmf, jmf[:])
                nr = kb.sb([128, 4], F32, "hnr"); sk = kb.sb([128, 2], F32, "hsk")
                kb.dma(nr[:], self.h_nrm[:], reads=[self.h_nrm], writes=[nr]); kb.dma(sk[:], self.h_sk[:], reads=[self.h_sk], writes=[sk])
                zp = kb.sb([128, nb + 2 * pad, 128], BF16, "zp")
                if pad:
                    kb.memset(zp, zp[:, 0:pad, :], 0.0); kb.memset(zp, zp[:, pad + nb:, :], 0.0)
                yT = kb.sb([128, nb, 128], F32, "yT")
                HP = min(2 * nb - 1, 32)
                Hs = [kb.sb([128, HP * 128], BF16, "Hs") for _ in range(4)]
                hi_ = 0
                zf = [kb.sb([128, 128], F32, "zf") for _ in range(2)]; zb = [kb.sb([128, 128], BF16, "zb") for _ in range(2)]
                zt = [kb.sb([128, 128], BF16, "zt") for _ in range(2)]
                g_ = [kb.sb([128, 128], F32, "hg") for _ in range(2)]; yo = [kb.sb([128, 128], F32, "hyo") for _ in range(2)]
                y16 = [kb.sb([128, 128], BF16, "hy16") for _ in range(2)]
                for o in range(2):
                    src = HV if o == 0 else ZC
                    for J in range(nb):
                        a = zf[J % 2]; b_ = zb[J % 2]; c_ = zt[J % 2]
                        if o == 0:
                            kb.dma(a[:], HV[0, :, s0 + J * 128:s0 + (J + 1) * 128], reads=[HV], writes=[a])
                        else:
                            kb.dma(a[:], ZC[:, s0 + J * 128:s0 + (J + 1) * 128], reads=[ZC], writes=[a])
                        kb.copy(b_, b_[:], a, a[:])
                        P1 = self.bank[J % 2]
                        pv = P1[:].bitcast(BF16)
                        kb.op("pe", lambda e: e.transpose(pv[:, 0:128], b_[:], self.identb[:]), reads=[b_, self.identb], writes=[P1])
                        kb.copy(c_, c_[:], P1, pv[:, 0:128], e="act")
                        P2 = self.bank[2 + J % 2]
                        kb.mm(P2, P2[:, 0:128], jm, jm[:], c_, c_[:])
                        kb.copy(zp, zp[:, pad + J, :], P2, P2[:, 0:128])
                    for c in range(128):
                        Y = self.bank[4 + c % 4]
                        for p0, pn in groups_of(2 * nb - 1, HP):
                            H = Hs[hi_ % 4]; hi_ += 1
                            hsrc = bass.AP(tensor=TA[L][o].h, offset=c * (2 * L + 254) + 127 + p0 * 128, ap=[[1, 128], [1, pn * 128]])
                            kb.dma(H[:, 0:pn * 128], hsrc, reads=[TA[L][o]], writes=[H], q="sp")
                            for dq in range(pn):
                                dp = p0 + dq
                                kb.mm(Y, Y[:, 0:nb], H, H[:, dq * 128:(dq + 1) * 128], zp, zp[:, 2 * pad - dp:2 * pad - dp + nb, c],
                                      start=(dp == 0), stop=(dp == 2 * nb - 2))
                        kb.copy(yT, yT[:, :, c], Y, Y[:, 0:nb], e=("dve", "act")[c % 2])
                    for I in range(nb):
                        P1 = self.bank[I % 2]
                        kb.op("pe", lambda e: e.transpose(P1[:, 0:128], yT[:, I, :], self.ident[:]), reads=[yT, self.ident], writes=[P1])
                        a = zf[I % 2]; gt = g_[I % 2]; y_ = yo[I % 2]
                        c0 = s0 + I * 128
                        if o == 0:
                            kb.dma(a[:], HV[0, :, c0:c0 + 128], reads=[HV], writes=[a])
                        else:
                            kb.dma(a[:], ZC[:, c0:c0 + 128], reads=[ZC], writes=[a])
                        kb.dma(gt[:], HV[1 + o, :, c0:c0 + 128], reads=[HV], writes=[gt])
                        kb.ts(y_, y_[:], P1, P1[:, 0:128], nr[:, 2 * si_ + o:2 * si_ + o + 1], None, ALU.mult, extra=[nr])
                        kb.stt(y_, y_[:], a, a[:], sk[:, o:o + 1], y_, y_[:], ALU.mult, ALU.add, extra=[sk])
                        if o == 0:
                            kb.tt(y_, y_[:], y_, y_[:], gt, gt[:], ALU.mult)
                            kb.dma(ZC[:, c0:c0 + 128], y_[:], reads=[y_], writes=[ZC])
                        else:
                            yb = y16[I % 2]
                            kb.tt(yb, yb[:], y_, y_[:], gt, gt[:], ALU.mult)
                            yt, yap = self.YT_my.cols(c0, 128)
                            kb.dma(yap[0:128, :], yb[:], reads=[yb], writes=[yt])


def build_full(depth=DEPTH, nl=NL):
    P = Prog(nl, depth, ())
    kb = P.kb
    P.setup_consts()
    P.setup_mod()
    P.ln_alloc()
    P.alloc_A()
    P.alloc_mix()
    P.alloc_tail()
    P.alloc_moe()
    xT = P.inp("xT", [2, 128, P.ntok])
    x_in = xT
    for l in range(depth):
        lam_init = 0.8 - 0.6 * math.exp(-0.3 * l)
        P.phase_A(l, x_in, 0, 1, P.hT_my, P.hT_all)
        P.phase_B(l)
        P.phase_hyena(l)
        P.phase_gdn(l)
        P.phase_diff(l, lam_init)
        P.phase_na(l)
        for (c0, cn, tm), (_, _, ta) in zip(P.YT_my.ch, P.YT_allc.ch):
            kb.allgather(tm, ta, QUADS)
        P.phase_merge(l, x_in)
        P.phase_moe(l)
        x_in = P.x2T
    o = P.out("o_x", [2, 128, nl])
    for j in range(2):
        kb.dma(o[j, :, :], P.x2T[j, :, 0:nl], reads=[P.x2T], writes=[o])
    kb.finish([o])
    return P


def core_bg(r): return r // 4, r % 4
def prep_common(inp, r, nl, l_list=(0,)):
    b, g = core_bg(r)
    m = {}
    sel = np.zeros((8, 2), np.float32); sel[0::2, 0] = 1; sel[1::2, 1] = 1
    m["c_sel8"] = sel
    cc = np.stack([inp["c"][b], inp["c_ctx"]], -1)
    m["cT"] = np.ascontiguousarray(cc.reshape(8, 128, 2).transpose(1, 0, 2))
    cols = np.concatenate([w * 1024 + 256 * g + np.arange(256) for w in range(6)])
    for l in l_list:
        wm = inp["w_mod"][l][:, cols]
        m[f"wmod{l}"] = np.ascontiguousarray(wm.reshape(8, 128, 1536).transpose(1, 0, 2))
        m[f"bmod{l}"] = np.ascontiguousarray(inp["b_mod"][l][cols].reshape(12, 128).T)
    xs = np.concatenate([inp["x"][b, :nl], inp["ctx"][b]], 0)
    m["xT"] = np.ascontiguousarray(xs[:, 256 * g:256 * g + 256].T.reshape(2, 128, nl + 256))
    return m

SPLIT = [1536, 1536, 512, 16, 16, 1536, 1536, 4096]
OFF = np.concatenate([[0], np.cumsum(SPLIT)])
def wg_cols(g):
    o_hy, o_gq, o_gz, o_ga, o_gb, o_d, o_n, o_gate = OFF[:8]
    c = []
    for part in range(3): c.append(o_hy + part * 512 + 128 * g + np.arange(128))
    for part in range(3): c.append(o_gq + part * 512 + 128 * g + np.arange(128))
    c.append(o_gz + 128 * g + np.arange(128))
    for part in range(3): c.append(o_d + part * 512 + 128 * g + np.arange(128))
    for part in range(3): c.append(o_n + part * 512 + 128 * g + np.arange(128))
    ab = []
    for base in (o_ga, o_gb):
        for d in range(2):
            for h in range(2): ab.append(base + d * 8 + 2 * g + h)
    c.append(np.array(ab))
    return np.concatenate(c)
def rope_tables(nl):
    t = np.arange(nl); row = (t // 64).astype(np.float32); col = (t % 64).astype(np.float32)
    inv = (10000.0 ** (-np.arange(16, dtype=np.float32) / 16)).astype(np.float32)
    ar = row[:, None] * inv; ac = col[:, None] * inv
    cosd = np.ones((64, nl + 256), np.float32); sind = np.zeros((64, nl + 256), np.float32)
    for hh, ang in enumerate((ar, ac)):
        for t2 in range(2):
            cosd[hh * 32 + t2 * 16: hh * 32 + t2 * 16 + 16, :nl] = np.cos(ang).T
            sind[hh * 32 + t2 * 16: hh * 32 + t2 * 16 + 16, :nl] = np.sin(ang).T
    return np.concatenate([cosd, cosd], 0), np.concatenate([sind, sind], 0)
def na_consts():
    cols = np.arange(64)
    cstart = np.clip(cols - 8, 0, 48)
    col_ok = (cols[None, :] >= cstart[:, None]) & (cols[None, :] < cstart[:, None] + 16)
    dc = np.clip(cols[None, :] - cols[:, None], -15, 15) + 15
    return col_ok.T.astype(np.float32).copy(), dc
def prep_mix(inp, r, nl, l):
    b, g = core_bg(r); m = {}
    w = inp["w_in"][l][:, wg_cols(g)]
    m[f"wg{l}"] = np.ascontiguousarray(w.reshape(8, 128, -1).transpose(1, 0, 2))
    c, s_ = rope_tables(nl); m["c_cosT"] = c; m["c_sinT"] = s_
    m["c_ident"] = np.eye(128, dtype=np.float32)
    m[f"dlam{l}"] = inp["diff_lam"][l]
    m[f"dnorm{l}"] = inp["diff_norm"][l][None, :]
    mask, dc = na_consts(); m["c_namask"] = mask
    rp = inp["na_rpb"][l][2 * g:2 * g + 2]
    bias = rp[:, :, dc]
    m[f"nabias{l}"] = np.ascontiguousarray(bias.transpose(3, 0, 1, 2).reshape(64, -1))
    return m

def prep_tail(inp, r, nl, l, ref=None):
    import ml_dtypes
    b, g = core_bg(r); m = {}
    og = OFF[7]
    gc = np.concatenate([og + mm * 1024 + 256 * g + np.arange(256) for mm in range(4)])
    wgate = inp["w_in"][l][:, gc]
    m[f"wgate{l}"] = np.ascontiguousarray(wgate.reshape(8, 128, 1024).transpose(1, 0, 2))
    bp = inp["branch_proj"][l][:, :, 256 * g:256 * g + 256]
    bp = bp.reshape(4, 4, 128, 256).transpose(2, 1, 0, 3)
    m[f"bp{l}"] = np.ascontiguousarray(bp.reshape(128, 16, 256))
    wo = inp["w_out"][l][:, 256 * g:256 * g + 256]
    m[f"wout{l}"] = np.ascontiguousarray(wo.reshape(8, 128, 256).transpose(1, 0, 2))
    f = 256 * g + np.arange(256)
    lnp = np.stack([inp["ln_g"][l, 0][f], inp["ln_b"][l, 0][f], inp["ln_g"][l, 1][f], inp["ln_b"][l, 1][f]], -1)
    m[f"lnp{l}"] = np.ascontiguousarray(lnp.reshape(2, 128, 4).transpose(1, 0, 2))
    if ref is not None:
        ys = []
        for g2 in range(4):
            for nm in ("ya", "yb", "yc", "yd"):
                full = np.concatenate([ref[nm + "_x"][b, :nl], ref[nm + "_c"][b]], 0)
                ys.append(full[:, 128 * g2:128 * g2 + 128].T)
        m["yt_ref"] = np.concatenate(ys, 0).astype(ml_dtypes.bfloat16)
    return m

def prep_moe(inp, r, nl, l, ref=None):
    b, g = core_bg(r); m = {}
    f = 256 * g + np.arange(256)
    m["rw"] = np.ascontiguousarray(inp["router_w"][f].reshape(2, 128, 16).transpose(1, 0, 2))
    m["rb"] = inp["router_b"][None, :]
    comb = np.zeros((64, 16), np.float32)
    for rr in range(4):
        comb[rr * 16 + np.arange(16), np.arange(16)] = 1
    m["c_comb"] = comb
    es = np.zeros((16, 4, 128), np.float32)
    for i in range(4): es[4 * g + i, i, :] = 1
    m["c_esel"] = es.reshape(16, 512)
    E = [4 * g + i for i in range(4)]
    m[f"ew1_{l}"] = np.ascontiguousarray(inp["exp_w1"][l][E].reshape(4, 8, 128, 512).transpose(0, 2, 1, 3).reshape(4, 128, 4096))
    m[f"ew3_{l}"] = np.ascontiguousarray(inp["exp_w3"][l][E].reshape(4, 8, 128, 512).transpose(0, 2, 1, 3).reshape(4, 128, 4096))
    m[f"ew2_{l}"] = np.ascontiguousarray(inp["exp_w2"][l][E].reshape(4, 4, 128, 1024).transpose(0, 2, 1, 3).reshape(4, 128, 4096))
    m["c_ident"] = np.eye(128, dtype=np.float32)
    if ref is not None:
        x1 = np.concatenate([ref['x1'][b, :nl], ref['xc1'][b]], 0)[:, f].T
        m["x1_ref"] = np.ascontiguousarray(x1.reshape(2, 128, -1))
    return m

def prep_gdn(inp, r, nl, l):
    b, g = core_bg(r); m = {}
    gc = np.concatenate([inp["gdn_conv"][l][part * 512 + 128 * g + np.arange(128)] for part in range(3)], 1)
    m[f"gconv{l}"] = np.ascontiguousarray(gc)
    al = [inp["gdn_a_log"][l][d, 2 * g + h] for d in range(2) for h in range(2)]
    dtb = [inp["gdn_dt_bias"][l][d, 2 * g + h] for d in range(2) for h in range(2)]
    m[f"gpar{l}"] = np.array([al + dtb], np.float32)
    m[f"gnorm{l}"] = np.concatenate([inp["gdn_norm"][l]] * 2)[None, :].astype(np.float32)
    p = np.arange(64)
    MU = (p[:, None] <= p[None, :]).astype(np.float32); ML = (p[:, None] >= p[None, :]).astype(np.float32)
    I = np.eye(64, dtype=np.float32); ON = np.ones((64, 64), np.float32)
    m["c_gdn"] = np.ascontiguousarray(np.stack([MU, ML, I, ON, MU - I, ML - I, ON], 1).reshape(64, 7 * 64))
    bo = np.zeros((128, 128), np.float32); bo[:64, :64] = 1; bo[64:, 64:] = 1
    m["c_bones"] = bo
    return m

def hy_feats(L):
    f32 = np.float32
    t = np.linspace(0.0, 1.0, L, dtype=f32)[:, None]
    ang = (f32(2.0 * np.pi) * np.arange(L, dtype=f32)[:, None] / f32(L)).astype(f32)
    bands = np.linspace(1e-4, 15, 16, dtype=f32)[None, :]
    feats = np.concatenate([t, np.cos(bands * ang), -np.sin(bands * ang)], -1).astype(f32)
    F = np.concatenate([feats.T, feats[::-1].T], 1)
    T = np.concatenate([t[:, 0], t[::-1, 0]])[None, :]
    return np.ascontiguousarray(F), np.ascontiguousarray(T.astype(f32))
def prep_hy(inp, r, nl, l):
    b, g = core_bg(r); m = {}
    ch = 128 * g + np.arange(128)
    m[f"hconv{l}"] = np.ascontiguousarray(np.concatenate([inp["hy_conv"][l][part * 512 + ch] for part in range(3)], 1))
    m[f"hw1_{l}"] = inp["hy_w1"][l]; m[f"hw2_{l}"] = inp["hy_w2"][l]
    m[f"hw3_{l}"] = np.ascontiguousarray(np.concatenate([inp["hy_w3"][l][:, dd * 1024 + o * 512 + ch] for dd in range(2) for o in range(2)], 1))
    m[f"hpar{l}"] = np.ascontiguousarray(np.stack([inp["hy_freq"][l], inp["hy_b1"][l], inp["hy_b2"][l]], 1))
    dl = [inp["hy_deltas"][l][dd, o, ch] for dd in range(2) for o in range(2)] + [inp["hy_skip"][l][o, ch] for o in range(2)]
    m[f"hdel{l}"] = np.ascontiguousarray(np.stack(dl, 1))
    m["c_jm"] = np.eye(128, dtype=np.float32)[::-1].copy()
    for L in (nl, 256):
        F, T = hy_feats(L); m[f"c_hyF{L}"] = F; m[f"c_hyT{L}"] = T
    return m


def kernel(**inputs):
    inp = {k: np.asarray(v) for k, v in inputs.items()}
    depth = DEPTH
    P = build_full(depth, NL)
    maps = []
    for r in range(8):
        m = prep_common(inp, r, NL, l_list=tuple(range(depth)))
        for l in range(depth):
            m.update(prep_mix(inp, r, NL, l)); m.update(prep_gdn(inp, r, NL, l)); m.update(prep_hy(inp, r, NL, l))
            m.update(prep_tail(inp, r, NL, l)); m.update(prep_moe(inp, r, NL, l))
        missing = sorted(set(P.inputs) - set(m))
        assert not missing, missing
        maps.append({k: np.ascontiguousarray(v) for k, v in m.items() if k in P.inputs})
    res = run_bass_kernel_spmd(P.nc, maps, core_ids=list(range(8)))
    out = np.zeros((2, NL, D), np.float32)
    for r in range(8):
        b, g = r // 4, r % 4
        o = np.asarray(res.results[r]["o_x"])
        out[b, :, 256 * g:256 * g + 256] = o.reshape(256, NL).T
    return out
```
